# Optimizing a Trainium2 kernel written in Bass

```python
import math
import jax
import jax.numpy as jnp
from jax import lax
import numpy as np

D_MODEL = 1024
BATCH = 1
SEQ = 16384
DEPTH = 1

CHUNK = 64
Q_BLOCK = 128
A_HEADS = 8
A_HEAD_DIM = 64
B_HEADS = 8
B_HEAD_DIM = 128
IDX_HEADS = 16
IDX_DIM = 64
TOPK_MAX = 256
N_EXPERTS = 32
TOP_K = 4
D_FF = 1024
SWIGLU_ALPHA = 1.702
SWIGLU_LIMIT = 7.0
MOE_BLOCK = 128
LN_EPS = 1e-5
DEEPNORM_ALPHA = (2.0 * DEPTH) ** 0.25
DEEPNORM_BETA = (8.0 * DEPTH) ** -0.25

A_QK = A_HEADS * 2 * A_HEAD_DIM
A_V = A_HEADS * 2 * A_HEAD_DIM
B_W = B_HEADS * B_HEAD_DIM
IDX_Q = IDX_HEADS * IDX_DIM
SPLIT_SIZES = (A_QK, A_QK, A_V, B_W, B_W, B_W, IDX_Q, IDX_DIM, IDX_HEADS, D_MODEL, D_MODEL)
SPLIT_POINTS = tuple(int(v) for v in np.cumsum(SPLIT_SIZES)[:-1])
PROJ_WIDTH = int(sum(SPLIT_SIZES))

kernel_name = 'hybrid_diffattn_dsa_moe_block'


def alibi_slopes(n):
    return 2.0 ** (-8.0 * jnp.arange(1, n + 1, dtype=jnp.float32) / n)


def chunk_end(pos):
    return (pos // CHUNK + 1) * CHUNK


def layer_norm(x, g=None, b=None):
    xf = x.astype(jnp.float32)
    mu = jnp.mean(xf, axis=-1, keepdims=True)
    var = jnp.mean(jnp.square(xf - mu), axis=-1, keepdims=True)
    y = (xf - mu) * lax.rsqrt(var + LN_EPS)
    if g is not None:
        y = y * g.astype(jnp.float32) + b.astype(jnp.float32)
    return y.astype(x.dtype)


def diff_attention(q, k, v, lam, lam_init, norm_g):
    B, S, H, _, d = q.shape
    nb = S // Q_BLOCK
    slopes = alibi_slopes(H)
    kpos = jnp.arange(S)
    scale = d ** -0.5

    def block(i):
        q_blk = lax.dynamic_slice_in_dim(q, i * Q_BLOCK, Q_BLOCK, axis=1)
        qpos = i * Q_BLOCK + jnp.arange(Q_BLOCK)
        s = jnp.einsum('bqhcd,bkhcd->bhcqk', q_blk, k, preferred_element_type=jnp.float32) * scale
        dist = jnp.abs(qpos[:, None] - kpos[None, :]).astype(jnp.float32)
        allowed = kpos[None, :] < chunk_end(qpos)[:, None]
        s = jnp.where(allowed, s - slopes[:, None, None, None] * dist, -jnp.inf)
        p = jax.nn.softmax(s, axis=-1)
        w = p[:, :, 0] - lam * p[:, :, 1]
        return jnp.einsum('bhqk,bkhe->bqhe', w.astype(v.dtype), v)

    o = lax.map(block, jnp.arange(nb))
    o = jnp.moveaxis(o, 0, 1).reshape(B, S, H, 2 * d)
    of = o.astype(jnp.float32)
    of = of * lax.rsqrt(jnp.mean(jnp.square(of), axis=-1, keepdims=True) + LN_EPS)
    of = of * norm_g.astype(jnp.float32) * (1.0 - lam_init)
    return of.astype(v.dtype).reshape(B, S, H * 2 * d)


def dsa_attention(q, k, v, qi, ki, wi):
    B, S, H, E = q.shape
    nb = S // Q_BLOCK
    topk = min(TOPK_MAX, S // 4)
    slopes = alibi_slopes(H)
    kpos = jnp.arange(S)
    gather = jax.vmap(lambda a, j: a[j])

    def block(i):
        sl = lambda a: lax.dynamic_slice_in_dim(a, i * Q_BLOCK, Q_BLOCK, axis=1)
        q_blk, qi_blk, wi_blk = sl(q), sl(qi), sl(wi)
        qpos = i * Q_BLOCK + jnp.arange(Q_BLOCK)
        end = chunk_end(qpos)
        allowed = kpos[None, :] < end[:, None]
        logits = jnp.einsum('bqgd,bkd->bqgk', qi_blk, ki, preferred_element_type=jnp.float32) * (IDX_DIM ** -0.5)
        w_h = wi_blk.astype(jnp.float32) * (IDX_HEADS ** -0.5)
        score = jnp.einsum('bqg,bqgk->bqk', w_h, jax.nn.relu(logits))
        score = jnp.where(allowed, score, -jnp.inf)
        _, idx = lax.top_k(score, topk)
        valid = idx < end[None, :, None]
        k_sel = gather(k, idx)
        v_sel = gather(v, idx)
        s = jnp.einsum('bqhe,bqkhe->bhqk', q_blk, k_sel, preferred_element_type=jnp.float32) * (E ** -0.5)
        dist = jnp.abs(qpos[None, :, None] - idx).astype(jnp.float32)
        s = s - slopes[None, :, None, None] * dist[:, None]
        s = jnp.where(valid[:, None], s, -jnp.inf)
        p = jax.nn.softmax(s, axis=-1)
        return jnp.einsum('bhqk,bqkhe->bqhe', p.astype(v.dtype), v_sel)

    o = lax.map(block, jnp.arange(nb))
    return jnp.moveaxis(o, 0, 1).reshape(B, S, H * E)


def moe_ffn(h, w_r, b_r, w1, b1, w2, b2):
    B, S, D = h.shape
    t = h.reshape(-1, D)
    logits = jnp.matmul(t, w_r, preferred_element_type=jnp.float32) + b_r.astype(jnp.float32)
    vals, idx = lax.top_k(logits, TOP_K)
    wts = jax.nn.softmax(vals, axis=-1)
    gate = jnp.einsum('nk,nke->ne', wts, jax.nn.one_hot(idx, N_EXPERTS, dtype=jnp.float32)).astype(t.dtype)
    n = t.shape[0]
    nb = n // MOE_BLOCK
    tb = t.reshape(nb, MOE_BLOCK, D)
    gb = gate.reshape(nb, MOE_BLOCK, N_EXPERTS)

    def block(args):
        xb, g = args
        u = jnp.einsum('td,edf->tef', xb, w1) + b1
        x_glu = jnp.minimum(u[..., 0::2], SWIGLU_LIMIT)
        x_lin = jnp.clip(u[..., 1::2], -SWIGLU_LIMIT, SWIGLU_LIMIT)
        a = x_glu * jax.nn.sigmoid(SWIGLU_ALPHA * x_glu) * (x_lin + 1.0)
        a = a * g[:, :, None]
        return jnp.einsum('tef,efd->td', a, w2) + jnp.matmul(g, b2)

    y = lax.map(block, (tb, gb))
    return y.reshape(B, S, D)


def setup_inputs(seed: int = 0) -> dict:
    key = jax.random.key(seed)
    ks = jax.random.split(key, 24)
    D = D_MODEL
    nrm = lambda k, shape, s: jax.random.normal(k, shape, jnp.float32) * s
    return {
        'x': nrm(ks[0], (BATCH, SEQ, D), 1.0),
        'c': nrm(ks[1], (BATCH, D), 1.0),
        'w_ada': nrm(ks[2], (DEPTH, D, 6 * D), 0.5 * D ** -0.5),
        'b_ada': nrm(ks[3], (DEPTH, 6 * D), 0.02),
        'w_in': nrm(ks[4], (DEPTH, D, PROJ_WIDTH), D ** -0.5),
        'lam_q1': nrm(ks[5], (DEPTH, A_HEAD_DIM), 0.1),
        'lam_k1': nrm(ks[6], (DEPTH, A_HEAD_DIM), 0.1),
        'lam_q2': nrm(ks[7], (DEPTH, A_HEAD_DIM), 0.1),
        'lam_k2': nrm(ks[8], (DEPTH, A_HEAD_DIM), 0.1),
        'diff_norm_g': 1.0 + nrm(ks[9], (DEPTH, 2 * A_HEAD_DIM), 0.02),
        'w_branch_a': nrm(ks[10], (DEPTH, A_V, D), A_V ** -0.5),
        'w_branch_b': nrm(ks[11], (DEPTH, B_W, D), B_W ** -0.5),
        'w_out': nrm(ks[12], (DEPTH, D, D), DEEPNORM_BETA * D ** -0.5),
        'ln1_g': 1.0 + nrm(ks[13], (DEPTH, D), 0.02),
        'ln1_b': nrm(ks[14], (DEPTH, D), 0.02),
        'w_router': nrm(ks[15], (DEPTH, D, N_EXPERTS), D ** -0.5),
        'b_router': nrm(ks[16], (DEPTH, N_EXPERTS), 0.01),
        'w_e1': nrm(ks[17], (DEPTH, N_EXPERTS, D, 2 * D_FF), D ** -0.5),
        'b_e1': nrm(ks[18], (DEPTH, N_EXPERTS, 2 * D_FF), 0.01),
        'w_e2': nrm(ks[19], (DEPTH, N_EXPERTS, D_FF, D), DEEPNORM_BETA * D_FF ** -0.5),
        'b_e2': nrm(ks[20], (DEPTH, N_EXPERTS, D), 0.01),
        'ln2_g': 1.0 + nrm(ks[21], (DEPTH, D), 0.02),
        'ln2_b': nrm(ks[22], (DEPTH, D), 0.02),
    }


def reference(x, c, w_ada, b_ada, w_in, lam_q1, lam_k1, lam_q2, lam_k2, diff_norm_g,
              w_branch_a, w_branch_b, w_out, ln1_g, ln1_b, w_router, b_router,
              w_e1, b_e1, w_e2, b_e2, ln2_g, ln2_b):
    B, S, D = x.shape
    c_act = jax.nn.silu(c)
    for l in range(DEPTH):
        mod = jnp.matmul(c_act, w_ada[l]) + b_ada[l]
        sh_a, sc_a, g_a, sh_f, sc_f, g_f = [m[:, None, :] for m in jnp.split(mod, 6, axis=-1)]

        u = layer_norm(x) * (1.0 + sc_a) + sh_a
        proj = jnp.matmul(u, w_in[l])
        (qa, ka, va, qb, kb, vb, qi, ki, wi, ga, gb) = jnp.split(proj, SPLIT_POINTS, axis=-1)

        lam_init = 0.8 - 0.6 * math.exp(-0.3 * l)
        lam = (jnp.exp(jnp.sum(lam_q1[l].astype(jnp.float32) * lam_k1[l].astype(jnp.float32)))
               - jnp.exp(jnp.sum(lam_q2[l].astype(jnp.float32) * lam_k2[l].astype(jnp.float32)))
               + lam_init)
        ya = diff_attention(qa.reshape(B, S, A_HEADS, 2, A_HEAD_DIM),
                            ka.reshape(B, S, A_HEADS, 2, A_HEAD_DIM),
                            va.reshape(B, S, A_HEADS, 2 * A_HEAD_DIM),
                            lam, lam_init, diff_norm_g[l])
        yb = dsa_attention(qb.reshape(B, S, B_HEADS, B_HEAD_DIM),
                           kb.reshape(B, S, B_HEADS, B_HEAD_DIM),
                           vb.reshape(B, S, B_HEADS, B_HEAD_DIM),
                           qi.reshape(B, S, IDX_HEADS, IDX_DIM), ki, wi)
        merged = (jax.nn.sigmoid(ga) * jnp.matmul(ya, w_branch_a[l])
                  + jax.nn.sigmoid(gb) * jnp.matmul(yb, w_branch_b[l]))
        mix_out = jnp.matmul(merged, w_out[l])
        x = layer_norm(DEEPNORM_ALPHA * x + g_a * mix_out, ln1_g[l], ln1_b[l])

        v = layer_norm(x) * (1.0 + sc_f) + sh_f
        ffn_out = moe_ffn(v, w_router[l], b_router[l], w_e1[l], b_e1[l], w_e2[l], b_e2[l])
        x = layer_norm(DEEPNORM_ALPHA * x + g_f * ffn_out.astype(x.dtype), ln2_g[l], ln2_b[l])
    return x
```

```python
import os
import numpy as np
import ml_dtypes
from contextlib import ExitStack
import concourse.bass as bass
import concourse.mybir as mybir
from concourse.bass_utils import run_bass_kernel_spmd

F32 = mybir.dt.float32
BF16 = mybir.dt.bfloat16
I32 = mybir.dt.int32
AF = mybir.ActivationFunctionType
ALU = mybir.AluOpType
AX = mybir.AxisListType
NPBF = ml_dtypes.bfloat16

D = 1024
NCORE = 8
NEXP = 32
DFF = 1024
TOPK = 256
LN_EPS = 1e-5
ALPHA = 2.0 ** 0.25
LAM_INIT = 0.2
NEG = -30000.0
NBISECT = 24
SWIGLU_ALPHA = 1.702
SWIGLU_LIMIT = 7.0
C_QA, C_KA, C_VA, C_QB, C_KB, C_VB, C_QI, C_KI, C_WI, C_GA, C_GB = (
    0, 1024, 2048, 3072, 4096, 5120, 6144, 7168, 7232, 7248, 8272)
PROJ_W = 9296


class Res:
    __slots__ = ("w", "r", "name")

    def __init__(self, name=""):
        self.w = None
        self.r = {}
        self.name = name


class Eng:
    def __init__(self, name, eng, sem):
        self.name = name
        self.eng = eng
        self.sem = sem
        self.cnt = 0
        self.seen = {}


class Trk:
    def __init__(self, nc, es):
        self.nc = nc
        mk = lambda n: es.enter_context(nc.semaphore(n))
        self.E = {n: Eng(n, e, mk("s_" + n)) for n, e in [
            ("pe", nc.tensor), ("act", nc.scalar), ("dve", nc.vector),
            ("pool", nc.gpsimd), ("sp", nc.sync)]}
        self.dsems = {q: [[mk(f"d_{q}{i}"), 0] for i in range(n)]
                      for q, n in [("sp", 16), ("pool", 10), ("act", 4)]}
        self.dnext = {q: 0 for q in self.dsems}
        self.nwait = 0

    def _waits(self, E, reads, writes):
        need = {}

        def add(tok, raw):
            if tok is None:
                return
            sem, val = tok
            if sem is E.sem and E.name == "pe":
                return
            k = id(sem)
            if k not in need or need[k][1] < val:
                need[k] = (sem, val)
        for r in reads:
            add(r.w, True)
        for w in writes:
            add(w.w, False)
            for tok in w.r.values():
                add(tok, False)
        for k, (sem, val) in need.items():
            if E.seen.get(k, 0) < val:
                E.eng.wait_ge(sem, val)
                E.seen[k] = val
                self.nwait += 1

    @staticmethod
    def _mark(tok, reads, writes):
        k = id(tok[0])
        for r in reads:
            r.r[k] = tok
        for w in writes:
            w.w = tok
            w.r = {}

    def op(self, en, fn, reads=(), writes=()):
        E = self.E[en]
        self._waits(E, reads, writes)
        ins = fn(E.eng)
        E.cnt += 1
        ins.then_inc(E.sem, 1)
        self._mark((E.sem, E.cnt), reads, writes)

    def dma(self, q, out, in_, reads=(), writes=(), **kw):
        E = self.E[q]
        self._waits(E, reads, writes)
        slots = self.dsems[q]
        i = self.dnext[q]
        self.dnext[q] = (i + 1) % len(slots)
        sem, val = slots[i]
        k = id(sem)
        if val > 0 and E.seen.get(k, 0) < val:
            E.eng.wait_ge(sem, val)
            E.seen[k] = val
        ins = E.eng.dma_start(out=out, in_=in_, **kw)
        val += 16
        slots[i][1] = val
        ins.then_inc(sem, 16)
        self._mark((sem, val), reads, writes)

    def barrier(self, all_res):
        toks = {}
        for r in all_res:
            for tok in [r.w] + list(r.r.values()):
                if tok is None:
                    continue
                k = id(tok[0])
                if k not in toks or toks[k][1] < tok[1]:
                    toks[k] = tok
        for E in self.E.values():
            for k, (sem, val) in toks.items():
                if sem is E.sem:
                    continue
                if E.seen.get(k, 0) < val:
                    E.eng.wait_ge(sem, val)
                    E.seen[k] = val


def alibi_slopes(n=8):
    return [2.0 ** (-8.0 * (h + 1) / n) for h in range(n)]


class MK:
    def __init__(self, nslot, debug=False, phases=99):
        self.nslot = nslot
        self.ntile = 8 * nslot
        self.S = 128 * self.ntile
        self.NQ = 128 * nslot
        self.debug = debug
        self.phases = phases
        self.nc = bass.Bass("TRN2", target_bir_lowering=False)
        self.res_all = []

    def R(self, name=""):
        r = Res(name)
        self.res_all.append(r)
        return r

    def I(self, name):
        if name not in self.in_aps:
            shape, dt = self.in_specs[name]
            self.in_aps[name] = self.nc.dram_tensor(name, list(shape), dt, kind="ExternalInput").ap()
        return self.in_aps[name]

    def dscr(self, name, shape, dt):
        kind = "ExternalOutput" if self.debug else "Internal"
        t = self.nc.dram_tensor(name, list(shape), dt, kind=kind).ap()
        return t

    def sb(self, es, name, shape, dt):
        return es.enter_context(self.nc.sbuf_tensor(name, list(shape), dt))

    def barrier(self):
        self.t.barrier(self.res_all)

    def build(self):
        nc = self.nc
        S, NQ, nslot, ntile = self.S, self.NQ, self.nslot, self.ntile
        self.in_specs = {
            "x": ([S, D], F32), "c": ([D], F32), "w_ada": ([D, 6 * D], F32), "b_ada": ([6 * D], F32),
            "w_in": ([D, PROJ_W], F32), "lamv": ([4, 64], F32), "diff_norm_g": ([128], F32),
            "w_branch_a": ([D, D], F32), "w_branch_b": ([D, D], F32), "w_out": ([D, D], F32),
            "ln1_g": ([D], F32), "ln1_b": ([D], F32), "w_router": ([D, NEXP], F32), "b_router": ([NEXP], F32),
            "w_e1": ([NEXP, D, 2 * DFF], F32), "b_e1": ([NEXP, 2 * DFF], F32),
            "w_e2": ([NEXP, DFF, D], F32), "b_e2": ([NEXP, D], F32), "ln2_g": ([D], F32), "ln2_b": ([D], F32),
            "c_identb": ([128, 128], BF16), "c_identf": ([128, 128], F32), "c_kaug": ([7, S], BF16),
            "c_qaug": ([7, nslot, 8, 128], BF16), "c_dg": ([128, 8, 128], BF16), "c_dmask": ([128, 128], F32),
            "c_dummy": ([128, 8], F32), "c_iota": ([128, 512], F32), "c_slopetab": ([2, 8, 128], F32),
            "c_pidx1": ([128, 1], F32),
        }
        self.in_aps = {}
        self.out = nc.dram_tensor("out", [NQ, D], F32, kind="ExternalOutput").ap()
        self.d_mod = self.dscr("d_mod", [6 * D], F32)
        self.d_kat = self.dscr("d_kat", [16, 64, S], BF16)
        self.d_va = self.dscr("d_va", [S, 8, 132], BF16)
        self.d_kbt = self.dscr("d_kbt", [8, 128, S], BF16)
        self.d_vb = self.dscr("d_vb", [S, 8, 132], BF16)
        self.d_kit = self.dscr("d_kit", [64, S], BF16)
        self.d_qat = self.dscr("d_qat", [16, 64, NQ], BF16)
        self.d_qbt = self.dscr("d_qbt", [8, 128, NQ], BF16)
        self.d_qit = self.dscr("d_qit", [16, 64, NQ], BF16)
        self.d_sgn = self.dscr("d_sgn", [NQ, 16], F32)
        self.d_gate = self.dscr("d_gate", [NQ, 2 * D], F32)
        self.d_ya = self.dscr("d_ya", [NQ, D], BF16)
        self.d_yb = self.dscr("d_yb", [NQ, D], BF16)
        self.d_x1 = self.dscr("d_x1", [NQ, D], F32)

        with ExitStack() as es:
            self.t = Trk(nc, es)
            self.ps = [es.enter_context(nc.psum_tensor(f"ps{i}", [128, 512], F32)) for i in range(8)]
            self.psr = [self.R(f"ps{i}") for i in range(8)]
            self.identb = self.sb(es, "identb", [128, 128], BF16)
            self.identf = self.sb(es, "identf", [128, 128], F32)
            self.r_const = self.R("const")
            self.t.dma("sp", self.identb[:], self.I("c_identb"), writes=[self.r_const])
            self.t.dma("sp", self.identf[:], self.I("c_identf"), writes=[self.r_const])
            self.modT = self.sb(es, "modT", [128, 48], F32)
            self.r_mod = self.R("mod")
            self.r_out = self.R("out")
            self.phase0()
            self.barrier()
            if self.phases >= 1:
                self.phase1a()
                self.barrier()
                self.phase1b()
                self.barrier()
            if self.phases >= 2:
                self.phase2()
                self.barrier()
            if self.phases >= 3:
                self.phase3()
                self.barrier()
            if self.phases >= 4:
                self.phase4()
            self.final_wait()
        return nc

    def final_wait(self):
        self.barrier()

    def phase0(self):
        nc, t = self.nc, self.t
        with ExitStack() as es:
            cT = self.sb(es, "cT", [128, 8], F32)
            cact = self.sb(es, "cact", [128, 8], F32)
            wbuf = [self.sb(es, f"wada{i}", [128, 8, 512], F32) for i in range(2)]
            wr = [self.R() for _ in range(2)]
            brow = self.sb(es, "brow", [1, 6 * D], F32)
            mrow = self.sb(es, "mrow", [1, 6 * D], F32)
            r_c, r_b, r_m = self.R(), self.R(), self.R()
            t.dma("sp", cT[:], self.I("c").rearrange("(c p) -> p c", p=128), writes=[r_c],
                  allow_slow_non_contiguous=True)
            t.dma("sp", brow[:], self.I("b_ada").rearrange("(o n) -> o n", o=1), writes=[r_b])
            t.op("act", lambda e: e.activation(out=cact[:], in_=cT[:], func=AF.Silu),
                 reads=[r_c], writes=[r_c])
            wv = self.I("w_ada").rearrange("(c p) n -> p c n", p=128)
            for g in range(12):
                b = g % 2
                t.dma("sp", wbuf[b][:], wv[:, :, g * 512:(g + 1) * 512], writes=[wr[b]])
                pb = g % 2

                def mm(e, b=b, pb=pb):
                    for c in range(8):
                        ins = e.matmul(self.ps[pb][0:1, :], lhsT=cact[:, c:c + 1], rhs=wbuf[b][:, c, :],
                                       start=(c == 0), stop=(c == 7))
                    return ins
                t.op("pe", mm, reads=[r_c, wr[b]], writes=[self.psr[pb]])
                t.op("dve", lambda e, g=g, pb=pb: e.tensor_tensor(
                    out=mrow[:, g * 512:(g + 1) * 512], in0=self.ps[pb][0:1, :],
                    in1=brow[:, g * 512:(g + 1) * 512], op=ALU.add),
                    reads=[self.psr[pb], r_b], writes=[r_m])
            r_d = self.R()
            t.dma("sp", self.d_mod.rearrange("(o n) -> o n", o=1), mrow[:], reads=[r_m], writes=[r_d])
            t.dma("sp", self.modT[:], self.d_mod.rearrange("(m p) -> p m", p=128), reads=[r_d],
                  writes=[self.r_mod], allow_slow_non_contiguous=True)
            self.r_dmod = r_d
            self.barrier()

    def ln_tile(self, xin_ap, xt, r_xt, xn, r_xn, stats, mv, r_st, out_xnT, r_out, pbank, q="sp"):
        t = self.t
        t.dma(q, xt[:], xin_ap, writes=[r_xt])
        for hh in range(2):
            t.op("dve", lambda e, hh=hh: e.bn_stats(out=stats[:, hh, :], in_=xt[:, hh * 512:(hh + 1) * 512]),
                 reads=[r_xt], writes=[r_st])
        t.op("dve", lambda e: e.bn_aggr(out=mv[:, 0:2], in_=stats[:].rearrange("p a b -> p (a b)")),
             reads=[r_st], writes=[r_st])
        t.op("dve", lambda e: e.tensor_scalar(out=mv[:, 2:3], in0=mv[:, 1:2], scalar1=LN_EPS, scalar2=None,
                                              op0=ALU.add), reads=[r_st], writes=[r_st])
        t.op("act", lambda e: e.activation(out=mv[:, 2:3], in_=mv[:, 2:3], func=AF.Sqrt),
             reads=[r_st], writes=[r_st])
        t.op("dve", lambda e: e.reciprocal(out=mv[:, 3:4], in_=mv[:, 2:3]), reads=[r_st], writes=[r_st])
        t.op("dve", lambda e: e.tensor_scalar(out=xn[:], in0=xt[:], scalar1=mv[:, 0:1], scalar2=mv[:, 3:4],
                                              op0=ALU.subtract, op1=ALU.mult),
             reads=[r_xt, r_st], writes=[r_xn])
        pbf = self.ps[pbank].bitcast(BF16)

        def tr(e):
            for c in range(8):
                ins = e.transpose(pbf[:, c * 128:(c + 1) * 128], xn[:, c * 128:(c + 1) * 128], self.identb[:])
            return ins
        t.op("pe", tr, reads=[r_xn, self.r_const], writes=[self.psr[pbank]])
        t.op("act", lambda e: e.copy(out=out_xnT, in_=pbf[:, :].rearrange("p (c n) -> p c n", c=8)),
             reads=[self.psr[pbank]], writes=[r_out])

    def prep_w(self, es, tag, colranges, sc_off, sh_off):
        nc, t = self.nc, self.t
        ncols = sum(l for _, l in colranges)
        wsb = self.sb(es, "w_" + tag, [128, 8, ncols], BF16)
        r_w = self.R()
        wv = self.I("w_in").rearrange("(c p) n -> p c n", p=128)
        o = 0
        for (s0, l) in colranges:
            for a in range(0, l, 512):
                b = min(l, a + 512)
                t.dma("pool", wsb[:, :, o + a:o + b], wv[:, :, s0 + a:s0 + b], writes=[r_w])
            o += l
        nch = (ncols + 127) // 128
        biasT = self.sb(es, "bT_" + tag, [128, nch], F32)
        biasrow = self.sb(es, "br_" + tag, [1, ncols], BF16)
        onep = self.sb(es, "onep_" + tag, [128, 8], F32)
        shb = self.sb(es, "shb_" + tag, [128, 8], BF16)
        r_b = self.R()
        t.op("dve", lambda e: e.tensor_scalar(out=onep[:], in0=self.modT[:, sc_off:sc_off + 8], scalar1=1.0,
                                              scalar2=None, op0=ALU.add), reads=[self.r_mod], writes=[r_b])
        t.op("dve", lambda e: e.tensor_copy(out=shb[:], in_=self.modT[:, sh_off:sh_off + 8]),
             reads=[self.r_mod], writes=[r_b])
        for ch in range(nch):
            w0 = ch * 128
            wl = min(128, ncols - w0)
            pb = ch % 2

            def mm(e, w0=w0, wl=wl, pb=pb):
                for c in range(8):
                    ins = e.matmul(self.ps[pb][0:wl, 0:1], lhsT=wsb[:, c, w0:w0 + wl], rhs=shb[:, c:c + 1],
                                   start=(c == 0), stop=(c == 7))
                return ins
            t.op("pe", mm, reads=[r_w, r_b], writes=[self.psr[pb]])
            t.op("dve", lambda e, ch=ch, wl=wl, pb=pb: e.tensor_copy(out=biasT[0:wl, ch:ch + 1],
                                                                      in_=self.ps[pb][0:wl, 0:1]),
                 reads=[self.psr[pb]], writes=[r_b])
        for a in range(0, ncols, 512):
            b = min(ncols, a + 512)
            pb = 2 + (a // 512) % 2

            def mm2(e, a=a, b=b, pb=pb):
                for c in range(8):
                    ins = e.matmul(self.ps[pb][0:1, 0:b - a], lhsT=shb[:, c:c + 1], rhs=wsb[:, c, a:b],
                                   start=(c == 0), stop=(c == 7))
                return ins
            t.op("pe", mm2, reads=[r_w, r_b], writes=[self.psr[pb]])
            t.op("dve", lambda e, a=a, b=b, pb=pb: e.tensor_copy(out=biasrow[:, a:b], in_=self.ps[pb][0:1, 0:b - a]),
                 reads=[self.psr[pb]], writes=[r_b])
        for c in range(8):
            en = "dve" if c % 2 == 0 else "pool"
            t.op(en, lambda e, c=c: e.tensor_scalar(out=wsb[:, c, :], in0=wsb[:, c, :], scalar1=onep[:, c:c + 1],
                                                     scalar2=None, op0=ALU.mult),
                 reads=[r_w, r_b], writes=[r_w])
        return wsb, r_w, biasT, biasrow, r_b

    def phase1a(self):
        nc, t = self.nc, self.t
        S = self.S
        with ExitStack() as es:
            wsb, r_w, biasT, biasrow, r_b = self.prep_w(
                es, "p1a", [(C_KA, 1024), (C_KB, 1024), (C_KI, 64), (C_VA, 1024), (C_VB, 1024)], 8, 0)
            VOFF = 2112
            ones = self.sb(es, "ones1", [1, 128], BF16)
            r_ones = self.R()
            t.op("dve", lambda e: e.memset(ones[:], 1.0), writes=[r_ones])
            xt = [self.sb(es, f"xt{i}", [128, D], F32) for i in range(2)]
            r_xt = [self.R() for _ in range(2)]
            xn = [self.sb(es, f"xn{i}", [128, D], BF16) for i in range(2)]
            r_xn = [self.R() for _ in range(2)]
            stats = [self.sb(es, f"st{i}", [128, 2, 6], F32) for i in range(2)]
            mv = [self.sb(es, f"mv{i}", [128, 4], F32) for i in range(2)]
            r_st = [self.R() for _ in range(2)]
            xnT = [self.sb(es, f"xnT{i}", [128, 8, 512], BF16) for i in range(2)]
            r_xnT = [self.R() for _ in range(2)]
            kst = [self.sb(es, f"kst{i}", [128, 512], BF16) for i in range(4)]
            r_kst = [self.R() for _ in range(4)]
            vst = [self.sb(es, f"vst{i}", [128, 4, 132], BF16) for i in range(4)]
            r_vst = [self.R() for _ in range(4)]
            for i in range(4):
                t.op("pool", lambda e, i=i: e.memset(vst[i][:, :, 128:132], 0.0), writes=[r_vst[i]])
                t.op("pool", lambda e, i=i: e.memset(vst[i][:, :, 128:129], 1.0), writes=[r_vst[i]])
            ngrp = S // 512
            ki = 0
            vi = 0
            tcount = 0
            ev = 0
            def ln_group(g):
                gb = g % 2
                for tt in range(4):
                    b = tcnt[0] % 2
                    tok0 = g * 512 + tt * 128
                    self.ln_tile(self.I("x")[tok0:tok0 + 128, :], xt[b], r_xt[b], xn[b], r_xn[b], stats[b], mv[b],
                                 r_st[b], xnT[gb][:, :, tt * 128:(tt + 1) * 128], r_xnT[gb], pbank=b)
                    tcnt[0] += 1
            tcnt = [0]
            ln_group(0)
            for g in range(ngrp):
                gb = g % 2
                if g + 1 < ngrp:
                    ln_group(g + 1)
                for ch in range(17):
                    wl = 128 if ch < 16 else 64
                    pb = 2 + ch % 3

                    def mm(e, ch=ch, wl=wl, pb=pb, gb=gb):
                        for c in range(8):
                            ins = e.matmul(self.ps[pb][0:wl, :], lhsT=wsb[:, c, ch * 128:ch * 128 + wl],
                                           rhs=xnT[gb][:, c, :], start=(c == 0), stop=(c == 7))
                        return ins
                    t.op("pe", mm, reads=[r_w, r_xnT[gb]], writes=[self.psr[pb]])
                    s = ki % 4
                    ki += 1
                    en = "act" if ev % 2 == 0 else "dve"
                    ev += 1
                    if en == "act":
                        t.op("act", lambda e, s=s, wl=wl, pb=pb, ch=ch: e.activation(
                            out=kst[s][0:wl, :], in_=self.ps[pb][0:wl, :], func=AF.Identity,
                            bias=biasT[0:wl, ch:ch + 1], scale=1.0),
                            reads=[self.psr[pb], r_b], writes=[r_kst[s]])
                    else:
                        t.op("dve", lambda e, s=s, wl=wl, pb=pb, ch=ch: e.tensor_scalar(
                            out=kst[s][0:wl, :], in0=self.ps[pb][0:wl, :], scalar1=biasT[0:wl, ch:ch + 1],
                            scalar2=None, op0=ALU.add),
                            reads=[self.psr[pb], r_b], writes=[r_kst[s]])
                    tsl = slice(g * 512, (g + 1) * 512)
                    if ch < 8:
                        t.dma("sp", self.d_kat[2 * ch:2 * ch + 2, :, tsl].rearrange("m d n -> (m d) n"),
                              kst[s][:, :], reads=[r_kst[s]])
                    elif ch < 16:
                        t.dma("sp", self.d_kbt[ch - 8, :, tsl], kst[s][:, :], reads=[r_kst[s]])
                    else:
                        t.dma("sp", self.d_kit[:, tsl], kst[s][0:64, :], reads=[r_kst[s]])
                for tt in range(4):
                    for vg in range(4):
                        pb = 5 + (tt * 4 + vg) % 3
                        c0 = VOFF + vg * 512

                        def mm(e, tt=tt, c0=c0, pb=pb, gb=gb):
                            for c in range(8):
                                e.matmul(self.ps[pb][:, :], lhsT=xnT[gb][:, c, tt * 128:(tt + 1) * 128],
                                         rhs=wsb[:, c, c0:c0 + 512], start=(c == 0), stop=False)
                            return e.matmul(self.ps[pb][:, :], lhsT=ones[0:1, :], rhs=biasrow[0:1, c0:c0 + 512],
                                            start=False, stop=True)
                        t.op("pe", mm, reads=[r_w, r_xnT[gb], r_b, r_ones], writes=[self.psr[pb]])
                        s = vi % 4
                        vi += 1
                        en = "act" if ev % 2 == 0 else "dve"
                        ev += 1
                        psv = self.ps[pb][:, :].rearrange("p (h e) -> p h e", h=4)
                        if en == "act":
                            t.op("act", lambda e, s=s, psv=psv: e.copy(out=vst[s][:, :, 0:128], in_=psv),
                                 reads=[self.psr[pb]], writes=[r_vst[s]])
                        else:
                            t.op("dve", lambda e, s=s, psv=psv: e.tensor_copy(out=vst[s][:, :, 0:128], in_=psv),
                                 reads=[self.psr[pb]], writes=[r_vst[s]])
                        tok0 = g * 512 + tt * 128
                        dst = self.d_va if vg < 2 else self.d_vb
                        t.dma("sp", dst[tok0:tok0 + 128, (vg % 2) * 4:(vg % 2) * 4 + 4, :], vst[s][:],
                              reads=[r_vst[s]])
            self.barrier()

    def phase1b(self):
        nc, t = self.nc, self.t
        nslot, NQ = self.nslot, self.NQ
        with ExitStack() as es:
            wsb, r_w, biasT, biasrow, r_b = self.prep_w(
                es, "p1b", [(C_QA, 1024), (C_QB, 1024), (C_QI, 1024), (C_WI, 16), (C_GA, 1024), (C_GB, 1024)], 8, 0)
            O_QI, O_WI, O_GA = 2048, 3072, 3088
            ones = self.sb(es, "ones1b", [1, 128], BF16)
            r_ones = self.R()
            t.op("dve", lambda e: e.memset(ones[:], 1.0), writes=[r_ones])
            xt = [self.sb(es, f"xtb{i}", [128, D], F32) for i in range(2)]
            r_xt = [self.R() for _ in range(2)]
            xn = [self.sb(es, f"xnb{i}", [128, D], BF16) for i in range(2)]
            r_xn = [self.R() for _ in range(2)]
            stats = [self.sb(es, f"stb{i}", [128, 2, 6], F32) for i in range(2)]
            mv = [self.sb(es, f"mvb{i}", [128, 4], F32) for i in range(2)]
            r_st = [self.R() for _ in range(2)]
            G = min(4, nslot)
            xnT = [self.sb(es, f"xnTb{i}", [128, 8, 128 * G], BF16) for i in range(2)]
            r_xnT = [self.R() for _ in range(2)]
            kst = [self.sb(es, f"kstb{i}", [128, 128 * G], BF16) for i in range(4)]
            r_kst = [self.R() for _ in range(4)]
            gst = [self.sb(es, f"gst{i}", [128, 512], F32) for i in range(3)]
            r_gst = [self.R() for _ in range(3)]
            wis = [self.sb(es, f"wis{i}", [128, 3, 16], F32) for i in range(2)]
            r_wis = [self.R() for _ in range(2)]
            qis = [self.sb(es, f"qis{i}", [128, 1024], BF16) for i in range(2)]
            r_qis = [self.R() for _ in range(2)]
            qit = [self.sb(es, f"qit{i}", [128, 8, 128], BF16) for i in range(2)]
            r_qit = [self.R() for _ in range(2)]
            ki = 0
            gi = 0
            tcount = 0
            for g in range(nslot // G):
                gb = g % 2
                NT = 128 * G
                for tt in range(G):
                    b = tcount % 2
                    j = g * G + tt
                    tok0 = (8 * j + 7) * 128
                    self.ln_tile(self.I("x")[tok0:tok0 + 128, :], xt[b], r_xt[b], xn[b], r_xn[b], stats[b], mv[b],
                                 r_st[b], xnT[gb][:, :, tt * 128:(tt + 1) * 128], r_xnT[gb], pbank=b)
                    tcount += 1
                q0 = g * NT
                for ch in range(16):
                    pb = 2 + ch % 3

                    def mm(e, ch=ch, pb=pb, gb=gb, NT=NT):
                        for c in range(8):
                            ins = e.matmul(self.ps[pb][:, 0:NT], lhsT=wsb[:, c, ch * 128:ch * 128 + 128],
                                           rhs=xnT[gb][:, c, :], start=(c == 0), stop=(c == 7))
                        return ins
                    t.op("pe", mm, reads=[r_w, r_xnT[gb]], writes=[self.psr[pb]])
                    s = ki % 4
                    ki += 1
                    scale = 0.125 if ch < 8 else 128.0 ** -0.5
                    t.op("dve", lambda e, s=s, pb=pb, ch=ch, scale=scale, NT=NT: e.tensor_scalar(
                        out=kst[s][:, 0:NT], in0=self.ps[pb][:, 0:NT], scalar1=biasT[:, ch:ch + 1], scalar2=scale,
                        op0=ALU.add, op1=ALU.mult), reads=[self.psr[pb], r_b], writes=[r_kst[s]])
                    if ch < 8:
                        t.dma("sp", self.d_qat[2 * ch:2 * ch + 2, :, q0:q0 + NT].rearrange("m d n -> (m d) n"),
                              kst[s][:, 0:NT], reads=[r_kst[s]])
                    else:
                        t.dma("sp", self.d_qbt[ch - 8, :, q0:q0 + NT], kst[s][:, 0:NT], reads=[r_kst[s]])
                for tt in range(G):
                    j = g * G + tt
                    tq0 = j * 128
                    lhs = lambda c, tt=tt, gb=gb: xnT[gb][:, c, tt * 128:(tt + 1) * 128]
                    wb = j % 2
                    pb = 5

                    def mmw(e, lhs=lhs, pb=pb):
                        for c in range(8):
                            e.matmul(self.ps[pb][:, 0:16], lhsT=lhs(c), rhs=wsb[:, c, O_WI:O_WI + 16],
                                     start=(c == 0), stop=False)
                        return e.matmul(self.ps[pb][:, 0:16], lhsT=ones[0:1, :], rhs=biasrow[0:1, O_WI:O_WI + 16],
                                        start=False, stop=True)
                    t.op("pe", mmw, reads=[r_w, r_xnT[gb], r_b, r_ones], writes=[self.psr[pb]])
                    t.op("dve", lambda e, wb=wb, pb=pb: e.tensor_copy(out=wis[wb][:, 0, :], in_=self.ps[pb][:, 0:16]),
                         reads=[self.psr[pb]], writes=[r_wis[wb]])
                    t.op("act", lambda e, wb=wb: e.activation(out=wis[wb][:, 1, :], in_=wis[wb][:, 0, :],
                                                              func=AF.Abs, scale=1.0 / 32.0),
                         reads=[r_wis[wb]], writes=[r_wis[wb]])
                    t.op("act", lambda e, wb=wb: e.activation(out=wis[wb][:, 2, :], in_=wis[wb][:, 0, :], func=AF.Sign),
                         reads=[r_wis[wb]], writes=[r_wis[wb]])
                    t.dma("sp", self.d_sgn[tq0:tq0 + 128, :], wis[wb][:, 2, :], reads=[r_wis[wb]])
                    for qg in range(2):
                        pb = 6 + qg
                        c0 = O_QI + qg * 512

                        def mmq(e, lhs=lhs, pb=pb, c0=c0):
                            for c in range(8):
                                e.matmul(self.ps[pb][:, :], lhsT=lhs(c), rhs=wsb[:, c, c0:c0 + 512],
                                         start=(c == 0), stop=False)
                            return e.matmul(self.ps[pb][:, :], lhsT=ones[0:1, :], rhs=biasrow[0:1, c0:c0 + 512],
                                            start=False, stop=True)
                        t.op("pe", mmq, reads=[r_w, r_xnT[gb], r_b, r_ones], writes=[self.psr[pb]])
                        t.op("dve", lambda e, wb=wb, pb=pb, qg=qg: e.tensor_tensor(
                            out=qis[wb][:, qg * 512:(qg + 1) * 512].rearrange("p (h d) -> p h d", h=8),
                            in0=self.ps[pb][:, :].rearrange("p (h d) -> p h d", h=8),
                            in1=wis[wb][:, 1, qg * 8:(qg + 1) * 8].unsqueeze(2).broadcast_to([128, 8, 64]),
                            op=ALU.mult), reads=[self.psr[pb], r_wis[wb]], writes=[r_qis[wb]])
                    pbf = self.ps[2 + (j % 3)].bitcast(BF16)

                    def tr(e, wb=wb, pbf=pbf):
                        for c in range(8):
                            ins = e.transpose(pbf[:, c * 128:(c + 1) * 128], qis[wb][:, c * 128:(c + 1) * 128],
                                              self.identb[:])
                        return ins
                    t.op("pe", tr, reads=[r_qis[wb], self.r_const], writes=[self.psr[2 + (j % 3)]])
                    t.op("act", lambda e, wb=wb, pbf=pbf: e.copy(
                        out=qit[wb][:], in_=pbf[:, :].rearrange("p (c n) -> p c n", c=8)),
                        reads=[self.psr[2 + (j % 3)]], writes=[r_qit[wb]])
                    for c in range(8):
                        t.dma("sp", self.d_qit[2 * c:2 * c + 2, :, tq0:tq0 + 128].rearrange("m d n -> (m d) n"),
                              qit[wb][:, c, :], reads=[r_qit[wb]])
                    for gg in range(4):
                        pb = 5 + gg % 3
                        c0 = O_GA + gg * 512

                        def mmg(e, lhs=lhs, pb=pb, c0=c0):
                            for c in range(8):
                                e.matmul(self.ps[pb][:, :], lhsT=lhs(c), rhs=wsb[:, c, c0:c0 + 512],
                                         start=(c == 0), stop=False)
                            return e.matmul(self.ps[pb][:, :], lhsT=ones[0:1, :], rhs=biasrow[0:1, c0:c0 + 512],
                                            start=False, stop=True)
                        t.op("pe", mmg, reads=[r_w, r_xnT[gb], r_b, r_ones], writes=[self.psr[pb]])
                        s = gi % 3
                        gi += 1
                        t.op("act", lambda e, s=s, pb=pb: e.activation(out=gst[s][:], in_=self.ps[pb][:, :],
                                                                        func=AF.Sigmoid),
                             reads=[self.psr[pb]], writes=[r_gst[s]])
                        t.dma("sp", self.d_gate[tq0:tq0 + 128, gg * 512:(gg + 1) * 512], gst[s][:],
                              reads=[r_gst[s]])
            self.barrier()

    def phase2(self):
        nc, t = self.nc, self.t
        STOP = int(os.environ.get('P2STOP', '99'))
        EXP = os.environ.get('EXP', '')
        S, nslot = self.S, self.nslot
        PS = self.ps
        PR = self.psr
        with ExitStack() as es:
            dg = self.sb(es, "dg", [128, 8, 128], BF16)
            dmask = self.sb(es, "dmask", [128, 128], F32)
            dummy = self.sb(es, "dummyc", [128, 8], F32)
            iota = self.sb(es, "iotac", [128, 512], F32)
            slopetab = self.sb(es, "slopetab", [2, 8, 128], F32)
            pidx1 = self.sb(es, "pidx1", [128, 1], F32)
            nlam = self.sb(es, "nlam", [128, 1], F32)
            gbc = self.sb(es, "gbc", [128, 128], F32)
            r_c2 = self.R("c2")
            for dst, nm in [(dg, "c_dg"), (dmask, "c_dmask"), (dummy, "c_dummy"), (iota, "c_iota"),
                            (slopetab, "c_slopetab"), (pidx1, "c_pidx1")]:
                t.dma("sp", dst[:], self.I(nm), writes=[r_c2])
            lv = self.sb(es, "lv", [1, 4, 64], F32)
            lsm = self.sb(es, "lsm", [1, 8], F32)
            onesf = self.sb(es, "onesf", [1, 128], F32)
            r_l = self.R("lam")
            t.dma("sp", lv[:], self.I("lamv").rearrange("(o a) d -> o a d", o=1), writes=[r_l])
            t.op("dve", lambda e: e.memset(onesf[:], 1.0), writes=[r_l])
            t.op("dve", lambda e: e.tensor_tensor(out=lv[:, 0, :], in0=lv[:, 0, :], in1=lv[:, 1, :], op=ALU.mult),
                 reads=[r_l], writes=[r_l])
            t.op("dve", lambda e: e.tensor_tensor(out=lv[:, 2, :], in0=lv[:, 2, :], in1=lv[:, 3, :], op=ALU.mult),
                 reads=[r_l], writes=[r_l])
            t.op("dve", lambda e: e.reduce_sum(out=lsm[:, 0:1], in_=lv[:, 0, :], axis=AX.X), reads=[r_l], writes=[r_l])
            t.op("dve", lambda e: e.reduce_sum(out=lsm[:, 1:2], in_=lv[:, 2, :], axis=AX.X), reads=[r_l], writes=[r_l])
            t.op("act", lambda e: e.activation(out=lsm[:, 2:4], in_=lsm[:, 0:2], func=AF.Exp), reads=[r_l], writes=[r_l])
            t.op("dve", lambda e: e.tensor_tensor(out=lsm[:, 4:5], in0=lsm[:, 3:4], in1=lsm[:, 2:3], op=ALU.subtract),
                 reads=[r_l], writes=[r_l])
            t.op("dve", lambda e: e.tensor_scalar(out=lsm[:, 5:6], in0=lsm[:, 4:5], scalar1=-LAM_INIT, scalar2=None,
                                                  op0=ALU.add), reads=[r_l], writes=[r_l])
            t.op("pe", lambda e: e.matmul(PS[7][:, 0:1], lhsT=onesf[0:1, :], rhs=lsm[0:1, 5:6], start=True, stop=True),
                 reads=[r_l], writes=[PR[7]])
            t.op("dve", lambda e: e.tensor_copy(out=nlam[:], in_=PS[7][:, 0:1]), reads=[PR[7]], writes=[r_c2])
            t.dma("sp", gbc[:], self.I("diff_norm_g").partition_broadcast(128), writes=[r_c2])
            t.op("dve", lambda e: e.tensor_scalar(out=gbc[:], in0=gbc[:], scalar1=1.0 - LAM_INIT, scalar2=None,
                                                  op0=ALU.mult), reads=[r_c2], writes=[r_c2])

            if STOP <= 0:
                self.barrier()
                return
            score = self.sb(es, "score", [128, S], F32)
            maskT = score.bitcast(BF16)
            r_sm = self.R("score")
            mask = self.sb(es, "mask", [128, S], BF16)
            r_mask = self.R("mask")
            qi_sb = self.sb(es, "qi_sb", [64, 16, 128], BF16)
            r_qi = self.R()
            sgn = self.sb(es, "sgn", [128, 16], F32)
            dsg = self.sb(es, "dsg", [128, 16, 128], BF16)
            r_dsg = self.R()
            kit_sb = [self.sb(es, f"kit{i}", [64, 1024], BF16) for i in range(2)]
            r_kit = [self.R() for _ in range(2)]
            rbuf = [self.sb(es, f"rbuf{i}", [128, 512], BF16) for i in range(4)]
            r_rbuf = [self.R() for _ in range(4)]
            sv = self.sb(es, "sv", [128, 48], F32)
            svi = self.sb(es, "svi", [128, 4], I32)
            r_sv = self.R()
            am = self.sb(es, "am", [128, 40], F32)
            r_am = self.R()
            tmp512 = self.sb(es, "tmp512", [128, 512], F32)
            r_tmp = self.R()
            ab = self.sb(es, "ab", [128, 2], BF16)
            qb_sb = self.sb(es, "qb_sb", [128, 8, 128], BF16)
            r_qb = self.R()
            qbaug = self.sb(es, "qbaug", [128, 8, 128], BF16)
            r_qbaug = self.R()
            qa_sb = self.sb(es, "qa_sb", [69, 16, 128], BF16)
            r_qa = self.R()
            NKB = 3
            kbuf = [self.sb(es, f"kbuf{i}", [128, 4, 1024], BF16) for i in range(NKB)]
            r_kbuf = [self.R() for _ in range(NKB)]
            vbuf = [self.sb(es, f"vbuf{i}", [128, 8, 4, 132], BF16) for i in range(NKB)]
            r_vbuf = [self.R() for _ in range(NKB)]
            kaug_sb = [self.sb(es, f"kaug{i}", [128, 1024], BF16) for i in range(NKB)]
            r_kaug = [self.R() for _ in range(NKB)]
            pbuf = [self.sb(es, f"pbuf{i}", [128, 4, 128], BF16) for i in range(4)]
            r_pbuf = [self.R() for _ in range(4)]
            ysb = [self.sb(es, f"ysb{i}", [128, D], BF16) for i in range(2)]
            r_ysb = [self.R() for _ in range(2)]
            junk = self.sb(es, "junk128", [128, 128], F32)
            r_junk = self.R()
            sv2 = self.sb(es, "sv2", [128, 16], F32)
            oraw = self.sb(es, "oraw", [128, D], F32)
            r_oraw = self.R()
            ssq = self.sb(es, "ssq", [128, 16], F32)
            r_ssq = self.R()
            r_sv2 = [self.R() for _ in range(2)]
            for i in range(NKB):
                t.op("pool", lambda e, i=i: e.memset(kaug_sb[i][:], 0.0), writes=[r_kaug[i]])
            t.op("pool", lambda e: e.memset(qbaug[:], 0.0), writes=[r_qbaug])
            kcnt = [0]
            pcnt = [0]
            scnt = [0]
            kitc = [0]
            rcnt = [0]

            for j in range(nslot):
                tq = 8 * j + 7
                NT = tq + 1
                N = NT * 128
                q0 = j * 128
                ngrp = NT // 4
                t.dma("sp", qi_sb[:], self.d_qit[:, :, q0:q0 + 128].rearrange("h d n -> d h n"), writes=[r_qi])
                t.dma("sp", sgn[:], self.d_sgn[q0:q0 + 128, :], writes=[r_dsg])
                for h in range(16):
                    en = "dve" if (h % 2 == 0 or os.environ.get("NOPOOL")) else "pool"
                    t.op(en, lambda e, h=h: e.tensor_scalar(out=dsg[:, h, :], in0=self.identb[:], scalar1=sgn[:, h:h + 1],
                                                            scalar2=None, op0=ALU.mult),
                         reads=[r_dsg, self.r_const], writes=[r_dsg])
                for kg in range(ngrp):
                    if kg % 2 == 0:
                        kb = kitc[0] % 2
                        kitc[0] += 1
                        w = min(1024, N - kg * 512)
                        t.dma("sp", kit_sb[kb][:, 0:w], self.d_kit[:, kg * 512:kg * 512 + w], writes=[r_kit[kb]])
                    koff = (kg % 2) * 512

                    def logits(h, kb=kb, koff=koff):
                        lb = 4 + h % 3
                        t.op("pe", lambda e: e.matmul(PS[lb][:, :], lhsT=qi_sb[:, h, :], rhs=kit_sb[kb][:, koff:koff + 512],
                                                      start=True, stop=True),
                             reads=[r_qi, r_kit[kb]], writes=[PR[lb]])

                    def relu(h):
                        lb = 4 + h % 3
                        rb = rcnt[0] % 4
                        rcnt[0] += 1
                        if h % 2 == 0 and "b" not in EXP:
                            t.op("act", lambda e: e.activation(out=rbuf[rb][:], in_=PS[lb][:, :], func=AF.Relu),
                                 reads=[PR[lb]], writes=[r_rbuf[rb]])
                        else:
                            t.op("dve", lambda e: e.tensor_scalar(out=rbuf[rb][:], in0=PS[lb][:, :], scalar1=0.0,
                                                                  scalar2=None, op0=ALU.max),
                                 reads=[PR[lb]], writes=[r_rbuf[rb]])
                        return rb

                    def hsum(h, rb):
                        t.op("pe", lambda e: e.matmul(PS[7][:, :], lhsT=dsg[:, h, :], rhs=rbuf[rb][:],
                                                      start=(h == 0), stop=(h == 15)),
                             reads=[r_dsg, r_rbuf[rb]], writes=[PR[7]])
                    if "e" in EXP:
                        continue
                    logits(0)
                    logits(1)
                    for h in range(16):
                        if h + 2 < 16:
                            logits(h + 2)
                        rb = relu(h)
                        if "d" not in EXP:
                            hsum(h, rb)
                    if "d" in EXP:
                        continue
                    if "f" in EXP:
                        continue
                    sl = slice(kg * 512, (kg + 1) * 512)
                    if "g" in EXP:
                        pass
                    elif "a" in EXP:
                        t.op("dve", lambda e, kg=kg: e.reduce_max(out=am[:, kg:kg + 1], in_=PS[7][:, :], axis=AX.X),
                             reads=[PR[7]], writes=[r_am])
                    else:
                        t.op("dve", lambda e, kg=kg: e.tensor_reduce(out=am[:, kg:kg + 1], in_=PS[7][:, :], axis=AX.X,
                                                                     op=ALU.max, apply_absolute_value=True),
                             reads=[PR[7]], writes=[r_am])
                    if "h" not in EXP:
                        if "I" not in EXP:
                            t.op("dve", lambda e, sl=sl: e.tensor_copy(out=score[:, sl], in_=PS[7][:, :]),
                                 reads=[PR[7]], writes=[r_sm])
                        else:
                            t.op("act", lambda e, sl=sl: e.activation(out=score[:, sl], in_=PS[7][:, :], func=AF.Identity),
                                 reads=[PR[7]], writes=[r_sm])
                    for tl in range(4):
                        if "c" in EXP:
                            break
                        tt = kg * 4 + tl
                        ts_ = slice(tt * 128, (tt + 1) * 128)
                        if tt < 7:
                            t.op("dve", lambda e, ts_=ts_, tt=tt: e.tensor_scalar(
                                out=score[:, ts_], in0=score[:, ts_], scalar1=dummy[:, tt:tt + 1], scalar2=None,
                                op0=ALU.add), reads=[r_sm, r_c2], writes=[r_sm])
                        if tt == NT - 1:
                            t.op("dve", lambda e, ts_=ts_: e.tensor_tensor(out=score[:, ts_], in0=score[:, ts_],
                                                                           in1=dmask[:], op=ALU.add),
                                 reads=[r_sm, r_c2], writes=[r_sm])
                if STOP <= 1:
                    continue
                LO, W0, MID, CNT, PRED, AMX = 0, 1, 2, 3, 4, 7
                col = lambda i: sv[:, i:i + 1]
                t.op("dve", lambda e: e.reduce_max(out=col(AMX), in_=am[:, 0:ngrp], axis=AX.X), reads=[r_am], writes=[r_sv])
                t.op("dve", lambda e: e.tensor_scalar(out=col(LO), in0=col(AMX), scalar1=1.0, scalar2=-1.0, op0=ALU.add, op1=ALU.mult),
                     reads=[r_sv], writes=[r_sv])
                t.op("dve", lambda e: e.tensor_scalar(out=col(W0), in0=col(AMX), scalar1=1.0, scalar2=2.0, op0=ALU.add, op1=ALU.mult),
                     reads=[r_sv], writes=[r_sv])
                bit = [0]

                def bisect(nit):
                    for it in range(nit):
                        f = 0.5 ** (bit[0] + 1)
                        bit[0] += 1
                        t.op("dve", lambda e, f=f: e.scalar_tensor_tensor(out=col(MID), in0=col(W0), scalar=f, in1=col(LO),
                                                                          op0=ALU.mult, op1=ALU.add), reads=[r_sv], writes=[r_sv])
                        t.op("dve", lambda e: e.tensor_scalar(out=mask[:, 0:N], in0=score[:, 0:N], scalar1=col(MID), scalar2=None,
                                                              op0=ALU.is_gt, op1=ALU.add, accum_out=col(CNT)),
                             reads=[r_sm, r_sv], writes=[r_mask, r_sv])
                        t.op("dve", lambda e, f=f: e.tensor_scalar(out=col(PRED), in0=col(CNT), scalar1=float(TOPK) - 0.5, scalar2=f,
                                                                   op0=ALU.is_gt, op1=ALU.mult), reads=[r_sv], writes=[r_sv])
                        t.op("dve", lambda e: e.scalar_tensor_tensor(out=col(LO), in0=col(W0), scalar=col(PRED), in1=col(LO),
                                                                     op0=ALU.mult, op1=ALU.add), reads=[r_sv], writes=[r_sv])

                t.dma("sp", qb_sb[:], self.d_qbt[:, :, q0:q0 + 128].rearrange("h d n -> d h n"), writes=[r_qb])
                t.dma("sp", qa_sb[0:64, :, :], self.d_qat[:, :, q0:q0 + 128].rearrange("m d n -> d m n"), writes=[r_qa])
                for m_ in range(2):
                    t.dma("sp", qa_sb[64:69, :, :].rearrange("r (h m) n -> r h m n", m=2)[:, :, m_, :],
                          self.I("c_qaug")[2:7, j, :, :], writes=[r_qa])

                def attn_group(kind, gi, ab):
                    b0, b1_ = (2, 3) if ab == 0 else (4, 5)
                    accs = [PS[b0][:, 0:129], PS[b0][:, 129:258], PS[b0][:, 258:387], PS[b1_][:, 0:129]]
                    first_in_bank = [True, False, False, True]
                    RA = [PR[b0], PR[b1_]]
                    pendq = []
                    for tg in range(NT // 8):
                        kb = kcnt[0] % NKB
                        kcnt[0] += 1
                        ksl = slice(tg * 1024, (tg + 1) * 1024)
                        if kind == "dsa":
                            t.dma("sp", kbuf[kb][:, :, :], self.d_kbt[4 * gi:4 * gi + 4, :, ksl].rearrange("h d n -> d h n"),
                                  writes=[r_kbuf[kb]])
                            t.dma("sp", kaug_sb[kb][0:7, :], self.I("c_kaug")[:, ksl], writes=[r_kaug[kb]])
                            t.dma("sp", vbuf[kb][:, :, :, :].rearrange("p t h e -> p t (h e)"),
                                  self.d_vb[ksl, 4 * gi:4 * gi + 4, :].rearrange("(t p) h e -> p t (h e)", p=128),
                                  writes=[r_vbuf[kb]])
                        else:
                            t.dma("sp", kbuf[kb][0:64, :, :], self.d_kat[4 * gi:4 * gi + 4, :, ksl].rearrange("m d n -> d m n"),
                                  writes=[r_kbuf[kb]])
                            t.dma("sp", kbuf[kb][64:69, :, :], self.I("c_kaug")[2:7, ksl].unsqueeze(1).broadcast_to([5, 4, 1024]),
                                  writes=[r_kbuf[kb]])
                            t.dma("sp", vbuf[kb][:, :, 0:2, :].rearrange("p t h e -> p t (h e)"),
                                  self.d_va[ksl, 2 * gi:2 * gi + 2, :].rearrange("(t p) h e -> p t (h e)", p=128),
                                  writes=[r_vbuf[kb]])
                        for tl in range(8):
                            tt = tg * 8 + tl
                            sb_ = (0, 1, 6)[scnt[0] % 3]
                            scnt[0] += 1
                            diag = (tt == NT - 1)

                            def qk(e, kb=kb, tl=tl, sb_=sb_, diag=diag):
                                tsl = slice(tl * 128, (tl + 1) * 128)
                                for i in range(4):
                                    reg = PS[sb_][:, i * 128:(i + 1) * 128]
                                    if kind == "dsa":
                                        ins = e.matmul(reg, lhsT=kbuf[kb][:, i, tsl], rhs=qb_sb[:, 4 * gi + i, :],
                                                       start=(i == 0), stop=False, skip_group_check=True)
                                        hh = 4 * gi + i
                                    else:
                                        ins = e.matmul(reg, lhsT=kbuf[kb][0:69, i, tsl], rhs=qa_sb[0:69, 4 * gi + i, :],
                                                       start=True, stop=not diag)
                                        hh = 2 * gi + i // 2
                                    if diag:
                                        ins = e.matmul(reg, lhsT=self.identb[:], rhs=dg[:, hh, :], start=False,
                                                       stop=(kind != "dsa"), skip_group_check=(kind == "dsa"))
                                if kind == "dsa":
                                    ins = e.matmul(PS[sb_][:, :], lhsT=kaug_sb[kb][:, tsl],
                                                   rhs=qbaug[:, 4 * gi:4 * gi + 4, :].rearrange("r h n -> r (h n)"),
                                                   start=False, stop=True, skip_group_check=True)
                                return ins
                            rd = [r_kbuf[kb], self.r_const, r_c2] + ([r_kaug[kb], r_qbaug, r_qb] if kind == "dsa" else [r_qa])
                            t.op("pe", qk, reads=rd, writes=[PR[sb_]])
                            pb_ = pcnt[0] % 4
                            pcnt[0] += 1
                            t.op("act", lambda e, sb_=sb_, pb_=pb_: e.activation(
                                out=pbuf[pb_][:].rearrange("p h n -> p (h n)"), in_=PS[sb_][:, :], func=AF.Exp),
                                reads=[PR[sb_]], writes=[r_pbuf[pb_]])
                            if kind == "dsa":
                                t.op("dve", lambda e, pb_=pb_, tt=tt: e.scalar_tensor_tensor(
                                    out=pbuf[pb_][:], in0=pbuf[pb_][:], scalar=1e30,
                                    in1=maskT[:, tt * 128:(tt + 1) * 128].unsqueeze(1).broadcast_to([128, 4, 128]),
                                    op0=ALU.min, op1=ALU.mult), reads=[r_pbuf[pb_], r_sm], writes=[r_pbuf[pb_]])

                            def pv(e, kb=kb, tl=tl, pb_=pb_, tt=tt):
                                for i in range(4):
                                    vh = i if kind == "dsa" else i // 2
                                    ins = e.matmul(accs[i], lhsT=pbuf[pb_][:, i, :], rhs=vbuf[kb][:, tl, vh, 0:129],
                                                   start=(tt == 0 and first_in_bank[i]), stop=(tt == NT - 1),
                                                   skip_group_check=True)
                                return ins
                            pendq.append(lambda pv=pv, kb=kb, pb_=pb_: t.op(
                                "pe", pv, reads=[r_pbuf[pb_], r_vbuf[kb]], writes=RA))
                            if len(pendq) > 2:
                                pendq.pop(0)()
                    while pendq:
                        pendq.pop(0)()
                    s0 = 8 * ab
                    if kind == "dsa":
                        for i in range(4):
                            hh = 4 * gi + i
                            t.op("dve", lambda e, i=i: e.reciprocal(out=sv2[:, s0 + i:s0 + i + 1], in_=accs[i][:, 128:129]),
                                 reads=RA, writes=[r_sv2[ab]])
                            t.op("dve", lambda e, i=i, hh=hh: e.tensor_scalar(
                                out=ysb[1][:, hh * 128:(hh + 1) * 128], in0=accs[i][:, 0:128], scalar1=sv2[:, s0 + i:s0 + i + 1],
                                scalar2=None, op0=ALU.mult), reads=RA + [r_sv2[ab]], writes=[r_ysb[1]])
                    else:
                        for hl in range(2):
                            hh = 2 * gi + hl
                            a0, a1 = accs[2 * hl], accs[2 * hl + 1]
                            c0 = s0 + 4 * hl
                            of_ = oraw[:, hh * 128:(hh + 1) * 128]
                            r_of_ = r_oraw
                            t.op("dve", lambda e, a0=a0, c0=c0: e.reciprocal(out=sv2[:, c0:c0 + 1], in_=a0[:, 128:129]),
                                 reads=RA, writes=[r_sv2[ab]])
                            t.op("dve", lambda e, a1=a1, c0=c0: e.reciprocal(out=sv2[:, c0 + 1:c0 + 2], in_=a1[:, 128:129]),
                                 reads=RA, writes=[r_sv2[ab]])
                            t.op("dve", lambda e, c0=c0: e.tensor_tensor(out=sv2[:, c0 + 1:c0 + 2], in0=sv2[:, c0 + 1:c0 + 2],
                                                                         in1=nlam[:], op=ALU.mult),
                                 reads=[r_sv2[ab], r_c2], writes=[r_sv2[ab]])
                            t.op("dve", lambda e, a0=a0, c0=c0, of_=of_: e.tensor_scalar(
                                out=of_, in0=a0[:, 0:128], scalar1=sv2[:, c0:c0 + 1], scalar2=None, op0=ALU.mult),
                                reads=RA + [r_sv2[ab]], writes=[r_of_])
                            t.op("dve", lambda e, a1=a1, c0=c0, of_=of_: e.scalar_tensor_tensor(
                                out=of_, in0=a1[:, 0:128], scalar=sv2[:, c0 + 1:c0 + 2], in1=of_,
                                op0=ALU.mult, op1=ALU.add), reads=RA + [r_sv2[ab], r_of_], writes=[r_of_])

                nb_per = NBISECT // 4
                for gi in range(4):
                    bisect(nb_per)
                    attn_group("diff", gi, gi % 2)
                bisect(NBISECT - 4 * nb_per)
                for hh in range(8):
                    t.op("act", lambda e, hh=hh: e.activation(out=junk[:], in_=oraw[:, hh * 128:(hh + 1) * 128], func=AF.Square,
                                                              accum_out=ssq[:, hh:hh + 1]),
                         reads=[r_oraw], writes=[r_ssq, r_junk])
                t.op("dve", lambda e: e.tensor_scalar(out=ssq[:, 0:8], in0=ssq[:, 0:8], scalar1=1.0 / 128.0, scalar2=LN_EPS,
                                                      op0=ALU.mult, op1=ALU.add), reads=[r_ssq], writes=[r_ssq])
                t.op("act", lambda e: e.activation(out=ssq[:, 0:8], in_=ssq[:, 0:8], func=AF.Sqrt), reads=[r_ssq], writes=[r_ssq])
                t.op("dve", lambda e: e.reciprocal(out=ssq[:, 8:16], in_=ssq[:, 0:8]), reads=[r_ssq], writes=[r_ssq])
                for hh in range(8):
                    t.op("dve", lambda e, hh=hh: e.scalar_tensor_tensor(
                        out=ysb[0][:, hh * 128:(hh + 1) * 128], in0=oraw[:, hh * 128:(hh + 1) * 128], scalar=ssq[:, 8 + hh:9 + hh],
                        in1=gbc[:], op0=ALU.mult, op1=ALU.mult), reads=[r_oraw, r_ssq, r_c2], writes=[r_ysb[0]])
                t.dma("sp", self.d_ya[q0:q0 + 128, :], ysb[0][:], reads=[r_ysb[0]])
                t.op("dve", lambda e: e.tensor_scalar(out=mask[:, 0:N], in0=score[:, 0:N], scalar1=col(LO), scalar2=None,
                                                      op0=ALU.is_gt), reads=[r_sm, r_sv], writes=[r_mask])
                for kg in range(ngrp):
                    t.op("dve", lambda e, kg=kg: e.scalar_tensor_tensor(
                        out=tmp512[:], in0=iota[:], scalar=float(kg * 512), in1=mask[:, kg * 512:(kg + 1) * 512],
                        op0=ALU.add, op1=ALU.mult), reads=[r_mask, r_c2], writes=[r_tmp])
                    t.op("dve", lambda e, kg=kg: e.reduce_max(out=am[:, kg:kg + 1], in_=tmp512[:], axis=AX.X),
                         reads=[r_tmp], writes=[r_am])
                MP, DD, AF_, BF_ = 8, 9, 10, 11
                t.op("dve", lambda e: e.reduce_max(out=col(MP), in_=am[:, 0:ngrp], axis=AX.X), reads=[r_am], writes=[r_sv])
                t.op("dve", lambda e: e.tensor_scalar(out=col(DD), in0=col(MP), scalar1=pidx1[:, 0:1], scalar2=float(128 * tq),
                                                      op0=ALU.subtract, op1=ALU.subtract), reads=[r_sv, r_c2], writes=[r_sv])
                t.op("act", lambda e: e.activation(out=col(DD), in_=col(DD), func=AF.Abs), reads=[r_sv], writes=[r_sv])
                t.op("dve", lambda e: e.tensor_copy(out=svi[:, 0:1], in_=col(DD)), reads=[r_sv], writes=[r_sv])
                t.op("dve", lambda e: e.tensor_single_scalar(out=svi[:, 1:2], in_=svi[:, 0:1], scalar=7,
                                                             op=ALU.arith_shift_right), reads=[r_sv], writes=[r_sv])
                t.op("dve", lambda e: e.tensor_copy(out=col(AF_), in_=svi[:, 1:2]), reads=[r_sv], writes=[r_sv])
                t.op("dve", lambda e: e.scalar_tensor_tensor(out=col(BF_), in0=col(AF_), scalar=-128.0, in1=col(DD),
                                                             op0=ALU.mult, op1=ALU.add), reads=[r_sv], writes=[r_sv])
                t.op("dve", lambda e: e.tensor_copy(out=ab[:, 0:2], in_=sv[:, AF_:AF_ + 2]), reads=[r_sv], writes=[r_sv])
                t.dma("sp", qbaug[2:7, :, :], self.I("c_qaug")[2:7, j, :, :], writes=[r_qbaug])
                t.op("pe", lambda e: e.matmul(PS[7][0:2, 0:128], lhsT=ab[:, 0:2], rhs=self.identb[:], start=True, stop=True),
                     reads=[r_sv, self.r_const], writes=[PR[7]])
                t.op("dve", lambda e: e.tensor_tensor(out=qbaug[0:2, :, :],
                                                      in0=PS[7][0:2, 0:128].unsqueeze(1).broadcast_to([2, 8, 128]),
                                                      in1=slopetab[:], op=ALU.mult),
                     reads=[PR[7], r_c2], writes=[r_qbaug])
                pbf = PS[6].bitcast(BF16)
                for g4 in range(ngrp):
                    def tr(e, g4=g4):
                        for tl in range(4):
                            tt = g4 * 4 + tl
                            ins = e.transpose(pbf[:, tl * 128:(tl + 1) * 128], mask[:, tt * 128:(tt + 1) * 128], self.identb[:])
                        return ins
                    t.op("pe", tr, reads=[r_mask, self.r_const], writes=[PR[6]])
                    if g4 % 2 == 0:
                        t.op("act", lambda e, g4=g4: e.copy(out=maskT[:, g4 * 512:(g4 + 1) * 512], in_=pbf[:, 0:512]),
                             reads=[PR[6]], writes=[r_sm])
                    else:
                        t.op("dve", lambda e, g4=g4: e.tensor_copy(out=maskT[:, g4 * 512:(g4 + 1) * 512], in_=pbf[:, 0:512]),
                             reads=[PR[6]], writes=[r_sm])
                for gi in range(2):
                    attn_group("dsa", gi, gi % 2)
                t.dma("sp", self.d_yb[q0:q0 + 128, :], ysb[1][:], reads=[r_ysb[1]])
            self.barrier()

    def load_w_bf16(self, es, name, src_ap, r):
        wsb = self.sb(es, name, [128, 8, 1024], BF16)
        v = src_ap.rearrange("(c p) n -> p c n", p=128)
        for a in range(0, 1024, 512):
            self.t.dma("pool", wsb[:, :, a:a + 512], v[:, :, a:a + 512], writes=[r])
        return wsb

    def ln_stats(self, xin, r_x, stats, mv, r_st):
        t = self.t
        for hh in range(2):
            t.op("dve", lambda e, hh=hh: e.bn_stats(out=stats[:, hh, :], in_=xin[:, hh * 512:(hh + 1) * 512]),
                 reads=[r_x], writes=[r_st])
        t.op("dve", lambda e: e.bn_aggr(out=mv[:, 0:2], in_=stats[:].rearrange("p a b -> p (a b)")),
             reads=[r_st], writes=[r_st])
        t.op("dve", lambda e: e.tensor_scalar(out=mv[:, 2:3], in0=mv[:, 1:2], scalar1=LN_EPS, scalar2=None,
                                              op0=ALU.add), reads=[r_st], writes=[r_st])
        t.op("act", lambda e: e.activation(out=mv[:, 2:3], in_=mv[:, 2:3], func=AF.Sqrt),
             reads=[r_st], writes=[r_st])
        t.op("dve", lambda e: e.reciprocal(out=mv[:, 3:4], in_=mv[:, 2:3]), reads=[r_st], writes=[r_st])

    def phase3(self):
        nc, t = self.nc, self.t
        PS, PR = self.ps, self.psr
        nslot = self.nslot
        with ExitStack() as es:
            r_w = self.R()
            wa = self.load_w_bf16(es, "wa", self.I("w_branch_a"), r_w)
            wb = self.load_w_bf16(es, "wb", self.I("w_branch_b"), r_w)
            wo = self.load_w_bf16(es, "wo", self.I("w_out"), r_w)
            g1 = self.sb(es, "g1bc", [128, D], F32)
            b1 = self.sb(es, "b1bc", [128, D], F32)
            r_c = self.R()
            self.ga_bc = self.sb(es, "ga_bc", [128, D], F32)
            t.dma("sp", self.ga_bc[:], self.d_mod[2 * D:3 * D].partition_broadcast(128), reads=[self.r_dmod], writes=[self.r_mod])
            t.dma("sp", g1[:], self.I("ln1_g").partition_broadcast(128), writes=[r_c])
            t.dma("sp", b1[:], self.I("ln1_b").partition_broadcast(128), writes=[r_c])
            yab = [self.sb(es, f"yab{i}", [128, 2, D], BF16) for i in range(2)]
            r_yab = [self.R() for _ in range(2)]
            yT = [self.sb(es, f"yT{i}", [128, 2, 8, 128], BF16) for i in range(2)]
            r_yT = [self.R() for _ in range(2)]
            gate = [self.sb(es, f"gate{i}", [128, 2 * D], F32) for i in range(2)]
            r_gate = [self.R() for _ in range(2)]
            xt = [self.sb(es, f"x3_{i}", [128, D], F32) for i in range(2)]
            r_xt = [self.R() for _ in range(2)]
            m1 = self.sb(es, "m1", [128, D], F32)
            m2 = self.sb(es, "m2", [128, D], F32)
            mg = self.sb(es, "mg", [128, D], BF16)
            mgT = self.sb(es, "mgT", [128, 8, 128], BF16)
            r_m = self.R()
            r_mg = self.R()
            r_mgT = self.R()
            xnew = self.sb(es, "xnew", [128, D], F32)
            r_xn = self.R()
            x1 = [self.sb(es, f"x1_{i}", [128, D], F32) for i in range(2)]
            r_x1 = [self.R() for _ in range(2)]
            stats = self.sb(es, "st3", [128, 2, 6], F32)
            mv = self.sb(es, "mv3", [128, 4], F32)
            r_st = self.R()
            for j in range(nslot):
                b = j % 2
                q0 = j * 128
                tok0 = (8 * j + 7) * 128
                t.dma("sp", yab[b][:, 0, :], self.d_ya[q0:q0 + 128, :], writes=[r_yab[b]])
                t.dma("sp", yab[b][:, 1, :], self.d_yb[q0:q0 + 128, :], writes=[r_yab[b]])
                t.dma("sp", gate[b][:], self.d_gate[q0:q0 + 128, :], writes=[r_gate[b]])
                t.dma("sp", xt[b][:], self.I("x")[tok0:tok0 + 128, :], writes=[r_xt[b]])
                for br in range(2):
                    pbf = PS[br].bitcast(BF16)

                    def tr(e, br=br, b=b, pbf=pbf):
                        for c in range(8):
                            ins = e.transpose(pbf[:, c * 128:(c + 1) * 128], yab[b][:, br, c * 128:(c + 1) * 128], self.identb[:])
                        return ins
                    t.op("pe", tr, reads=[r_yab[b], self.r_const], writes=[PR[br]])
                    t.op("act", lambda e, br=br, b=b, pbf=pbf: e.copy(
                        out=yT[b][:, br, :, :], in_=pbf[:, :].rearrange("p (c n) -> p c n", c=8)),
                        reads=[PR[br]], writes=[r_yT[b]])
                for cg in range(2):
                    csl = slice(cg * 512, (cg + 1) * 512)
                    for br, w_ in ((0, wa), (1, wb)):
                        pb = 2 + br

                        def mm(e, br=br, w_=w_, pb=pb, b=b, csl=csl):
                            for c in range(8):
                                ins = e.matmul(PS[pb][:, :], lhsT=yT[b][:, br, c, :], rhs=w_[:, c, csl],
                                               start=(c == 0), stop=(c == 7))
                            return ins
                        t.op("pe", mm, reads=[r_yT[b], r_w], writes=[PR[pb]])
                    t.op("dve", lambda e, b=b, csl=csl, cg=cg: e.tensor_tensor(
                        out=m1[:, csl], in0=PS[2][:, :], in1=gate[b][:, cg * 512:(cg + 1) * 512], op=ALU.mult),
                        reads=[PR[2], r_gate[b]], writes=[r_m])
                    t.op("dve", lambda e, b=b, csl=csl, cg=cg: e.tensor_tensor(
                        out=m2[:, csl], in0=PS[3][:, :], in1=gate[b][:, D + cg * 512:D + (cg + 1) * 512], op=ALU.mult),
                        reads=[PR[3], r_gate[b]], writes=[r_m])
                    t.op("dve", lambda e, csl=csl: e.tensor_tensor(out=mg[:, csl], in0=m1[:, csl], in1=m2[:, csl], op=ALU.add),
                         reads=[r_m], writes=[r_mg])
                pbf = PS[4].bitcast(BF16)

                def tr2(e, pbf=pbf):
                    for c in range(8):
                        ins = e.transpose(pbf[:, c * 128:(c + 1) * 128], mg[:, c * 128:(c + 1) * 128], self.identb[:])
                    return ins
                t.op("pe", tr2, reads=[r_mg, self.r_const], writes=[PR[4]])
                t.op("act", lambda e, pbf=pbf: e.copy(out=mgT[:], in_=pbf[:, :].rearrange("p (c n) -> p c n", c=8)),
                     reads=[PR[4]], writes=[r_mgT])
                for cg in range(2):
                    csl = slice(cg * 512, (cg + 1) * 512)
                    pb = 5 + cg

                    def mm3(e, pb=pb, csl=csl):
                        for c in range(8):
                            ins = e.matmul(PS[pb][:, :], lhsT=mgT[:, c, :], rhs=wo[:, c, csl], start=(c == 0), stop=(c == 7))
                        return ins
                    t.op("pe", mm3, reads=[r_mgT, r_w], writes=[PR[pb]])
                    t.op("dve", lambda e, pb=pb, csl=csl: e.tensor_tensor(out=m1[:, csl], in0=PS[pb][:, :],
                                                                          in1=self.ga_bc[:, csl], op=ALU.mult),
                         reads=[PR[pb], self.r_mod], writes=[r_m])
                    t.op("dve", lambda e, b=b, csl=csl: e.scalar_tensor_tensor(
                        out=xnew[:, csl], in0=xt[b][:, csl], scalar=ALPHA, in1=m1[:, csl], op0=ALU.mult, op1=ALU.add),
                        reads=[r_xt[b], r_m], writes=[r_xn])
                self.ln_stats(xnew, r_xn, stats, mv, r_st)
                t.op("dve", lambda e, b=b: e.tensor_scalar(out=x1[b][:], in0=xnew[:], scalar1=mv[:, 0:1], scalar2=mv[:, 3:4],
                                                           op0=ALU.subtract, op1=ALU.mult),
                     reads=[r_xn, r_st], writes=[r_x1[b]])
                t.op("pool", lambda e, b=b: e.tensor_tensor(out=x1[b][:], in0=x1[b][:], in1=g1[:], op=ALU.mult),
                     reads=[r_x1[b], r_c], writes=[r_x1[b]])
                t.op("pool", lambda e, b=b: e.tensor_tensor(out=x1[b][:], in0=x1[b][:], in1=b1[:], op=ALU.add),
                     reads=[r_x1[b], r_c], writes=[r_x1[b]])
                t.dma("sp", self.d_x1[q0:q0 + 128, :], x1[b][:], reads=[r_x1[b]])
            self.barrier()

    def phase4(self):
        nc, t = self.nc, self.t
        PS, PR = self.ps, self.psr
        NQ = self.NQ
        HT = min(1024, NQ)
        nhalf = NQ // HT
        TG = min(512, HT)
        ntg = HT // TG
        ntt = HT // 128
        with ExitStack() as es:
            scf = self.sb(es, "scf_bc", [128, D], F32)
            shf = self.sb(es, "shf_bc", [128, D], F32)
            g2 = self.sb(es, "g2bc", [128, D], F32)
            b2l = self.sb(es, "b2lbc", [128, D], F32)
            wr = self.sb(es, "wr", [128, 8, NEXP], F32)
            brr = self.sb(es, "brr", [1, NEXP], F32)
            onesf = self.sb(es, "onesf4", [1, 128], F32)
            b2w = self.sb(es, "b2w", [NEXP, D], F32)
            b1raw = self.sb(es, "b1raw", [NEXP, 2 * DFF], F32)
            b1g = self.sb(es, "b1g", [128, 8, NEXP], F32)
            b1l = self.sb(es, "b1l", [128, 8, NEXP], F32)
            r_c = self.R()
            self.gf_bc = self.sb(es, "gf_bc", [128, D], F32)
            t.dma("sp", self.gf_bc[:], self.d_mod[5 * D:6 * D].partition_broadcast(128), reads=[self.r_dmod], writes=[self.r_mod])
            t.dma("sp", scf[:], self.d_mod[4 * D:5 * D].partition_broadcast(128), writes=[r_c])
            t.dma("sp", shf[:], self.d_mod[3 * D:4 * D].partition_broadcast(128), writes=[r_c])
            t.dma("sp", g2[:], self.I("ln2_g").partition_broadcast(128), writes=[r_c])
            t.dma("sp", b2l[:], self.I("ln2_b").partition_broadcast(128), writes=[r_c])
            t.dma("sp", wr[:], self.I("w_router").rearrange("(c p) n -> p c n", p=128), writes=[r_c])
            t.dma("sp", brr[:], self.I("b_router").rearrange("(o n) -> o n", o=1), writes=[r_c])
            t.dma("sp", b2w[:], self.I("b_e2"), writes=[r_c])
            t.dma("sp", b1raw[:], self.I("b_e1"), writes=[r_c])
            t.op("dve", lambda e: e.memset(onesf[:], 1.0), writes=[r_c])
            t.op("dve", lambda e: e.tensor_scalar(out=scf[:], in0=scf[:], scalar1=1.0, scalar2=None, op0=ALU.add),
                 reads=[r_c], writes=[r_c])
            b1v = b1raw[:].rearrange("e (p f two) -> e p f two", p=8, two=2)
            for p in range(8):
                for two, dst in ((0, b1g), (1, b1l)):
                    t.op("pe", lambda e, p=p, two=two: e.transpose(PS[7][:, 0:NEXP], b1v[:, p, :, two], self.identf[0:NEXP, 0:NEXP]),
                         reads=[r_c, self.r_const], writes=[PR[7]])
                    t.op("dve", lambda e, p=p, dst=dst, two=two: e.tensor_scalar(
                        out=dst[:, p, :], in0=PS[7][:, 0:NEXP], scalar1=float(two), scalar2=None, op0=ALU.add),
                        reads=[PR[7]], writes=[r_c])
            vT = self.sb(es, "vT", [128, 8, HT], BF16)
            r_vT = self.R()
            yacc = self.sb(es, "yacc", [128, ntt, D], F32)
            r_y = self.R()
            gate = self.sb(es, "gate4", [128, ntt, NEXP], F32)
            r_g = self.R()
            aT = [self.sb(es, f"aT{i}", [128, 8, HT], BF16) for i in range(2)]
            r_aT = [self.R() for _ in range(2)]
            w1p = [self.sb(es, f"w1p{i}", [128, 8, 256], BF16) for i in range(3)]
            r_w1 = [self.R() for _ in range(3)]
            w2e = [self.sb(es, f"w2e{i}", [128, 8, D], BF16) for i in range(2)]
            r_w2 = [self.R() for _ in range(2)]
            glu = [self.sb(es, f"glu{i}", [128, TG], F32) for i in range(3)]
            sig = [self.sb(es, f"sig{i}", [128, TG], F32) for i in range(3)]
            lin = [self.sb(es, f"lin{i}", [128, TG], F32) for i in range(3)]
            r_elg = [self.R() for _ in range(3)]
            r_ell = [self.R() for _ in range(3)]
            r_els = [self.R() for _ in range(3)]
            xt = [self.sb(es, f"x4_{i}", [128, D], F32) for i in range(2)]
            r_xt = [self.R() for _ in range(2)]
            vf = self.sb(es, "vf", [128, D], F32)
            vb16 = self.sb(es, "vb16", [128, D], BF16)
            vTf = self.sb(es, "vTf", [128, 8, 128], F32)
            r_v = self.R()
            stats = self.sb(es, "st4", [128, 2, 6], F32)
            mv = self.sb(es, "mv4", [128, 4], F32)
            r_st = self.R()
            rt = self.sb(es, "rt", [128, 4, NEXP], F32)
            m8 = self.sb(es, "m8", [128, 16], F32)
            gT = self.sb(es, "gT", [NEXP, 128], F32)
            r_rt = self.R()
            w1cnt = [0]
            elc = [0]
            for hf in range(nhalf):
                h0 = hf * HT
                for tt in range(ntt):
                    b = tt % 2
                    q0 = h0 + tt * 128
                    t.dma("sp", xt[b][:], self.d_x1[q0:q0 + 128, :], writes=[r_xt[b]])
                    self.ln_stats(xt[b], r_xt[b], stats, mv, r_st)
                    t.op("dve", lambda e, b=b: e.tensor_scalar(out=vf[:], in0=xt[b][:], scalar1=mv[:, 0:1], scalar2=mv[:, 3:4],
                                                               op0=ALU.subtract, op1=ALU.mult),
                         reads=[r_xt[b], r_st], writes=[r_v])
                    t.op("pool", lambda e: e.tensor_tensor(out=vf[:], in0=vf[:], in1=scf[:], op=ALU.mult),
                         reads=[r_v, r_c], writes=[r_v])
                    t.op("pool", lambda e: e.tensor_tensor(out=vf[:], in0=vf[:], in1=shf[:], op=ALU.add),
                         reads=[r_v, r_c], writes=[r_v])
                    t.op("dve", lambda e: e.tensor_copy(out=vb16[:], in_=vf[:]), reads=[r_v], writes=[r_v])
                    pbf = PS[0].bitcast(BF16)

                    def tr(e, pbf=pbf):
                        for c in range(8):
                            ins = e.transpose(pbf[:, c * 128:(c + 1) * 128], vb16[:, c * 128:(c + 1) * 128], self.identb[:])
                        return ins
                    t.op("pe", tr, reads=[r_v, self.r_const], writes=[PR[0]])
                    t.op("act", lambda e, tt=tt, pbf=pbf: e.copy(out=vT[:, :, tt * 128:(tt + 1) * 128],
                                                                 in_=pbf[:, :].rearrange("p (c n) -> p c n", c=8)),
                         reads=[PR[0]], writes=[r_vT])
                    for half2 in range(2):
                        def trf(e, half2=half2):
                            for c4 in range(4):
                                c = half2 * 4 + c4
                                ins = e.transpose(PS[1 + half2][:, c4 * 128:(c4 + 1) * 128], vf[:, c * 128:(c + 1) * 128], self.identf[:])
                            return ins
                        t.op("pe", trf, reads=[r_v, self.r_const], writes=[PR[1 + half2]])
                        t.op("dve", lambda e, half2=half2: e.tensor_copy(
                            out=vTf[:, half2 * 4:half2 * 4 + 4, :], in_=PS[1 + half2][:, :].rearrange("p (c n) -> p c n", c=4)),
                            reads=[PR[1 + half2]], writes=[r_v])

                    def mmr(e):
                        for c in range(8):
                            e.matmul(PS[3][:, 0:NEXP], lhsT=vTf[:, c, :], rhs=wr[:, c, :], start=(c == 0), stop=False)
                        return e.matmul(PS[3][:, 0:NEXP], lhsT=onesf[0:1, :], rhs=brr[0:1, :], start=False, stop=True)
                    t.op("pe", mmr, reads=[r_v, r_c], writes=[PR[3]])
                    LG, SEL, EX = 0, 1, 2
                    t.op("dve", lambda e: e.tensor_copy(out=rt[:, LG, :], in_=PS[3][:, 0:NEXP]), reads=[PR[3]], writes=[r_rt])
                    t.op("dve", lambda e: e.max(out=m8[:, 0:8], in_=rt[:, LG, :]), reads=[r_rt], writes=[r_rt])
                    t.op("dve", lambda e: e.tensor_scalar(out=rt[:, SEL, :], in0=rt[:, LG, :], scalar1=m8[:, 3:4], scalar2=None,
                                                          op0=ALU.is_ge), reads=[r_rt], writes=[r_rt])
                    t.op("dve", lambda e: e.tensor_scalar(out=m8[:, 8:9], in0=m8[:, 0:1], scalar1=-1.0, scalar2=None,
                                                          op0=ALU.mult), reads=[r_rt], writes=[r_rt])
                    t.op("act", lambda e: e.activation(out=rt[:, EX, :], in_=rt[:, LG, :], func=AF.Exp, bias=m8[:, 8:9], scale=1.0),
                         reads=[r_rt], writes=[r_rt])
                    t.op("dve", lambda e: e.tensor_tensor(out=rt[:, EX, :], in0=rt[:, EX, :], in1=rt[:, SEL, :], op=ALU.mult),
                         reads=[r_rt], writes=[r_rt])
                    t.op("dve", lambda e: e.reduce_sum(out=m8[:, 9:10], in_=rt[:, EX, :], axis=AX.X), reads=[r_rt], writes=[r_rt])
                    t.op("dve", lambda e: e.reciprocal(out=m8[:, 10:11], in_=m8[:, 9:10]), reads=[r_rt], writes=[r_rt])
                    t.op("dve", lambda e, tt=tt: e.tensor_scalar(out=gate[:, tt, :], in0=rt[:, EX, :], scalar1=m8[:, 10:11],
                                                                 scalar2=None, op0=ALU.mult), reads=[r_rt], writes=[r_g])
                    t.op("pe", lambda e, tt=tt: e.transpose(PS[3][0:NEXP, 128:256], gate[:, tt, :], self.identf[:]),
                         reads=[r_g, self.r_const], writes=[PR[3]])
                    t.op("dve", lambda e: e.tensor_copy(out=gT[:], in_=PS[3][0:NEXP, 128:256]), reads=[PR[3]], writes=[r_rt])
                    for cg in range(2):
                        t.op("pe", lambda e, cg=cg: e.matmul(PS[4 + cg][:, :], lhsT=gT[:, :], rhs=b2w[:, cg * 512:(cg + 1) * 512],
                                                            start=True, stop=True), reads=[r_rt, r_c], writes=[PR[4 + cg]])
                        t.op("dve", lambda e, cg=cg, tt=tt: e.tensor_copy(out=yacc[:, tt, cg * 512:(cg + 1) * 512], in_=PS[4 + cg][:, :]),
                             reads=[PR[4 + cg]], writes=[r_y])

                def stageA(e_):
                    ab_ = e_ % 2
                    for p in range(8):
                        wb_ = w1cnt[0] % 3
                        w1cnt[0] += 1
                        t.dma("pool", w1p[wb_][:], self.I("w_e1")[e_, :, p * 256:(p + 1) * 256].rearrange("(c q) n -> q c n", q=128),
                              writes=[r_w1[wb_]])
                        for tg in range(ntg):
                            tsl = slice(tg * TG, (tg + 1) * TG)
                            pg, pl = (0, 1) if (p * ntg + tg) % 2 == 0 else (2, 3)

                            def mm(e, wb_=wb_, tsl=tsl, pg=pg, pl=pl):
                                for two, pb in ((0, pg), (1, pl)):
                                    for c in range(8):
                                        ins = e.matmul(PS[pb][:, 0:TG], lhsT=w1p[wb_][:, c, two::2], rhs=vT[:, c, tsl],
                                                       start=(c == 0), stop=(c == 7))
                                return ins
                            t.op("pe", mm, reads=[r_w1[wb_], r_vT], writes=[PR[pg], PR[pl]])
                            k = elc[0] % 3
                            elc[0] += 1
                            t.op("dve", lambda e, k=k, pg=pg, p=p, e_=e_: e.tensor_scalar(
                                out=glu[k][:], in0=PS[pg][:, 0:TG], scalar1=b1g[:, p, e_:e_ + 1], scalar2=SWIGLU_LIMIT,
                                op0=ALU.add, op1=ALU.min), reads=[PR[pg], r_c], writes=[r_elg[k]])
                            t.op("dve", lambda e, k=k, pl=pl, p=p, e_=e_: e.tensor_scalar(
                                out=lin[k][:], in0=PS[pl][:, 0:TG], scalar1=b1l[:, p, e_:e_ + 1], scalar2=1.0 - SWIGLU_LIMIT,
                                op0=ALU.add, op1=ALU.max), reads=[PR[pl], r_c], writes=[r_ell[k]])
                            t.op("act", lambda e, k=k: e.activation(out=sig[k][:], in_=glu[k][:], func=AF.Sigmoid, scale=SWIGLU_ALPHA),
                                 reads=[r_elg[k]], writes=[r_els[k]])
                            t.op("dve", lambda e, k=k: e.scalar_tensor_tensor(
                                out=lin[k][:], in0=lin[k][:], scalar=1.0 + SWIGLU_LIMIT, in1=glu[k][:], op0=ALU.min, op1=ALU.mult),
                                reads=[r_ell[k], r_elg[k]], writes=[r_ell[k]])
                            t.op("dve", lambda e, k=k, ab_=ab_, p=p, tsl=tsl: e.tensor_tensor(
                                out=aT[ab_][:, p, tsl], in0=lin[k][:], in1=sig[k][:], op=ALU.mult),
                                reads=[r_ell[k], r_els[k]], writes=[r_aT[ab_]])

                def stageB(e_):
                    ab_ = e_ % 2
                    for tt in range(ntt):
                        for cg in range(2):
                            pb = 4 + (tt * 2 + cg) % 3

                            def mm(e, tt=tt, cg=cg, pb=pb):
                                for p in range(8):
                                    ins = e.matmul(PS[pb][:, :], lhsT=aT[ab_][:, p, tt * 128:(tt + 1) * 128],
                                                   rhs=w2e[ab_][:, p, cg * 512:(cg + 1) * 512], start=(p == 0), stop=(p == 7))
                                return ins
                            t.op("pe", mm, reads=[r_aT[ab_], r_w2[ab_]], writes=[PR[pb]])
                            t.op("dve", lambda e, tt=tt, cg=cg, pb=pb: e.scalar_tensor_tensor(
                                out=yacc[:, tt, cg * 512:(cg + 1) * 512], in0=PS[pb][:, :], scalar=gate[:, tt, e_:e_ + 1],
                                in1=yacc[:, tt, cg * 512:(cg + 1) * 512], op0=ALU.mult, op1=ALU.add),
                                reads=[PR[pb], r_g, r_y], writes=[r_y])

                def loadw2(e_):
                    ab_ = e_ % 2
                    v = self.I("w_e2")[e_].rearrange("(c q) n -> q c n", q=128)
                    for a in range(0, 1024, 512):
                        t.dma("pool", w2e[ab_][:, :, a:a + 512], v[:, :, a:a + 512], writes=[r_w2[ab_]])
                loadw2(0)
                stageA(0)
                for e_ in range(NEXP):
                    if e_ + 1 < NEXP:
                        loadw2(e_ + 1)
                        stageA(e_ + 1)
                    stageB(e_)
                for tt in range(ntt):
                    b = tt % 2
                    q0 = h0 + tt * 128
                    t.dma("sp", xt[b][:], self.d_x1[q0:q0 + 128, :], writes=[r_xt[b]])
                    t.op("pool", lambda e, tt=tt: e.tensor_tensor(out=yacc[:, tt, :], in0=yacc[:, tt, :], in1=self.gf_bc[:], op=ALU.mult),
                         reads=[r_y, self.r_mod], writes=[r_y])
                    t.op("dve", lambda e, b=b, tt=tt: e.scalar_tensor_tensor(out=vf[:], in0=xt[b][:], scalar=ALPHA, in1=yacc[:, tt, :],
                                                                            op0=ALU.mult, op1=ALU.add),
                         reads=[r_xt[b], r_y], writes=[r_v])
                    self.ln_stats(vf, r_v, stats, mv, r_st)
                    t.op("dve", lambda e, b=b: e.tensor_scalar(out=xt[b][:], in0=vf[:], scalar1=mv[:, 0:1], scalar2=mv[:, 3:4],
                                                               op0=ALU.subtract, op1=ALU.mult),
                         reads=[r_v, r_st], writes=[r_xt[b]])
                    t.op("pool", lambda e, b=b: e.tensor_tensor(out=xt[b][:], in0=xt[b][:], in1=g2[:], op=ALU.mult),
                         reads=[r_xt[b], r_c], writes=[r_xt[b]])
                    t.op("pool", lambda e, b=b: e.tensor_tensor(out=xt[b][:], in0=xt[b][:], in1=b2l[:], op=ALU.add),
                         reads=[r_xt[b], r_c], writes=[r_xt[b]])
                    t.dma("sp", self.out[q0:q0 + 128, :], xt[b][:], reads=[r_xt[b]], writes=[self.r_out])
            self.barrier()


def make_consts(nslot, c):
    ntile = 8 * nslot
    S = 128 * ntile
    slopes = alibi_slopes(8)
    k = np.arange(S)
    tt = k // 128
    pk = k % 128
    ndummy = 7 - c
    kaug = np.zeros((7, S), np.float32)
    kaug[0] = 1.0
    kaug[1] = 1.0
    kaug[2] = 128.0 * tt
    kaug[3] = 1.0
    kaug[4] = 1.0
    kaug[5] = pk
    kaug[6] = (tt < ndummy).astype(np.float32)
    qaug = np.zeros((7, nslot, 8, 128), np.float32)
    ql = np.arange(128)
    for j in range(nslot):
        tq = 8 * j + 7
        for h in range(8):
            s = slopes[h]
            qaug[2, j, h] = s
            qaug[3, j, h] = -s * 128.0 * tq
            qaug[4, j, h] = -s * ql
            qaug[5, j, h] = s
            qaug[6, j, h] = NEG
    kk = np.arange(128)[:, None]
    qq = np.arange(128)[None, :]
    cend = (qq // 64 + 1) * 64
    dg = np.zeros((128, 8, 128), np.float32)
    for h in range(8):
        s = slopes[h]
        m = np.where(kk > qq, -2.0 * s * (kk - qq), 0.0)
        m = np.where(kk >= cend, NEG, m)
        dg[:, h, :] = m
    dmask = np.where(kk.T >= 0, 0.0, 0.0) * 0.0
    qq2 = np.arange(128)[:, None]
    kk2 = np.arange(128)[None, :]
    dmask = np.where(kk2 < (qq2 // 64 + 1) * 64, 0.0, -1e9).astype(np.float32)
    dummy = np.zeros((128, 8), np.float32)
    dummy[:, :ndummy] = -1e9
    iota = np.broadcast_to(np.arange(1, 513, dtype=np.float32)[None, :], (128, 512)).copy()
    slopetab = np.zeros((2, 8, 128), np.float32)
    for h in range(8):
        slopetab[0, h] = slopes[h] * 128.0
        slopetab[1, h] = slopes[h]
    return {
        "c_identb": np.eye(128, dtype=np.float32).astype(NPBF),
        "c_identf": np.eye(128, dtype=np.float32),
        "c_kaug": kaug.astype(NPBF),
        "c_qaug": qaug.astype(NPBF),
        "c_dg": dg.astype(NPBF),
        "c_dmask": dmask,
        "c_dummy": dummy,
        "c_iota": iota,
        "c_slopetab": slopetab,
        "c_pidx1": np.arange(1, 129, dtype=np.float32).reshape(128, 1),
    }


def make_in_maps(inputs, nslot, used=None):
    S = 128 * 8 * nslot
    f = lambda a: np.ascontiguousarray(np.asarray(a, dtype=np.float32))
    x = f(inputs["x"])[0]
    assert x.shape[0] == S
    shared = {
        "c": f(inputs["c"])[0],
        "w_ada": f(inputs["w_ada"])[0],
        "b_ada": f(inputs["b_ada"])[0],
        "w_in": f(inputs["w_in"])[0],
        "lamv": np.stack([f(inputs[k])[0] for k in ("lam_q1", "lam_k1", "lam_q2", "lam_k2")]),
        "diff_norm_g": f(inputs["diff_norm_g"])[0],
        "w_branch_a": f(inputs["w_branch_a"])[0],
        "w_branch_b": f(inputs["w_branch_b"])[0],
        "w_out": f(inputs["w_out"])[0],
        "ln1_g": f(inputs["ln1_g"])[0],
        "ln1_b": f(inputs["ln1_b"])[0],
        "w_router": f(inputs["w_router"])[0],
        "b_router": f(inputs["b_router"])[0],
        "w_e1": f(inputs["w_e1"])[0],
        "b_e1": f(inputs["b_e1"])[0],
        "w_e2": f(inputs["w_e2"])[0],
        "b_e2": f(inputs["b_e2"])[0],
        "ln2_g": f(inputs["ln2_g"])[0],
        "ln2_b": f(inputs["ln2_b"])[0],
    }
    maps = []
    for c in range(NCORE):
        m = dict(shared)
        m["x"] = np.ascontiguousarray(np.roll(x, 128 * (7 - c), axis=0))
        m.update(make_consts(nslot, c))
        maps.append({k: v for k, v in m.items() if used is None or k in used})
    return maps


_CACHE = {}


def run(inputs, nslot, debug=False, phases=99, trace=False):
    key = (nslot, debug, phases)
    mk = MK(nslot, debug=debug, phases=phases)
    nc = mk.build()
    in_maps = make_in_maps(inputs, nslot, used=set(mk.in_aps.keys()))
    res = run_bass_kernel_spmd(nc, in_maps, core_ids=list(range(NCORE)), trace=trace)
    return res


def kernel(**inputs):
    nslot = 16
    res = run(inputs, nslot)
    S = 128 * 8 * nslot
    out = np.zeros((1, S, D), np.float32)
    for c in range(NCORE):
        o = np.asarray(res.results[c]["out"], dtype=np.float32)
        for j in range(nslot):
            rt = 8 * j + c
            out[0, rt * 128:(rt + 1) * 128, :] = o[j * 128:(j + 1) * 128, :]
    return out
```

```python
import os
import numpy as np
import ml_dtypes
from contextlib import ExitStack
import concourse.bass as bass
import concourse.mybir as mybir
from concourse.bass_utils import run_bass_kernel_spmd

F32 = mybir.dt.float32
BF16 = mybir.dt.bfloat16
I32 = mybir.dt.int32
AF = mybir.ActivationFunctionType
ALU = mybir.AluOpType
AX = mybir.AxisListType
NPBF = ml_dtypes.bfloat16

D = 1024
NCORE = 8
NEXP = 32
DFF = 1024
TOPK = 256
LN_EPS = 1e-5
ALPHA = 2.0 ** 0.25
LAM_INIT = 0.2
NEG = -30000.0
NBISECT = 24
SWIGLU_ALPHA = 1.702
SWIGLU_LIMIT = 7.0
C_QA, C_KA, C_VA, C_QB, C_KB, C_VB, C_QI, C_KI, C_WI, C_GA, C_GB = (
    0, 1024, 2048, 3072, 4096, 5120, 6144, 7168, 7232, 7248, 8272)
PROJ_W = 9296


class Res:
    __slots__ = ("w", "r", "name")

    def __init__(self, name=""):
        self.w = None
        self.r = {}
        self.name = name


class Eng:
    def __init__(self, name, eng, sem):
        self.name = name
        self.eng = eng
        self.sem = sem
        self.cnt = 0
        self.seen = {}


class Trk:
    def __init__(self, nc, es):
        self.nc = nc
        mk = lambda n: es.enter_context(nc.semaphore(n))
        self.E = {n: Eng(n, e, mk("s_" + n)) for n, e in [
            ("pe", nc.tensor), ("act", nc.scalar), ("dve", nc.vector),
            ("pool", nc.gpsimd), ("sp", nc.sync)]}
        self.dsems = {q: [[mk(f"d_{q}{i}"), 0] for i in range(n)]
                      for q, n in [("sp", 16), ("pool", 10), ("act", 4)]}
        self.dnext = {q: 0 for q in self.dsems}
        self.nwait = 0

    def _waits(self, E, reads, writes):
        need = {}

        def add(tok, raw):
            if tok is None:
                return
            sem, val = tok
            if sem is E.sem and E.name == "pe":
                return
            k = id(sem)
            if k not in need or need[k][1] < val:
                need[k] = (sem, val)
        for r in reads:
            add(r.w, True)
        for w in writes:
            add(w.w, False)
            for tok in w.r.values():
                add(tok, False)
        for k, (sem, val) in need.items():
            if E.seen.get(k, 0) < val:
                E.eng.wait_ge(sem, val)
                E.seen[k] = val
                self.nwait += 1

    @staticmethod
    def _mark(tok, reads, writes):
        k = id(tok[0])
        for r in reads:
            r.r[k] = tok
        for w in writes:
            w.w = tok
            w.r = {}

    def op(self, en, fn, reads=(), writes=()):
        E = self.E[en]
        self._waits(E, reads, writes)
        ins = fn(E.eng)
        E.cnt += 1
        ins.then_inc(E.sem, 1)
        self._mark((E.sem, E.cnt), reads, writes)

    def dma(self, q, out, in_, reads=(), writes=(), **kw):
        E = self.E[q]
        self._waits(E, reads, writes)
        slots = self.dsems[q]
        i = self.dnext[q]
        self.dnext[q] = (i + 1) % len(slots)
        sem, val = slots[i]
        k = id(sem)
        if val > 0 and E.seen.get(k, 0) < val:
            E.eng.wait_ge(sem, val)
            E.seen[k] = val
        ins = E.eng.dma_start(out=out, in_=in_, **kw)
        val += 16
        slots[i][1] = val
        ins.then_inc(sem, 16)
        self._mark((sem, val), reads, writes)

    def barrier(self, all_res):
        toks = {}
        for r in all_res:
            for tok in [r.w] + list(r.r.values()):
                if tok is None:
                    continue
                k = id(tok[0])
                if k not in toks or toks[k][1] < tok[1]:
                    toks[k] = tok
        for E in self.E.values():
            for k, (sem, val) in toks.items():
                if sem is E.sem:
                    continue
                if E.seen.get(k, 0) < val:
                    E.eng.wait_ge(sem, val)
                    E.seen[k] = val


def alibi_slopes(n=8):
    return [2.0 ** (-8.0 * (h + 1) / n) for h in range(n)]


class MK:
    def __init__(self, nslot, debug=False, phases=99):
        self.nslot = nslot
        self.ntile = 8 * nslot
        self.S = 128 * self.ntile
        self.NQ = 128 * nslot
        self.debug = debug
        self.phases = phases
        self.nc = bass.Bass("TRN2", target_bir_lowering=False)
        self.res_all = []

    def R(self, name=""):
        r = Res(name)
        self.res_all.append(r)
        return r

    def I(self, name):
        if name not in self.in_aps:
            shape, dt = self.in_specs[name]
            self.in_aps[name] = self.nc.dram_tensor(name, list(shape), dt, kind="ExternalInput").ap()
        return self.in_aps[name]

    def dscr(self, name, shape, dt):
        kind = "ExternalOutput" if self.debug else "Internal"
        t = self.nc.dram_tensor(name, list(shape), dt, kind=kind).ap()
        return t

    def sb(self, es, name, shape, dt):
        return es.enter_context(self.nc.sbuf_tensor(name, list(shape), dt))

    def barrier(self):
        self.t.barrier(self.res_all)

    def build(self):
        nc = self.nc
        S, NQ, nslot, ntile = self.S, self.NQ, self.nslot, self.ntile
        self.in_specs = {
            "x": ([S, D], F32), "c": ([D], F32), "w_ada": ([D, 6 * D], F32), "b_ada": ([6 * D], F32),
            "w_in": ([D, PROJ_W], F32), "lamv": ([4, 64], F32), "diff_norm_g": ([128], F32),
            "w_branch_a": ([D, D], F32), "w_branch_b": ([D, D], F32), "w_out": ([D, D], F32),
            "ln1_g": ([D], F32), "ln1_b": ([D], F32), "w_router": ([D, NEXP], F32), "b_router": ([NEXP], F32),
            "w_e1": ([NEXP, D, 2 * DFF], F32), "b_e1": ([NEXP, 2 * DFF], F32),
            "w_e2": ([NEXP, DFF, D], F32), "b_e2": ([NEXP, D], F32), "ln2_g": ([D], F32), "ln2_b": ([D], F32),
            "c_identb": ([128, 128], BF16), "c_identf": ([128, 128], F32), "c_kaug": ([7, S], BF16),
            "c_qaug": ([7, nslot, 8, 128], BF16), "c_dg": ([128, 8, 128], BF16), "c_dmask": ([128, 128], F32),
            "c_dummy": ([128, 8], F32), "c_iota": ([128, 512], F32), "c_slopetab": ([2, 8, 128], F32),
            "c_pidx1": ([128, 1], F32),
        }
        self.in_aps = {}
        self.out = nc.dram_tensor("out", [NQ, D], F32, kind="ExternalOutput").ap()
        self.d_mod = self.dscr("d_mod", [6 * D], F32)
        self.d_kat = self.dscr("d_kat", [16, 64, S], BF16)
        self.d_va = self.dscr("d_va", [S, 8, 132], BF16)
        self.d_kbt = self.dscr("d_kbt", [8, 128, S], BF16)
        self.d_vb = self.dscr("d_vb", [S, 8, 132], BF16)
        self.d_kit = self.dscr("d_kit", [64, S], BF16)
        self.d_qat = self.dscr("d_qat", [16, 64, NQ], BF16)
        self.d_qbt = self.dscr("d_qbt", [8, 128, NQ], BF16)
        self.d_qit = self.dscr("d_qit", [16, 64, NQ], BF16)
        self.d_sgn = self.dscr("d_sgn", [NQ, 16], F32)
        self.d_gate = self.dscr("d_gate", [NQ, 2 * D], F32)
        self.d_ya = self.dscr("d_ya", [NQ, D], BF16)
        self.d_yb = self.dscr("d_yb", [NQ, D], BF16)
        self.d_x1 = self.dscr("d_x1", [NQ, D], F32)

        with ExitStack() as es:
            self.t = Trk(nc, es)
            self.ps = [es.enter_context(nc.psum_tensor(f"ps{i}", [128, 512], F32)) for i in range(8)]
            self.psr = [self.R(f"ps{i}") for i in range(8)]
            self.identb = self.sb(es, "identb", [128, 128], BF16)
            self.identf = self.sb(es, "identf", [128, 128], F32)
            self.r_const = self.R("const")
            self.t.dma("sp", self.identb[:], self.I("c_identb"), writes=[self.r_const])
            self.t.dma("sp", self.identf[:], self.I("c_identf"), writes=[self.r_const])
            self.modT = self.sb(es, "modT", [128, 48], F32)
            self.r_mod = self.R("mod")
            self.r_out = self.R("out")
            self.phase0()
            self.barrier()
            if self.phases >= 1:
                self.phase1a()
                self.barrier()
                self.phase1b()
                self.barrier()
            if self.phases >= 2:
                self.phase2()
                self.barrier()
            if self.phases >= 3:
                self.phase3()
                self.barrier()
            if self.phases >= 4:
                self.phase4()
            self.final_wait()
        return nc

    def final_wait(self):
        self.barrier()

    def phase0(self):
        nc, t = self.nc, self.t
        with ExitStack() as es:
            cT = self.sb(es, "cT", [128, 8], F32)
            cact = self.sb(es, "cact", [128, 8], F32)
            wbuf = [self.sb(es, f"wada{i}", [128, 8, 512], F32) for i in range(2)]
            wr = [self.R() for _ in range(2)]
            brow = self.sb(es, "brow", [1, 6 * D], F32)
            mrow = self.sb(es, "mrow", [1, 6 * D], F32)
            r_c, r_b, r_m = self.R(), self.R(), self.R()
            t.dma("sp", cT[:], self.I("c").rearrange("(c p) -> p c", p=128), writes=[r_c],
                  allow_slow_non_contiguous=True)
            t.dma("sp", brow[:], self.I("b_ada").rearrange("(o n) -> o n", o=1), writes=[r_b])
            t.op("act", lambda e: e.activation(out=cact[:], in_=cT[:], func=AF.Silu),
                 reads=[r_c], writes=[r_c])
            wv = self.I("w_ada").rearrange("(c p) n -> p c n", p=128)
            for g in range(12):
                b = g % 2
                t.dma("sp", wbuf[b][:], wv[:, :, g * 512:(g + 1) * 512], writes=[wr[b]])
                pb = g % 2

                def mm(e, b=b, pb=pb):
                    for c in range(8):
                        ins = e.matmul(self.ps[pb][0:1, :], lhsT=cact[:, c:c + 1], rhs=wbuf[b][:, c, :],
                                       start=(c == 0), stop=(c == 7))
                    return ins
                t.op("pe", mm, reads=[r_c, wr[b]], writes=[self.psr[pb]])
                t.op("dve", lambda e, g=g, pb=pb: e.tensor_tensor(
                    out=mrow[:, g * 512:(g + 1) * 512], in0=self.ps[pb][0:1, :],
                    in1=brow[:, g * 512:(g + 1) * 512], op=ALU.add),
                    reads=[self.psr[pb], r_b], writes=[r_m])
            r_d = self.R()
            t.dma("sp", self.d_mod.rearrange("(o n) -> o n", o=1), mrow[:], reads=[r_m], writes=[r_d])
            t.dma("sp", self.modT[:], self.d_mod.rearrange("(m p) -> p m", p=128), reads=[r_d],
                  writes=[self.r_mod], allow_slow_non_contiguous=True)
            self.r_dmod = r_d
            self.barrier()

    def ln_pre(self, xin_ap, xt, r_xt, xn, r_xn, stats, mv, r_st, q="sp"):
        t = self.t
        t.dma(q, xt[:], xin_ap, writes=[r_xt])
        for hh in range(2):
            t.op("dve", lambda e, hh=hh: e.bn_stats(out=stats[:, hh, :], in_=xt[:, hh * 512:(hh + 1) * 512]),
                 reads=[r_xt], writes=[r_st])
        t.op("dve", lambda e: e.bn_aggr(out=mv[:, 0:2], in_=stats[:].rearrange("p a b -> p (a b)")),
             reads=[r_st], writes=[r_st])
        t.op("dve", lambda e: e.tensor_scalar(out=mv[:, 2:3], in0=mv[:, 1:2], scalar1=LN_EPS, scalar2=None,
                                              op0=ALU.add), reads=[r_st], writes=[r_st])
        t.op("act", lambda e: e.activation(out=mv[:, 2:3], in_=mv[:, 2:3], func=AF.Sqrt),
             reads=[r_st], writes=[r_st])
        t.op("dve", lambda e: e.reciprocal(out=mv[:, 3:4], in_=mv[:, 2:3]), reads=[r_st], writes=[r_st])
        t.op("dve", lambda e: e.tensor_scalar(out=xn[:], in0=xt[:], scalar1=mv[:, 0:1], scalar2=mv[:, 3:4],
                                              op0=ALU.subtract, op1=ALU.mult),
             reads=[r_xt, r_st], writes=[r_xn])

    def ln_post(self, xn, r_xn, out_xnT, r_out, pbank):
        t = self.t
        pbf = self.ps[pbank].bitcast(BF16)

        def tr(e):
            for c in range(8):
                ins = e.transpose(pbf[:, c * 128:(c + 1) * 128], xn[:, c * 128:(c + 1) * 128], self.identb[:])
            return ins
        t.op("pe", tr, reads=[r_xn, self.r_const], writes=[self.psr[pbank]])
        t.op("act", lambda e: e.copy(out=out_xnT, in_=pbf[:, :].rearrange("p (c n) -> p c n", c=8)),
             reads=[self.psr[pbank]], writes=[r_out])

    def ln_tile(self, xin_ap, xt, r_xt, xn, r_xn, stats, mv, r_st, out_xnT, r_out, pbank, q="sp"):
        self.ln_pre(xin_ap, xt, r_xt, xn, r_xn, stats, mv, r_st, q=q)
        self.ln_post(xn, r_xn, out_xnT, r_out, pbank)

    def prep_w(self, es, tag, colranges, sc_off, sh_off):
        nc, t = self.nc, self.t
        ncols = sum(l for _, l in colranges)
        wsb = self.sb(es, "w_" + tag, [128, 8, ncols], BF16)
        r_w = self.R()
        wv = self.I("w_in").rearrange("(c p) n -> p c n", p=128)
        o = 0
        for (s0, l) in colranges:
            for a in range(0, l, 512):
                b = min(l, a + 512)
                t.dma("pool", wsb[:, :, o + a:o + b], wv[:, :, s0 + a:s0 + b], writes=[r_w])
            o += l
        nch = (ncols + 127) // 128
        biasT = self.sb(es, "bT_" + tag, [128, nch], F32)
        biasrow = self.sb(es, "br_" + tag, [1, ncols], BF16)
        onep = self.sb(es, "onep_" + tag, [128, 8], F32)
        shb = self.sb(es, "shb_" + tag, [128, 8], BF16)
        r_b = self.R()
        t.op("dve", lambda e: e.tensor_scalar(out=onep[:], in0=self.modT[:, sc_off:sc_off + 8], scalar1=1.0,
                                              scalar2=None, op0=ALU.add), reads=[self.r_mod], writes=[r_b])
        t.op("dve", lambda e: e.tensor_copy(out=shb[:], in_=self.modT[:, sh_off:sh_off + 8]),
             reads=[self.r_mod], writes=[r_b])
        for ch in range(nch):
            w0 = ch * 128
            wl = min(128, ncols - w0)
            pb = ch % 2

            def mm(e, w0=w0, wl=wl, pb=pb):
                for c in range(8):
                    ins = e.matmul(self.ps[pb][0:wl, 0:1], lhsT=wsb[:, c, w0:w0 + wl], rhs=shb[:, c:c + 1],
                                   start=(c == 0), stop=(c == 7))
                return ins
            t.op("pe", mm, reads=[r_w, r_b], writes=[self.psr[pb]])
            t.op("dve", lambda e, ch=ch, wl=wl, pb=pb: e.tensor_copy(out=biasT[0:wl, ch:ch + 1],
                                                                      in_=self.ps[pb][0:wl, 0:1]),
                 reads=[self.psr[pb]], writes=[r_b])
        for a in range(0, ncols, 512):
            b = min(ncols, a + 512)
            pb = 2 + (a // 512) % 2

            def mm2(e, a=a, b=b, pb=pb):
                for c in range(8):
                    ins = e.matmul(self.ps[pb][0:1, 0:b - a], lhsT=shb[:, c:c + 1], rhs=wsb[:, c, a:b],
                                   start=(c == 0), stop=(c == 7))
                return ins
            t.op("pe", mm2, reads=[r_w, r_b], writes=[self.psr[pb]])
            t.op("dve", lambda e, a=a, b=b, pb=pb: e.tensor_copy(out=biasrow[:, a:b], in_=self.ps[pb][0:1, 0:b - a]),
                 reads=[self.psr[pb]], writes=[r_b])
        for c in range(8):
            en = "dve" if c % 2 == 0 else "pool"
            t.op(en, lambda e, c=c: e.tensor_scalar(out=wsb[:, c, :], in0=wsb[:, c, :], scalar1=onep[:, c:c + 1],
                                                     scalar2=None, op0=ALU.mult),
                 reads=[r_w, r_b], writes=[r_w])
        return wsb, r_w, biasT, biasrow, r_b

    def phase1a(self):
        nc, t = self.nc, self.t
        S = self.S
        with ExitStack() as es:
            wsb, r_w, biasT, biasrow, r_b = self.prep_w(
                es, "p1a", [(C_KA, 1024), (C_KB, 1024), (C_KI, 64), (C_VA, 1024), (C_VB, 1024)], 8, 0)
            VOFF = 2112
            ones = self.sb(es, "ones1", [1, 128], BF16)
            r_ones = self.R()
            t.op("dve", lambda e: e.memset(ones[:], 1.0), writes=[r_ones])
            xt = [self.sb(es, f"xt{i}", [128, D], F32) for i in range(2)]
            r_xt = [self.R() for _ in range(2)]
            xn = [self.sb(es, f"xn{i}", [128, D], BF16) for i in range(2)]
            r_xn = [self.R() for _ in range(2)]
            stats = [self.sb(es, f"st{i}", [128, 2, 6], F32) for i in range(2)]
            mv = [self.sb(es, f"mv{i}", [128, 4], F32) for i in range(2)]
            r_st = [self.R() for _ in range(2)]
            xnT = [self.sb(es, f"xnT{i}", [128, 8, 512], BF16) for i in range(2)]
            r_xnT = [self.R() for _ in range(2)]
            kst = [self.sb(es, f"kst{i}", [128, 512], BF16) for i in range(4)]
            r_kst = [self.R() for _ in range(4)]
            vst = [self.sb(es, f"vst{i}", [128, 4, 132], BF16) for i in range(4)]
            r_vst = [self.R() for _ in range(4)]
            for i in range(4):
                t.op("pool", lambda e, i=i: e.memset(vst[i][:, :, 128:132], 0.0), writes=[r_vst[i]])
                t.op("pool", lambda e, i=i: e.memset(vst[i][:, :, 128:129], 1.0), writes=[r_vst[i]])
            ngrp = S // 512
            ki = 0
            vi = 0
            tcount = 0
            ev = 0
            xn4 = [self.sb(es, f"xn4_{i}", [128, D], BF16) for i in range(8)]
            r_xn4 = [self.R() for _ in range(8)]

            def ln_group_pre(g):
                for tt in range(4):
                    b = tcnt[0] % 2
                    tcnt[0] += 1
                    k = (g % 2) * 4 + tt
                    tok0 = g * 512 + tt * 128
                    self.ln_pre(self.I("x")[tok0:tok0 + 128, :], xt[b], r_xt[b], xn4[k], r_xn4[k], stats[b], mv[b], r_st[b])

            def ln_group_post(g):
                gb = g % 2
                for tt in range(4):
                    k = (g % 2) * 4 + tt
                    self.ln_post(xn4[k], r_xn4[k], xnT[gb][:, :, tt * 128:(tt + 1) * 128], r_xnT[gb], pbank=tt % 2)
            tcnt = [0]
            ln_group_pre(0)
            ln_group_post(0)
            for g in range(ngrp):
                gb = g % 2
                if g + 1 < ngrp:
                    ln_group_pre(g + 1)
                for ch in range(17):
                    wl = 128 if ch < 16 else 64
                    pb = 2 + ch % 3

                    def mm(e, ch=ch, wl=wl, pb=pb, gb=gb):
                        for c in range(8):
                            ins = e.matmul(self.ps[pb][0:wl, :], lhsT=wsb[:, c, ch * 128:ch * 128 + wl],
                                           rhs=xnT[gb][:, c, :], start=(c == 0), stop=(c == 7))
                        return ins
                    t.op("pe", mm, reads=[r_w, r_xnT[gb]], writes=[self.psr[pb]])
                    s = ki % 4
                    ki += 1
                    en = "act" if ev % 4 != 3 else "dve"
                    ev += 1
                    if en == "act":
                        t.op("act", lambda e, s=s, wl=wl, pb=pb, ch=ch: e.activation(
                            out=kst[s][0:wl, :], in_=self.ps[pb][0:wl, :], func=AF.Identity,
                            bias=biasT[0:wl, ch:ch + 1], scale=1.0),
                            reads=[self.psr[pb], r_b], writes=[r_kst[s]])
                    else:
                        t.op("dve", lambda e, s=s, wl=wl, pb=pb, ch=ch: e.tensor_scalar(
                            out=kst[s][0:wl, :], in0=self.ps[pb][0:wl, :], scalar1=biasT[0:wl, ch:ch + 1],
                            scalar2=None, op0=ALU.add),
                            reads=[self.psr[pb], r_b], writes=[r_kst[s]])
                    tsl = slice(g * 512, (g + 1) * 512)
                    if ch < 8:
                        t.dma("sp", self.d_kat[2 * ch:2 * ch + 2, :, tsl].rearrange("m d n -> (m d) n"),
                              kst[s][:, :], reads=[r_kst[s]])
                    elif ch < 16:
                        t.dma("sp", self.d_kbt[ch - 8, :, tsl], kst[s][:, :], reads=[r_kst[s]])
                    else:
                        t.dma("sp", self.d_kit[:, tsl], kst[s][0:64, :], reads=[r_kst[s]])
                if g + 1 < ngrp:
                    ln_group_post(g + 1)
                for tt in range(4):
                    for vg in range(4):
                        pb = 5 + (tt * 4 + vg) % 3
                        c0 = VOFF + vg * 512

                        def mm(e, tt=tt, c0=c0, pb=pb, gb=gb):
                            for c in range(8):
                                e.matmul(self.ps[pb][:, :], lhsT=xnT[gb][:, c, tt * 128:(tt + 1) * 128],
                                         rhs=wsb[:, c, c0:c0 + 512], start=(c == 0), stop=False)
                            return e.matmul(self.ps[pb][:, :], lhsT=ones[0:1, :], rhs=biasrow[0:1, c0:c0 + 512],
                                            start=False, stop=True)
                        t.op("pe", mm, reads=[r_w, r_xnT[gb], r_b, r_ones], writes=[self.psr[pb]])
                        s = vi % 4
                        vi += 1
                        en = "act" if ev % 4 != 3 else "dve"
                        ev += 1
                        psv = self.ps[pb][:, :].rearrange("p (h e) -> p h e", h=4)
                        if en == "act":
                            t.op("act", lambda e, s=s, psv=psv: e.copy(out=vst[s][:, :, 0:128], in_=psv),
                                 reads=[self.psr[pb]], writes=[r_vst[s]])
                        else:
                            t.op("dve", lambda e, s=s, psv=psv: e.tensor_copy(out=vst[s][:, :, 0:128], in_=psv),
                                 reads=[self.psr[pb]], writes=[r_vst[s]])
                        tok0 = g * 512 + tt * 128
                        dst = self.d_va if vg < 2 else self.d_vb
                        t.dma("sp", dst[tok0:tok0 + 128, (vg % 2) * 4:(vg % 2) * 4 + 4, :], vst[s][:],
                              reads=[r_vst[s]])
            self.barrier()

    def phase1b(self):
        nc, t = self.nc, self.t
        nslot, NQ = self.nslot, self.NQ
        with ExitStack() as es:
            wsb, r_w, biasT, biasrow, r_b = self.prep_w(
                es, "p1b", [(C_QA, 1024), (C_QB, 1024), (C_QI, 1024), (C_WI, 16), (C_GA, 1024), (C_GB, 1024)], 8, 0)
            O_QI, O_WI, O_GA = 2048, 3072, 3088
            ones = self.sb(es, "ones1b", [1, 128], BF16)
            r_ones = self.R()
            t.op("dve", lambda e: e.memset(ones[:], 1.0), writes=[r_ones])
            xt = [self.sb(es, f"xtb{i}", [128, D], F32) for i in range(2)]
            r_xt = [self.R() for _ in range(2)]
            xn = [self.sb(es, f"xnb{i}", [128, D], BF16) for i in range(2)]
            r_xn = [self.R() for _ in range(2)]
            stats = [self.sb(es, f"stb{i}", [128, 2, 6], F32) for i in range(2)]
            mv = [self.sb(es, f"mvb{i}", [128, 4], F32) for i in range(2)]
            r_st = [self.R() for _ in range(2)]
            G = min(4, nslot)
            xnT = [self.sb(es, f"xnTb{i}", [128, 8, 128 * G], BF16) for i in range(2)]
            r_xnT = [self.R() for _ in range(2)]
            kst = [self.sb(es, f"kstb{i}", [128, 128 * G], BF16) for i in range(4)]
            r_kst = [self.R() for _ in range(4)]
            gst = [self.sb(es, f"gst{i}", [128, 512], F32) for i in range(3)]
            r_gst = [self.R() for _ in range(3)]
            wis = [self.sb(es, f"wis{i}", [128, 3, 16], F32) for i in range(2)]
            r_wis = [self.R() for _ in range(2)]
            qis = [self.sb(es, f"qis{i}", [128, 1024], BF16) for i in range(2)]
            r_qis = [self.R() for _ in range(2)]
            qit = [self.sb(es, f"qit{i}", [128, 8, 128], BF16) for i in range(2)]
            r_qit = [self.R() for _ in range(2)]
            ki = 0
            gi = 0
            tcount = 0
            for g in range(nslot // G):
                gb = g % 2
                NT = 128 * G
                for tt in range(G):
                    b = tcount % 2
                    j = g * G + tt
                    tok0 = (8 * j + 7) * 128
                    self.ln_tile(self.I("x")[tok0:tok0 + 128, :], xt[b], r_xt[b], xn[b], r_xn[b], stats[b], mv[b],
                                 r_st[b], xnT[gb][:, :, tt * 128:(tt + 1) * 128], r_xnT[gb], pbank=b)
                    tcount += 1
                q0 = g * NT
                for ch in range(16):
                    pb = 2 + ch % 3

                    def mm(e, ch=ch, pb=pb, gb=gb, NT=NT):
                        for c in range(8):
                            ins = e.matmul(self.ps[pb][:, 0:NT], lhsT=wsb[:, c, ch * 128:ch * 128 + 128],
                                           rhs=xnT[gb][:, c, :], start=(c == 0), stop=(c == 7))
                        return ins
                    t.op("pe", mm, reads=[r_w, r_xnT[gb]], writes=[self.psr[pb]])
                    s = ki % 4
                    ki += 1
                    scale = 0.125 if ch < 8 else 128.0 ** -0.5
                    t.op("dve", lambda e, s=s, pb=pb, ch=ch, scale=scale, NT=NT: e.tensor_scalar(
                        out=kst[s][:, 0:NT], in0=self.ps[pb][:, 0:NT], scalar1=biasT[:, ch:ch + 1], scalar2=scale,
                        op0=ALU.add, op1=ALU.mult), reads=[self.psr[pb], r_b], writes=[r_kst[s]])
                    if ch < 8:
                        t.dma("sp", self.d_qat[2 * ch:2 * ch + 2, :, q0:q0 + NT].rearrange("m d n -> (m d) n"),
                              kst[s][:, 0:NT], reads=[r_kst[s]])
                    else:
                        t.dma("sp", self.d_qbt[ch - 8, :, q0:q0 + NT], kst[s][:, 0:NT], reads=[r_kst[s]])
                for tt in range(G):
                    j = g * G + tt
                    tq0 = j * 128
                    lhs = lambda c, tt=tt, gb=gb: xnT[gb][:, c, tt * 128:(tt + 1) * 128]
                    wb = j % 2
                    pb = 5

                    def mmw(e, lhs=lhs, pb=pb):
                        for c in range(8):
                            e.matmul(self.ps[pb][:, 0:16], lhsT=lhs(c), rhs=wsb[:, c, O_WI:O_WI + 16],
                                     start=(c == 0), stop=False)
                        return e.matmul(self.ps[pb][:, 0:16], lhsT=ones[0:1, :], rhs=biasrow[0:1, O_WI:O_WI + 16],
                                        start=False, stop=True)
                    t.op("pe", mmw, reads=[r_w, r_xnT[gb], r_b, r_ones], writes=[self.psr[pb]])
                    t.op("dve", lambda e, wb=wb, pb=pb: e.tensor_copy(out=wis[wb][:, 0, :], in_=self.ps[pb][:, 0:16]),
                         reads=[self.psr[pb]], writes=[r_wis[wb]])
                    t.op("act", lambda e, wb=wb: e.activation(out=wis[wb][:, 1, :], in_=wis[wb][:, 0, :],
                                                              func=AF.Abs, scale=1.0 / 32.0),
                         reads=[r_wis[wb]], writes=[r_wis[wb]])
                    t.op("act", lambda e, wb=wb: e.activation(out=wis[wb][:, 2, :], in_=wis[wb][:, 0, :], func=AF.Sign),
                         reads=[r_wis[wb]], writes=[r_wis[wb]])
                    t.dma("sp", self.d_sgn[tq0:tq0 + 128, :], wis[wb][:, 2, :], reads=[r_wis[wb]])
                    for qg in range(2):
                        pb = 6 + qg
                        c0 = O_QI + qg * 512

                        def mmq(e, lhs=lhs, pb=pb, c0=c0):
                            for c in range(8):
                                e.matmul(self.ps[pb][:, :], lhsT=lhs(c), rhs=wsb[:, c, c0:c0 + 512],
                                         start=(c == 0), stop=False)
                            return e.matmul(self.ps[pb][:, :], lhsT=ones[0:1, :], rhs=biasrow[0:1, c0:c0 + 512],
                                            start=False, stop=True)
                        t.op("pe", mmq, reads=[r_w, r_xnT[gb], r_b, r_ones], writes=[self.psr[pb]])
                        t.op("dve", lambda e, wb=wb, pb=pb, qg=qg: e.tensor_tensor(
                            out=qis[wb][:, qg * 512:(qg + 1) * 512].rearrange("p (h d) -> p h d", h=8),
                            in0=self.ps[pb][:, :].rearrange("p (h d) -> p h d", h=8),
                            in1=wis[wb][:, 1, qg * 8:(qg + 1) * 8].unsqueeze(2).broadcast_to([128, 8, 64]),
                            op=ALU.mult), reads=[self.psr[pb], r_wis[wb]], writes=[r_qis[wb]])
                    pbf = self.ps[2 + (j % 3)].bitcast(BF16)

                    def tr(e, wb=wb, pbf=pbf):
                        for c in range(8):
                            ins = e.transpose(pbf[:, c * 128:(c + 1) * 128], qis[wb][:, c * 128:(c + 1) * 128],
                                              self.identb[:])
                        return ins
                    t.op("pe", tr, reads=[r_qis[wb], self.r_const], writes=[self.psr[2 + (j % 3)]])
                    t.op("act", lambda e, wb=wb, pbf=pbf: e.copy(
                        out=qit[wb][:], in_=pbf[:, :].rearrange("p (c n) -> p c n", c=8)),
                        reads=[self.psr[2 + (j % 3)]], writes=[r_qit[wb]])
                    for c in range(8):
                        t.dma("sp", self.d_qit[2 * c:2 * c + 2, :, tq0:tq0 + 128].rearrange("m d n -> (m d) n"),
                              qit[wb][:, c, :], reads=[r_qit[wb]])
                    for gg in range(4):
                        pb = 5 + gg % 3
                        c0 = O_GA + gg * 512

                        def mmg(e, lhs=lhs, pb=pb, c0=c0):
                            for c in range(8):
                                e.matmul(self.ps[pb][:, :], lhsT=lhs(c), rhs=wsb[:, c, c0:c0 + 512],
                                         start=(c == 0), stop=False)
                            return e.matmul(self.ps[pb][:, :], lhsT=ones[0:1, :], rhs=biasrow[0:1, c0:c0 + 512],
                                            start=False, stop=True)
                        t.op("pe", mmg, reads=[r_w, r_xnT[gb], r_b, r_ones], writes=[self.psr[pb]])
                        s = gi % 3
                        gi += 1
                        t.op("act", lambda e, s=s, pb=pb: e.activation(out=gst[s][:], in_=self.ps[pb][:, :],
                                                                        func=AF.Sigmoid),
                             reads=[self.psr[pb]], writes=[r_gst[s]])
                        t.dma("sp", self.d_gate[tq0:tq0 + 128, gg * 512:(gg + 1) * 512], gst[s][:],
                              reads=[r_gst[s]])
            self.barrier()

    def phase2(self):
        nc, t = self.nc, self.t
        STOP = int(os.environ.get('P2STOP', '99'))
        EXP = os.environ.get('EXP', '')
        S, nslot = self.S, self.nslot
        PS = self.ps
        PR = self.psr
        with ExitStack() as es:
            dg = self.sb(es, "dg", [128, 8, 128], BF16)
            dmask = self.sb(es, "dmask", [128, 128], F32)
            dummy = self.sb(es, "dummyc", [128, 8], F32)
            iota = self.sb(es, "iotac", [128, 512], F32)
            slopetab = self.sb(es, "slopetab", [2, 8, 128], F32)
            pidx1 = self.sb(es, "pidx1", [128, 1], F32)
            nlam = self.sb(es, "nlam", [128, 1], F32)
            gbc = self.sb(es, "gbc", [128, 128], F32)
            r_c2 = self.R("c2")
            for dst, nm in [(dg, "c_dg"), (dmask, "c_dmask"), (dummy, "c_dummy"), (iota, "c_iota"),
                            (slopetab, "c_slopetab"), (pidx1, "c_pidx1")]:
                t.dma("sp", dst[:], self.I(nm), writes=[r_c2])
            lv = self.sb(es, "lv", [1, 4, 64], F32)
            lsm = self.sb(es, "lsm", [1, 8], F32)
            onesf = self.sb(es, "onesf", [1, 128], F32)
            r_l = self.R("lam")
            t.dma("sp", lv[:], self.I("lamv").rearrange("(o a) d -> o a d", o=1), writes=[r_l])
            t.op("dve", lambda e: e.memset(onesf[:], 1.0), writes=[r_l])
            t.op("dve", lambda e: e.tensor_tensor(out=lv[:, 0, :], in0=lv[:, 0, :], in1=lv[:, 1, :], op=ALU.mult),
                 reads=[r_l], writes=[r_l])
            t.op("dve", lambda e: e.tensor_tensor(out=lv[:, 2, :], in0=lv[:, 2, :], in1=lv[:, 3, :], op=ALU.mult),
                 reads=[r_l], writes=[r_l])
            t.op("dve", lambda e: e.reduce_sum(out=lsm[:, 0:1], in_=lv[:, 0, :], axis=AX.X), reads=[r_l], writes=[r_l])
            t.op("dve", lambda e: e.reduce_sum(out=lsm[:, 1:2], in_=lv[:, 2, :], axis=AX.X), reads=[r_l], writes=[r_l])
            t.op("act", lambda e: e.activation(out=lsm[:, 2:4], in_=lsm[:, 0:2], func=AF.Exp), reads=[r_l], writes=[r_l])
            t.op("dve", lambda e: e.tensor_tensor(out=lsm[:, 4:5], in0=lsm[:, 3:4], in1=lsm[:, 2:3], op=ALU.subtract),
                 reads=[r_l], writes=[r_l])
            t.op("dve", lambda e: e.tensor_scalar(out=lsm[:, 5:6], in0=lsm[:, 4:5], scalar1=-LAM_INIT, scalar2=None,
                                                  op0=ALU.add), reads=[r_l], writes=[r_l])
            t.op("pe", lambda e: e.matmul(PS[7][:, 0:1], lhsT=onesf[0:1, :], rhs=lsm[0:1, 5:6], start=True, stop=True),
                 reads=[r_l], writes=[PR[7]])
            t.op("dve", lambda e: e.tensor_copy(out=nlam[:], in_=PS[7][:, 0:1]), reads=[PR[7]], writes=[r_c2])
            t.dma("sp", gbc[:], self.I("diff_norm_g").partition_broadcast(128), writes=[r_c2])
            t.op("dve", lambda e: e.tensor_scalar(out=gbc[:], in0=gbc[:], scalar1=1.0 - LAM_INIT, scalar2=None,
                                                  op0=ALU.mult), reads=[r_c2], writes=[r_c2])

            if STOP <= 0:
                self.barrier()
                return
            score = self.sb(es, "score", [128, S], F32)
            maskT = score.bitcast(BF16)
            r_sm = self.R("score")
            mask = self.sb(es, "mask", [128, S], BF16)
            r_mask = self.R("mask")
            qi_sb = self.sb(es, "qi_sb", [64, 16, 128], BF16)
            r_qi = self.R()
            sgn = self.sb(es, "sgn", [128, 16], F32)
            dsg = self.sb(es, "dsg", [128, 16, 128], BF16)
            r_dsg = self.R()
            kit_sb = [self.sb(es, f"kit{i}", [64, 1024], BF16) for i in range(2)]
            r_kit = [self.R() for _ in range(2)]
            rbuf = [self.sb(es, f"rbuf{i}", [128, 512], BF16) for i in range(4)]
            r_rbuf = [self.R() for _ in range(4)]
            sv = self.sb(es, "sv", [128, 48], F32)
            svi = self.sb(es, "svi", [128, 4], I32)
            r_sv = self.R()
            am = self.sb(es, "am", [128, 40], F32)
            r_am = self.R()
            tmp512 = self.sb(es, "tmp512", [128, 512], F32)
            r_tmp = self.R()
            ab = self.sb(es, "ab", [128, 2], BF16)
            qb_sb = self.sb(es, "qb_sb", [128, 8, 128], BF16)
            r_qb = self.R()
            qbaug = self.sb(es, "qbaug", [128, 8, 128], BF16)
            r_qbaug = self.R()
            qa_sb = self.sb(es, "qa_sb", [69, 16, 128], BF16)
            r_qa = self.R()
            NKB = 3
            kbuf = [self.sb(es, f"kbuf{i}", [128, 4, 1024], BF16) for i in range(NKB)]
            r_kbuf = [self.R() for _ in range(NKB)]
            vbuf = [self.sb(es, f"vbuf{i}", [128, 8, 4, 132], BF16) for i in range(NKB)]
            r_vbuf = [self.R() for _ in range(NKB)]
            kaug_sb = [self.sb(es, f"kaug{i}", [128, 1024], BF16) for i in range(NKB)]
            r_kaug = [self.R() for _ in range(NKB)]
            pbuf = [self.sb(es, f"pbuf{i}", [128, 4, 128], BF16) for i in range(5)]
            r_pbuf = [self.R() for _ in range(5)]
            ysb = [self.sb(es, f"ysb{i}", [128, D], BF16) for i in range(2)]
            r_ysb = [self.R() for _ in range(2)]
            junk = self.sb(es, "junk128", [128, 128], F32)
            r_junk = self.R()
            sv2 = self.sb(es, "sv2", [128, 16], F32)
            oraw = self.sb(es, "oraw", [128, D], F32)
            r_oraw = self.R()
            ssq = self.sb(es, "ssq", [128, 16], F32)
            r_ssq = self.R()
            r_sv2 = [self.R() for _ in range(2)]
            for i in range(NKB):
                t.op("pool", lambda e, i=i: e.memset(kaug_sb[i][:], 0.0), writes=[r_kaug[i]])
            t.op("pool", lambda e: e.memset(qbaug[:], 0.0), writes=[r_qbaug])
            kcnt = [0]
            pcnt = [0]
            scnt = [0]
            kitc = [0]
            rcnt = [0]

            for j in range(nslot):
                tq = 8 * j + 7
                NT = tq + 1
                N = NT * 128
                q0 = j * 128
                ngrp = NT // 4
                t.dma("sp", qi_sb[:], self.d_qit[:, :, q0:q0 + 128].rearrange("h d n -> d h n"), writes=[r_qi])
                t.dma("sp", sgn[:], self.d_sgn[q0:q0 + 128, :], writes=[r_dsg])
                for h in range(16):
                    en = "dve" if (h % 2 == 0 or os.environ.get("NOPOOL")) else "pool"
                    t.op(en, lambda e, h=h: e.tensor_scalar(out=dsg[:, h, :], in0=self.identb[:], scalar1=sgn[:, h:h + 1],
                                                            scalar2=None, op0=ALU.mult),
                         reads=[r_dsg, self.r_const], writes=[r_dsg])
                for kg in range(ngrp):
                    if kg % 2 == 0:
                        kb = kitc[0] % 2
                        kitc[0] += 1
                        w = min(1024, N - kg * 512)
                        t.dma("sp", kit_sb[kb][:, 0:w], self.d_kit[:, kg * 512:kg * 512 + w], writes=[r_kit[kb]])
                    koff = (kg % 2) * 512

                    def logits(h, kb=kb, koff=koff):
                        lb = 4 + h % 3
                        t.op("pe", lambda e: e.matmul(PS[lb][:, :], lhsT=qi_sb[:, h, :], rhs=kit_sb[kb][:, koff:koff + 512],
                                                      start=True, stop=True),
                             reads=[r_qi, r_kit[kb]], writes=[PR[lb]])

                    def relu(h):
                        lb = 4 + h % 3
                        rb = rcnt[0] % 4
                        rcnt[0] += 1
                        if h % 2 == 0 and "b" not in EXP:
                            t.op("act", lambda e: e.activation(out=rbuf[rb][:], in_=PS[lb][:, :], func=AF.Relu),
                                 reads=[PR[lb]], writes=[r_rbuf[rb]])
                        else:
                            t.op("dve", lambda e: e.tensor_scalar(out=rbuf[rb][:], in0=PS[lb][:, :], scalar1=0.0,
                                                                  scalar2=None, op0=ALU.max),
                                 reads=[PR[lb]], writes=[r_rbuf[rb]])
                        return rb

                    def hsum(h, rb):
                        t.op("pe", lambda e: e.matmul(PS[7][:, :], lhsT=dsg[:, h, :], rhs=rbuf[rb][:],
                                                      start=(h == 0), stop=(h == 15)),
                             reads=[r_dsg, r_rbuf[rb]], writes=[PR[7]])
                    if "e" in EXP:
                        continue
                    logits(0)
                    logits(1)
                    for h in range(16):
                        if h + 2 < 16:
                            logits(h + 2)
                        rb = relu(h)
                        if "d" not in EXP:
                            hsum(h, rb)
                    if "d" in EXP:
                        continue
                    if "f" in EXP:
                        continue
                    sl = slice(kg * 512, (kg + 1) * 512)
                    if "g" in EXP:
                        pass
                    elif "a" in EXP:
                        t.op("dve", lambda e, kg=kg: e.reduce_max(out=am[:, kg:kg + 1], in_=PS[7][:, :], axis=AX.X),
                             reads=[PR[7]], writes=[r_am])
                    else:
                        t.op("dve", lambda e, kg=kg: e.tensor_reduce(out=am[:, kg:kg + 1], in_=PS[7][:, :], axis=AX.X,
                                                                     op=ALU.max, apply_absolute_value=True),
                             reads=[PR[7]], writes=[r_am])
                    if "h" not in EXP:
                        if "I" not in EXP:
                            t.op("dve", lambda e, sl=sl: e.tensor_copy(out=score[:, sl], in_=PS[7][:, :]),
                                 reads=[PR[7]], writes=[r_sm])
                        else:
                            t.op("act", lambda e, sl=sl: e.activation(out=score[:, sl], in_=PS[7][:, :], func=AF.Identity),
                                 reads=[PR[7]], writes=[r_sm])
                    for tl in range(4):
                        if "c" in EXP:
                            break
                        tt = kg * 4 + tl
                        ts_ = slice(tt * 128, (tt + 1) * 128)
                        if tt < 7:
                            t.op("dve", lambda e, ts_=ts_, tt=tt: e.tensor_scalar(
                                out=score[:, ts_], in0=score[:, ts_], scalar1=dummy[:, tt:tt + 1], scalar2=None,
                                op0=ALU.add), reads=[r_sm, r_c2], writes=[r_sm])
                        if tt == NT - 1:
                            t.op("dve", lambda e, ts_=ts_: e.tensor_tensor(out=score[:, ts_], in0=score[:, ts_],
                                                                           in1=dmask[:], op=ALU.add),
                                 reads=[r_sm, r_c2], writes=[r_sm])
                if STOP <= 1:
                    continue
                LO, W0, MID, CNT, PRED, AMX = 0, 1, 2, 3, 4, 7
                col = lambda i: sv[:, i:i + 1]
                t.op("dve", lambda e: e.reduce_max(out=col(AMX), in_=am[:, 0:ngrp], axis=AX.X), reads=[r_am], writes=[r_sv])
                t.op("dve", lambda e: e.tensor_scalar(out=col(LO), in0=col(AMX), scalar1=1.0, scalar2=-1.0, op0=ALU.add, op1=ALU.mult),
                     reads=[r_sv], writes=[r_sv])
                t.op("dve", lambda e: e.tensor_scalar(out=col(W0), in0=col(AMX), scalar1=1.0, scalar2=2.0, op0=ALU.add, op1=ALU.mult),
                     reads=[r_sv], writes=[r_sv])
                bit = [0]

                def bisect(nit):
                    for it in range(nit):
                        f = 0.5 ** (bit[0] + 1)
                        bit[0] += 1
                        t.op("dve", lambda e, f=f: e.scalar_tensor_tensor(out=col(MID), in0=col(W0), scalar=f, in1=col(LO),
                                                                          op0=ALU.mult, op1=ALU.add), reads=[r_sv], writes=[r_sv])
                        t.op("dve", lambda e: e.tensor_scalar(out=mask[:, 0:N], in0=score[:, 0:N], scalar1=col(MID), scalar2=None,
                                                              op0=ALU.is_gt, op1=ALU.add, accum_out=col(CNT)),
                             reads=[r_sm, r_sv], writes=[r_mask, r_sv])
                        t.op("dve", lambda e, f=f: e.tensor_scalar(out=col(PRED), in0=col(CNT), scalar1=float(TOPK) - 0.5, scalar2=f,
                                                                   op0=ALU.is_gt, op1=ALU.mult), reads=[r_sv], writes=[r_sv])
                        t.op("dve", lambda e: e.scalar_tensor_tensor(out=col(LO), in0=col(W0), scalar=col(PRED), in1=col(LO),
                                                                     op0=ALU.mult, op1=ALU.add), reads=[r_sv], writes=[r_sv])

                t.dma("sp", qb_sb[:], self.d_qbt[:, :, q0:q0 + 128].rearrange("h d n -> d h n"), writes=[r_qb])
                t.dma("sp", qa_sb[0:64, :, :], self.d_qat[:, :, q0:q0 + 128].rearrange("m d n -> d m n"), writes=[r_qa])
                for m_ in range(2):
                    t.dma("sp", qa_sb[64:69, :, :].rearrange("r (h m) n -> r h m n", m=2)[:, :, m_, :],
                          self.I("c_qaug")[2:7, j, :, :], writes=[r_qa])

                def attn_group(kind, gi, ab):
                    b0, b1_ = (2, 3) if ab == 0 else (4, 5)
                    accs = [PS[b0][:, 0:129], PS[b0][:, 129:258], PS[b0][:, 258:387], PS[b1_][:, 0:129]]
                    first_in_bank = [True, False, False, True]
                    RA = [PR[b0], PR[b1_]]
                    pendq = []
                    for tg in range(NT // 8):
                        kb = kcnt[0] % NKB
                        kcnt[0] += 1
                        ksl = slice(tg * 1024, (tg + 1) * 1024)
                        if kind == "dsa":
                            t.dma("sp", kbuf[kb][:, :, :], self.d_kbt[4 * gi:4 * gi + 4, :, ksl].rearrange("h d n -> d h n"),
                                  writes=[r_kbuf[kb]])
                            t.dma("sp", kaug_sb[kb][0:7, :], self.I("c_kaug")[:, ksl], writes=[r_kaug[kb]])
                            t.dma("sp", vbuf[kb][:, :, :, :].rearrange("p t h e -> p t (h e)"),
                                  self.d_vb[ksl, 4 * gi:4 * gi + 4, :].rearrange("(t p) h e -> p t (h e)", p=128),
                                  writes=[r_vbuf[kb]])
                        else:
                            t.dma("sp", kbuf[kb][0:64, :, :], self.d_kat[4 * gi:4 * gi + 4, :, ksl].rearrange("m d n -> d m n"),
                                  writes=[r_kbuf[kb]])
                            t.dma("sp", kbuf[kb][64:69, :, :], self.I("c_kaug")[2:7, ksl].unsqueeze(1).broadcast_to([5, 4, 1024]),
                                  writes=[r_kbuf[kb]])
                            t.dma("sp", vbuf[kb][:, :, 0:2, :].rearrange("p t h e -> p t (h e)"),
                                  self.d_va[ksl, 2 * gi:2 * gi + 2, :].rearrange("(t p) h e -> p t (h e)", p=128),
                                  writes=[r_vbuf[kb]])
                        for tl in range(8):
                            tt = tg * 8 + tl
                            sb_ = (0, 1, 6, 7)[scnt[0] % 4]
                            scnt[0] += 1
                            diag = (tt == NT - 1)

                            def qk(e, kb=kb, tl=tl, sb_=sb_, diag=diag):
                                tsl = slice(tl * 128, (tl + 1) * 128)
                                for i in range(4):
                                    reg = PS[sb_][:, i * 128:(i + 1) * 128]
                                    if kind == "dsa":
                                        ins = e.matmul(reg, lhsT=kbuf[kb][:, i, tsl], rhs=qb_sb[:, 4 * gi + i, :],
                                                       start=(i == 0), stop=False, skip_group_check=True)
                                        hh = 4 * gi + i
                                    else:
                                        ins = e.matmul(reg, lhsT=kbuf[kb][0:69, i, tsl], rhs=qa_sb[0:69, 4 * gi + i, :],
                                                       start=True, stop=not diag)
                                        hh = 2 * gi + i // 2
                                    if diag:
                                        ins = e.matmul(reg, lhsT=self.identb[:], rhs=dg[:, hh, :], start=False,
                                                       stop=(kind != "dsa"), skip_group_check=(kind == "dsa"))
                                if kind == "dsa":
                                    ins = e.matmul(PS[sb_][:, :], lhsT=kaug_sb[kb][:, tsl],
                                                   rhs=qbaug[:, 4 * gi:4 * gi + 4, :].rearrange("r h n -> r (h n)"),
                                                   start=False, stop=True, skip_group_check=True)
                                return ins
                            rd = [r_kbuf[kb], self.r_const, r_c2] + ([r_kaug[kb], r_qbaug, r_qb] if kind == "dsa" else [r_qa])
                            t.op("pe", qk, reads=rd, writes=[PR[sb_]])
                            pb_ = pcnt[0] % 5
                            pcnt[0] += 1
                            t.op("act", lambda e, sb_=sb_, pb_=pb_: e.activation(
                                out=pbuf[pb_][:].rearrange("p h n -> p (h n)"), in_=PS[sb_][:, :], func=AF.Exp),
                                reads=[PR[sb_]], writes=[r_pbuf[pb_]])
                            if kind == "dsa":
                                t.op("dve", lambda e, pb_=pb_, tt=tt: e.scalar_tensor_tensor(
                                    out=pbuf[pb_][:], in0=pbuf[pb_][:], scalar=1e30,
                                    in1=maskT[:, tt * 128:(tt + 1) * 128].unsqueeze(1).broadcast_to([128, 4, 128]),
                                    op0=ALU.min, op1=ALU.mult), reads=[r_pbuf[pb_], r_sm], writes=[r_pbuf[pb_]])

                            def pv(e, kb=kb, tl=tl, pb_=pb_, tt=tt):
                                for i in range(4):
                                    vh = i if kind == "dsa" else i // 2
                                    ins = e.matmul(accs[i], lhsT=pbuf[pb_][:, i, :], rhs=vbuf[kb][:, tl, vh, 0:129],
                                                   start=(tt == 0 and first_in_bank[i]), stop=(tt == NT - 1),
                                                   skip_group_check=True)
                                return ins
                            pendq.append(lambda pv=pv, kb=kb, pb_=pb_: t.op(
                                "pe", pv, reads=[r_pbuf[pb_], r_vbuf[kb]], writes=RA))
                            if len(pendq) > 3:
                                pendq.pop(0)()
                    while pendq:
                        pendq.pop(0)()
                    s0 = 8 * ab
                    if kind == "dsa":
                        for i in range(4):
                            hh = 4 * gi + i
                            t.op("dve", lambda e, i=i: e.reciprocal(out=sv2[:, s0 + i:s0 + i + 1], in_=accs[i][:, 128:129]),
                                 reads=RA, writes=[r_sv2[ab]])
                            t.op("dve", lambda e, i=i, hh=hh: e.tensor_scalar(
                                out=ysb[1][:, hh * 128:(hh + 1) * 128], in0=accs[i][:, 0:128], scalar1=sv2[:, s0 + i:s0 + i + 1],
                                scalar2=None, op0=ALU.mult), reads=RA + [r_sv2[ab]], writes=[r_ysb[1]])
                    else:
                        for hl in range(2):
                            hh = 2 * gi + hl
                            a0, a1 = accs[2 * hl], accs[2 * hl + 1]
                            c0 = s0 + 4 * hl
                            of_ = oraw[:, hh * 128:(hh + 1) * 128]
                            r_of_ = r_oraw
                            t.op("dve", lambda e, a0=a0, c0=c0: e.reciprocal(out=sv2[:, c0:c0 + 1], in_=a0[:, 128:129]),
                                 reads=RA, writes=[r_sv2[ab]])
                            t.op("dve", lambda e, a1=a1, c0=c0: e.reciprocal(out=sv2[:, c0 + 1:c0 + 2], in_=a1[:, 128:129]),
                                 reads=RA, writes=[r_sv2[ab]])
                            t.op("dve", lambda e, c0=c0: e.tensor_tensor(out=sv2[:, c0 + 1:c0 + 2], in0=sv2[:, c0 + 1:c0 + 2],
                                                                         in1=nlam[:], op=ALU.mult),
                                 reads=[r_sv2[ab], r_c2], writes=[r_sv2[ab]])
                            t.op("dve", lambda e, a0=a0, c0=c0, of_=of_: e.tensor_scalar(
                                out=of_, in0=a0[:, 0:128], scalar1=sv2[:, c0:c0 + 1], scalar2=None, op0=ALU.mult),
                                reads=RA + [r_sv2[ab]], writes=[r_of_])
                            t.op("dve", lambda e, a1=a1, c0=c0, of_=of_: e.scalar_tensor_tensor(
                                out=of_, in0=a1[:, 0:128], scalar=sv2[:, c0 + 1:c0 + 2], in1=of_,
                                op0=ALU.mult, op1=ALU.add), reads=RA + [r_sv2[ab], r_of_], writes=[r_of_])

                nb_per = NBISECT // 4
                for gi in range(4):
                    bisect(nb_per)
                    attn_group("diff", gi, gi % 2)
                bisect(NBISECT - 4 * nb_per)
                for hh in range(8):
                    t.op("act", lambda e, hh=hh: e.activation(out=junk[:], in_=oraw[:, hh * 128:(hh + 1) * 128], func=AF.Square,
                                                              accum_out=ssq[:, hh:hh + 1]),
                         reads=[r_oraw], writes=[r_ssq, r_junk])
                t.op("dve", lambda e: e.tensor_scalar(out=ssq[:, 0:8], in0=ssq[:, 0:8], scalar1=1.0 / 128.0, scalar2=LN_EPS,
                                                      op0=ALU.mult, op1=ALU.add), reads=[r_ssq], writes=[r_ssq])
                t.op("act", lambda e: e.activation(out=ssq[:, 0:8], in_=ssq[:, 0:8], func=AF.Sqrt), reads=[r_ssq], writes=[r_ssq])
                t.op("dve", lambda e: e.reciprocal(out=ssq[:, 8:16], in_=ssq[:, 0:8]), reads=[r_ssq], writes=[r_ssq])
                for hh in range(8):
                    t.op("dve", lambda e, hh=hh: e.scalar_tensor_tensor(
                        out=ysb[0][:, hh * 128:(hh + 1) * 128], in0=oraw[:, hh * 128:(hh + 1) * 128], scalar=ssq[:, 8 + hh:9 + hh],
                        in1=gbc[:], op0=ALU.mult, op1=ALU.mult), reads=[r_oraw, r_ssq, r_c2], writes=[r_ysb[0]])
                t.dma("sp", self.d_ya[q0:q0 + 128, :], ysb[0][:], reads=[r_ysb[0]])
                t.op("dve", lambda e: e.tensor_scalar(out=mask[:, 0:N], in0=score[:, 0:N], scalar1=col(LO), scalar2=None,
                                                      op0=ALU.is_gt), reads=[r_sm, r_sv], writes=[r_mask])
                for kg in range(ngrp):
                    t.op("dve", lambda e, kg=kg: e.scalar_tensor_tensor(
                        out=tmp512[:], in0=iota[:], scalar=float(kg * 512), in1=mask[:, kg * 512:(kg + 1) * 512],
                        op0=ALU.add, op1=ALU.mult), reads=[r_mask, r_c2], writes=[r_tmp])
                    t.op("dve", lambda e, kg=kg: e.reduce_max(out=am[:, kg:kg + 1], in_=tmp512[:], axis=AX.X),
                         reads=[r_tmp], writes=[r_am])
                MP, DD, AF_, BF_ = 8, 9, 10, 11
                t.op("dve", lambda e: e.reduce_max(out=col(MP), in_=am[:, 0:ngrp], axis=AX.X), reads=[r_am], writes=[r_sv])
                t.op("dve", lambda e: e.tensor_scalar(out=col(DD), in0=col(MP), scalar1=pidx1[:, 0:1], scalar2=float(128 * tq),
                                                      op0=ALU.subtract, op1=ALU.subtract), reads=[r_sv, r_c2], writes=[r_sv])
                t.op("act", lambda e: e.activation(out=col(DD), in_=col(DD), func=AF.Abs), reads=[r_sv], writes=[r_sv])
                t.op("dve", lambda e: e.tensor_copy(out=svi[:, 0:1], in_=col(DD)), reads=[r_sv], writes=[r_sv])
                t.op("dve", lambda e: e.tensor_single_scalar(out=svi[:, 1:2], in_=svi[:, 0:1], scalar=7,
                                                             op=ALU.arith_shift_right), reads=[r_sv], writes=[r_sv])
                t.op("dve", lambda e: e.tensor_copy(out=col(AF_), in_=svi[:, 1:2]), reads=[r_sv], writes=[r_sv])
                t.op("dve", lambda e: e.scalar_tensor_tensor(out=col(BF_), in0=col(AF_), scalar=-128.0, in1=col(DD),
                                                             op0=ALU.mult, op1=ALU.add), reads=[r_sv], writes=[r_sv])
                t.op("dve", lambda e: e.tensor_copy(out=ab[:, 0:2], in_=sv[:, AF_:AF_ + 2]), reads=[r_sv], writes=[r_sv])
                t.dma("sp", qbaug[2:7, :, :], self.I("c_qaug")[2:7, j, :, :], writes=[r_qbaug])
                t.op("pe", lambda e: e.matmul(PS[7][0:2, 0:128], lhsT=ab[:, 0:2], rhs=self.identb[:], start=True, stop=True),
                     reads=[r_sv, self.r_const], writes=[PR[7]])
                t.op("dve", lambda e: e.tensor_tensor(out=qbaug[0:2, :, :],
                                                      in0=PS[7][0:2, 0:128].unsqueeze(1).broadcast_to([2, 8, 128]),
                                                      in1=slopetab[:], op=ALU.mult),
                     reads=[PR[7], r_c2], writes=[r_qbaug])
                pbf = PS[6].bitcast(BF16)
                for g4 in range(ngrp):
                    def tr(e, g4=g4):
                        for tl in range(4):
                            tt = g4 * 4 + tl
                            ins = e.transpose(pbf[:, tl * 128:(tl + 1) * 128], mask[:, tt * 128:(tt + 1) * 128], self.identb[:])
                        return ins
                    t.op("pe", tr, reads=[r_mask, self.r_const], writes=[PR[6]])
                    if g4 % 2 == 0:
                        t.op("act", lambda e, g4=g4: e.copy(out=maskT[:, g4 * 512:(g4 + 1) * 512], in_=pbf[:, 0:512]),
                             reads=[PR[6]], writes=[r_sm])
                    else:
                        t.op("dve", lambda e, g4=g4: e.tensor_copy(out=maskT[:, g4 * 512:(g4 + 1) * 512], in_=pbf[:, 0:512]),
                             reads=[PR[6]], writes=[r_sm])
                for gi in range(2):
                    attn_group("dsa", gi, gi % 2)
                t.dma("sp", self.d_yb[q0:q0 + 128, :], ysb[1][:], reads=[r_ysb[1]])
            self.barrier()

    def load_w_bf16(self, es, name, src_ap, r):
        wsb = self.sb(es, name, [128, 8, 1024], BF16)
        v = src_ap.rearrange("(c p) n -> p c n", p=128)
        for a in range(0, 1024, 512):
            self.t.dma("pool", wsb[:, :, a:a + 512], v[:, :, a:a + 512], writes=[r])
        return wsb

    def ln_stats(self, xin, r_x, stats, mv, r_st):
        t = self.t
        for hh in range(2):
            t.op("dve", lambda e, hh=hh: e.bn_stats(out=stats[:, hh, :], in_=xin[:, hh * 512:(hh + 1) * 512]),
                 reads=[r_x], writes=[r_st])
        t.op("dve", lambda e: e.bn_aggr(out=mv[:, 0:2], in_=stats[:].rearrange("p a b -> p (a b)")),
             reads=[r_st], writes=[r_st])
        t.op("dve", lambda e: e.tensor_scalar(out=mv[:, 2:3], in0=mv[:, 1:2], scalar1=LN_EPS, scalar2=None,
                                              op0=ALU.add), reads=[r_st], writes=[r_st])
        t.op("act", lambda e: e.activation(out=mv[:, 2:3], in_=mv[:, 2:3], func=AF.Sqrt),
             reads=[r_st], writes=[r_st])
        t.op("dve", lambda e: e.reciprocal(out=mv[:, 3:4], in_=mv[:, 2:3]), reads=[r_st], writes=[r_st])

    def phase3(self):
        nc, t = self.nc, self.t
        PS, PR = self.ps, self.psr
        nslot = self.nslot
        with ExitStack() as es:
            r_w = self.R()
            wa = self.load_w_bf16(es, "wa", self.I("w_branch_a"), r_w)
            wb = self.load_w_bf16(es, "wb", self.I("w_branch_b"), r_w)
            wo = self.load_w_bf16(es, "wo", self.I("w_out"), r_w)
            g1 = self.sb(es, "g1bc", [128, D], F32)
            b1 = self.sb(es, "b1bc", [128, D], F32)
            r_c = self.R()
            self.ga_bc = self.sb(es, "ga_bc", [128, D], F32)
            t.dma("sp", self.ga_bc[:], self.d_mod[2 * D:3 * D].partition_broadcast(128), reads=[self.r_dmod], writes=[self.r_mod])
            t.dma("sp", g1[:], self.I("ln1_g").partition_broadcast(128), writes=[r_c])
            t.dma("sp", b1[:], self.I("ln1_b").partition_broadcast(128), writes=[r_c])
            yab = [self.sb(es, f"yab{i}", [128, 2, D], BF16) for i in range(2)]
            r_yab = [self.R() for _ in range(2)]
            yT = [self.sb(es, f"yT{i}", [128, 2, 8, 128], BF16) for i in range(2)]
            r_yT = [self.R() for _ in range(2)]
            gate = [self.sb(es, f"gate{i}", [128, 2 * D], F32) for i in range(2)]
            r_gate = [self.R() for _ in range(2)]
            xt = [self.sb(es, f"x3_{i}", [128, D], F32) for i in range(2)]
            r_xt = [self.R() for _ in range(2)]
            m1 = self.sb(es, "m1", [128, D], F32)
            m2 = self.sb(es, "m2", [128, D], F32)
            mg = self.sb(es, "mg", [128, D], BF16)
            mgT = self.sb(es, "mgT", [128, 8, 128], BF16)
            r_m = self.R()
            r_mg = self.R()
            r_mgT = self.R()
            xnew = self.sb(es, "xnew", [128, D], F32)
            r_xn = self.R()
            x1 = [self.sb(es, f"x1_{i}", [128, D], F32) for i in range(2)]
            r_x1 = [self.R() for _ in range(2)]
            stats = self.sb(es, "st3", [128, 2, 6], F32)
            mv = self.sb(es, "mv3", [128, 4], F32)
            r_st = self.R()
            for j in range(nslot):
                b = j % 2
                q0 = j * 128
                tok0 = (8 * j + 7) * 128
                t.dma("sp", yab[b][:, 0, :], self.d_ya[q0:q0 + 128, :], writes=[r_yab[b]])
                t.dma("sp", yab[b][:, 1, :], self.d_yb[q0:q0 + 128, :], writes=[r_yab[b]])
                t.dma("sp", gate[b][:], self.d_gate[q0:q0 + 128, :], writes=[r_gate[b]])
                t.dma("sp", xt[b][:], self.I("x")[tok0:tok0 + 128, :], writes=[r_xt[b]])
                for br in range(2):
                    pbf = PS[br].bitcast(BF16)

                    def tr(e, br=br, b=b, pbf=pbf):
                        for c in range(8):
                            ins = e.transpose(pbf[:, c * 128:(c + 1) * 128], yab[b][:, br, c * 128:(c + 1) * 128], self.identb[:])
                        return ins
                    t.op("pe", tr, reads=[r_yab[b], self.r_const], writes=[PR[br]])
                    t.op("act", lambda e, br=br, b=b, pbf=pbf: e.copy(
                        out=yT[b][:, br, :, :], in_=pbf[:, :].rearrange("p (c n) -> p c n", c=8)),
                        reads=[PR[br]], writes=[r_yT[b]])
                for cg in range(2):
                    csl = slice(cg * 512, (cg + 1) * 512)
                    for br, w_ in ((0, wa), (1, wb)):
                        pb = 2 + br

                        def mm(e, br=br, w_=w_, pb=pb, b=b, csl=csl):
                            for c in range(8):
                                ins = e.matmul(PS[pb][:, :], lhsT=yT[b][:, br, c, :], rhs=w_[:, c, csl],
                                               start=(c == 0), stop=(c == 7))
                            return ins
                        t.op("pe", mm, reads=[r_yT[b], r_w], writes=[PR[pb]])
                    t.op("dve", lambda e, b=b, csl=csl, cg=cg: e.tensor_tensor(
                        out=m1[:, csl], in0=PS[2][:, :], in1=gate[b][:, cg * 512:(cg + 1) * 512], op=ALU.mult),
                        reads=[PR[2], r_gate[b]], writes=[r_m])
                    t.op("dve", lambda e, b=b, csl=csl, cg=cg: e.tensor_tensor(
                        out=m2[:, csl], in0=PS[3][:, :], in1=gate[b][:, D + cg * 512:D + (cg + 1) * 512], op=ALU.mult),
                        reads=[PR[3], r_gate[b]], writes=[r_m])
                    t.op("dve", lambda e, csl=csl: e.tensor_tensor(out=mg[:, csl], in0=m1[:, csl], in1=m2[:, csl], op=ALU.add),
                         reads=[r_m], writes=[r_mg])
                pbf = PS[4].bitcast(BF16)

                def tr2(e, pbf=pbf):
                    for c in range(8):
                        ins = e.transpose(pbf[:, c * 128:(c + 1) * 128], mg[:, c * 128:(c + 1) * 128], self.identb[:])
                    return ins
                t.op("pe", tr2, reads=[r_mg, self.r_const], writes=[PR[4]])
                t.op("act", lambda e, pbf=pbf: e.copy(out=mgT[:], in_=pbf[:, :].rearrange("p (c n) -> p c n", c=8)),
                     reads=[PR[4]], writes=[r_mgT])
                for cg in range(2):
                    csl = slice(cg * 512, (cg + 1) * 512)
                    pb = 5 + cg

                    def mm3(e, pb=pb, csl=csl):
                        for c in range(8):
                            ins = e.matmul(PS[pb][:, :], lhsT=mgT[:, c, :], rhs=wo[:, c, csl], start=(c == 0), stop=(c == 7))
                        return ins
                    t.op("pe", mm3, reads=[r_mgT, r_w], writes=[PR[pb]])
                    t.op("dve", lambda e, pb=pb, csl=csl: e.tensor_tensor(out=m1[:, csl], in0=PS[pb][:, :],
                                                                          in1=self.ga_bc[:, csl], op=ALU.mult),
                         reads=[PR[pb], self.r_mod], writes=[r_m])
                    t.op("dve", lambda e, b=b, csl=csl: e.scalar_tensor_tensor(
                        out=xnew[:, csl], in0=xt[b][:, csl], scalar=ALPHA, in1=m1[:, csl], op0=ALU.mult, op1=ALU.add),
                        reads=[r_xt[b], r_m], writes=[r_xn])
                self.ln_stats(xnew, r_xn, stats, mv, r_st)
                t.op("dve", lambda e, b=b: e.tensor_scalar(out=x1[b][:], in0=xnew[:], scalar1=mv[:, 0:1], scalar2=mv[:, 3:4],
                                                           op0=ALU.subtract, op1=ALU.mult),
                     reads=[r_xn, r_st], writes=[r_x1[b]])
                t.op("pool", lambda e, b=b: e.tensor_tensor(out=x1[b][:], in0=x1[b][:], in1=g1[:], op=ALU.mult),
                     reads=[r_x1[b], r_c], writes=[r_x1[b]])
                t.op("pool", lambda e, b=b: e.tensor_tensor(out=x1[b][:], in0=x1[b][:], in1=b1[:], op=ALU.add),
                     reads=[r_x1[b], r_c], writes=[r_x1[b]])
                t.dma("sp", self.d_x1[q0:q0 + 128, :], x1[b][:], reads=[r_x1[b]])
            self.barrier()

    def phase4(self):
        nc, t = self.nc, self.t
        PS, PR = self.ps, self.psr
        NQ = self.NQ
        HT = min(1024, NQ)
        nhalf = NQ // HT
        TG = min(512, HT)
        ntg = HT // TG
        ntt = HT // 128
        with ExitStack() as es:
            scf = self.sb(es, "scf_bc", [128, D], F32)
            shf = self.sb(es, "shf_bc", [128, D], F32)
            g2 = scf
            b2l = shf
            wr = self.sb(es, "wr", [128, 8, NEXP], F32)
            brr = self.sb(es, "brr", [1, NEXP], F32)
            onesf = self.sb(es, "onesf4", [1, 128], F32)
            b2w = self.sb(es, "b2w", [NEXP, D], F32)
            b1raw = self.sb(es, "b1raw", [NEXP, 2 * DFF], F32)
            b1g = self.sb(es, "b1g", [128, 8, NEXP], F32)
            b1l = self.sb(es, "b1l", [128, 8, NEXP], F32)
            r_c = self.R()
            self.gf_bc = self.sb(es, "gf_bc", [128, D], F32)
            t.dma("sp", self.gf_bc[:], self.d_mod[5 * D:6 * D].partition_broadcast(128), reads=[self.r_dmod], writes=[self.r_mod])
            r_md = self.R()
            t.dma("sp", wr[:], self.I("w_router").rearrange("(c p) n -> p c n", p=128), writes=[r_c])
            t.dma("sp", brr[:], self.I("b_router").rearrange("(o n) -> o n", o=1), writes=[r_c])
            t.dma("sp", b2w[:], self.I("b_e2"), writes=[r_c])
            t.dma("sp", b1raw[:], self.I("b_e1"), writes=[r_c])
            t.op("dve", lambda e: e.memset(onesf[:], 1.0), writes=[r_c])
            b1v = b1raw[:].rearrange("e (p f two) -> e p f two", p=8, two=2)
            for p in range(8):
                for two, dst in ((0, b1g), (1, b1l)):
                    t.op("pe", lambda e, p=p, two=two: e.transpose(PS[7][:, 0:NEXP], b1v[:, p, :, two], self.identf[0:NEXP, 0:NEXP]),
                         reads=[r_c, self.r_const], writes=[PR[7]])
                    t.op("dve", lambda e, p=p, dst=dst, two=two: e.tensor_scalar(
                        out=dst[:, p, :], in0=PS[7][:, 0:NEXP], scalar1=float(two), scalar2=None, op0=ALU.add),
                        reads=[PR[7]], writes=[r_c])
            vT = self.sb(es, "vT", [128, 8, HT], BF16)
            r_vT = self.R()
            yacc = self.sb(es, "yacc", [128, ntt, D], F32)
            r_y = self.R()
            gate = self.sb(es, "gate4", [128, ntt, NEXP], F32)
            r_g = self.R()
            aT = [self.sb(es, f"aT{i}", [128, 8, HT], BF16) for i in range(2)]
            r_aT = [self.R() for _ in range(2)]
            w1p = [self.sb(es, f"w1p{i}", [128, 8, 256], BF16) for i in range(5)]
            r_w1 = [self.R() for _ in range(5)]
            w2e = [self.sb(es, f"w2e{i}", [128, 8, D], BF16) for i in range(2)]
            r_w2 = [self.R() for _ in range(2)]
            glu = [self.sb(es, f"glu{i}", [128, TG], F32) for i in range(2)]
            sig = [self.sb(es, f"sig{i}", [128, TG], F32) for i in range(2)]
            lin = [self.sb(es, f"lin{i}", [128, TG], F32) for i in range(2)]
            r_elg = [self.R() for _ in range(3)]
            r_ell = [self.R() for _ in range(3)]
            r_els = [self.R() for _ in range(3)]
            xt = [self.sb(es, f"x4_{i}", [128, D], F32) for i in range(2)]
            r_xt = [self.R() for _ in range(2)]
            vf = self.sb(es, "vf", [128, D], F32)
            vb16 = self.sb(es, "vb16", [128, D], BF16)
            vTf = self.sb(es, "vTf", [128, 8, 128], F32)
            r_v = self.R()
            stats = self.sb(es, "st4", [128, 2, 6], F32)
            mv = self.sb(es, "mv4", [128, 4], F32)
            r_st = self.R()
            rt = self.sb(es, "rt", [128, 4, NEXP], F32)
            m8 = self.sb(es, "m8", [128, 16], F32)
            gT = self.sb(es, "gT", [NEXP, 128], F32)
            r_rt = self.R()
            w1cnt = [0]
            wfc = [0]
            elc = [0]
            for hf in range(nhalf):
                h0 = hf * HT
                t.dma("sp", scf[:], self.d_mod[4 * D:5 * D].partition_broadcast(128), writes=[r_md])
                t.dma("sp", shf[:], self.d_mod[3 * D:4 * D].partition_broadcast(128), writes=[r_md])
                t.op("dve", lambda e: e.tensor_scalar(out=scf[:], in0=scf[:], scalar1=1.0, scalar2=None, op0=ALU.add),
                     reads=[r_md], writes=[r_md])
                for tt in range(ntt):
                    b = tt % 2
                    q0 = h0 + tt * 128
                    t.dma("sp", xt[b][:], self.d_x1[q0:q0 + 128, :], writes=[r_xt[b]])
                    self.ln_stats(xt[b], r_xt[b], stats, mv, r_st)
                    t.op("dve", lambda e, b=b: e.tensor_scalar(out=vf[:], in0=xt[b][:], scalar1=mv[:, 0:1], scalar2=mv[:, 3:4],
                                                               op0=ALU.subtract, op1=ALU.mult),
                         reads=[r_xt[b], r_st], writes=[r_v])
                    t.op("pool", lambda e: e.tensor_tensor(out=vf[:], in0=vf[:], in1=scf[:], op=ALU.mult),
                         reads=[r_v, r_md], writes=[r_v])
                    t.op("pool", lambda e: e.tensor_tensor(out=vf[:], in0=vf[:], in1=shf[:], op=ALU.add),
                         reads=[r_v, r_md], writes=[r_v])
                    t.op("dve", lambda e: e.tensor_copy(out=vb16[:], in_=vf[:]), reads=[r_v], writes=[r_v])
                    pbf = PS[0].bitcast(BF16)

                    def tr(e, pbf=pbf):
                        for c in range(8):
                            ins = e.transpose(pbf[:, c * 128:(c + 1) * 128], vb16[:, c * 128:(c + 1) * 128], self.identb[:])
                        return ins
                    t.op("pe", tr, reads=[r_v, self.r_const], writes=[PR[0]])
                    t.op("act", lambda e, tt=tt, pbf=pbf: e.copy(out=vT[:, :, tt * 128:(tt + 1) * 128],
                                                                 in_=pbf[:, :].rearrange("p (c n) -> p c n", c=8)),
                         reads=[PR[0]], writes=[r_vT])
                    for half2 in range(2):
                        def trf(e, half2=half2):
                            for c4 in range(4):
                                c = half2 * 4 + c4
                                ins = e.transpose(PS[1 + half2][:, c4 * 128:(c4 + 1) * 128], vf[:, c * 128:(c + 1) * 128], self.identf[:])
                            return ins
                        t.op("pe", trf, reads=[r_v, self.r_const], writes=[PR[1 + half2]])
                        t.op("dve", lambda e, half2=half2: e.tensor_copy(
                            out=vTf[:, half2 * 4:half2 * 4 + 4, :], in_=PS[1 + half2][:, :].rearrange("p (c n) -> p c n", c=4)),
                            reads=[PR[1 + half2]], writes=[r_v])

                    def mmr(e):
                        for c in range(8):
                            e.matmul(PS[3][:, 0:NEXP], lhsT=vTf[:, c, :], rhs=wr[:, c, :], start=(c == 0), stop=False)
                        return e.matmul(PS[3][:, 0:NEXP], lhsT=onesf[0:1, :], rhs=brr[0:1, :], start=False, stop=True)
                    t.op("pe", mmr, reads=[r_v, r_c], writes=[PR[3]])
                    LG, SEL, EX = 0, 1, 2
                    t.op("dve", lambda e: e.tensor_copy(out=rt[:, LG, :], in_=PS[3][:, 0:NEXP]), reads=[PR[3]], writes=[r_rt])
                    t.op("dve", lambda e: e.max(out=m8[:, 0:8], in_=rt[:, LG, :]), reads=[r_rt], writes=[r_rt])
                    t.op("dve", lambda e: e.tensor_scalar(out=rt[:, SEL, :], in0=rt[:, LG, :], scalar1=m8[:, 3:4], scalar2=None,
                                                          op0=ALU.is_ge), reads=[r_rt], writes=[r_rt])
                    t.op("dve", lambda e: e.tensor_scalar(out=m8[:, 8:9], in0=m8[:, 0:1], scalar1=-1.0, scalar2=None,
                                                          op0=ALU.mult), reads=[r_rt], writes=[r_rt])
                    t.op("act", lambda e: e.activation(out=rt[:, EX, :], in_=rt[:, LG, :], func=AF.Exp, bias=m8[:, 8:9], scale=1.0),
                         reads=[r_rt], writes=[r_rt])
                    t.op("dve", lambda e: e.tensor_tensor(out=rt[:, EX, :], in0=rt[:, EX, :], in1=rt[:, SEL, :], op=ALU.mult),
                         reads=[r_rt], writes=[r_rt])
                    t.op("dve", lambda e: e.reduce_sum(out=m8[:, 9:10], in_=rt[:, EX, :], axis=AX.X), reads=[r_rt], writes=[r_rt])
                    t.op("dve", lambda e: e.reciprocal(out=m8[:, 10:11], in_=m8[:, 9:10]), reads=[r_rt], writes=[r_rt])
                    t.op("dve", lambda e, tt=tt: e.tensor_scalar(out=gate[:, tt, :], in0=rt[:, EX, :], scalar1=m8[:, 10:11],
                                                                 scalar2=None, op0=ALU.mult), reads=[r_rt], writes=[r_g])
                    t.op("pe", lambda e, tt=tt: e.transpose(PS[3][0:NEXP, 128:256], gate[:, tt, :], self.identf[:]),
                         reads=[r_g, self.r_const], writes=[PR[3]])
                    t.op("dve", lambda e: e.tensor_copy(out=gT[:], in_=PS[3][0:NEXP, 128:256]), reads=[PR[3]], writes=[r_rt])
                    for cg in range(2):
                        t.op("pe", lambda e, cg=cg: e.matmul(PS[4 + cg][:, :], lhsT=gT[:, :], rhs=b2w[:, cg * 512:(cg + 1) * 512],
                                                            start=True, stop=True), reads=[r_rt, r_c], writes=[PR[4 + cg]])
                        t.op("dve", lambda e, cg=cg, tt=tt: e.tensor_copy(out=yacc[:, tt, cg * 512:(cg + 1) * 512], in_=PS[4 + cg][:, :]),
                             reads=[PR[4 + cg]], writes=[r_y])

                def stageA(e_):
                    ab_ = e_ % 2
                    for p in range(8):
                        wb_ = w1cnt[0] % 5
                        w1cnt[0] += 1
                        t.dma("pool", w1p[wb_][:], self.I("w_e1")[e_, :, p * 256:(p + 1) * 256].rearrange("(c q) n -> q c n", q=128),
                              writes=[r_w1[wb_]])
                        for tg in range(ntg):
                            tsl = slice(tg * TG, (tg + 1) * TG)
                            pg, pl = (0, 1) if (p * ntg + tg) % 2 == 0 else (2, 3)

                            def mm(e, wb_=wb_, tsl=tsl, pg=pg, pl=pl):
                                for two, pb in ((0, pg), (1, pl)):
                                    for c in range(8):
                                        ins = e.matmul(PS[pb][:, 0:TG], lhsT=w1p[wb_][:, c, two::2], rhs=vT[:, c, tsl],
                                                       start=(c == 0), stop=(c == 7))
                                return ins
                            t.op("pe", mm, reads=[r_w1[wb_], r_vT], writes=[PR[pg], PR[pl]])
                            k = elc[0] % 2
                            elc[0] += 1
                            t.op("dve", lambda e, k=k, pg=pg, p=p, e_=e_: e.tensor_scalar(
                                out=glu[k][:], in0=PS[pg][:, 0:TG], scalar1=b1g[:, p, e_:e_ + 1], scalar2=SWIGLU_LIMIT,
                                op0=ALU.add, op1=ALU.min), reads=[PR[pg], r_c], writes=[r_elg[k]])
                            t.op("dve", lambda e, k=k, pl=pl, p=p, e_=e_: e.tensor_scalar(
                                out=lin[k][:], in0=PS[pl][:, 0:TG], scalar1=b1l[:, p, e_:e_ + 1], scalar2=1.0 - SWIGLU_LIMIT,
                                op0=ALU.add, op1=ALU.max), reads=[PR[pl], r_c], writes=[r_ell[k]])
                            t.op("act", lambda e, k=k: e.activation(out=sig[k][:], in_=glu[k][:], func=AF.Sigmoid, scale=SWIGLU_ALPHA),
                                 reads=[r_elg[k]], writes=[r_els[k]])
                            t.op("dve", lambda e, k=k: e.scalar_tensor_tensor(
                                out=lin[k][:], in0=lin[k][:], scalar=1.0 + SWIGLU_LIMIT, in1=glu[k][:], op0=ALU.min, op1=ALU.mult),
                                reads=[r_ell[k], r_elg[k]], writes=[r_ell[k]])
                            t.op("dve", lambda e, k=k, ab_=ab_, p=p, tsl=tsl: e.tensor_tensor(
                                out=aT[ab_][:, p, tsl], in0=lin[k][:], in1=sig[k][:], op=ALU.mult),
                                reads=[r_ell[k], r_els[k]], writes=[r_aT[ab_]])

                def stageB(e_):
                    ab_ = e_ % 2
                    for tt in range(ntt):
                        for cg in range(2):
                            pb = 4 + (tt * 2 + cg) % 3

                            def mm(e, tt=tt, cg=cg, pb=pb):
                                for p in range(8):
                                    ins = e.matmul(PS[pb][:, :], lhsT=aT[ab_][:, p, tt * 128:(tt + 1) * 128],
                                                   rhs=w2e[ab_][:, p, cg * 512:(cg + 1) * 512], start=(p == 0), stop=(p == 7))
                                return ins
                            t.op("pe", mm, reads=[r_aT[ab_], r_w2[ab_]], writes=[PR[pb]])
                            t.op("dve", lambda e, tt=tt, cg=cg, pb=pb: e.scalar_tensor_tensor(
                                out=yacc[:, tt, cg * 512:(cg + 1) * 512], in0=PS[pb][:, :], scalar=gate[:, tt, e_:e_ + 1],
                                in1=yacc[:, tt, cg * 512:(cg + 1) * 512], op0=ALU.mult, op1=ALU.add),
                                reads=[PR[pb], r_g, r_y], writes=[r_y])

                def loadw2(e_):
                    ab_ = e_ % 2
                    v = self.I("w_e2")[e_].rearrange("(c q) n -> q c n", q=128)
                    for a in range(0, 1024, 512):
                        t.dma("pool", w2e[ab_][:, :, a:a + 512], v[:, :, a:a + 512], writes=[r_w2[ab_]])
                loadw2(0)
                stageA(0)
                for e_ in range(NEXP):
                    if e_ + 1 < NEXP:
                        loadw2(e_ + 1)
                        stageA(e_ + 1)
                    stageB(e_)
                t.dma("sp", g2[:], self.I("ln2_g").partition_broadcast(128), writes=[r_md])
                t.dma("sp", b2l[:], self.I("ln2_b").partition_broadcast(128), writes=[r_md])
                for tt in range(ntt):
                    b = tt % 2
                    q0 = h0 + tt * 128
                    t.dma("sp", xt[b][:], self.d_x1[q0:q0 + 128, :], writes=[r_xt[b]])
                    t.op("pool", lambda e, tt=tt: e.tensor_tensor(out=yacc[:, tt, :], in0=yacc[:, tt, :], in1=self.gf_bc[:], op=ALU.mult),
                         reads=[r_y, self.r_mod], writes=[r_y])
                    t.op("dve", lambda e, b=b, tt=tt: e.scalar_tensor_tensor(out=vf[:], in0=xt[b][:], scalar=ALPHA, in1=yacc[:, tt, :],
                                                                            op0=ALU.mult, op1=ALU.add),
                         reads=[r_xt[b], r_y], writes=[r_v])
                    self.ln_stats(vf, r_v, stats, mv, r_st)
                    t.op("dve", lambda e, b=b: e.tensor_scalar(out=xt[b][:], in0=vf[:], scalar1=mv[:, 0:1], scalar2=mv[:, 3:4],
                                                               op0=ALU.subtract, op1=ALU.mult),
                         reads=[r_v, r_st], writes=[r_xt[b]])
                    t.op("pool", lambda e, b=b: e.tensor_tensor(out=xt[b][:], in0=xt[b][:], in1=g2[:], op=ALU.mult),
                         reads=[r_xt[b], r_md], writes=[r_xt[b]])
                    t.op("pool", lambda e, b=b: e.tensor_tensor(out=xt[b][:], in0=xt[b][:], in1=b2l[:], op=ALU.add),
                         reads=[r_xt[b], r_md], writes=[r_xt[b]])
                    t.dma("sp", self.out[q0:q0 + 128, :], xt[b][:], reads=[r_xt[b]], writes=[self.r_out])
            self.barrier()


def make_consts(nslot, c):
    ntile = 8 * nslot
    S = 128 * ntile
    slopes = alibi_slopes(8)
    k = np.arange(S)
    tt = k // 128
    pk = k % 128
    ndummy = 7 - c
    kaug = np.zeros((7, S), np.float32)
    kaug[0] = 1.0
    kaug[1] = 1.0
    kaug[2] = 128.0 * tt
    kaug[3] = 1.0
    kaug[4] = 1.0
    kaug[5] = pk
    kaug[6] = (tt < ndummy).astype(np.float32)
    qaug = np.zeros((7, nslot, 8, 128), np.float32)
    ql = np.arange(128)
    for j in range(nslot):
        tq = 8 * j + 7
        for h in range(8):
            s = slopes[h]
            qaug[2, j, h] = s
            qaug[3, j, h] = -s * 128.0 * tq
            qaug[4, j, h] = -s * ql
            qaug[5, j, h] = s
            qaug[6, j, h] = NEG
    kk = np.arange(128)[:, None]
    qq = np.arange(128)[None, :]
    cend = (qq // 64 + 1) * 64
    dg = np.zeros((128, 8, 128), np.float32)
    for h in range(8):
        s = slopes[h]
        m = np.where(kk > qq, -2.0 * s * (kk - qq), 0.0)
        m = np.where(kk >= cend, NEG, m)
        dg[:, h, :] = m
    dmask = np.where(kk.T >= 0, 0.0, 0.0) * 0.0
    qq2 = np.arange(128)[:, None]
    kk2 = np.arange(128)[None, :]
    dmask = np.where(kk2 < (qq2 // 64 + 1) * 64, 0.0, -1e9).astype(np.float32)
    dummy = np.zeros((128, 8), np.float32)
    dummy[:, :ndummy] = -1e9
    iota = np.broadcast_to(np.arange(1, 513, dtype=np.float32)[None, :], (128, 512)).copy()
    slopetab = np.zeros((2, 8, 128), np.float32)
    for h in range(8):
        slopetab[0, h] = slopes[h] * 128.0
        slopetab[1, h] = slopes[h]
    return {
        "c_identb": np.eye(128, dtype=np.float32).astype(NPBF),
        "c_identf": np.eye(128, dtype=np.float32),
        "c_kaug": kaug.astype(NPBF),
        "c_qaug": qaug.astype(NPBF),
        "c_dg": dg.astype(NPBF),
        "c_dmask": dmask,
        "c_dummy": dummy,
        "c_iota": iota,
        "c_slopetab": slopetab,
        "c_pidx1": np.arange(1, 129, dtype=np.float32).reshape(128, 1),
    }


def make_in_maps(inputs, nslot, used=None):
    S = 128 * 8 * nslot
    f = lambda a: np.ascontiguousarray(np.asarray(a, dtype=np.float32))
    x = f(inputs["x"])[0]
    assert x.shape[0] == S
    shared = {
        "c": f(inputs["c"])[0],
        "w_ada": f(inputs["w_ada"])[0],
        "b_ada": f(inputs["b_ada"])[0],
        "w_in": f(inputs["w_in"])[0],
        "lamv": np.stack([f(inputs[k])[0] for k in ("lam_q1", "lam_k1", "lam_q2", "lam_k2")]),
        "diff_norm_g": f(inputs["diff_norm_g"])[0],
        "w_branch_a": f(inputs["w_branch_a"])[0],
        "w_branch_b": f(inputs["w_branch_b"])[0],
        "w_out": f(inputs["w_out"])[0],
        "ln1_g": f(inputs["ln1_g"])[0],
        "ln1_b": f(inputs["ln1_b"])[0],
        "w_router": f(inputs["w_router"])[0],
        "b_router": f(inputs["b_router"])[0],
        "w_e1": f(inputs["w_e1"])[0],
        "b_e1": f(inputs["b_e1"])[0],
        "w_e2": f(inputs["w_e2"])[0],
        "b_e2": f(inputs["b_e2"])[0],
        "ln2_g": f(inputs["ln2_g"])[0],
        "ln2_b": f(inputs["ln2_b"])[0],
    }
    maps = []
    for c in range(NCORE):
        m = dict(shared)
        m["x"] = np.ascontiguousarray(np.roll(x, 128 * (7 - c), axis=0))
        m.update(make_consts(nslot, c))
        maps.append({k: v for k, v in m.items() if used is None or k in used})
    return maps


_CACHE = {}


def run(inputs, nslot, debug=False, phases=99, trace=False):
    key = (nslot, debug, phases)
    mk = MK(nslot, debug=debug, phases=phases)
    nc = mk.build()
    in_maps = make_in_maps(inputs, nslot, used=set(mk.in_aps.keys()))
    res = run_bass_kernel_spmd(nc, in_maps, core_ids=list(range(NCORE)), trace=trace)
    return res


def kernel(**inputs):
    nslot = 16
    res = run(inputs, nslot)
    S = 128 * 8 * nslot
    out = np.zeros((1, S, D), np.float32)
    for c in range(NCORE):
        o = np.asarray(res.results[c]["out"], dtype=np.float32)
        for j in range(nslot):
            rt = 8 * j + c
            out[0, rt * 128:(rt + 1) * 128, :] = o[j * 128:(j + 1) * 128, :]
    return out
```

```python
import os
import numpy as np
import ml_dtypes
from contextlib import ExitStack
import concourse.bass as bass
import concourse.mybir as mybir
from concourse.bass_utils import run_bass_kernel_spmd

F32 = mybir.dt.float32
BF16 = mybir.dt.bfloat16
I32 = mybir.dt.int32
AF = mybir.ActivationFunctionType
ALU = mybir.AluOpType
AX = mybir.AxisListType
NPBF = ml_dtypes.bfloat16

D = 1024
NCORE = 8
NEXP = 32
DFF = 1024
TOPK = 256
LN_EPS = 1e-5
ALPHA = 2.0 ** 0.25
LAM_INIT = 0.2
NEG = -30000.0
NBISECT = 24
SWIGLU_ALPHA = 1.702
SWIGLU_LIMIT = 7.0
C_QA, C_KA, C_VA, C_QB, C_KB, C_VB, C_QI, C_KI, C_WI, C_GA, C_GB = (
    0, 1024, 2048, 3072, 4096, 5120, 6144, 7168, 7232, 7248, 8272)
PROJ_W = 9296


class Res:
    __slots__ = ("w", "r", "name")

    def __init__(self, name=""):
        self.w = None
        self.r = {}
        self.name = name


class Eng:
    def __init__(self, name, eng, sem):
        self.name = name
        self.eng = eng
        self.sem = sem
        self.cnt = 0
        self.seen = {}


class Trk:
    def __init__(self, nc, es):
        self.nc = nc
        mk = lambda n: es.enter_context(nc.semaphore(n))
        self.E = {n: Eng(n, e, mk("s_" + n)) for n, e in [
            ("pe", nc.tensor), ("act", nc.scalar), ("dve", nc.vector),
            ("pool", nc.gpsimd), ("sp", nc.sync)]}
        self.dsems = {q: [[mk(f"d_{q}{i}"), 0] for i in range(n)]
                      for q, n in [("sp", 16), ("pool", 10), ("act", 4)]}
        self.dnext = {q: 0 for q in self.dsems}
        self.nwait = 0

    def _waits(self, E, reads, writes):
        need = {}

        def add(tok, raw):
            if tok is None:
                return
            sem, val = tok
            if sem is E.sem and E.name == "pe":
                return
            k = id(sem)
            if k not in need or need[k][1] < val:
                need[k] = (sem, val)
        for r in reads:
            add(r.w, True)
        for w in writes:
            add(w.w, False)
            for tok in w.r.values():
                add(tok, False)
        for k, (sem, val) in need.items():
            if E.seen.get(k, 0) < val:
                E.eng.wait_ge(sem, val)
                E.seen[k] = val
                self.nwait += 1

    @staticmethod
    def _mark(tok, reads, writes):
        k = id(tok[0])
        for r in reads:
            r.r[k] = tok
        for w in writes:
            w.w = tok
            w.r = {}

    def op(self, en, fn, reads=(), writes=()):
        E = self.E[en]
        self._waits(E, reads, writes)
        ins = fn(E.eng)
        E.cnt += 1
        ins.then_inc(E.sem, 1)
        self._mark((E.sem, E.cnt), reads, writes)

    def dma(self, q, out, in_, reads=(), writes=(), **kw):
        E = self.E[q]
        self._waits(E, reads, writes)
        slots = self.dsems[q]
        i = self.dnext[q]
        self.dnext[q] = (i + 1) % len(slots)
        sem, val = slots[i]
        k = id(sem)
        if val > 0 and E.seen.get(k, 0) < val:
            E.eng.wait_ge(sem, val)
            E.seen[k] = val
        ins = E.eng.dma_start(out=out, in_=in_, **kw)
        val += 16
        slots[i][1] = val
        ins.then_inc(sem, 16)
        self._mark((sem, val), reads, writes)

    def barrier(self, all_res):
        toks = {}
        for r in all_res:
            for tok in [r.w] + list(r.r.values()):
                if tok is None:
                    continue
                k = id(tok[0])
                if k not in toks or toks[k][1] < tok[1]:
                    toks[k] = tok
        for E in self.E.values():
            for k, (sem, val) in toks.items():
                if sem is E.sem:
                    continue
                if E.seen.get(k, 0) < val:
                    E.eng.wait_ge(sem, val)
                    E.seen[k] = val


def alibi_slopes(n=8):
    return [2.0 ** (-8.0 * (h + 1) / n) for h in range(n)]


class MK:
    def __init__(self, nslot, debug=False, phases=99):
        self.nslot = nslot
        self.ntile = 8 * nslot
        self.S = 128 * self.ntile
        self.NQ = 128 * nslot
        self.debug = debug
        self.phases = phases
        self.nc = bass.Bass("TRN2", target_bir_lowering=False)
        self.res_all = []

    def R(self, name=""):
        r = Res(name)
        self.res_all.append(r)
        return r

    def I(self, name):
        if name not in self.in_aps:
            shape, dt = self.in_specs[name]
            self.in_aps[name] = self.nc.dram_tensor(name, list(shape), dt, kind="ExternalInput").ap()
        return self.in_aps[name]

    def dscr(self, name, shape, dt):
        kind = "ExternalOutput" if self.debug else "Internal"
        t = self.nc.dram_tensor(name, list(shape), dt, kind=kind).ap()
        return t

    def sb(self, es, name, shape, dt):
        return es.enter_context(self.nc.sbuf_tensor(name, list(shape), dt))

    def barrier(self):
        self.t.barrier(self.res_all)

    def build(self):
        nc = self.nc
        S, NQ, nslot, ntile = self.S, self.NQ, self.nslot, self.ntile
        self.in_specs = {
            "x": ([S, D], F32), "c": ([D], F32), "w_ada": ([D, 6 * D], F32), "b_ada": ([6 * D], F32),
            "w_in": ([D, PROJ_W], F32), "lamv": ([4, 64], F32), "diff_norm_g": ([128], F32),
            "w_branch_a": ([D, D], F32), "w_branch_b": ([D, D], F32), "w_out": ([D, D], F32),
            "ln1_g": ([D], F32), "ln1_b": ([D], F32), "w_router": ([D, NEXP], F32), "b_router": ([NEXP], F32),
            "w_e1": ([NEXP, D, 2 * DFF], F32), "b_e1": ([NEXP, 2 * DFF], F32),
            "w_e2": ([NEXP, DFF, D], F32), "b_e2": ([NEXP, D], F32), "ln2_g": ([D], F32), "ln2_b": ([D], F32),
            "c_identb": ([128, 128], BF16), "c_identf": ([128, 128], F32), "c_kaug": ([7, S], BF16),
            "c_qaug": ([7, nslot, 8, 128], BF16), "c_dg": ([128, 8, 128], BF16), "c_dmask": ([128, 128], F32),
            "c_dummy": ([128, 8], F32), "c_iota": ([128, 512], F32), "c_slopetab": ([2, 8, 128], F32),
            "c_pidx1": ([128, 1], F32),
        }
        self.in_aps = {}
        self.out = nc.dram_tensor("out", [NQ, D], F32, kind="ExternalOutput").ap()
        self.d_mod = self.dscr("d_mod", [6 * D], F32)
        self.d_kat = self.dscr("d_kat", [16, 64, S], BF16)
        self.d_va = self.dscr("d_va", [S, 8, 132], BF16)
        self.d_kbt = self.dscr("d_kbt", [8, 128, S], BF16)
        self.d_vb = self.dscr("d_vb", [S, 8, 132], BF16)
        self.d_kit = self.dscr("d_kit", [64, S], BF16)
        self.d_qat = self.dscr("d_qat", [16, 64, NQ], BF16)
        self.d_qbt = self.dscr("d_qbt", [8, 128, NQ], BF16)
        self.d_qit = self.dscr("d_qit", [16, 64, NQ], BF16)
        self.d_sgn = self.dscr("d_sgn", [NQ, 16], F32)
        self.d_gate = self.dscr("d_gate", [NQ, 2 * D], F32)
        self.d_ya = self.dscr("d_ya", [NQ, D], BF16)
        self.d_yb = self.dscr("d_yb", [NQ, D], BF16)
        self.d_x1 = self.dscr("d_x1", [NQ, D], F32)

        with ExitStack() as es:
            self.t = Trk(nc, es)
            self.ps = [es.enter_context(nc.psum_tensor(f"ps{i}", [128, 512], F32)) for i in range(8)]
            self.psr = [self.R(f"ps{i}") for i in range(8)]
            self.identb = self.sb(es, "identb", [128, 128], BF16)
            self.identf = self.sb(es, "identf", [128, 128], F32)
            self.r_const = self.R("const")
            self.t.dma("sp", self.identb[:], self.I("c_identb"), writes=[self.r_const])
            self.t.dma("sp", self.identf[:], self.I("c_identf"), writes=[self.r_const])
            self.modT = self.sb(es, "modT", [128, 48], F32)
            self.r_mod = self.R("mod")
            self.r_out = self.R("out")
            self.phase0()
            self.barrier()
            if self.phases >= 1:
                self.phase1a()
                self.barrier()
                self.phase1b()
                self.barrier()
            if self.phases >= 2:
                self.phase2()
                self.barrier()
            if self.phases >= 3:
                self.phase3()
                self.barrier()
            if self.phases >= 4:
                self.phase4()
            self.final_wait()
        return nc

    def final_wait(self):
        self.barrier()

    def phase0(self):
        nc, t = self.nc, self.t
        with ExitStack() as es:
            cT = self.sb(es, "cT", [128, 8], F32)
            cact = self.sb(es, "cact", [128, 8], F32)
            wbuf = [self.sb(es, f"wada{i}", [128, 8, 512], F32) for i in range(2)]
            wr = [self.R() for _ in range(2)]
            brow = self.sb(es, "brow", [1, 6 * D], F32)
            mrow = self.sb(es, "mrow", [1, 6 * D], F32)
            r_c, r_b, r_m = self.R(), self.R(), self.R()
            t.dma("sp", cT[:], self.I("c").rearrange("(c p) -> p c", p=128), writes=[r_c],
                  allow_slow_non_contiguous=True)
            t.dma("sp", brow[:], self.I("b_ada").rearrange("(o n) -> o n", o=1), writes=[r_b])
            t.op("act", lambda e: e.activation(out=cact[:], in_=cT[:], func=AF.Silu),
                 reads=[r_c], writes=[r_c])
            wv = self.I("w_ada").rearrange("(c p) n -> p c n", p=128)
            for g in range(12):
                b = g % 2
                t.dma("sp", wbuf[b][:], wv[:, :, g * 512:(g + 1) * 512], writes=[wr[b]])
                pb = g % 2

                def mm(e, b=b, pb=pb):
                    for c in range(8):
                        ins = e.matmul(self.ps[pb][0:1, :], lhsT=cact[:, c:c + 1], rhs=wbuf[b][:, c, :],
                                       start=(c == 0), stop=(c == 7))
                    return ins
                t.op("pe", mm, reads=[r_c, wr[b]], writes=[self.psr[pb]])
                t.op("dve", lambda e, g=g, pb=pb: e.tensor_tensor(
                    out=mrow[:, g * 512:(g + 1) * 512], in0=self.ps[pb][0:1, :],
                    in1=brow[:, g * 512:(g + 1) * 512], op=ALU.add),
                    reads=[self.psr[pb], r_b], writes=[r_m])
            r_d = self.R()
            t.dma("sp", self.d_mod.rearrange("(o n) -> o n", o=1), mrow[:], reads=[r_m], writes=[r_d])
            t.dma("sp", self.modT[:], self.d_mod.rearrange("(m p) -> p m", p=128), reads=[r_d],
                  writes=[self.r_mod], allow_slow_non_contiguous=True)
            self.r_dmod = r_d
            self.barrier()

    def ln_pre(self, xin_ap, xt, r_xt, xn, r_xn, stats, mv, r_st, q="sp"):
        t = self.t
        t.dma(q, xt[:], xin_ap, writes=[r_xt])
        for hh in range(2):
            t.op("dve", lambda e, hh=hh: e.bn_stats(out=stats[:, hh, :], in_=xt[:, hh * 512:(hh + 1) * 512]),
                 reads=[r_xt], writes=[r_st])
        t.op("dve", lambda e: e.bn_aggr(out=mv[:, 0:2], in_=stats[:].rearrange("p a b -> p (a b)")),
             reads=[r_st], writes=[r_st])
        t.op("dve", lambda e: e.tensor_scalar(out=mv[:, 2:3], in0=mv[:, 1:2], scalar1=LN_EPS, scalar2=None,
                                              op0=ALU.add), reads=[r_st], writes=[r_st])
        t.op("act", lambda e: e.activation(out=mv[:, 2:3], in_=mv[:, 2:3], func=AF.Sqrt),
             reads=[r_st], writes=[r_st])
        t.op("dve", lambda e: e.reciprocal(out=mv[:, 3:4], in_=mv[:, 2:3]), reads=[r_st], writes=[r_st])
        t.op("dve", lambda e: e.tensor_scalar(out=xn[:], in0=xt[:], scalar1=mv[:, 0:1], scalar2=mv[:, 3:4],
                                              op0=ALU.subtract, op1=ALU.mult),
             reads=[r_xt, r_st], writes=[r_xn])

    def ln_post(self, xn, r_xn, out_xnT, r_out, pbank):
        t = self.t
        pbf = self.ps[pbank].bitcast(BF16)

        def tr(e):
            for c in range(8):
                ins = e.transpose(pbf[:, c * 128:(c + 1) * 128], xn[:, c * 128:(c + 1) * 128], self.identb[:])
            return ins
        t.op("pe", tr, reads=[r_xn, self.r_const], writes=[self.psr[pbank]])
        t.op("act", lambda e: e.copy(out=out_xnT, in_=pbf[:, :].rearrange("p (c n) -> p c n", c=8)),
             reads=[self.psr[pbank]], writes=[r_out])

    def ln_tile(self, xin_ap, xt, r_xt, xn, r_xn, stats, mv, r_st, out_xnT, r_out, pbank, q="sp"):
        self.ln_pre(xin_ap, xt, r_xt, xn, r_xn, stats, mv, r_st, q=q)
        self.ln_post(xn, r_xn, out_xnT, r_out, pbank)

    def prep_w(self, es, tag, colranges, sc_off, sh_off):
        nc, t = self.nc, self.t
        ncols = sum(l for _, l in colranges)
        wsb = self.sb(es, "w_" + tag, [128, 8, ncols], BF16)
        r_w = self.R()
        wv = self.I("w_in").rearrange("(c p) n -> p c n", p=128)
        o = 0
        for (s0, l) in colranges:
            for a in range(0, l, 512):
                b = min(l, a + 512)
                t.dma("pool", wsb[:, :, o + a:o + b], wv[:, :, s0 + a:s0 + b], writes=[r_w])
            o += l
        nch = (ncols + 127) // 128
        biasT = self.sb(es, "bT_" + tag, [128, nch], F32)
        biasrow = self.sb(es, "br_" + tag, [1, ncols], BF16)
        onep = self.sb(es, "onep_" + tag, [128, 8], F32)
        shb = self.sb(es, "shb_" + tag, [128, 8], BF16)
        r_b = self.R()
        t.op("dve", lambda e: e.tensor_scalar(out=onep[:], in0=self.modT[:, sc_off:sc_off + 8], scalar1=1.0,
                                              scalar2=None, op0=ALU.add), reads=[self.r_mod], writes=[r_b])
        t.op("dve", lambda e: e.tensor_copy(out=shb[:], in_=self.modT[:, sh_off:sh_off + 8]),
             reads=[self.r_mod], writes=[r_b])
        for ch in range(nch):
            w0 = ch * 128
            wl = min(128, ncols - w0)
            pb = ch % 2

            def mm(e, w0=w0, wl=wl, pb=pb):
                for c in range(8):
                    ins = e.matmul(self.ps[pb][0:wl, 0:1], lhsT=wsb[:, c, w0:w0 + wl], rhs=shb[:, c:c + 1],
                                   start=(c == 0), stop=(c == 7))
                return ins
            t.op("pe", mm, reads=[r_w, r_b], writes=[self.psr[pb]])
            t.op("dve", lambda e, ch=ch, wl=wl, pb=pb: e.tensor_copy(out=biasT[0:wl, ch:ch + 1],
                                                                      in_=self.ps[pb][0:wl, 0:1]),
                 reads=[self.psr[pb]], writes=[r_b])
        for a in range(0, ncols, 512):
            b = min(ncols, a + 512)
            pb = 2 + (a // 512) % 2

            def mm2(e, a=a, b=b, pb=pb):
                for c in range(8):
                    ins = e.matmul(self.ps[pb][0:1, 0:b - a], lhsT=shb[:, c:c + 1], rhs=wsb[:, c, a:b],
                                   start=(c == 0), stop=(c == 7))
                return ins
            t.op("pe", mm2, reads=[r_w, r_b], writes=[self.psr[pb]])
            t.op("dve", lambda e, a=a, b=b, pb=pb: e.tensor_copy(out=biasrow[:, a:b], in_=self.ps[pb][0:1, 0:b - a]),
                 reads=[self.psr[pb]], writes=[r_b])
        for c in range(8):
            en = "dve" if c % 2 == 0 else "pool"
            t.op(en, lambda e, c=c: e.tensor_scalar(out=wsb[:, c, :], in0=wsb[:, c, :], scalar1=onep[:, c:c + 1],
                                                     scalar2=None, op0=ALU.mult),
                 reads=[r_w, r_b], writes=[r_w])
        return wsb, r_w, biasT, biasrow, r_b

    def phase1a(self):
        nc, t = self.nc, self.t
        S = self.S
        with ExitStack() as es:
            wsb, r_w, biasT, biasrow, r_b = self.prep_w(
                es, "p1a", [(C_KA, 1024), (C_KB, 1024), (C_KI, 64), (C_VA, 1024), (C_VB, 1024)], 8, 0)
            VOFF = 2112
            ones = self.sb(es, "ones1", [1, 128], BF16)
            r_ones = self.R()
            t.op("dve", lambda e: e.memset(ones[:], 1.0), writes=[r_ones])
            xt = [self.sb(es, f"xt{i}", [128, D], F32) for i in range(2)]
            r_xt = [self.R() for _ in range(2)]
            xn = [self.sb(es, f"xn{i}", [128, D], BF16) for i in range(2)]
            r_xn = [self.R() for _ in range(2)]
            stats = [self.sb(es, f"st{i}", [128, 2, 6], F32) for i in range(2)]
            mv = [self.sb(es, f"mv{i}", [128, 4], F32) for i in range(2)]
            r_st = [self.R() for _ in range(2)]
            xnT = [self.sb(es, f"xnT{i}", [128, 8, 512], BF16) for i in range(2)]
            r_xnT = [self.R() for _ in range(2)]
            kst = [self.sb(es, f"kst{i}", [128, 512], BF16) for i in range(4)]
            r_kst = [self.R() for _ in range(4)]
            vst = [self.sb(es, f"vst{i}", [128, 4, 132], BF16) for i in range(4)]
            r_vst = [self.R() for _ in range(4)]
            for i in range(4):
                t.op("pool", lambda e, i=i: e.memset(vst[i][:, :, 128:132], 0.0), writes=[r_vst[i]])
                t.op("pool", lambda e, i=i: e.memset(vst[i][:, :, 128:129], 1.0), writes=[r_vst[i]])
            ngrp = S // 512
            ki = 0
            vi = 0
            tcount = 0
            ev = 0
            xn4 = [self.sb(es, f"xn4_{i}", [128, D], BF16) for i in range(8)]
            r_xn4 = [self.R() for _ in range(8)]

            def ln_group_pre(g):
                for tt in range(4):
                    b = tcnt[0] % 2
                    tcnt[0] += 1
                    k = (g % 2) * 4 + tt
                    tok0 = g * 512 + tt * 128
                    self.ln_pre(self.I("x")[tok0:tok0 + 128, :], xt[b], r_xt[b], xn4[k], r_xn4[k], stats[b], mv[b], r_st[b])

            def ln_group_post(g):
                gb = g % 2
                for tt in range(4):
                    k = (g % 2) * 4 + tt
                    self.ln_post(xn4[k], r_xn4[k], xnT[gb][:, :, tt * 128:(tt + 1) * 128], r_xnT[gb], pbank=tt % 2)
            tcnt = [0]
            ln_group_pre(0)
            ln_group_post(0)
            for g in range(ngrp):
                gb = g % 2
                if g + 1 < ngrp:
                    ln_group_pre(g + 1)
                for ch in range(17):
                    wl = 128 if ch < 16 else 64
                    pb = 2 + ch % 3

                    def mm(e, ch=ch, wl=wl, pb=pb, gb=gb):
                        for c in range(8):
                            ins = e.matmul(self.ps[pb][0:wl, :], lhsT=wsb[:, c, ch * 128:ch * 128 + wl],
                                           rhs=xnT[gb][:, c, :], start=(c == 0), stop=(c == 7))
                        return ins
                    t.op("pe", mm, reads=[r_w, r_xnT[gb]], writes=[self.psr[pb]])
                    s = ki % 4
                    ki += 1
                    en = "act" if ev % 4 != 3 else "dve"
                    ev += 1
                    if en == "act":
                        t.op("act", lambda e, s=s, wl=wl, pb=pb, ch=ch: e.activation(
                            out=kst[s][0:wl, :], in_=self.ps[pb][0:wl, :], func=AF.Identity,
                            bias=biasT[0:wl, ch:ch + 1], scale=1.0),
                            reads=[self.psr[pb], r_b], writes=[r_kst[s]])
                    else:
                        t.op("dve", lambda e, s=s, wl=wl, pb=pb, ch=ch: e.tensor_scalar(
                            out=kst[s][0:wl, :], in0=self.ps[pb][0:wl, :], scalar1=biasT[0:wl, ch:ch + 1],
                            scalar2=None, op0=ALU.add),
                            reads=[self.psr[pb], r_b], writes=[r_kst[s]])
                    tsl = slice(g * 512, (g + 1) * 512)
                    if ch < 8:
                        t.dma("sp", self.d_kat[2 * ch:2 * ch + 2, :, tsl].rearrange("m d n -> (m d) n"),
                              kst[s][:, :], reads=[r_kst[s]])
                    elif ch < 16:
                        t.dma("sp", self.d_kbt[ch - 8, :, tsl], kst[s][:, :], reads=[r_kst[s]])
                    else:
                        t.dma("sp", self.d_kit[:, tsl], kst[s][0:64, :], reads=[r_kst[s]])
                if g + 1 < ngrp:
                    ln_group_post(g + 1)
                for tt in range(4):
                    for vg in range(4):
                        pb = 5 + (tt * 4 + vg) % 3
                        c0 = VOFF + vg * 512

                        def mm(e, tt=tt, c0=c0, pb=pb, gb=gb):
                            for c in range(8):
                                e.matmul(self.ps[pb][:, :], lhsT=xnT[gb][:, c, tt * 128:(tt + 1) * 128],
                                         rhs=wsb[:, c, c0:c0 + 512], start=(c == 0), stop=False)
                            return e.matmul(self.ps[pb][:, :], lhsT=ones[0:1, :], rhs=biasrow[0:1, c0:c0 + 512],
                                            start=False, stop=True)
                        t.op("pe", mm, reads=[r_w, r_xnT[gb], r_b, r_ones], writes=[self.psr[pb]])
                        s = vi % 4
                        vi += 1
                        en = "act" if ev % 4 != 3 else "dve"
                        ev += 1
                        psv = self.ps[pb][:, :].rearrange("p (h e) -> p h e", h=4)
                        if en == "act":
                            t.op("act", lambda e, s=s, psv=psv: e.copy(out=vst[s][:, :, 0:128], in_=psv),
                                 reads=[self.psr[pb]], writes=[r_vst[s]])
                        else:
                            t.op("dve", lambda e, s=s, psv=psv: e.tensor_copy(out=vst[s][:, :, 0:128], in_=psv),
                                 reads=[self.psr[pb]], writes=[r_vst[s]])
                        tok0 = g * 512 + tt * 128
                        dst = self.d_va if vg < 2 else self.d_vb
                        t.dma("sp", dst[tok0:tok0 + 128, (vg % 2) * 4:(vg % 2) * 4 + 4, :], vst[s][:],
                              reads=[r_vst[s]])
            self.barrier()

    def phase1b(self):
        nc, t = self.nc, self.t
        nslot, NQ = self.nslot, self.NQ
        with ExitStack() as es:
            wsb, r_w, biasT, biasrow, r_b = self.prep_w(
                es, "p1b", [(C_QA, 1024), (C_QB, 1024), (C_QI, 1024), (C_WI, 16), (C_GA, 1024), (C_GB, 1024)], 8, 0)
            O_QI, O_WI, O_GA = 2048, 3072, 3088
            ones = self.sb(es, "ones1b", [1, 128], BF16)
            r_ones = self.R()
            t.op("dve", lambda e: e.memset(ones[:], 1.0), writes=[r_ones])
            xt = [self.sb(es, f"xtb{i}", [128, D], F32) for i in range(2)]
            r_xt = [self.R() for _ in range(2)]
            xn = [self.sb(es, f"xnb{i}", [128, D], BF16) for i in range(2)]
            r_xn = [self.R() for _ in range(2)]
            stats = [self.sb(es, f"stb{i}", [128, 2, 6], F32) for i in range(2)]
            mv = [self.sb(es, f"mvb{i}", [128, 4], F32) for i in range(2)]
            r_st = [self.R() for _ in range(2)]
            G = min(4, nslot)
            xnT = [self.sb(es, f"xnTb{i}", [128, 8, 128 * G], BF16) for i in range(2)]
            r_xnT = [self.R() for _ in range(2)]
            kst = [self.sb(es, f"kstb{i}", [128, 128 * G], BF16) for i in range(4)]
            r_kst = [self.R() for _ in range(4)]
            gst = [self.sb(es, f"gst{i}", [128, 512], F32) for i in range(3)]
            r_gst = [self.R() for _ in range(3)]
            wis = [self.sb(es, f"wis{i}", [128, 3, 16], F32) for i in range(2)]
            r_wis = [self.R() for _ in range(2)]
            qis = [self.sb(es, f"qis{i}", [128, 1024], BF16) for i in range(2)]
            r_qis = [self.R() for _ in range(2)]
            qit = [self.sb(es, f"qit{i}", [128, 8, 128], BF16) for i in range(2)]
            r_qit = [self.R() for _ in range(2)]
            ki = 0
            gi = 0
            tcount = 0
            for g in range(nslot // G):
                gb = g % 2
                NT = 128 * G
                for tt in range(G):
                    b = tcount % 2
                    j = g * G + tt
                    tok0 = (8 * j + 7) * 128
                    self.ln_tile(self.I("x")[tok0:tok0 + 128, :], xt[b], r_xt[b], xn[b], r_xn[b], stats[b], mv[b],
                                 r_st[b], xnT[gb][:, :, tt * 128:(tt + 1) * 128], r_xnT[gb], pbank=b)
                    tcount += 1
                q0 = g * NT
                for ch in range(16):
                    pb = 2 + ch % 3

                    def mm(e, ch=ch, pb=pb, gb=gb, NT=NT):
                        for c in range(8):
                            ins = e.matmul(self.ps[pb][:, 0:NT], lhsT=wsb[:, c, ch * 128:ch * 128 + 128],
                                           rhs=xnT[gb][:, c, :], start=(c == 0), stop=(c == 7))
                        return ins
                    t.op("pe", mm, reads=[r_w, r_xnT[gb]], writes=[self.psr[pb]])
                    s = ki % 4
                    ki += 1
                    scale = 0.125 if ch < 8 else 128.0 ** -0.5
                    t.op("dve", lambda e, s=s, pb=pb, ch=ch, scale=scale, NT=NT: e.tensor_scalar(
                        out=kst[s][:, 0:NT], in0=self.ps[pb][:, 0:NT], scalar1=biasT[:, ch:ch + 1], scalar2=scale,
                        op0=ALU.add, op1=ALU.mult), reads=[self.psr[pb], r_b], writes=[r_kst[s]])
                    if ch < 8:
                        t.dma("sp", self.d_qat[2 * ch:2 * ch + 2, :, q0:q0 + NT].rearrange("m d n -> (m d) n"),
                              kst[s][:, 0:NT], reads=[r_kst[s]])
                    else:
                        t.dma("sp", self.d_qbt[ch - 8, :, q0:q0 + NT], kst[s][:, 0:NT], reads=[r_kst[s]])
                for tt in range(G):
                    j = g * G + tt
                    tq0 = j * 128
                    lhs = lambda c, tt=tt, gb=gb: xnT[gb][:, c, tt * 128:(tt + 1) * 128]
                    wb = j % 2
                    pb = 5

                    def mmw(e, lhs=lhs, pb=pb):
                        for c in range(8):
                            e.matmul(self.ps[pb][:, 0:16], lhsT=lhs(c), rhs=wsb[:, c, O_WI:O_WI + 16],
                                     start=(c == 0), stop=False)
                        return e.matmul(self.ps[pb][:, 0:16], lhsT=ones[0:1, :], rhs=biasrow[0:1, O_WI:O_WI + 16],
                                        start=False, stop=True)
                    t.op("pe", mmw, reads=[r_w, r_xnT[gb], r_b, r_ones], writes=[self.psr[pb]])
                    t.op("dve", lambda e, wb=wb, pb=pb: e.tensor_copy(out=wis[wb][:, 0, :], in_=self.ps[pb][:, 0:16]),
                         reads=[self.psr[pb]], writes=[r_wis[wb]])
                    t.op("act", lambda e, wb=wb: e.activation(out=wis[wb][:, 1, :], in_=wis[wb][:, 0, :],
                                                              func=AF.Abs, scale=1.0 / 32.0),
                         reads=[r_wis[wb]], writes=[r_wis[wb]])
                    t.op("act", lambda e, wb=wb: e.activation(out=wis[wb][:, 2, :], in_=wis[wb][:, 0, :], func=AF.Sign),
                         reads=[r_wis[wb]], writes=[r_wis[wb]])
                    t.dma("sp", self.d_sgn[tq0:tq0 + 128, :], wis[wb][:, 2, :], reads=[r_wis[wb]])
                    for qg in range(2):
                        pb = 6 + qg
                        c0 = O_QI + qg * 512

                        def mmq(e, lhs=lhs, pb=pb, c0=c0):
                            for c in range(8):
                                e.matmul(self.ps[pb][:, :], lhsT=lhs(c), rhs=wsb[:, c, c0:c0 + 512],
                                         start=(c == 0), stop=False)
                            return e.matmul(self.ps[pb][:, :], lhsT=ones[0:1, :], rhs=biasrow[0:1, c0:c0 + 512],
                                            start=False, stop=True)
                        t.op("pe", mmq, reads=[r_w, r_xnT[gb], r_b, r_ones], writes=[self.psr[pb]])
                        t.op("dve", lambda e, wb=wb, pb=pb, qg=qg: e.tensor_tensor(
                            out=qis[wb][:, qg * 512:(qg + 1) * 512].rearrange("p (h d) -> p h d", h=8),
                            in0=self.ps[pb][:, :].rearrange("p (h d) -> p h d", h=8),
                            in1=wis[wb][:, 1, qg * 8:(qg + 1) * 8].unsqueeze(2).broadcast_to([128, 8, 64]),
                            op=ALU.mult), reads=[self.psr[pb], r_wis[wb]], writes=[r_qis[wb]])
                    pbf = self.ps[2 + (j % 3)].bitcast(BF16)

                    def tr(e, wb=wb, pbf=pbf):
                        for c in range(8):
                            ins = e.transpose(pbf[:, c * 128:(c + 1) * 128], qis[wb][:, c * 128:(c + 1) * 128],
                                              self.identb[:])
                        return ins
                    t.op("pe", tr, reads=[r_qis[wb], self.r_const], writes=[self.psr[2 + (j % 3)]])
                    t.op("act", lambda e, wb=wb, pbf=pbf: e.copy(
                        out=qit[wb][:], in_=pbf[:, :].rearrange("p (c n) -> p c n", c=8)),
                        reads=[self.psr[2 + (j % 3)]], writes=[r_qit[wb]])
                    for c in range(8):
                        t.dma("sp", self.d_qit[2 * c:2 * c + 2, :, tq0:tq0 + 128].rearrange("m d n -> (m d) n"),
                              qit[wb][:, c, :], reads=[r_qit[wb]])
                    for gg in range(4):
                        pb = 5 + gg % 3
                        c0 = O_GA + gg * 512

                        def mmg(e, lhs=lhs, pb=pb, c0=c0):
                            for c in range(8):
                                e.matmul(self.ps[pb][:, :], lhsT=lhs(c), rhs=wsb[:, c, c0:c0 + 512],
                                         start=(c == 0), stop=False)
                            return e.matmul(self.ps[pb][:, :], lhsT=ones[0:1, :], rhs=biasrow[0:1, c0:c0 + 512],
                                            start=False, stop=True)
                        t.op("pe", mmg, reads=[r_w, r_xnT[gb], r_b, r_ones], writes=[self.psr[pb]])
                        s = gi % 3
                        gi += 1
                        t.op("act", lambda e, s=s, pb=pb: e.activation(out=gst[s][:], in_=self.ps[pb][:, :],
                                                                        func=AF.Sigmoid),
                             reads=[self.psr[pb]], writes=[r_gst[s]])
                        t.dma("sp", self.d_gate[tq0:tq0 + 128, gg * 512:(gg + 1) * 512], gst[s][:],
                              reads=[r_gst[s]])
            self.barrier()

    def phase2(self):
        nc, t = self.nc, self.t
        STOP = int(os.environ.get('P2STOP', '99'))
        EXP = os.environ.get('EXP', '')
        S, nslot = self.S, self.nslot
        PS = self.ps
        PR = self.psr
        with ExitStack() as es:
            dg = self.sb(es, "dg", [128, 8, 128], BF16)
            dmask = self.sb(es, "dmask", [128, 128], F32)
            dummy = self.sb(es, "dummyc", [128, 8], F32)
            iota = self.sb(es, "iotac", [128, 512], F32)
            slopetab = self.sb(es, "slopetab", [2, 8, 128], F32)
            pidx1 = self.sb(es, "pidx1", [128, 1], F32)
            nlam = self.sb(es, "nlam", [128, 1], F32)
            gbc = self.sb(es, "gbc", [128, 128], F32)
            r_c2 = self.R("c2")
            for dst, nm in [(dg, "c_dg"), (dmask, "c_dmask"), (dummy, "c_dummy"), (iota, "c_iota"),
                            (slopetab, "c_slopetab"), (pidx1, "c_pidx1")]:
                t.dma("sp", dst[:], self.I(nm), writes=[r_c2])
            lv = self.sb(es, "lv", [1, 4, 64], F32)
            lsm = self.sb(es, "lsm", [1, 8], F32)
            onesf = self.sb(es, "onesf", [1, 128], F32)
            r_l = self.R("lam")
            t.dma("sp", lv[:], self.I("lamv").rearrange("(o a) d -> o a d", o=1), writes=[r_l])
            t.op("dve", lambda e: e.memset(onesf[:], 1.0), writes=[r_l])
            t.op("dve", lambda e: e.tensor_tensor(out=lv[:, 0, :], in0=lv[:, 0, :], in1=lv[:, 1, :], op=ALU.mult),
                 reads=[r_l], writes=[r_l])
            t.op("dve", lambda e: e.tensor_tensor(out=lv[:, 2, :], in0=lv[:, 2, :], in1=lv[:, 3, :], op=ALU.mult),
                 reads=[r_l], writes=[r_l])
            t.op("dve", lambda e: e.reduce_sum(out=lsm[:, 0:1], in_=lv[:, 0, :], axis=AX.X), reads=[r_l], writes=[r_l])
            t.op("dve", lambda e: e.reduce_sum(out=lsm[:, 1:2], in_=lv[:, 2, :], axis=AX.X), reads=[r_l], writes=[r_l])
            t.op("act", lambda e: e.activation(out=lsm[:, 2:4], in_=lsm[:, 0:2], func=AF.Exp), reads=[r_l], writes=[r_l])
            t.op("dve", lambda e: e.tensor_tensor(out=lsm[:, 4:5], in0=lsm[:, 3:4], in1=lsm[:, 2:3], op=ALU.subtract),
                 reads=[r_l], writes=[r_l])
            t.op("dve", lambda e: e.tensor_scalar(out=lsm[:, 5:6], in0=lsm[:, 4:5], scalar1=-LAM_INIT, scalar2=None,
                                                  op0=ALU.add), reads=[r_l], writes=[r_l])
            t.op("pe", lambda e: e.matmul(PS[7][:, 0:1], lhsT=onesf[0:1, :], rhs=lsm[0:1, 5:6], start=True, stop=True),
                 reads=[r_l], writes=[PR[7]])
            t.op("dve", lambda e: e.tensor_copy(out=nlam[:], in_=PS[7][:, 0:1]), reads=[PR[7]], writes=[r_c2])
            t.dma("sp", gbc[:], self.I("diff_norm_g").partition_broadcast(128), writes=[r_c2])
            t.op("dve", lambda e: e.tensor_scalar(out=gbc[:], in0=gbc[:], scalar1=1.0 - LAM_INIT, scalar2=None,
                                                  op0=ALU.mult), reads=[r_c2], writes=[r_c2])

            if STOP <= 0:
                self.barrier()
                return
            score = self.sb(es, "score", [128, S], F32)
            maskT = score.bitcast(BF16)
            r_sm = self.R("score")
            mask = self.sb(es, "mask", [128, S], BF16)
            r_mask = self.R("mask")
            qi_sb = self.sb(es, "qi_sb", [64, 16, 128], BF16)
            r_qi = self.R()
            sgn = self.sb(es, "sgn", [128, 16], F32)
            dsg = self.sb(es, "dsg", [128, 16, 128], BF16)
            r_dsg = self.R()
            kit_sb = [self.sb(es, f"kit{i}", [64, 1024], BF16) for i in range(2)]
            r_kit = [self.R() for _ in range(2)]
            rbuf = [self.sb(es, f"rbuf{i}", [128, 512], BF16) for i in range(4)]
            r_rbuf = [self.R() for _ in range(4)]
            sv = self.sb(es, "sv", [128, 48], F32)
            svi = self.sb(es, "svi", [128, 4], I32)
            r_sv = self.R()
            am = self.sb(es, "am", [128, 40], F32)
            r_am = self.R()
            tmp512 = self.sb(es, "tmp512", [128, 512], F32)
            r_tmp = self.R()
            ab = self.sb(es, "ab", [128, 2], BF16)
            qb_sb = self.sb(es, "qb_sb", [128, 8, 128], BF16)
            r_qb = self.R()
            qbaug = self.sb(es, "qbaug", [128, 8, 128], BF16)
            r_qbaug = self.R()
            qa_sb = self.sb(es, "qa_sb", [69, 16, 128], BF16)
            r_qa = self.R()
            NKB = 3
            kbuf = [self.sb(es, f"kbuf{i}", [128, 4, 1024], BF16) for i in range(NKB)]
            r_kbuf = [self.R() for _ in range(NKB)]
            vbuf = [self.sb(es, f"vbuf{i}", [128, 8, 4, 132], BF16) for i in range(NKB)]
            r_vbuf = [self.R() for _ in range(NKB)]
            kaug_sb = [self.sb(es, f"kaug{i}", [128, 1024], BF16) for i in range(NKB)]
            r_kaug = [self.R() for _ in range(NKB)]
            pbuf = [self.sb(es, f"pbuf{i}", [128, 4, 128], BF16) for i in range(5)]
            r_pbuf = [self.R() for _ in range(5)]
            ysb = [self.sb(es, f"ysb{i}", [128, D], BF16) for i in range(2)]
            r_ysb = [self.R() for _ in range(2)]
            junk = self.sb(es, "junk128", [128, 128], F32)
            r_junk = self.R()
            sv2 = self.sb(es, "sv2", [128, 16], F32)
            oraw = self.sb(es, "oraw", [128, D], F32)
            r_oraw = self.R()
            ssq = self.sb(es, "ssq", [128, 16], F32)
            r_ssq = self.R()
            r_sv2 = [self.R() for _ in range(2)]
            for i in range(NKB):
                t.op("pool", lambda e, i=i: e.memset(kaug_sb[i][:], 0.0), writes=[r_kaug[i]])
            t.op("pool", lambda e: e.memset(qbaug[:], 0.0), writes=[r_qbaug])
            kcnt = [0]
            pcnt = [0]
            scnt = [0]
            kitc = [0]
            rcnt = [0]

            for j in range(nslot):
                tq = 8 * j + 7
                NT = tq + 1
                N = NT * 128
                q0 = j * 128
                ngrp = NT // 4
                t.dma("sp", qi_sb[:], self.d_qit[:, :, q0:q0 + 128].rearrange("h d n -> d h n"), writes=[r_qi])
                t.dma("sp", sgn[:], self.d_sgn[q0:q0 + 128, :], writes=[r_dsg])
                for h in range(16):
                    en = "dve" if (h % 2 == 0 or os.environ.get("NOPOOL")) else "pool"
                    t.op(en, lambda e, h=h: e.tensor_scalar(out=dsg[:, h, :], in0=self.identb[:], scalar1=sgn[:, h:h + 1],
                                                            scalar2=None, op0=ALU.mult),
                         reads=[r_dsg, self.r_const], writes=[r_dsg])
                for kg in range(ngrp):
                    if kg % 2 == 0:
                        kb = kitc[0] % 2
                        kitc[0] += 1
                        w = min(1024, N - kg * 512)
                        t.dma("sp", kit_sb[kb][:, 0:w], self.d_kit[:, kg * 512:kg * 512 + w], writes=[r_kit[kb]])
                    koff = (kg % 2) * 512

                    def logits(h, kb=kb, koff=koff):
                        lb = 4 + h % 3
                        t.op("pe", lambda e: e.matmul(PS[lb][:, :], lhsT=qi_sb[:, h, :], rhs=kit_sb[kb][:, koff:koff + 512],
                                                      start=True, stop=True),
                             reads=[r_qi, r_kit[kb]], writes=[PR[lb]])

                    def relu(h):
                        lb = 4 + h % 3
                        rb = rcnt[0] % 4
                        rcnt[0] += 1
                        if h % 4 != 3 and "b" not in EXP:
                            t.op("act", lambda e: e.activation(out=rbuf[rb][:], in_=PS[lb][:, :], func=AF.Relu),
                                 reads=[PR[lb]], writes=[r_rbuf[rb]])
                        else:
                            t.op("dve", lambda e: e.tensor_scalar(out=rbuf[rb][:], in0=PS[lb][:, :], scalar1=0.0,
                                                                  scalar2=None, op0=ALU.max),
                                 reads=[PR[lb]], writes=[r_rbuf[rb]])
                        return rb

                    def hsum(h, rb):
                        t.op("pe", lambda e: e.matmul(PS[7][:, :], lhsT=dsg[:, h, :], rhs=rbuf[rb][:],
                                                      start=(h == 0), stop=(h == 15)),
                             reads=[r_dsg, r_rbuf[rb]], writes=[PR[7]])
                    if "e" in EXP:
                        continue
                    logits(0)
                    logits(1)
                    for h in range(16):
                        if h + 2 < 16:
                            logits(h + 2)
                        rb = relu(h)
                        if "d" not in EXP:
                            hsum(h, rb)
                    if "d" in EXP:
                        continue
                    if "f" in EXP:
                        continue
                    sl = slice(kg * 512, (kg + 1) * 512)
                    if "g" in EXP:
                        pass
                    elif "a" in EXP:
                        t.op("dve", lambda e, kg=kg: e.reduce_max(out=am[:, kg:kg + 1], in_=PS[7][:, :], axis=AX.X),
                             reads=[PR[7]], writes=[r_am])
                    else:
                        t.op("dve", lambda e, kg=kg: e.tensor_reduce(out=am[:, kg:kg + 1], in_=PS[7][:, :], axis=AX.X,
                                                                     op=ALU.max, apply_absolute_value=True),
                             reads=[PR[7]], writes=[r_am])
                    if "h" not in EXP:
                        if "I" not in EXP:
                            t.op("dve", lambda e, sl=sl: e.tensor_copy(out=score[:, sl], in_=PS[7][:, :]),
                                 reads=[PR[7]], writes=[r_sm])
                        else:
                            t.op("act", lambda e, sl=sl: e.activation(out=score[:, sl], in_=PS[7][:, :], func=AF.Identity),
                                 reads=[PR[7]], writes=[r_sm])
                    for tl in range(4):
                        if "c" in EXP:
                            break
                        tt = kg * 4 + tl
                        ts_ = slice(tt * 128, (tt + 1) * 128)
                        if tt < 7:
                            t.op("dve", lambda e, ts_=ts_, tt=tt: e.tensor_scalar(
                                out=score[:, ts_], in0=score[:, ts_], scalar1=dummy[:, tt:tt + 1], scalar2=None,
                                op0=ALU.add), reads=[r_sm, r_c2], writes=[r_sm])
                        if tt == NT - 1:
                            t.op("dve", lambda e, ts_=ts_: e.tensor_tensor(out=score[:, ts_], in0=score[:, ts_],
                                                                           in1=dmask[:], op=ALU.add),
                                 reads=[r_sm, r_c2], writes=[r_sm])
                if STOP <= 1:
                    continue
                LO, W0, MID, CNT, PRED, AMX = 0, 1, 2, 3, 4, 7
                col = lambda i: sv[:, i:i + 1]
                t.op("dve", lambda e: e.reduce_max(out=col(AMX), in_=am[:, 0:ngrp], axis=AX.X), reads=[r_am], writes=[r_sv])
                t.op("dve", lambda e: e.tensor_scalar(out=col(LO), in0=col(AMX), scalar1=1.0, scalar2=-1.0, op0=ALU.add, op1=ALU.mult),
                     reads=[r_sv], writes=[r_sv])
                t.op("dve", lambda e: e.tensor_scalar(out=col(W0), in0=col(AMX), scalar1=1.0, scalar2=2.0, op0=ALU.add, op1=ALU.mult),
                     reads=[r_sv], writes=[r_sv])
                bit = [0]

                def bisect(nit):
                    for it in range(nit):
                        f = 0.5 ** (bit[0] + 1)
                        bit[0] += 1
                        t.op("dve", lambda e, f=f: e.scalar_tensor_tensor(out=col(MID), in0=col(W0), scalar=f, in1=col(LO),
                                                                          op0=ALU.mult, op1=ALU.add), reads=[r_sv], writes=[r_sv])
                        t.op("dve", lambda e: e.tensor_scalar(out=mask[:, 0:N], in0=score[:, 0:N], scalar1=col(MID), scalar2=None,
                                                              op0=ALU.is_gt, op1=ALU.add, accum_out=col(CNT)),
                             reads=[r_sm, r_sv], writes=[r_mask, r_sv])
                        t.op("dve", lambda e, f=f: e.tensor_scalar(out=col(PRED), in0=col(CNT), scalar1=float(TOPK) - 0.5, scalar2=f,
                                                                   op0=ALU.is_gt, op1=ALU.mult), reads=[r_sv], writes=[r_sv])
                        t.op("dve", lambda e: e.scalar_tensor_tensor(out=col(LO), in0=col(W0), scalar=col(PRED), in1=col(LO),
                                                                     op0=ALU.mult, op1=ALU.add), reads=[r_sv], writes=[r_sv])

                t.dma("sp", qb_sb[:], self.d_qbt[:, :, q0:q0 + 128].rearrange("h d n -> d h n"), writes=[r_qb])
                t.dma("sp", qa_sb[0:64, :, :], self.d_qat[:, :, q0:q0 + 128].rearrange("m d n -> d m n"), writes=[r_qa])
                for m_ in range(2):
                    t.dma("sp", qa_sb[64:69, :, :].rearrange("r (h m) n -> r h m n", m=2)[:, :, m_, :],
                          self.I("c_qaug")[2:7, j, :, :], writes=[r_qa])

                def attn_group(kind, gi, ab):
                    b0, b1_ = (2, 3) if ab == 0 else (4, 5)
                    accs = [PS[b0][:, 0:129], PS[b0][:, 129:258], PS[b0][:, 258:387], PS[b1_][:, 0:129]]
                    first_in_bank = [True, False, False, True]
                    RA = [PR[b0], PR[b1_]]
                    pendq = []
                    for tg in range(NT // 8):
                        kb = kcnt[0] % NKB
                        kcnt[0] += 1
                        ksl = slice(tg * 1024, (tg + 1) * 1024)
                        if kind == "dsa":
                            t.dma("sp", kbuf[kb][:, :, :], self.d_kbt[4 * gi:4 * gi + 4, :, ksl].rearrange("h d n -> d h n"),
                                  writes=[r_kbuf[kb]])
                            t.dma("sp", kaug_sb[kb][0:7, :], self.I("c_kaug")[:, ksl], writes=[r_kaug[kb]])
                            t.dma("sp", vbuf[kb][:, :, :, :].rearrange("p t h e -> p t (h e)"),
                                  self.d_vb[ksl, 4 * gi:4 * gi + 4, :].rearrange("(t p) h e -> p t (h e)", p=128),
                                  writes=[r_vbuf[kb]])
                        else:
                            t.dma("sp", kbuf[kb][0:64, :, :], self.d_kat[4 * gi:4 * gi + 4, :, ksl].rearrange("m d n -> d m n"),
                                  writes=[r_kbuf[kb]])
                            t.dma("sp", kbuf[kb][64:69, :, :], self.I("c_kaug")[2:7, ksl].unsqueeze(1).broadcast_to([5, 4, 1024]),
                                  writes=[r_kbuf[kb]])
                            t.dma("sp", vbuf[kb][:, :, 0:2, :].rearrange("p t h e -> p t (h e)"),
                                  self.d_va[ksl, 2 * gi:2 * gi + 2, :].rearrange("(t p) h e -> p t (h e)", p=128),
                                  writes=[r_vbuf[kb]])
                        for tl in range(8):
                            tt = tg * 8 + tl
                            sb_ = (0, 1, 6, 7)[scnt[0] % 4]
                            scnt[0] += 1
                            diag = (tt == NT - 1)

                            def qk(e, kb=kb, tl=tl, sb_=sb_, diag=diag):
                                tsl = slice(tl * 128, (tl + 1) * 128)
                                for i in range(4):
                                    reg = PS[sb_][:, i * 128:(i + 1) * 128]
                                    if kind == "dsa":
                                        ins = e.matmul(reg, lhsT=kbuf[kb][:, i, tsl], rhs=qb_sb[:, 4 * gi + i, :],
                                                       start=(i == 0), stop=False, skip_group_check=True)
                                        hh = 4 * gi + i
                                    else:
                                        ins = e.matmul(reg, lhsT=kbuf[kb][0:69, i, tsl], rhs=qa_sb[0:69, 4 * gi + i, :],
                                                       start=True, stop=not diag)
                                        hh = 2 * gi + i // 2
                                    if diag:
                                        ins = e.matmul(reg, lhsT=self.identb[:], rhs=dg[:, hh, :], start=False,
                                                       stop=(kind != "dsa"), skip_group_check=(kind == "dsa"))
                                if kind == "dsa":
                                    ins = e.matmul(PS[sb_][:, :], lhsT=kaug_sb[kb][:, tsl],
                                                   rhs=qbaug[:, 4 * gi:4 * gi + 4, :].rearrange("r h n -> r (h n)"),
                                                   start=False, stop=True, skip_group_check=True)
                                return ins
                            rd = [r_kbuf[kb], self.r_const, r_c2] + ([r_kaug[kb], r_qbaug, r_qb] if kind == "dsa" else [r_qa])
                            t.op("pe", qk, reads=rd, writes=[PR[sb_]])
                            pb_ = pcnt[0] % 5
                            pcnt[0] += 1
                            t.op("act", lambda e, sb_=sb_, pb_=pb_: e.activation(
                                out=pbuf[pb_][:].rearrange("p h n -> p (h n)"), in_=PS[sb_][:, :], func=AF.Exp),
                                reads=[PR[sb_]], writes=[r_pbuf[pb_]])
                            if kind == "dsa":
                                t.op("dve", lambda e, pb_=pb_, tt=tt: e.scalar_tensor_tensor(
                                    out=pbuf[pb_][:], in0=pbuf[pb_][:], scalar=1e30,
                                    in1=maskT[:, tt * 128:(tt + 1) * 128].unsqueeze(1).broadcast_to([128, 4, 128]),
                                    op0=ALU.min, op1=ALU.mult), reads=[r_pbuf[pb_], r_sm], writes=[r_pbuf[pb_]])

                            def pv(e, kb=kb, tl=tl, pb_=pb_, tt=tt):
                                for i in range(4):
                                    vh = i if kind == "dsa" else i // 2
                                    ins = e.matmul(accs[i], lhsT=pbuf[pb_][:, i, :], rhs=vbuf[kb][:, tl, vh, 0:129],
                                                   start=(tt == 0 and first_in_bank[i]), stop=(tt == NT - 1),
                                                   skip_group_check=True)
                                return ins
                            pendq.append(lambda pv=pv, kb=kb, pb_=pb_: t.op(
                                "pe", pv, reads=[r_pbuf[pb_], r_vbuf[kb]], writes=RA))
                            if len(pendq) > 3:
                                pendq.pop(0)()
                    while pendq:
                        pendq.pop(0)()
                    s0 = 8 * ab
                    if kind == "dsa":
                        for i in range(4):
                            hh = 4 * gi + i
                            t.op("dve", lambda e, i=i: e.reciprocal(out=sv2[:, s0 + i:s0 + i + 1], in_=accs[i][:, 128:129]),
                                 reads=RA, writes=[r_sv2[ab]])
                            t.op("dve", lambda e, i=i, hh=hh: e.tensor_scalar(
                                out=ysb[1][:, hh * 128:(hh + 1) * 128], in0=accs[i][:, 0:128], scalar1=sv2[:, s0 + i:s0 + i + 1],
                                scalar2=None, op0=ALU.mult), reads=RA + [r_sv2[ab]], writes=[r_ysb[1]])
                    else:
                        for hl in range(2):
                            hh = 2 * gi + hl
                            a0, a1 = accs[2 * hl], accs[2 * hl + 1]
                            c0 = s0 + 4 * hl
                            of_ = oraw[:, hh * 128:(hh + 1) * 128]
                            r_of_ = r_oraw
                            t.op("dve", lambda e, a0=a0, c0=c0: e.reciprocal(out=sv2[:, c0:c0 + 1], in_=a0[:, 128:129]),
                                 reads=RA, writes=[r_sv2[ab]])
                            t.op("dve", lambda e, a1=a1, c0=c0: e.reciprocal(out=sv2[:, c0 + 1:c0 + 2], in_=a1[:, 128:129]),
                                 reads=RA, writes=[r_sv2[ab]])
                            t.op("dve", lambda e, c0=c0: e.tensor_tensor(out=sv2[:, c0 + 1:c0 + 2], in0=sv2[:, c0 + 1:c0 + 2],
                                                                         in1=nlam[:], op=ALU.mult),
                                 reads=[r_sv2[ab], r_c2], writes=[r_sv2[ab]])
                            t.op("dve", lambda e, a0=a0, c0=c0, of_=of_: e.tensor_scalar(
                                out=of_, in0=a0[:, 0:128], scalar1=sv2[:, c0:c0 + 1], scalar2=None, op0=ALU.mult),
                                reads=RA + [r_sv2[ab]], writes=[r_of_])
                            t.op("dve", lambda e, a1=a1, c0=c0, of_=of_: e.scalar_tensor_tensor(
                                out=of_, in0=a1[:, 0:128], scalar=sv2[:, c0 + 1:c0 + 2], in1=of_,
                                op0=ALU.mult, op1=ALU.add), reads=RA + [r_sv2[ab], r_of_], writes=[r_of_])

                nb_per = NBISECT // 4
                for gi in range(4):
                    bisect(nb_per)
                    attn_group("diff", gi, gi % 2)
                bisect(NBISECT - 4 * nb_per)
                for hh in range(8):
                    t.op("act", lambda e, hh=hh: e.activation(out=junk[:], in_=oraw[:, hh * 128:(hh + 1) * 128], func=AF.Square,
                                                              accum_out=ssq[:, hh:hh + 1]),
                         reads=[r_oraw], writes=[r_ssq, r_junk])
                t.op("dve", lambda e: e.tensor_scalar(out=ssq[:, 0:8], in0=ssq[:, 0:8], scalar1=1.0 / 128.0, scalar2=LN_EPS,
                                                      op0=ALU.mult, op1=ALU.add), reads=[r_ssq], writes=[r_ssq])
                t.op("act", lambda e: e.activation(out=ssq[:, 0:8], in_=ssq[:, 0:8], func=AF.Sqrt), reads=[r_ssq], writes=[r_ssq])
                t.op("dve", lambda e: e.reciprocal(out=ssq[:, 8:16], in_=ssq[:, 0:8]), reads=[r_ssq], writes=[r_ssq])
                for hh in range(8):
                    t.op("dve", lambda e, hh=hh: e.scalar_tensor_tensor(
                        out=ysb[0][:, hh * 128:(hh + 1) * 128], in0=oraw[:, hh * 128:(hh + 1) * 128], scalar=ssq[:, 8 + hh:9 + hh],
                        in1=gbc[:], op0=ALU.mult, op1=ALU.mult), reads=[r_oraw, r_ssq, r_c2], writes=[r_ysb[0]])
                t.dma("sp", self.d_ya[q0:q0 + 128, :], ysb[0][:], reads=[r_ysb[0]])
                t.op("dve", lambda e: e.tensor_scalar(out=mask[:, 0:N], in0=score[:, 0:N], scalar1=col(LO), scalar2=None,
                                                      op0=ALU.is_gt), reads=[r_sm, r_sv], writes=[r_mask])
                for kg in range(ngrp):
                    t.op("dve", lambda e, kg=kg: e.scalar_tensor_tensor(
                        out=tmp512[:], in0=iota[:], scalar=float(kg * 512), in1=mask[:, kg * 512:(kg + 1) * 512],
                        op0=ALU.add, op1=ALU.mult), reads=[r_mask, r_c2], writes=[r_tmp])
                    t.op("dve", lambda e, kg=kg: e.reduce_max(out=am[:, kg:kg + 1], in_=tmp512[:], axis=AX.X),
                         reads=[r_tmp], writes=[r_am])
                MP, DD, AF_, BF_ = 8, 9, 10, 11
                t.op("dve", lambda e: e.reduce_max(out=col(MP), in_=am[:, 0:ngrp], axis=AX.X), reads=[r_am], writes=[r_sv])
                t.op("dve", lambda e: e.tensor_scalar(out=col(DD), in0=col(MP), scalar1=pidx1[:, 0:1], scalar2=float(128 * tq),
                                                      op0=ALU.subtract, op1=ALU.subtract), reads=[r_sv, r_c2], writes=[r_sv])
                t.op("act", lambda e: e.activation(out=col(DD), in_=col(DD), func=AF.Abs), reads=[r_sv], writes=[r_sv])
                t.op("dve", lambda e: e.tensor_copy(out=svi[:, 0:1], in_=col(DD)), reads=[r_sv], writes=[r_sv])
                t.op("dve", lambda e: e.tensor_single_scalar(out=svi[:, 1:2], in_=svi[:, 0:1], scalar=7,
                                                             op=ALU.arith_shift_right), reads=[r_sv], writes=[r_sv])
                t.op("dve", lambda e: e.tensor_copy(out=col(AF_), in_=svi[:, 1:2]), reads=[r_sv], writes=[r_sv])
                t.op("dve", lambda e: e.scalar_tensor_tensor(out=col(BF_), in0=col(AF_), scalar=-128.0, in1=col(DD),
                                                             op0=ALU.mult, op1=ALU.add), reads=[r_sv], writes=[r_sv])
                t.op("dve", lambda e: e.tensor_copy(out=ab[:, 0:2], in_=sv[:, AF_:AF_ + 2]), reads=[r_sv], writes=[r_sv])
                t.dma("sp", qbaug[2:7, :, :], self.I("c_qaug")[2:7, j, :, :], writes=[r_qbaug])
                t.op("pe", lambda e: e.matmul(PS[7][0:2, 0:128], lhsT=ab[:, 0:2], rhs=self.identb[:], start=True, stop=True),
                     reads=[r_sv, self.r_const], writes=[PR[7]])
                t.op("dve", lambda e: e.tensor_tensor(out=qbaug[0:2, :, :],
                                                      in0=PS[7][0:2, 0:128].unsqueeze(1).broadcast_to([2, 8, 128]),
                                                      in1=slopetab[:], op=ALU.mult),
                     reads=[PR[7], r_c2], writes=[r_qbaug])
                pbf = PS[6].bitcast(BF16)
                for g4 in range(ngrp):
                    def tr(e, g4=g4):
                        for tl in range(4):
                            tt = g4 * 4 + tl
                            ins = e.transpose(pbf[:, tl * 128:(tl + 1) * 128], mask[:, tt * 128:(tt + 1) * 128], self.identb[:])
                        return ins
                    t.op("pe", tr, reads=[r_mask, self.r_const], writes=[PR[6]])
                    if g4 % 2 == 0:
                        t.op("act", lambda e, g4=g4: e.copy(out=maskT[:, g4 * 512:(g4 + 1) * 512], in_=pbf[:, 0:512]),
                             reads=[PR[6]], writes=[r_sm])
                    else:
                        t.op("dve", lambda e, g4=g4: e.tensor_copy(out=maskT[:, g4 * 512:(g4 + 1) * 512], in_=pbf[:, 0:512]),
                             reads=[PR[6]], writes=[r_sm])
                for gi in range(2):
                    attn_group("dsa", gi, gi % 2)
                t.dma("sp", self.d_yb[q0:q0 + 128, :], ysb[1][:], reads=[r_ysb[1]])
            self.barrier()

    def load_w_bf16(self, es, name, src_ap, r):
        wsb = self.sb(es, name, [128, 8, 1024], BF16)
        v = src_ap.rearrange("(c p) n -> p c n", p=128)
        for a in range(0, 1024, 512):
            self.t.dma("pool", wsb[:, :, a:a + 512], v[:, :, a:a + 512], writes=[r])
        return wsb

    def ln_stats(self, xin, r_x, stats, mv, r_st):
        t = self.t
        for hh in range(2):
            t.op("dve", lambda e, hh=hh: e.bn_stats(out=stats[:, hh, :], in_=xin[:, hh * 512:(hh + 1) * 512]),
                 reads=[r_x], writes=[r_st])
        t.op("dve", lambda e: e.bn_aggr(out=mv[:, 0:2], in_=stats[:].rearrange("p a b -> p (a b)")),
             reads=[r_st], writes=[r_st])
        t.op("dve", lambda e: e.tensor_scalar(out=mv[:, 2:3], in0=mv[:, 1:2], scalar1=LN_EPS, scalar2=None,
                                              op0=ALU.add), reads=[r_st], writes=[r_st])
        t.op("act", lambda e: e.activation(out=mv[:, 2:3], in_=mv[:, 2:3], func=AF.Sqrt),
             reads=[r_st], writes=[r_st])
        t.op("dve", lambda e: e.reciprocal(out=mv[:, 3:4], in_=mv[:, 2:3]), reads=[r_st], writes=[r_st])

    def phase3(self):
        nc, t = self.nc, self.t
        PS, PR = self.ps, self.psr
        nslot = self.nslot
        with ExitStack() as es:
            r_w = self.R()
            wa = self.load_w_bf16(es, "wa", self.I("w_branch_a"), r_w)
            wb = self.load_w_bf16(es, "wb", self.I("w_branch_b"), r_w)
            wo = self.load_w_bf16(es, "wo", self.I("w_out"), r_w)
            g1 = self.sb(es, "g1bc", [128, D], F32)
            b1 = self.sb(es, "b1bc", [128, D], F32)
            r_c = self.R()
            self.ga_bc = self.sb(es, "ga_bc", [128, D], F32)
            t.dma("sp", self.ga_bc[:], self.d_mod[2 * D:3 * D].partition_broadcast(128), reads=[self.r_dmod], writes=[self.r_mod])
            t.dma("sp", g1[:], self.I("ln1_g").partition_broadcast(128), writes=[r_c])
            t.dma("sp", b1[:], self.I("ln1_b").partition_broadcast(128), writes=[r_c])
            yab = [self.sb(es, f"yab{i}", [128, 2, D], BF16) for i in range(2)]
            r_yab = [self.R() for _ in range(2)]
            yT = [self.sb(es, f"yT{i}", [128, 2, 8, 128], BF16) for i in range(2)]
            r_yT = [self.R() for _ in range(2)]
            gate = [self.sb(es, f"gate{i}", [128, 2 * D], F32) for i in range(2)]
            r_gate = [self.R() for _ in range(2)]
            xt = [self.sb(es, f"x3_{i}", [128, D], F32) for i in range(2)]
            r_xt = [self.R() for _ in range(2)]
            m1 = self.sb(es, "m1", [128, D], F32)
            m2 = self.sb(es, "m2", [128, D], F32)
            mg = self.sb(es, "mg", [128, D], BF16)
            mgT = self.sb(es, "mgT", [128, 8, 128], BF16)
            r_m = self.R()
            r_mg = self.R()
            r_mgT = self.R()
            xnew = self.sb(es, "xnew", [128, D], F32)
            r_xn = self.R()
            x1 = [self.sb(es, f"x1_{i}", [128, D], F32) for i in range(2)]
            r_x1 = [self.R() for _ in range(2)]
            stats = self.sb(es, "st3", [128, 2, 6], F32)
            mv = self.sb(es, "mv3", [128, 4], F32)
            r_st = self.R()
            for j in range(nslot):
                b = j % 2
                q0 = j * 128
                tok0 = (8 * j + 7) * 128
                t.dma("sp", yab[b][:, 0, :], self.d_ya[q0:q0 + 128, :], writes=[r_yab[b]])
                t.dma("sp", yab[b][:, 1, :], self.d_yb[q0:q0 + 128, :], writes=[r_yab[b]])
                t.dma("sp", gate[b][:], self.d_gate[q0:q0 + 128, :], writes=[r_gate[b]])
                t.dma("sp", xt[b][:], self.I("x")[tok0:tok0 + 128, :], writes=[r_xt[b]])
                for br in range(2):
                    pbf = PS[br].bitcast(BF16)

                    def tr(e, br=br, b=b, pbf=pbf):
                        for c in range(8):
                            ins = e.transpose(pbf[:, c * 128:(c + 1) * 128], yab[b][:, br, c * 128:(c + 1) * 128], self.identb[:])
                        return ins
                    t.op("pe", tr, reads=[r_yab[b], self.r_const], writes=[PR[br]])
                    t.op("act", lambda e, br=br, b=b, pbf=pbf: e.copy(
                        out=yT[b][:, br, :, :], in_=pbf[:, :].rearrange("p (c n) -> p c n", c=8)),
                        reads=[PR[br]], writes=[r_yT[b]])
                for cg in range(2):
                    csl = slice(cg * 512, (cg + 1) * 512)
                    for br, w_ in ((0, wa), (1, wb)):
                        pb = 2 + br

                        def mm(e, br=br, w_=w_, pb=pb, b=b, csl=csl):
                            for c in range(8):
                                ins = e.matmul(PS[pb][:, :], lhsT=yT[b][:, br, c, :], rhs=w_[:, c, csl],
                                               start=(c == 0), stop=(c == 7))
                            return ins
                        t.op("pe", mm, reads=[r_yT[b], r_w], writes=[PR[pb]])
                    t.op("dve", lambda e, b=b, csl=csl, cg=cg: e.tensor_tensor(
                        out=m1[:, csl], in0=PS[2][:, :], in1=gate[b][:, cg * 512:(cg + 1) * 512], op=ALU.mult),
                        reads=[PR[2], r_gate[b]], writes=[r_m])
                    t.op("dve", lambda e, b=b, csl=csl, cg=cg: e.tensor_tensor(
                        out=m2[:, csl], in0=PS[3][:, :], in1=gate[b][:, D + cg * 512:D + (cg + 1) * 512], op=ALU.mult),
                        reads=[PR[3], r_gate[b]], writes=[r_m])
                    t.op("dve", lambda e, csl=csl: e.tensor_tensor(out=mg[:, csl], in0=m1[:, csl], in1=m2[:, csl], op=ALU.add),
                         reads=[r_m], writes=[r_mg])
                pbf = PS[4].bitcast(BF16)

                def tr2(e, pbf=pbf):
                    for c in range(8):
                        ins = e.transpose(pbf[:, c * 128:(c + 1) * 128], mg[:, c * 128:(c + 1) * 128], self.identb[:])
                    return ins
                t.op("pe", tr2, reads=[r_mg, self.r_const], writes=[PR[4]])
                t.op("act", lambda e, pbf=pbf: e.copy(out=mgT[:], in_=pbf[:, :].rearrange("p (c n) -> p c n", c=8)),
                     reads=[PR[4]], writes=[r_mgT])
                for cg in range(2):
                    csl = slice(cg * 512, (cg + 1) * 512)
                    pb = 5 + cg

                    def mm3(e, pb=pb, csl=csl):
                        for c in range(8):
                            ins = e.matmul(PS[pb][:, :], lhsT=mgT[:, c, :], rhs=wo[:, c, csl], start=(c == 0), stop=(c == 7))
                        return ins
                    t.op("pe", mm3, reads=[r_mgT, r_w], writes=[PR[pb]])
                    t.op("dve", lambda e, pb=pb, csl=csl: e.tensor_tensor(out=m1[:, csl], in0=PS[pb][:, :],
                                                                          in1=self.ga_bc[:, csl], op=ALU.mult),
                         reads=[PR[pb], self.r_mod], writes=[r_m])
                    t.op("dve", lambda e, b=b, csl=csl: e.scalar_tensor_tensor(
                        out=xnew[:, csl], in0=xt[b][:, csl], scalar=ALPHA, in1=m1[:, csl], op0=ALU.mult, op1=ALU.add),
                        reads=[r_xt[b], r_m], writes=[r_xn])
                self.ln_stats(xnew, r_xn, stats, mv, r_st)
                t.op("dve", lambda e, b=b: e.tensor_scalar(out=x1[b][:], in0=xnew[:], scalar1=mv[:, 0:1], scalar2=mv[:, 3:4],
                                                           op0=ALU.subtract, op1=ALU.mult),
                     reads=[r_xn, r_st], writes=[r_x1[b]])
                t.op("pool", lambda e, b=b: e.tensor_tensor(out=x1[b][:], in0=x1[b][:], in1=g1[:], op=ALU.mult),
                     reads=[r_x1[b], r_c], writes=[r_x1[b]])
                t.op("pool", lambda e, b=b: e.tensor_tensor(out=x1[b][:], in0=x1[b][:], in1=b1[:], op=ALU.add),
                     reads=[r_x1[b], r_c], writes=[r_x1[b]])
                t.dma("sp", self.d_x1[q0:q0 + 128, :], x1[b][:], reads=[r_x1[b]])
            self.barrier()

    def phase4(self):
        nc, t = self.nc, self.t
        PS, PR = self.ps, self.psr
        NQ = self.NQ
        HT = min(1024, NQ)
        nhalf = NQ // HT
        TG = min(512, HT)
        ntg = HT // TG
        ntt = HT // 128
        with ExitStack() as es:
            scf = self.sb(es, "scf_bc", [128, D], F32)
            shf = self.sb(es, "shf_bc", [128, D], F32)
            g2 = scf
            b2l = shf
            wr = self.sb(es, "wr", [128, 8, NEXP], F32)
            brr = self.sb(es, "brr", [1, NEXP], F32)
            onesf = self.sb(es, "onesf4", [1, 128], F32)
            b2w = self.sb(es, "b2w", [NEXP, D], F32)
            b1raw = self.sb(es, "b1raw", [NEXP, 2 * DFF], F32)
            b1g = self.sb(es, "b1g", [128, 8, NEXP], F32)
            b1l = self.sb(es, "b1l", [128, 8, NEXP], F32)
            r_c = self.R()
            self.gf_bc = self.sb(es, "gf_bc", [128, D], F32)
            t.dma("sp", self.gf_bc[:], self.d_mod[5 * D:6 * D].partition_broadcast(128), reads=[self.r_dmod], writes=[self.r_mod])
            r_md = self.R()
            t.dma("sp", wr[:], self.I("w_router").rearrange("(c p) n -> p c n", p=128), writes=[r_c])
            t.dma("sp", brr[:], self.I("b_router").rearrange("(o n) -> o n", o=1), writes=[r_c])
            t.dma("sp", b2w[:], self.I("b_e2"), writes=[r_c])
            t.dma("sp", b1raw[:], self.I("b_e1"), writes=[r_c])
            t.op("dve", lambda e: e.memset(onesf[:], 1.0), writes=[r_c])
            b1v = b1raw[:].rearrange("e (p f two) -> e p f two", p=8, two=2)
            for p in range(8):
                for two, dst in ((0, b1g), (1, b1l)):
                    t.op("pe", lambda e, p=p, two=two: e.transpose(PS[7][:, 0:NEXP], b1v[:, p, :, two], self.identf[0:NEXP, 0:NEXP]),
                         reads=[r_c, self.r_const], writes=[PR[7]])
                    t.op("dve", lambda e, p=p, dst=dst, two=two: e.tensor_scalar(
                        out=dst[:, p, :], in0=PS[7][:, 0:NEXP], scalar1=float(two), scalar2=None, op0=ALU.add),
                        reads=[PR[7]], writes=[r_c])
            vT = self.sb(es, "vT", [128, 8, HT], BF16)
            r_vT = self.R()
            yacc = self.sb(es, "yacc", [128, ntt, D], F32)
            r_y = self.R()
            gate = self.sb(es, "gate4", [128, ntt, NEXP], F32)
            r_g = self.R()
            aT = [self.sb(es, f"aT{i}", [128, 8, HT], BF16) for i in range(2)]
            r_aT = [self.R() for _ in range(2)]
            w1p = [self.sb(es, f"w1p{i}", [128, 8, 256], BF16) for i in range(5)]
            r_w1 = [self.R() for _ in range(5)]
            w2e = [self.sb(es, f"w2e{i}", [128, 8, D], BF16) for i in range(2)]
            r_w2 = [self.R() for _ in range(2)]
            glu = [self.sb(es, f"glu{i}", [128, TG], F32) for i in range(2)]
            sig = [self.sb(es, f"sig{i}", [128, TG], F32) for i in range(2)]
            lin = [self.sb(es, f"lin{i}", [128, TG], F32) for i in range(2)]
            r_elg = [self.R() for _ in range(3)]
            r_ell = [self.R() for _ in range(3)]
            r_els = [self.R() for _ in range(3)]
            xt = [self.sb(es, f"x4_{i}", [128, D], F32) for i in range(2)]
            r_xt = [self.R() for _ in range(2)]
            vf = self.sb(es, "vf", [128, D], F32)
            vb16 = self.sb(es, "vb16", [128, D], BF16)
            vTf = self.sb(es, "vTf", [128, 8, 128], F32)
            r_v = self.R()
            stats = self.sb(es, "st4", [128, 2, 6], F32)
            mv = self.sb(es, "mv4", [128, 4], F32)
            r_st = self.R()
            rt = self.sb(es, "rt", [128, 4, NEXP], F32)
            m8 = self.sb(es, "m8", [128, 16], F32)
            gT = self.sb(es, "gT", [NEXP, 128], F32)
            r_rt = self.R()
            w1cnt = [0]
            wfc = [0]
            elc = [0]
            for hf in range(nhalf):
                h0 = hf * HT
                t.dma("sp", scf[:], self.d_mod[4 * D:5 * D].partition_broadcast(128), writes=[r_md])
                t.dma("sp", shf[:], self.d_mod[3 * D:4 * D].partition_broadcast(128), writes=[r_md])
                t.op("dve", lambda e: e.tensor_scalar(out=scf[:], in0=scf[:], scalar1=1.0, scalar2=None, op0=ALU.add),
                     reads=[r_md], writes=[r_md])
                for tt in range(ntt):
                    b = tt % 2
                    q0 = h0 + tt * 128
                    t.dma("sp", xt[b][:], self.d_x1[q0:q0 + 128, :], writes=[r_xt[b]])
                    self.ln_stats(xt[b], r_xt[b], stats, mv, r_st)
                    t.op("dve", lambda e, b=b: e.tensor_scalar(out=vf[:], in0=xt[b][:], scalar1=mv[:, 0:1], scalar2=mv[:, 3:4],
                                                               op0=ALU.subtract, op1=ALU.mult),
                         reads=[r_xt[b], r_st], writes=[r_v])
                    t.op("pool", lambda e: e.tensor_tensor(out=vf[:], in0=vf[:], in1=scf[:], op=ALU.mult),
                         reads=[r_v, r_md], writes=[r_v])
                    t.op("pool", lambda e: e.tensor_tensor(out=vf[:], in0=vf[:], in1=shf[:], op=ALU.add),
                         reads=[r_v, r_md], writes=[r_v])
                    t.op("dve", lambda e: e.tensor_copy(out=vb16[:], in_=vf[:]), reads=[r_v], writes=[r_v])
                    pbf = PS[0].bitcast(BF16)

                    def tr(e, pbf=pbf):
                        for c in range(8):
                            ins = e.transpose(pbf[:, c * 128:(c + 1) * 128], vb16[:, c * 128:(c + 1) * 128], self.identb[:])
                        return ins
                    t.op("pe", tr, reads=[r_v, self.r_const], writes=[PR[0]])
                    t.op("act", lambda e, tt=tt, pbf=pbf: e.copy(out=vT[:, :, tt * 128:(tt + 1) * 128],
                                                                 in_=pbf[:, :].rearrange("p (c n) -> p c n", c=8)),
                         reads=[PR[0]], writes=[r_vT])
                    for half2 in range(2):
                        def trf(e, half2=half2):
                            for c4 in range(4):
                                c = half2 * 4 + c4
                                ins = e.transpose(PS[1 + half2][:, c4 * 128:(c4 + 1) * 128], vf[:, c * 128:(c + 1) * 128], self.identf[:])
                            return ins
                        t.op("pe", trf, reads=[r_v, self.r_const], writes=[PR[1 + half2]])
                        t.op("dve", lambda e, half2=half2: e.tensor_copy(
                            out=vTf[:, half2 * 4:half2 * 4 + 4, :], in_=PS[1 + half2][:, :].rearrange("p (c n) -> p c n", c=4)),
                            reads=[PR[1 + half2]], writes=[r_v])

                    def mmr(e):
                        for c in range(8):
                            e.matmul(PS[3][:, 0:NEXP], lhsT=vTf[:, c, :], rhs=wr[:, c, :], start=(c == 0), stop=False)
                        return e.matmul(PS[3][:, 0:NEXP], lhsT=onesf[0:1, :], rhs=brr[0:1, :], start=False, stop=True)
                    t.op("pe", mmr, reads=[r_v, r_c], writes=[PR[3]])
                    LG, SEL, EX = 0, 1, 2
                    t.op("dve", lambda e: e.tensor_copy(out=rt[:, LG, :], in_=PS[3][:, 0:NEXP]), reads=[PR[3]], writes=[r_rt])
                    t.op("dve", lambda e: e.max(out=m8[:, 0:8], in_=rt[:, LG, :]), reads=[r_rt], writes=[r_rt])
                    t.op("dve", lambda e: e.tensor_scalar(out=rt[:, SEL, :], in0=rt[:, LG, :], scalar1=m8[:, 3:4], scalar2=None,
                                                          op0=ALU.is_ge), reads=[r_rt], writes=[r_rt])
                    t.op("dve", lambda e: e.tensor_scalar(out=m8[:, 8:9], in0=m8[:, 0:1], scalar1=-1.0, scalar2=None,
                                                          op0=ALU.mult), reads=[r_rt], writes=[r_rt])
                    t.op("act", lambda e: e.activation(out=rt[:, EX, :], in_=rt[:, LG, :], func=AF.Exp, bias=m8[:, 8:9], scale=1.0),
                         reads=[r_rt], writes=[r_rt])
                    t.op("dve", lambda e: e.tensor_tensor(out=rt[:, EX, :], in0=rt[:, EX, :], in1=rt[:, SEL, :], op=ALU.mult),
                         reads=[r_rt], writes=[r_rt])
                    t.op("dve", lambda e: e.reduce_sum(out=m8[:, 9:10], in_=rt[:, EX, :], axis=AX.X), reads=[r_rt], writes=[r_rt])
                    t.op("dve", lambda e: e.reciprocal(out=m8[:, 10:11], in_=m8[:, 9:10]), reads=[r_rt], writes=[r_rt])
                    t.op("dve", lambda e, tt=tt: e.tensor_scalar(out=gate[:, tt, :], in0=rt[:, EX, :], scalar1=m8[:, 10:11],
                                                                 scalar2=None, op0=ALU.mult), reads=[r_rt], writes=[r_g])
                    t.op("pe", lambda e, tt=tt: e.transpose(PS[3][0:NEXP, 128:256], gate[:, tt, :], self.identf[:]),
                         reads=[r_g, self.r_const], writes=[PR[3]])
                    t.op("dve", lambda e: e.tensor_copy(out=gT[:], in_=PS[3][0:NEXP, 128:256]), reads=[PR[3]], writes=[r_rt])
                    for cg in range(2):
                        t.op("pe", lambda e, cg=cg: e.matmul(PS[4 + cg][:, :], lhsT=gT[:, :], rhs=b2w[:, cg * 512:(cg + 1) * 512],
                                                            start=True, stop=True), reads=[r_rt, r_c], writes=[PR[4 + cg]])
                        t.op("dve", lambda e, cg=cg, tt=tt: e.tensor_copy(out=yacc[:, tt, cg * 512:(cg + 1) * 512], in_=PS[4 + cg][:, :]),
                             reads=[PR[4 + cg]], writes=[r_y])

                def stageA(e_):
                    ab_ = e_ % 2
                    for p in range(8):
                        wb_ = w1cnt[0] % 5
                        w1cnt[0] += 1
                        t.dma("pool", w1p[wb_][:], self.I("w_e1")[e_, :, p * 256:(p + 1) * 256].rearrange("(c q) n -> q c n", q=128),
                              writes=[r_w1[wb_]])
                        for tg in range(ntg):
                            tsl = slice(tg * TG, (tg + 1) * TG)
                            pg, pl = (0, 1) if (p * ntg + tg) % 2 == 0 else (2, 3)

                            def mm(e, wb_=wb_, tsl=tsl, pg=pg, pl=pl):
                                for two, pb in ((0, pg), (1, pl)):
                                    for c in range(8):
                                        ins = e.matmul(PS[pb][:, 0:TG], lhsT=w1p[wb_][:, c, two::2], rhs=vT[:, c, tsl],
                                                       start=(c == 0), stop=(c == 7))
                                return ins
                            t.op("pe", mm, reads=[r_w1[wb_], r_vT], writes=[PR[pg], PR[pl]])
                            k = elc[0] % 2
                            elc[0] += 1
                            t.op("dve", lambda e, k=k, pg=pg, p=p, e_=e_: e.tensor_scalar(
                                out=glu[k][:], in0=PS[pg][:, 0:TG], scalar1=b1g[:, p, e_:e_ + 1], scalar2=SWIGLU_LIMIT,
                                op0=ALU.add, op1=ALU.min), reads=[PR[pg], r_c], writes=[r_elg[k]])
                            t.op("dve", lambda e, k=k, pl=pl, p=p, e_=e_: e.tensor_scalar(
                                out=lin[k][:], in0=PS[pl][:, 0:TG], scalar1=b1l[:, p, e_:e_ + 1], scalar2=1.0 - SWIGLU_LIMIT,
                                op0=ALU.add, op1=ALU.max), reads=[PR[pl], r_c], writes=[r_ell[k]])
                            t.op("act", lambda e, k=k: e.activation(out=sig[k][:], in_=glu[k][:], func=AF.Sigmoid, scale=SWIGLU_ALPHA),
                                 reads=[r_elg[k]], writes=[r_els[k]])
                            t.op("dve", lambda e, k=k: e.scalar_tensor_tensor(
                                out=lin[k][:], in0=lin[k][:], scalar=1.0 + SWIGLU_LIMIT, in1=glu[k][:], op0=ALU.min, op1=ALU.mult),
                                reads=[r_ell[k], r_elg[k]], writes=[r_ell[k]])
                            t.op("dve", lambda e, k=k, ab_=ab_, p=p, tsl=tsl: e.tensor_tensor(
                                out=aT[ab_][:, p, tsl], in0=lin[k][:], in1=sig[k][:], op=ALU.mult),
                                reads=[r_ell[k], r_els[k]], writes=[r_aT[ab_]])

                def stageB(e_):
                    ab_ = e_ % 2
                    for tt in range(ntt):
                        for cg in range(2):
                            pb = 4 + (tt * 2 + cg) % 3

                            def mm(e, tt=tt, cg=cg, pb=pb):
                                for p in range(8):
                                    ins = e.matmul(PS[pb][:, :], lhsT=aT[ab_][:, p, tt * 128:(tt + 1) * 128],
                                                   rhs=w2e[ab_][:, p, cg * 512:(cg + 1) * 512], start=(p == 0), stop=(p == 7))
                                return ins
                            t.op("pe", mm, reads=[r_aT[ab_], r_w2[ab_]], writes=[PR[pb]])
                            t.op("dve", lambda e, tt=tt, cg=cg, pb=pb: e.scalar_tensor_tensor(
                                out=yacc[:, tt, cg * 512:(cg + 1) * 512], in0=PS[pb][:, :], scalar=gate[:, tt, e_:e_ + 1],
                                in1=yacc[:, tt, cg * 512:(cg + 1) * 512], op0=ALU.mult, op1=ALU.add),
                                reads=[PR[pb], r_g, r_y], writes=[r_y])

                def loadw2(e_):
                    ab_ = e_ % 2
                    v = self.I("w_e2")[e_].rearrange("(c q) n -> q c n", q=128)
                    for a in range(0, 1024, 512):
                        t.dma("pool", w2e[ab_][:, :, a:a + 512], v[:, :, a:a + 512], writes=[r_w2[ab_]])
                loadw2(0)
                stageA(0)
                for e_ in range(NEXP):
                    if e_ + 1 < NEXP:
                        loadw2(e_ + 1)
                        stageA(e_ + 1)
                    stageB(e_)
                t.dma("sp", g2[:], self.I("ln2_g").partition_broadcast(128), writes=[r_md])
                t.dma("sp", b2l[:], self.I("ln2_b").partition_broadcast(128), writes=[r_md])
                for tt in range(ntt):
                    b = tt % 2
                    q0 = h0 + tt * 128
                    t.dma("sp", xt[b][:], self.d_x1[q0:q0 + 128, :], writes=[r_xt[b]])
                    t.op("pool", lambda e, tt=tt: e.tensor_tensor(out=yacc[:, tt, :], in0=yacc[:, tt, :], in1=self.gf_bc[:], op=ALU.mult),
                         reads=[r_y, self.r_mod], writes=[r_y])
                    t.op("dve", lambda e, b=b, tt=tt: e.scalar_tensor_tensor(out=vf[:], in0=xt[b][:], scalar=ALPHA, in1=yacc[:, tt, :],
                                                                            op0=ALU.mult, op1=ALU.add),
                         reads=[r_xt[b], r_y], writes=[r_v])
                    self.ln_stats(vf, r_v, stats, mv, r_st)
                    t.op("dve", lambda e, b=b: e.tensor_scalar(out=xt[b][:], in0=vf[:], scalar1=mv[:, 0:1], scalar2=mv[:, 3:4],
                                                               op0=ALU.subtract, op1=ALU.mult),
                         reads=[r_v, r_st], writes=[r_xt[b]])
                    t.op("pool", lambda e, b=b: e.tensor_tensor(out=xt[b][:], in0=xt[b][:], in1=g2[:], op=ALU.mult),
                         reads=[r_xt[b], r_md], writes=[r_xt[b]])
                    t.op("pool", lambda e, b=b: e.tensor_tensor(out=xt[b][:], in0=xt[b][:], in1=b2l[:], op=ALU.add),
                         reads=[r_xt[b], r_md], writes=[r_xt[b]])
                    t.dma("sp", self.out[q0:q0 + 128, :], xt[b][:], reads=[r_xt[b]], writes=[self.r_out])
            self.barrier()


def make_consts(nslot, c):
    ntile = 8 * nslot
    S = 128 * ntile
    slopes = alibi_slopes(8)
    k = np.arange(S)
    tt = k // 128
    pk = k % 128
    ndummy = 7 - c
    kaug = np.zeros((7, S), np.float32)
    kaug[0] = 1.0
    kaug[1] = 1.0
    kaug[2] = 128.0 * tt
    kaug[3] = 1.0
    kaug[4] = 1.0
    kaug[5] = pk
    kaug[6] = (tt < ndummy).astype(np.float32)
    qaug = np.zeros((7, nslot, 8, 128), np.float32)
    ql = np.arange(128)
    for j in range(nslot):
        tq = 8 * j + 7
        for h in range(8):
            s = slopes[h]
            qaug[2, j, h] = s
            qaug[3, j, h] = -s * 128.0 * tq
            qaug[4, j, h] = -s * ql
            qaug[5, j, h] = s
            qaug[6, j, h] = NEG
    kk = np.arange(128)[:, None]
    qq = np.arange(128)[None, :]
    cend = (qq // 64 + 1) * 64
    dg = np.zeros((128, 8, 128), np.float32)
    for h in range(8):
        s = slopes[h]
        m = np.where(kk > qq, -2.0 * s * (kk - qq), 0.0)
        m = np.where(kk >= cend, NEG, m)
        dg[:, h, :] = m
    dmask = np.where(kk.T >= 0, 0.0, 0.0) * 0.0
    qq2 = np.arange(128)[:, None]
    kk2 = np.arange(128)[None, :]
    dmask = np.where(kk2 < (qq2 // 64 + 1) * 64, 0.0, -1e9).astype(np.float32)
    dummy = np.zeros((128, 8), np.float32)
    dummy[:, :ndummy] = -1e9
    iota = np.broadcast_to(np.arange(1, 513, dtype=np.float32)[None, :], (128, 512)).copy()
    slopetab = np.zeros((2, 8, 128), np.float32)
    for h in range(8):
        slopetab[0, h] = slopes[h] * 128.0
        slopetab[1, h] = slopes[h]
    return {
        "c_identb": np.eye(128, dtype=np.float32).astype(NPBF),
        "c_identf": np.eye(128, dtype=np.float32),
        "c_kaug": kaug.astype(NPBF),
        "c_qaug": qaug.astype(NPBF),
        "c_dg": dg.astype(NPBF),
        "c_dmask": dmask,
        "c_dummy": dummy,
        "c_iota": iota,
        "c_slopetab": slopetab,
        "c_pidx1": np.arange(1, 129, dtype=np.float32).reshape(128, 1),
    }


def make_in_maps(inputs, nslot, used=None):
    S = 128 * 8 * nslot
    f = lambda a: np.ascontiguousarray(np.asarray(a, dtype=np.float32))
    x = f(inputs["x"])[0]
    assert x.shape[0] == S
    shared = {
        "c": f(inputs["c"])[0],
        "w_ada": f(inputs["w_ada"])[0],
        "b_ada": f(inputs["b_ada"])[0],
        "w_in": f(inputs["w_in"])[0],
        "lamv": np.stack([f(inputs[k])[0] for k in ("lam_q1", "lam_k1", "lam_q2", "lam_k2")]),
        "diff_norm_g": f(inputs["diff_norm_g"])[0],
        "w_branch_a": f(inputs["w_branch_a"])[0],
        "w_branch_b": f(inputs["w_branch_b"])[0],
        "w_out": f(inputs["w_out"])[0],
        "ln1_g": f(inputs["ln1_g"])[0],
        "ln1_b": f(inputs["ln1_b"])[0],
        "w_router": f(inputs["w_router"])[0],
        "b_router": f(inputs["b_router"])[0],
        "w_e1": f(inputs["w_e1"])[0],
        "b_e1": f(inputs["b_e1"])[0],
        "w_e2": f(inputs["w_e2"])[0],
        "b_e2": f(inputs["b_e2"])[0],
        "ln2_g": f(inputs["ln2_g"])[0],
        "ln2_b": f(inputs["ln2_b"])[0],
    }
    maps = []
    for c in range(NCORE):
        m = dict(shared)
        m["x"] = np.ascontiguousarray(np.roll(x, 128 * (7 - c), axis=0))
        m.update(make_consts(nslot, c))
        maps.append({k: v for k, v in m.items() if used is None or k in used})
    return maps


_CACHE = {}


def run(inputs, nslot, debug=False, phases=99, trace=False):
    key = (nslot, debug, phases)
    mk = MK(nslot, debug=debug, phases=phases)
    nc = mk.build()
    in_maps = make_in_maps(inputs, nslot, used=set(mk.in_aps.keys()))
    res = run_bass_kernel_spmd(nc, in_maps, core_ids=list(range(NCORE)), trace=trace)
    return res


def kernel(**inputs):
    nslot = 16
    res = run(inputs, nslot)
    S = 128 * 8 * nslot
    out = np.zeros((1, S, D), np.float32)
    for c in range(NCORE):
        o = np.asarray(res.results[c]["out"], dtype=np.float32)
        for j in range(nslot):
            rt = 8 * j + c
            out[0, rt * 128:(rt + 1) * 128, :] = o[j * 128:(j + 1) * 128, :]
    return out
```

```python
import os
import numpy as np
import ml_dtypes
from contextlib import ExitStack
import concourse.bass as bass
import concourse.mybir as mybir
from concourse.bass_utils import run_bass_kernel_spmd

F32 = mybir.dt.float32
BF16 = mybir.dt.bfloat16
I32 = mybir.dt.int32
AF = mybir.ActivationFunctionType
ALU = mybir.AluOpType
AX = mybir.AxisListType
NPBF = ml_dtypes.bfloat16

D = 1024
NCORE = 8
NEXP = 32
DFF = 1024
TOPK = 256
LN_EPS = 1e-5
ALPHA = 2.0 ** 0.25
LAM_INIT = 0.2
NEG = -30000.0
NBISECT = 24
SWIGLU_ALPHA = 1.702
SWIGLU_LIMIT = 7.0
C_QA, C_KA, C_VA, C_QB, C_KB, C_VB, C_QI, C_KI, C_WI, C_GA, C_GB = (
    0, 1024, 2048, 3072, 4096, 5120, 6144, 7168, 7232, 7248, 8272)
PROJ_W = 9296


class Res:
    __slots__ = ("w", "r", "name")

    def __init__(self, name=""):
        self.w = None
        self.r = {}
        self.name = name


class Eng:
    def __init__(self, name, eng, sem):
        self.name = name
        self.eng = eng
        self.sem = sem
        self.cnt = 0
        self.seen = {}


class Trk:
    def __init__(self, nc, es):
        self.nc = nc
        mk = lambda n: es.enter_context(nc.semaphore(n))
        self.E = {n: Eng(n, e, mk("s_" + n)) for n, e in [
            ("pe", nc.tensor), ("act", nc.scalar), ("dve", nc.vector),
            ("pool", nc.gpsimd), ("sp", nc.sync)]}
        self.dsems = {q: [[mk(f"d_{q}{i}"), 0] for i in range(n)]
                      for q, n in [("sp", 16), ("pool", 10), ("act", 4)]}
        self.dnext = {q: 0 for q in self.dsems}
        self.nwait = 0

    def _waits(self, E, reads, writes):
        need = {}

        def add(tok, raw):
            if tok is None:
                return
            sem, val = tok
            if sem is E.sem and E.name == "pe":
                return
            k = id(sem)
            if k not in need or need[k][1] < val:
                need[k] = (sem, val)
        for r in reads:
            add(r.w, True)
        for w in writes:
            add(w.w, False)
            for tok in w.r.values():
                add(tok, False)
        for k, (sem, val) in need.items():
            if E.seen.get(k, 0) < val:
                E.eng.wait_ge(sem, val)
                E.seen[k] = val
                self.nwait += 1

    @staticmethod
    def _mark(tok, reads, writes):
        k = id(tok[0])
        for r in reads:
            r.r[k] = tok
        for w in writes:
            w.w = tok
            w.r = {}

    def op(self, en, fn, reads=(), writes=()):
        E = self.E[en]
        self._waits(E, reads, writes)
        ins = fn(E.eng)
        E.cnt += 1
        ins.then_inc(E.sem, 1)
        self._mark((E.sem, E.cnt), reads, writes)

    def dma(self, q, out, in_, reads=(), writes=(), **kw):
        E = self.E[q]
        self._waits(E, reads, writes)
        slots = self.dsems[q]
        i = self.dnext[q]
        self.dnext[q] = (i + 1) % len(slots)
        sem, val = slots[i]
        k = id(sem)
        if val > 0 and E.seen.get(k, 0) < val:
            E.eng.wait_ge(sem, val)
            E.seen[k] = val
        ins = E.eng.dma_start(out=out, in_=in_, **kw)
        val += 16
        slots[i][1] = val
        ins.then_inc(sem, 16)
        self._mark((sem, val), reads, writes)

    def barrier(self, all_res):
        toks = {}
        for r in all_res:
            for tok in [r.w] + list(r.r.values()):
                if tok is None:
                    continue
                k = id(tok[0])
                if k not in toks or toks[k][1] < tok[1]:
                    toks[k] = tok
        for E in self.E.values():
            for k, (sem, val) in toks.items():
                if sem is E.sem:
                    continue
                if E.seen.get(k, 0) < val:
                    E.eng.wait_ge(sem, val)
                    E.seen[k] = val


def alibi_slopes(n=8):
    return [2.0 ** (-8.0 * (h + 1) / n) for h in range(n)]


class MK:
    def __init__(self, nslot, debug=False, phases=99):
        self.nslot = nslot
        self.ntile = 8 * nslot
        self.S = 128 * self.ntile
        self.NQ = 128 * nslot
        self.debug = debug
        self.phases = phases
        self.nc = bass.Bass("TRN2", target_bir_lowering=False)
        self.res_all = []

    def R(self, name=""):
        r = Res(name)
        self.res_all.append(r)
        return r

    def I(self, name):
        if name not in self.in_aps:
            shape, dt = self.in_specs[name]
            self.in_aps[name] = self.nc.dram_tensor(name, list(shape), dt, kind="ExternalInput").ap()
        return self.in_aps[name]

    def dscr(self, name, shape, dt):
        kind = "ExternalOutput" if self.debug else "Internal"
        t = self.nc.dram_tensor(name, list(shape), dt, kind=kind).ap()
        return t

    def sb(self, es, name, shape, dt):
        return es.enter_context(self.nc.sbuf_tensor(name, list(shape), dt))

    def barrier(self):
        self.t.barrier(self.res_all)

    def build(self):
        nc = self.nc
        S, NQ, nslot, ntile = self.S, self.NQ, self.nslot, self.ntile
        self.in_specs = {
            "x": ([S, D], F32), "c": ([D], F32), "w_ada": ([D, 6 * D], F32), "b_ada": ([6 * D], F32),
            "w_in": ([D, PROJ_W], F32), "lamv": ([4, 64], F32), "diff_norm_g": ([128], F32),
            "w_branch_a": ([D, D], F32), "w_branch_b": ([D, D], F32), "w_out": ([D, D], F32),
            "ln1_g": ([D], F32), "ln1_b": ([D], F32), "w_router": ([D, NEXP], F32), "b_router": ([NEXP], F32),
            "w_e1": ([NEXP, D, 2 * DFF], F32), "b_e1": ([NEXP, 2 * DFF], F32),
            "w_e2": ([NEXP, DFF, D], F32), "b_e2": ([NEXP, D], F32), "ln2_g": ([D], F32), "ln2_b": ([D], F32),
            "c_identb": ([128, 128], BF16), "c_identf": ([128, 128], F32), "c_kaug": ([7, S], BF16),
            "c_qaug": ([7, nslot, 8, 128], BF16), "c_dg": ([128, 8, 128], BF16), "c_dmask": ([128, 128], F32),
            "c_dummy": ([128, 8], F32), "c_iota": ([128, 512], F32), "c_slopetab": ([2, 8, 128], F32),
            "c_pidx1": ([128, 1], F32),
        }
        self.in_aps = {}
        self.out = nc.dram_tensor("out", [NQ, D], F32, kind="ExternalOutput").ap()
        self.d_mod = self.dscr("d_mod", [6 * D], F32)
        self.d_kat = self.dscr("d_kat", [16, 64, S], BF16)
        self.d_va = self.dscr("d_va", [S, 8, 132], BF16)
        self.d_kbt = self.dscr("d_kbt", [8, 128, S], BF16)
        self.d_vb = self.dscr("d_vb", [S, 8, 132], BF16)
        self.d_kit = self.dscr("d_kit", [64, S], BF16)
        self.d_qat = self.dscr("d_qat", [16, 64, NQ], BF16)
        self.d_qbt = self.dscr("d_qbt", [8, 128, NQ], BF16)
        self.d_qit = self.dscr("d_qit", [16, 64, NQ], BF16)
        self.d_sgn = self.dscr("d_sgn", [NQ, 16], F32)
        self.d_gate = self.dscr("d_gate", [NQ, 2 * D], F32)
        self.d_ya = self.dscr("d_ya", [NQ, D], BF16)
        self.d_yb = self.dscr("d_yb", [NQ, D], BF16)
        self.d_x1 = self.dscr("d_x1", [NQ, D], F32)

        with ExitStack() as es:
            self.t = Trk(nc, es)
            self.ps = [es.enter_context(nc.psum_tensor(f"ps{i}", [128, 512], F32)) for i in range(8)]
            self.psr = [self.R(f"ps{i}") for i in range(8)]
            self.identb = self.sb(es, "identb", [128, 128], BF16)
            self.identf = self.sb(es, "identf", [128, 128], F32)
            self.r_const = self.R("const")
            self.t.dma("sp", self.identb[:], self.I("c_identb"), writes=[self.r_const])
            self.t.dma("sp", self.identf[:], self.I("c_identf"), writes=[self.r_const])
            self.modT = self.sb(es, "modT", [128, 48], F32)
            self.r_mod = self.R("mod")
            self.r_out = self.R("out")
            self.phase0()
            self.barrier()
            if self.phases >= 1:
                self.phase1a()
                self.barrier()
                self.phase1b()
                self.barrier()
            if self.phases >= 2:
                self.phase2()
                self.barrier()
            if self.phases >= 3:
                self.phase3()
                self.barrier()
            if self.phases >= 4:
                self.phase4()
            self.final_wait()
        return nc

    def final_wait(self):
        self.barrier()

    def phase0(self):
        nc, t = self.nc, self.t
        with ExitStack() as es:
            cT = self.sb(es, "cT", [128, 8], F32)
            cact = self.sb(es, "cact", [128, 8], F32)
            wbuf = [self.sb(es, f"wada{i}", [128, 8, 512], F32) for i in range(2)]
            wr = [self.R() for _ in range(2)]
            brow = self.sb(es, "brow", [1, 6 * D], F32)
            mrow = self.sb(es, "mrow", [1, 6 * D], F32)
            r_c, r_b, r_m = self.R(), self.R(), self.R()
            t.dma("sp", cT[:], self.I("c").rearrange("(c p) -> p c", p=128), writes=[r_c],
                  allow_slow_non_contiguous=True)
            t.dma("sp", brow[:], self.I("b_ada").rearrange("(o n) -> o n", o=1), writes=[r_b])
            t.op("act", lambda e: e.activation(out=cact[:], in_=cT[:], func=AF.Silu),
                 reads=[r_c], writes=[r_c])
            wv = self.I("w_ada").rearrange("(c p) n -> p c n", p=128)
            for g in range(12):
                b = g % 2
                t.dma("sp", wbuf[b][:], wv[:, :, g * 512:(g + 1) * 512], writes=[wr[b]])
                pb = g % 2

                def mm(e, b=b, pb=pb):
                    for c in range(8):
                        ins = e.matmul(self.ps[pb][0:1, :], lhsT=cact[:, c:c + 1], rhs=wbuf[b][:, c, :],
                                       start=(c == 0), stop=(c == 7))
                    return ins
                t.op("pe", mm, reads=[r_c, wr[b]], writes=[self.psr[pb]])
                t.op("dve", lambda e, g=g, pb=pb: e.tensor_tensor(
                    out=mrow[:, g * 512:(g + 1) * 512], in0=self.ps[pb][0:1, :],
                    in1=brow[:, g * 512:(g + 1) * 512], op=ALU.add),
                    reads=[self.psr[pb], r_b], writes=[r_m])
            r_d = self.R()
            t.dma("sp", self.d_mod.rearrange("(o n) -> o n", o=1), mrow[:], reads=[r_m], writes=[r_d])
            t.dma("sp", self.modT[:], self.d_mod.rearrange("(m p) -> p m", p=128), reads=[r_d],
                  writes=[self.r_mod], allow_slow_non_contiguous=True)
            self.r_dmod = r_d
            self.barrier()

    def ln_pre(self, xin_ap, xt, r_xt, xn, r_xn, stats, mv, r_st, q="sp"):
        t = self.t
        t.dma(q, xt[:], xin_ap, writes=[r_xt])
        for hh in range(2):
            t.op("dve", lambda e, hh=hh: e.bn_stats(out=stats[:, hh, :], in_=xt[:, hh * 512:(hh + 1) * 512]),
                 reads=[r_xt], writes=[r_st])
        t.op("dve", lambda e: e.bn_aggr(out=mv[:, 0:2], in_=stats[:].rearrange("p a b -> p (a b)")),
             reads=[r_st], writes=[r_st])
        t.op("dve", lambda e: e.tensor_scalar(out=mv[:, 2:3], in0=mv[:, 1:2], scalar1=LN_EPS, scalar2=None,
                                              op0=ALU.add), reads=[r_st], writes=[r_st])
        t.op("act", lambda e: e.activation(out=mv[:, 2:3], in_=mv[:, 2:3], func=AF.Sqrt),
             reads=[r_st], writes=[r_st])
        t.op("dve", lambda e: e.reciprocal(out=mv[:, 3:4], in_=mv[:, 2:3]), reads=[r_st], writes=[r_st])
        t.op("dve", lambda e: e.tensor_scalar(out=xn[:], in0=xt[:], scalar1=mv[:, 0:1], scalar2=mv[:, 3:4],
                                              op0=ALU.subtract, op1=ALU.mult),
             reads=[r_xt, r_st], writes=[r_xn])

    def ln_post(self, xn, r_xn, out_xnT, r_out, pbank):
        t = self.t
        pbf = self.ps[pbank].bitcast(BF16)

        def tr(e):
            for c in range(8):
                ins = e.transpose(pbf[:, c * 128:(c + 1) * 128], xn[:, c * 128:(c + 1) * 128], self.identb[:])
            return ins
        t.op("pe", tr, reads=[r_xn, self.r_const], writes=[self.psr[pbank]])
        t.op("act", lambda e: e.copy(out=out_xnT, in_=pbf[:, :].rearrange("p (c n) -> p c n", c=8)),
             reads=[self.psr[pbank]], writes=[r_out])

    def ln_tile(self, xin_ap, xt, r_xt, xn, r_xn, stats, mv, r_st, out_xnT, r_out, pbank, q="sp"):
        self.ln_pre(xin_ap, xt, r_xt, xn, r_xn, stats, mv, r_st, q=q)
        self.ln_post(xn, r_xn, out_xnT, r_out, pbank)

    def prep_w(self, es, tag, colranges, sc_off, sh_off):
        nc, t = self.nc, self.t
        ncols = sum(l for _, l in colranges)
        wsb = self.sb(es, "w_" + tag, [128, 8, ncols], BF16)
        r_w = self.R()
        wv = self.I("w_in").rearrange("(c p) n -> p c n", p=128)
        o = 0
        for (s0, l) in colranges:
            for a in range(0, l, 512):
                b = min(l, a + 512)
                t.dma("pool", wsb[:, :, o + a:o + b], wv[:, :, s0 + a:s0 + b], writes=[r_w])
            o += l
        nch = (ncols + 127) // 128
        biasT = self.sb(es, "bT_" + tag, [128, nch], F32)
        biasrow = self.sb(es, "br_" + tag, [1, ncols], BF16)
        onep = self.sb(es, "onep_" + tag, [128, 8], F32)
        shb = self.sb(es, "shb_" + tag, [128, 8], BF16)
        r_b = self.R()
        t.op("dve", lambda e: e.tensor_scalar(out=onep[:], in0=self.modT[:, sc_off:sc_off + 8], scalar1=1.0,
                                              scalar2=None, op0=ALU.add), reads=[self.r_mod], writes=[r_b])
        t.op("dve", lambda e: e.tensor_copy(out=shb[:], in_=self.modT[:, sh_off:sh_off + 8]),
             reads=[self.r_mod], writes=[r_b])
        for ch in range(nch):
            w0 = ch * 128
            wl = min(128, ncols - w0)
            pb = ch % 2

            def mm(e, w0=w0, wl=wl, pb=pb):
                for c in range(8):
                    ins = e.matmul(self.ps[pb][0:wl, 0:1], lhsT=wsb[:, c, w0:w0 + wl], rhs=shb[:, c:c + 1],
                                   start=(c == 0), stop=(c == 7))
                return ins
            t.op("pe", mm, reads=[r_w, r_b], writes=[self.psr[pb]])
            t.op("dve", lambda e, ch=ch, wl=wl, pb=pb: e.tensor_copy(out=biasT[0:wl, ch:ch + 1],
                                                                      in_=self.ps[pb][0:wl, 0:1]),
                 reads=[self.psr[pb]], writes=[r_b])
        for a in range(0, ncols, 512):
            b = min(ncols, a + 512)
            pb = 2 + (a // 512) % 2

            def mm2(e, a=a, b=b, pb=pb):
                for c in range(8):
                    ins = e.matmul(self.ps[pb][0:1, 0:b - a], lhsT=shb[:, c:c + 1], rhs=wsb[:, c, a:b],
                                   start=(c == 0), stop=(c == 7))
                return ins
            t.op("pe", mm2, reads=[r_w, r_b], writes=[self.psr[pb]])
            t.op("dve", lambda e, a=a, b=b, pb=pb: e.tensor_copy(out=biasrow[:, a:b], in_=self.ps[pb][0:1, 0:b - a]),
                 reads=[self.psr[pb]], writes=[r_b])
        for c in range(8):
            en = "dve" if c % 2 == 0 else "pool"
            t.op(en, lambda e, c=c: e.tensor_scalar(out=wsb[:, c, :], in0=wsb[:, c, :], scalar1=onep[:, c:c + 1],
                                                     scalar2=None, op0=ALU.mult),
                 reads=[r_w, r_b], writes=[r_w])
        return wsb, r_w, biasT, biasrow, r_b

    def phase1a(self):
        nc, t = self.nc, self.t
        S = self.S
        with ExitStack() as es:
            wsb, r_w, biasT, biasrow, r_b = self.prep_w(
                es, "p1a", [(C_KA, 1024), (C_KB, 1024), (C_KI, 64), (C_VA, 1024), (C_VB, 1024)], 8, 0)
            VOFF = 2112
            ones = self.sb(es, "ones1", [1, 128], BF16)
            r_ones = self.R()
            t.op("dve", lambda e: e.memset(ones[:], 1.0), writes=[r_ones])
            xt = [self.sb(es, f"xt{i}", [128, D], F32) for i in range(2)]
            r_xt = [self.R() for _ in range(2)]
            xn = [self.sb(es, f"xn{i}", [128, D], BF16) for i in range(2)]
            r_xn = [self.R() for _ in range(2)]
            stats = [self.sb(es, f"st{i}", [128, 2, 6], F32) for i in range(2)]
            mv = [self.sb(es, f"mv{i}", [128, 4], F32) for i in range(2)]
            r_st = [self.R() for _ in range(2)]
            xnT = [self.sb(es, f"xnT{i}", [128, 8, 512], BF16) for i in range(2)]
            r_xnT = [self.R() for _ in range(2)]
            kst = [self.sb(es, f"kst{i}", [128, 512], BF16) for i in range(4)]
            r_kst = [self.R() for _ in range(4)]
            vst = [self.sb(es, f"vst{i}", [128, 4, 132], BF16) for i in range(4)]
            r_vst = [self.R() for _ in range(4)]
            for i in range(4):
                t.op("pool", lambda e, i=i: e.memset(vst[i][:, :, 128:132], 0.0), writes=[r_vst[i]])
                t.op("pool", lambda e, i=i: e.memset(vst[i][:, :, 128:129], 1.0), writes=[r_vst[i]])
            ngrp = S // 512
            ki = 0
            vi = 0
            tcount = 0
            ev = 0
            xn4 = [self.sb(es, f"xn4_{i}", [128, D], BF16) for i in range(8)]
            r_xn4 = [self.R() for _ in range(8)]

            def ln_group_pre(g):
                for tt in range(4):
                    b = tcnt[0] % 2
                    tcnt[0] += 1
                    k = (g % 2) * 4 + tt
                    tok0 = g * 512 + tt * 128
                    self.ln_pre(self.I("x")[tok0:tok0 + 128, :], xt[b], r_xt[b], xn4[k], r_xn4[k], stats[b], mv[b], r_st[b])

            def ln_group_post(g):
                gb = g % 2
                for tt in range(4):
                    k = (g % 2) * 4 + tt
                    self.ln_post(xn4[k], r_xn4[k], xnT[gb][:, :, tt * 128:(tt + 1) * 128], r_xnT[gb], pbank=tt % 2)
            tcnt = [0]
            ln_group_pre(0)
            ln_group_post(0)
            for g in range(ngrp):
                gb = g % 2
                if g + 1 < ngrp:
                    ln_group_pre(g + 1)
                for ch in range(17):
                    wl = 128 if ch < 16 else 64
                    pb = 2 + ch % 3

                    def mm(e, ch=ch, wl=wl, pb=pb, gb=gb):
                        for c in range(8):
                            ins = e.matmul(self.ps[pb][0:wl, :], lhsT=wsb[:, c, ch * 128:ch * 128 + wl],
                                           rhs=xnT[gb][:, c, :], start=(c == 0), stop=(c == 7))
                        return ins
                    t.op("pe", mm, reads=[r_w, r_xnT[gb]], writes=[self.psr[pb]])
                    s = ki % 4
                    ki += 1
                    en = "act" if ev % 4 != 3 else "dve"
                    ev += 1
                    if en == "act":
                        t.op("act", lambda e, s=s, wl=wl, pb=pb, ch=ch: e.activation(
                            out=kst[s][0:wl, :], in_=self.ps[pb][0:wl, :], func=AF.Identity,
                            bias=biasT[0:wl, ch:ch + 1], scale=1.0),
                            reads=[self.psr[pb], r_b], writes=[r_kst[s]])
                    else:
                        t.op("dve", lambda e, s=s, wl=wl, pb=pb, ch=ch: e.tensor_scalar(
                            out=kst[s][0:wl, :], in0=self.ps[pb][0:wl, :], scalar1=biasT[0:wl, ch:ch + 1],
                            scalar2=None, op0=ALU.add),
                            reads=[self.psr[pb], r_b], writes=[r_kst[s]])
                    tsl = slice(g * 512, (g + 1) * 512)
                    if ch < 8:
                        t.dma("sp", self.d_kat[2 * ch:2 * ch + 2, :, tsl].rearrange("m d n -> (m d) n"),
                              kst[s][:, :], reads=[r_kst[s]])
                    elif ch < 16:
                        t.dma("sp", self.d_kbt[ch - 8, :, tsl], kst[s][:, :], reads=[r_kst[s]])
                    else:
                        t.dma("sp", self.d_kit[:, tsl], kst[s][0:64, :], reads=[r_kst[s]])
                if g + 1 < ngrp:
                    ln_group_post(g + 1)
                for tt in range(4):
                    for vg in range(4):
                        pb = 5 + (tt * 4 + vg) % 3
                        c0 = VOFF + vg * 512

                        def mm(e, tt=tt, c0=c0, pb=pb, gb=gb):
                            for c in range(8):
                                e.matmul(self.ps[pb][:, :], lhsT=xnT[gb][:, c, tt * 128:(tt + 1) * 128],
                                         rhs=wsb[:, c, c0:c0 + 512], start=(c == 0), stop=False)
                            return e.matmul(self.ps[pb][:, :], lhsT=ones[0:1, :], rhs=biasrow[0:1, c0:c0 + 512],
                                            start=False, stop=True)
                        t.op("pe", mm, reads=[r_w, r_xnT[gb], r_b, r_ones], writes=[self.psr[pb]])
                        s = vi % 4
                        vi += 1
                        en = "act" if ev % 4 != 3 else "dve"
                        ev += 1
                        psv = self.ps[pb][:, :].rearrange("p (h e) -> p h e", h=4)
                        if en == "act":
                            t.op("act", lambda e, s=s, psv=psv: e.copy(out=vst[s][:, :, 0:128], in_=psv),
                                 reads=[self.psr[pb]], writes=[r_vst[s]])
                        else:
                            t.op("dve", lambda e, s=s, psv=psv: e.tensor_copy(out=vst[s][:, :, 0:128], in_=psv),
                                 reads=[self.psr[pb]], writes=[r_vst[s]])
                        tok0 = g * 512 + tt * 128
                        dst = self.d_va if vg < 2 else self.d_vb
                        t.dma("sp", dst[tok0:tok0 + 128, (vg % 2) * 4:(vg % 2) * 4 + 4, :], vst[s][:],
                              reads=[r_vst[s]])
            self.barrier()

    def phase1b(self):
        nc, t = self.nc, self.t
        nslot, NQ = self.nslot, self.NQ
        with ExitStack() as es:
            wsb, r_w, biasT, biasrow, r_b = self.prep_w(
                es, "p1b", [(C_QA, 1024), (C_QB, 1024), (C_QI, 1024), (C_WI, 16), (C_GA, 1024), (C_GB, 1024)], 8, 0)
            O_QI, O_WI, O_GA = 2048, 3072, 3088
            ones = self.sb(es, "ones1b", [1, 128], BF16)
            r_ones = self.R()
            t.op("dve", lambda e: e.memset(ones[:], 1.0), writes=[r_ones])
            xt = [self.sb(es, f"xtb{i}", [128, D], F32) for i in range(2)]
            r_xt = [self.R() for _ in range(2)]
            xn = [self.sb(es, f"xnb{i}", [128, D], BF16) for i in range(2)]
            r_xn = [self.R() for _ in range(2)]
            stats = [self.sb(es, f"stb{i}", [128, 2, 6], F32) for i in range(2)]
            mv = [self.sb(es, f"mvb{i}", [128, 4], F32) for i in range(2)]
            r_st = [self.R() for _ in range(2)]
            G = min(4, nslot)
            xnT = [self.sb(es, f"xnTb{i}", [128, 8, 128 * G], BF16) for i in range(2)]
            r_xnT = [self.R() for _ in range(2)]
            kst = [self.sb(es, f"kstb{i}", [128, 128 * G], BF16) for i in range(4)]
            r_kst = [self.R() for _ in range(4)]
            gst = [self.sb(es, f"gst{i}", [128, 512], F32) for i in range(3)]
            r_gst = [self.R() for _ in range(3)]
            wis = [self.sb(es, f"wis{i}", [128, 3, 16], F32) for i in range(2)]
            r_wis = [self.R() for _ in range(2)]
            qis = [self.sb(es, f"qis{i}", [128, 1024], BF16) for i in range(2)]
            r_qis = [self.R() for _ in range(2)]
            qit = [self.sb(es, f"qit{i}", [128, 8, 128], BF16) for i in range(2)]
            r_qit = [self.R() for _ in range(2)]
            ki = 0
            gi = 0
            tcount = 0
            for g in range(nslot // G):
                gb = g % 2
                NT = 128 * G
                for tt in range(G):
                    b = tcount % 2
                    j = g * G + tt
                    tok0 = (8 * j + 7) * 128
                    self.ln_tile(self.I("x")[tok0:tok0 + 128, :], xt[b], r_xt[b], xn[b], r_xn[b], stats[b], mv[b],
                                 r_st[b], xnT[gb][:, :, tt * 128:(tt + 1) * 128], r_xnT[gb], pbank=b)
                    tcount += 1
                q0 = g * NT
                for ch in range(16):
                    pb = 2 + ch % 3

                    def mm(e, ch=ch, pb=pb, gb=gb, NT=NT):
                        for c in range(8):
                            ins = e.matmul(self.ps[pb][:, 0:NT], lhsT=wsb[:, c, ch * 128:ch * 128 + 128],
                                           rhs=xnT[gb][:, c, :], start=(c == 0), stop=(c == 7))
                        return ins
                    t.op("pe", mm, reads=[r_w, r_xnT[gb]], writes=[self.psr[pb]])
                    s = ki % 4
                    ki += 1
                    scale = 0.125 if ch < 8 else 128.0 ** -0.5
                    t.op("dve", lambda e, s=s, pb=pb, ch=ch, scale=scale, NT=NT: e.tensor_scalar(
                        out=kst[s][:, 0:NT], in0=self.ps[pb][:, 0:NT], scalar1=biasT[:, ch:ch + 1], scalar2=scale,
                        op0=ALU.add, op1=ALU.mult), reads=[self.psr[pb], r_b], writes=[r_kst[s]])
                    if ch < 8:
                        t.dma("sp", self.d_qat[2 * ch:2 * ch + 2, :, q0:q0 + NT].rearrange("m d n -> (m d) n"),
                              kst[s][:, 0:NT], reads=[r_kst[s]])
                    else:
                        t.dma("sp", self.d_qbt[ch - 8, :, q0:q0 + NT], kst[s][:, 0:NT], reads=[r_kst[s]])
                for tt in range(G):
                    j = g * G + tt
                    tq0 = j * 128
                    lhs = lambda c, tt=tt, gb=gb: xnT[gb][:, c, tt * 128:(tt + 1) * 128]
                    wb = j % 2
                    pb = 5

                    def mmw(e, lhs=lhs, pb=pb):
                        for c in range(8):
                            e.matmul(self.ps[pb][:, 0:16], lhsT=lhs(c), rhs=wsb[:, c, O_WI:O_WI + 16],
                                     start=(c == 0), stop=False)
                        return e.matmul(self.ps[pb][:, 0:16], lhsT=ones[0:1, :], rhs=biasrow[0:1, O_WI:O_WI + 16],
                                        start=False, stop=True)
                    t.op("pe", mmw, reads=[r_w, r_xnT[gb], r_b, r_ones], writes=[self.psr[pb]])
                    t.op("dve", lambda e, wb=wb, pb=pb: e.tensor_copy(out=wis[wb][:, 0, :], in_=self.ps[pb][:, 0:16]),
                         reads=[self.psr[pb]], writes=[r_wis[wb]])
                    t.op("act", lambda e, wb=wb: e.activation(out=wis[wb][:, 1, :], in_=wis[wb][:, 0, :],
                                                              func=AF.Abs, scale=1.0 / 32.0),
                         reads=[r_wis[wb]], writes=[r_wis[wb]])
                    t.op("act", lambda e, wb=wb: e.activation(out=wis[wb][:, 2, :], in_=wis[wb][:, 0, :], func=AF.Sign),
                         reads=[r_wis[wb]], writes=[r_wis[wb]])
                    t.dma("sp", self.d_sgn[tq0:tq0 + 128, :], wis[wb][:, 2, :], reads=[r_wis[wb]])
                    for qg in range(2):
                        pb = 6 + qg
                        c0 = O_QI + qg * 512

                        def mmq(e, lhs=lhs, pb=pb, c0=c0):
                            for c in range(8):
                                e.matmul(self.ps[pb][:, :], lhsT=lhs(c), rhs=wsb[:, c, c0:c0 + 512],
                                         start=(c == 0), stop=False)
                            return e.matmul(self.ps[pb][:, :], lhsT=ones[0:1, :], rhs=biasrow[0:1, c0:c0 + 512],
                                            start=False, stop=True)
                        t.op("pe", mmq, reads=[r_w, r_xnT[gb], r_b, r_ones], writes=[self.psr[pb]])
                        t.op("dve", lambda e, wb=wb, pb=pb, qg=qg: e.tensor_tensor(
                            out=qis[wb][:, qg * 512:(qg + 1) * 512].rearrange("p (h d) -> p h d", h=8),
                            in0=self.ps[pb][:, :].rearrange("p (h d) -> p h d", h=8),
                            in1=wis[wb][:, 1, qg * 8:(qg + 1) * 8].unsqueeze(2).broadcast_to([128, 8, 64]),
                            op=ALU.mult), reads=[self.psr[pb], r_wis[wb]], writes=[r_qis[wb]])
                    pbf = self.ps[2 + (j % 3)].bitcast(BF16)

                    def tr(e, wb=wb, pbf=pbf):
                        for c in range(8):
                            ins = e.transpose(pbf[:, c * 128:(c + 1) * 128], qis[wb][:, c * 128:(c + 1) * 128],
                                              self.identb[:])
                        return ins
                    t.op("pe", tr, reads=[r_qis[wb], self.r_const], writes=[self.psr[2 + (j % 3)]])
                    t.op("act", lambda e, wb=wb, pbf=pbf: e.copy(
                        out=qit[wb][:], in_=pbf[:, :].rearrange("p (c n) -> p c n", c=8)),
                        reads=[self.psr[2 + (j % 3)]], writes=[r_qit[wb]])
                    for c in range(8):
                        t.dma("sp", self.d_qit[2 * c:2 * c + 2, :, tq0:tq0 + 128].rearrange("m d n -> (m d) n"),
                              qit[wb][:, c, :], reads=[r_qit[wb]])
                    for gg in range(4):
                        pb = 5 + gg % 3
                        c0 = O_GA + gg * 512

                        def mmg(e, lhs=lhs, pb=pb, c0=c0):
                            for c in range(8):
                                e.matmul(self.ps[pb][:, :], lhsT=lhs(c), rhs=wsb[:, c, c0:c0 + 512],
                                         start=(c == 0), stop=False)
                            return e.matmul(self.ps[pb][:, :], lhsT=ones[0:1, :], rhs=biasrow[0:1, c0:c0 + 512],
                                            start=False, stop=True)
                        t.op("pe", mmg, reads=[r_w, r_xnT[gb], r_b, r_ones], writes=[self.psr[pb]])
                        s = gi % 3
                        gi += 1
                        t.op("act", lambda e, s=s, pb=pb: e.activation(out=gst[s][:], in_=self.ps[pb][:, :],
                                                                        func=AF.Sigmoid),
                             reads=[self.psr[pb]], writes=[r_gst[s]])
                        t.dma("sp", self.d_gate[tq0:tq0 + 128, gg * 512:(gg + 1) * 512], gst[s][:],
                              reads=[r_gst[s]])
            self.barrier()

    def phase2(self):
        nc, t = self.nc, self.t
        STOP = int(os.environ.get('P2STOP', '99'))
        EXP = os.environ.get('EXP', '')
        S, nslot = self.S, self.nslot
        PS = self.ps
        PR = self.psr
        with ExitStack() as es:
            dg = self.sb(es, "dg", [128, 8, 128], BF16)
            dmask = self.sb(es, "dmask", [128, 128], F32)
            dummy = self.sb(es, "dummyc", [128, 8], F32)
            iota = self.sb(es, "iotac", [128, 512], F32)
            slopetab = self.sb(es, "slopetab", [2, 8, 128], F32)
            pidx1 = self.sb(es, "pidx1", [128, 1], F32)
            nlam = self.sb(es, "nlam", [128, 1], F32)
            gbc = self.sb(es, "gbc", [128, 128], F32)
            r_c2 = self.R("c2")
            for dst, nm in [(dg, "c_dg"), (dmask, "c_dmask"), (dummy, "c_dummy"), (iota, "c_iota"),
                            (slopetab, "c_slopetab"), (pidx1, "c_pidx1")]:
                t.dma("sp", dst[:], self.I(nm), writes=[r_c2])
            lv = self.sb(es, "lv", [1, 4, 64], F32)
            lsm = self.sb(es, "lsm", [1, 8], F32)
            onesf = self.sb(es, "onesf", [1, 128], F32)
            r_l = self.R("lam")
            t.dma("sp", lv[:], self.I("lamv").rearrange("(o a) d -> o a d", o=1), writes=[r_l])
            t.op("dve", lambda e: e.memset(onesf[:], 1.0), writes=[r_l])
            t.op("dve", lambda e: e.tensor_tensor(out=lv[:, 0, :], in0=lv[:, 0, :], in1=lv[:, 1, :], op=ALU.mult),
                 reads=[r_l], writes=[r_l])
            t.op("dve", lambda e: e.tensor_tensor(out=lv[:, 2, :], in0=lv[:, 2, :], in1=lv[:, 3, :], op=ALU.mult),
                 reads=[r_l], writes=[r_l])
            t.op("dve", lambda e: e.reduce_sum(out=lsm[:, 0:1], in_=lv[:, 0, :], axis=AX.X), reads=[r_l], writes=[r_l])
            t.op("dve", lambda e: e.reduce_sum(out=lsm[:, 1:2], in_=lv[:, 2, :], axis=AX.X), reads=[r_l], writes=[r_l])
            t.op("act", lambda e: e.activation(out=lsm[:, 2:4], in_=lsm[:, 0:2], func=AF.Exp), reads=[r_l], writes=[r_l])
            t.op("dve", lambda e: e.tensor_tensor(out=lsm[:, 4:5], in0=lsm[:, 3:4], in1=lsm[:, 2:3], op=ALU.subtract),
                 reads=[r_l], writes=[r_l])
            t.op("dve", lambda e: e.tensor_scalar(out=lsm[:, 5:6], in0=lsm[:, 4:5], scalar1=-LAM_INIT, scalar2=None,
                                                  op0=ALU.add), reads=[r_l], writes=[r_l])
            t.op("pe", lambda e: e.matmul(PS[7][:, 0:1], lhsT=onesf[0:1, :], rhs=lsm[0:1, 5:6], start=True, stop=True),
                 reads=[r_l], writes=[PR[7]])
            t.op("dve", lambda e: e.tensor_copy(out=nlam[:], in_=PS[7][:, 0:1]), reads=[PR[7]], writes=[r_c2])
            t.dma("sp", gbc[:], self.I("diff_norm_g").partition_broadcast(128), writes=[r_c2])
            t.op("dve", lambda e: e.tensor_scalar(out=gbc[:], in0=gbc[:], scalar1=1.0 - LAM_INIT, scalar2=None,
                                                  op0=ALU.mult), reads=[r_c2], writes=[r_c2])

            if STOP <= 0:
                self.barrier()
                return
            score = self.sb(es, "score", [128, S], F32)
            maskT = score.bitcast(BF16)
            r_sm = self.R("score")
            mask = self.sb(es, "mask", [128, S], BF16)
            r_mask = self.R("mask")
            qi_sb = self.sb(es, "qi_sb", [64, 16, 128], BF16)
            r_qi = self.R()
            sgn = self.sb(es, "sgn", [128, 16], F32)
            dsg = self.sb(es, "dsg", [128, 16, 128], BF16)
            r_dsg = self.R()
            kit_sb = [self.sb(es, f"kit{i}", [64, 1024], BF16) for i in range(2)]
            r_kit = [self.R() for _ in range(2)]
            rbuf = [self.sb(es, f"rbuf{i}", [128, 512], BF16) for i in range(4)]
            r_rbuf = [self.R() for _ in range(4)]
            sv = self.sb(es, "sv", [128, 48], F32)
            svi = self.sb(es, "svi", [128, 4], I32)
            r_sv = self.R()
            am = self.sb(es, "am", [128, 40], F32)
            r_am = self.R()
            tmp512 = self.sb(es, "tmp512", [128, 512], F32)
            r_tmp = self.R()
            ab = self.sb(es, "ab", [128, 2], BF16)
            qb_sb = self.sb(es, "qb_sb", [128, 8, 128], BF16)
            r_qb = self.R()
            qbaug = self.sb(es, "qbaug", [128, 8, 128], BF16)
            r_qbaug = self.R()
            qa_sb = self.sb(es, "qa_sb", [69, 16, 128], BF16)
            r_qa = self.R()
            NKB = 3
            kbuf = [self.sb(es, f"kbuf{i}", [128, 4, 1024], BF16) for i in range(NKB)]
            r_kbuf = [self.R() for _ in range(NKB)]
            vbuf = [self.sb(es, f"vbuf{i}", [128, 8, 4, 132], BF16) for i in range(NKB)]
            r_vbuf = [self.R() for _ in range(NKB)]
            kaug_sb = [self.sb(es, f"kaug{i}", [128, 1024], BF16) for i in range(NKB)]
            r_kaug = [self.R() for _ in range(NKB)]
            pbuf = [self.sb(es, f"pbuf{i}", [128, 4, 128], BF16) for i in range(5)]
            r_pbuf = [self.R() for _ in range(5)]
            ysb = [self.sb(es, f"ysb{i}", [128, D], BF16) for i in range(2)]
            r_ysb = [self.R() for _ in range(2)]
            junk = self.sb(es, "junk128", [128, 128], F32)
            r_junk = self.R()
            sv2 = self.sb(es, "sv2", [128, 16], F32)
            oraw = self.sb(es, "oraw", [128, D], F32)
            r_oraw = self.R()
            ssq = self.sb(es, "ssq", [128, 16], F32)
            r_ssq = self.R()
            r_sv2 = [self.R() for _ in range(2)]
            for i in range(NKB):
                t.op("pool", lambda e, i=i: e.memset(kaug_sb[i][:], 0.0), writes=[r_kaug[i]])
            t.op("pool", lambda e: e.memset(qbaug[:], 0.0), writes=[r_qbaug])
            kcnt = [0]
            pcnt = [0]
            scnt = [0]
            kitc = [0]
            rcnt = [0]

            for j in range(nslot):
                tq = 8 * j + 7
                NT = tq + 1
                N = NT * 128
                q0 = j * 128
                ngrp = NT // 4
                t.dma("sp", qi_sb[:], self.d_qit[:, :, q0:q0 + 128].rearrange("h d n -> d h n"), writes=[r_qi])
                t.dma("sp", sgn[:], self.d_sgn[q0:q0 + 128, :], writes=[r_dsg])
                for h in range(16):
                    en = "dve" if (h % 2 == 0 or os.environ.get("NOPOOL")) else "pool"
                    t.op(en, lambda e, h=h: e.tensor_scalar(out=dsg[:, h, :], in0=self.identb[:], scalar1=sgn[:, h:h + 1],
                                                            scalar2=None, op0=ALU.mult),
                         reads=[r_dsg, self.r_const], writes=[r_dsg])
                for kg in range(ngrp):
                    if kg % 2 == 0:
                        kb = kitc[0] % 2
                        kitc[0] += 1
                        w = min(1024, N - kg * 512)
                        t.dma("sp", kit_sb[kb][:, 0:w], self.d_kit[:, kg * 512:kg * 512 + w], writes=[r_kit[kb]])
                    koff = (kg % 2) * 512

                    def logits(h, kb=kb, koff=koff):
                        lb = 4 + h % 3
                        t.op("pe", lambda e: e.matmul(PS[lb][:, :], lhsT=qi_sb[:, h, :], rhs=kit_sb[kb][:, koff:koff + 512],
                                                      start=True, stop=True),
                             reads=[r_qi, r_kit[kb]], writes=[PR[lb]])

                    def relu(h):
                        lb = 4 + h % 3
                        rb = rcnt[0] % 4
                        rcnt[0] += 1
                        if h % 2 == 0 and "b" not in EXP:
                            t.op("act", lambda e: e.activation(out=rbuf[rb][:], in_=PS[lb][:, :], func=AF.Relu),
                                 reads=[PR[lb]], writes=[r_rbuf[rb]])
                        else:
                            t.op("dve", lambda e: e.tensor_scalar(out=rbuf[rb][:], in0=PS[lb][:, :], scalar1=0.0,
                                                                  scalar2=None, op0=ALU.max),
                                 reads=[PR[lb]], writes=[r_rbuf[rb]])
                        return rb

                    def hsum(h, rb):
                        t.op("pe", lambda e: e.matmul(PS[7][:, :], lhsT=dsg[:, h, :], rhs=rbuf[rb][:],
                                                      start=(h == 0), stop=(h == 15)),
                             reads=[r_dsg, r_rbuf[rb]], writes=[PR[7]])
                    if "e" in EXP:
                        continue
                    logits(0)
                    logits(1)
                    for h in range(16):
                        if h + 2 < 16:
                            logits(h + 2)
                        rb = relu(h)
                        if "d" not in EXP:
                            hsum(h, rb)
                    if "d" in EXP:
                        continue
                    if "f" in EXP:
                        continue
                    sl = slice(kg * 512, (kg + 1) * 512)
                    if "g" in EXP:
                        pass
                    elif "a" in EXP:
                        t.op("dve", lambda e, kg=kg: e.reduce_max(out=am[:, kg:kg + 1], in_=PS[7][:, :], axis=AX.X),
                             reads=[PR[7]], writes=[r_am])
                    else:
                        t.op("dve", lambda e, kg=kg: e.tensor_reduce(out=am[:, kg:kg + 1], in_=PS[7][:, :], axis=AX.X,
                                                                     op=ALU.max, apply_absolute_value=True),
                             reads=[PR[7]], writes=[r_am])
                    if "h" not in EXP:
                        if "I" not in EXP:
                            t.op("dve", lambda e, sl=sl: e.tensor_copy(out=score[:, sl], in_=PS[7][:, :]),
                                 reads=[PR[7]], writes=[r_sm])
                        else:
                            t.op("act", lambda e, sl=sl: e.activation(out=score[:, sl], in_=PS[7][:, :], func=AF.Identity),
                                 reads=[PR[7]], writes=[r_sm])
                    for tl in range(4):
                        if "c" in EXP:
                            break
                        tt = kg * 4 + tl
                        ts_ = slice(tt * 128, (tt + 1) * 128)
                        if tt < 7:
                            t.op("dve", lambda e, ts_=ts_, tt=tt: e.tensor_scalar(
                                out=score[:, ts_], in0=score[:, ts_], scalar1=dummy[:, tt:tt + 1], scalar2=None,
                                op0=ALU.add), reads=[r_sm, r_c2], writes=[r_sm])
                        if tt == NT - 1:
                            t.op("dve", lambda e, ts_=ts_: e.tensor_tensor(out=score[:, ts_], in0=score[:, ts_],
                                                                           in1=dmask[:], op=ALU.add),
                                 reads=[r_sm, r_c2], writes=[r_sm])
                if STOP <= 1:
                    continue
                LO, W0, MID, CNT, PRED, AMX = 0, 1, 2, 3, 4, 7
                col = lambda i: sv[:, i:i + 1]
                t.op("dve", lambda e: e.reduce_max(out=col(AMX), in_=am[:, 0:ngrp], axis=AX.X), reads=[r_am], writes=[r_sv])
                t.op("dve", lambda e: e.tensor_scalar(out=col(LO), in0=col(AMX), scalar1=1.0, scalar2=-1.0, op0=ALU.add, op1=ALU.mult),
                     reads=[r_sv], writes=[r_sv])
                t.op("dve", lambda e: e.tensor_scalar(out=col(W0), in0=col(AMX), scalar1=1.0, scalar2=2.0, op0=ALU.add, op1=ALU.mult),
                     reads=[r_sv], writes=[r_sv])
                bit = [0]

                def bisect(nit):
                    for it in range(nit):
                        f = 0.5 ** (bit[0] + 1)
                        bit[0] += 1
                        t.op("dve", lambda e, f=f: e.scalar_tensor_tensor(out=col(MID), in0=col(W0), scalar=f, in1=col(LO),
                                                                          op0=ALU.mult, op1=ALU.add), reads=[r_sv], writes=[r_sv])
                        t.op("dve", lambda e: e.tensor_scalar(out=mask[:, 0:N], in0=score[:, 0:N], scalar1=col(MID), scalar2=None,
                                                              op0=ALU.is_gt, op1=ALU.add, accum_out=col(CNT)),
                             reads=[r_sm, r_sv], writes=[r_mask, r_sv])
                        t.op("dve", lambda e, f=f: e.tensor_scalar(out=col(PRED), in0=col(CNT), scalar1=float(TOPK) - 0.5, scalar2=f,
                                                                   op0=ALU.is_gt, op1=ALU.mult), reads=[r_sv], writes=[r_sv])
                        t.op("dve", lambda e: e.scalar_tensor_tensor(out=col(LO), in0=col(W0), scalar=col(PRED), in1=col(LO),
                                                                     op0=ALU.mult, op1=ALU.add), reads=[r_sv], writes=[r_sv])

                t.dma("sp", qb_sb[:], self.d_qbt[:, :, q0:q0 + 128].rearrange("h d n -> d h n"), writes=[r_qb])
                t.dma("sp", qa_sb[0:64, :, :], self.d_qat[:, :, q0:q0 + 128].rearrange("m d n -> d m n"), writes=[r_qa])
                for m_ in range(2):
                    t.dma("sp", qa_sb[64:69, :, :].rearrange("r (h m) n -> r h m n", m=2)[:, :, m_, :],
                          self.I("c_qaug")[2:7, j, :, :], writes=[r_qa])

                def attn_group(kind, gi, ab):
                    b0, b1_ = (2, 3) if ab == 0 else (4, 5)
                    accs = [PS[b0][:, 0:129], PS[b0][:, 129:258], PS[b0][:, 258:387], PS[b1_][:, 0:129]]
                    first_in_bank = [True, False, False, True]
                    RA = [PR[b0], PR[b1_]]
                    pendq = []
                    for tg in range(NT // 8):
                        kb = kcnt[0] % NKB
                        kcnt[0] += 1
                        ksl = slice(tg * 1024, (tg + 1) * 1024)
                        if kind == "dsa":
                            t.dma("sp", kbuf[kb][:, :, :], self.d_kbt[4 * gi:4 * gi + 4, :, ksl].rearrange("h d n -> d h n"),
                                  writes=[r_kbuf[kb]])
                            t.dma("sp", kaug_sb[kb][0:7, :], self.I("c_kaug")[:, ksl], writes=[r_kaug[kb]])
                            t.dma("sp", vbuf[kb][:, :, :, :].rearrange("p t h e -> p t (h e)"),
                                  self.d_vb[ksl, 4 * gi:4 * gi + 4, :].rearrange("(t p) h e -> p t (h e)", p=128),
                                  writes=[r_vbuf[kb]])
                        else:
                            t.dma("sp", kbuf[kb][0:64, :, :], self.d_kat[4 * gi:4 * gi + 4, :, ksl].rearrange("m d n -> d m n"),
                                  writes=[r_kbuf[kb]])
                            t.dma("sp", kbuf[kb][64:69, :, :], self.I("c_kaug")[2:7, ksl].unsqueeze(1).broadcast_to([5, 4, 1024]),
                                  writes=[r_kbuf[kb]])
                            t.dma("sp", vbuf[kb][:, :, 0:2, :].rearrange("p t h e -> p t (h e)"),
                                  self.d_va[ksl, 2 * gi:2 * gi + 2, :].rearrange("(t p) h e -> p t (h e)", p=128),
                                  writes=[r_vbuf[kb]])
                        for tl in range(8):
                            tt = tg * 8 + tl
                            sb_ = (0, 1, 6, 7)[scnt[0] % 4]
                            scnt[0] += 1
                            diag = (tt == NT - 1)

                            def qk(e, kb=kb, tl=tl, sb_=sb_, diag=diag):
                                tsl = slice(tl * 128, (tl + 1) * 128)
                                for i in range(4):
                                    reg = PS[sb_][:, i * 128:(i + 1) * 128]
                                    if kind == "dsa":
                                        ins = e.matmul(reg, lhsT=kbuf[kb][:, i, tsl], rhs=qb_sb[:, 4 * gi + i, :],
                                                       start=(i == 0), stop=False, skip_group_check=True)
                                        hh = 4 * gi + i
                                    else:
                                        ins = e.matmul(reg, lhsT=kbuf[kb][0:69, i, tsl], rhs=qa_sb[0:69, 4 * gi + i, :],
                                                       start=True, stop=not diag)
                                        hh = 2 * gi + i // 2
                                    if diag:
                                        ins = e.matmul(reg, lhsT=self.identb[:], rhs=dg[:, hh, :], start=False,
                                                       stop=(kind != "dsa"), skip_group_check=(kind == "dsa"))
                                if kind == "dsa":
                                    ins = e.matmul(PS[sb_][:, :], lhsT=kaug_sb[kb][:, tsl],
                                                   rhs=qbaug[:, 4 * gi:4 * gi + 4, :].rearrange("r h n -> r (h n)"),
                                                   start=False, stop=True, skip_group_check=True)
                                return ins
                            rd = [r_kbuf[kb], self.r_const, r_c2] + ([r_kaug[kb], r_qbaug, r_qb] if kind == "dsa" else [r_qa])
                            t.op("pe", qk, reads=rd, writes=[PR[sb_]])
                            pb_ = pcnt[0] % 5
                            pcnt[0] += 1
                            t.op("act", lambda e, sb_=sb_, pb_=pb_: e.activation(
                                out=pbuf[pb_][:].rearrange("p h n -> p (h n)"), in_=PS[sb_][:, :], func=AF.Exp),
                                reads=[PR[sb_]], writes=[r_pbuf[pb_]])
                            if kind == "dsa":
                                t.op("dve", lambda e, pb_=pb_, tt=tt: e.scalar_tensor_tensor(
                                    out=pbuf[pb_][:], in0=pbuf[pb_][:], scalar=1e30,
                                    in1=maskT[:, tt * 128:(tt + 1) * 128].unsqueeze(1).broadcast_to([128, 4, 128]),
                                    op0=ALU.min, op1=ALU.mult), reads=[r_pbuf[pb_], r_sm], writes=[r_pbuf[pb_]])

                            def pv(e, kb=kb, tl=tl, pb_=pb_, tt=tt):
                                for i in range(4):
                                    vh = i if kind == "dsa" else i // 2
                                    ins = e.matmul(accs[i], lhsT=pbuf[pb_][:, i, :], rhs=vbuf[kb][:, tl, vh, 0:129],
                                                   start=(tt == 0 and first_in_bank[i]), stop=(tt == NT - 1),
                                                   skip_group_check=True)
                                return ins
                            pendq.append(lambda pv=pv, kb=kb, pb_=pb_: t.op(
                                "pe", pv, reads=[r_pbuf[pb_], r_vbuf[kb]], writes=RA))
                            if len(pendq) > 3:
                                pendq.pop(0)()
                    while pendq:
                        pendq.pop(0)()
                    s0 = 8 * ab
                    if kind == "dsa":
                        for i in range(4):
                            hh = 4 * gi + i
                            t.op("dve", lambda e, i=i: e.reciprocal(out=sv2[:, s0 + i:s0 + i + 1], in_=accs[i][:, 128:129]),
                                 reads=RA, writes=[r_sv2[ab]])
                            t.op("dve", lambda e, i=i, hh=hh: e.tensor_scalar(
                                out=ysb[1][:, hh * 128:(hh + 1) * 128], in0=accs[i][:, 0:128], scalar1=sv2[:, s0 + i:s0 + i + 1],
                                scalar2=None, op0=ALU.mult), reads=RA + [r_sv2[ab]], writes=[r_ysb[1]])
                    else:
                        for hl in range(2):
                            hh = 2 * gi + hl
                            a0, a1 = accs[2 * hl], accs[2 * hl + 1]
                            c0 = s0 + 4 * hl
                            of_ = oraw[:, hh * 128:(hh + 1) * 128]
                            r_of_ = r_oraw
                            t.op("dve", lambda e, a0=a0, c0=c0: e.reciprocal(out=sv2[:, c0:c0 + 1], in_=a0[:, 128:129]),
                                 reads=RA, writes=[r_sv2[ab]])
                            t.op("dve", lambda e, a1=a1, c0=c0: e.reciprocal(out=sv2[:, c0 + 1:c0 + 2], in_=a1[:, 128:129]),
                                 reads=RA, writes=[r_sv2[ab]])
                            t.op("dve", lambda e, c0=c0: e.tensor_tensor(out=sv2[:, c0 + 1:c0 + 2], in0=sv2[:, c0 + 1:c0 + 2],
                                                                         in1=nlam[:], op=ALU.mult),
                                 reads=[r_sv2[ab], r_c2], writes=[r_sv2[ab]])
                            t.op("dve", lambda e, a0=a0, c0=c0, of_=of_: e.tensor_scalar(
                                out=of_, in0=a0[:, 0:128], scalar1=sv2[:, c0:c0 + 1], scalar2=None, op0=ALU.mult),
                                reads=RA + [r_sv2[ab]], writes=[r_of_])
                            t.op("dve", lambda e, a1=a1, c0=c0, of_=of_: e.scalar_tensor_tensor(
                                out=of_, in0=a1[:, 0:128], scalar=sv2[:, c0 + 1:c0 + 2], in1=of_,
                                op0=ALU.mult, op1=ALU.add), reads=RA + [r_sv2[ab], r_of_], writes=[r_of_])

                nb_per = NBISECT // 4
                for gi in range(4):
                    bisect(nb_per)
                    attn_group("diff", gi, gi % 2)
                bisect(NBISECT - 4 * nb_per)
                for hh in range(8):
                    t.op("act", lambda e, hh=hh: e.activation(out=junk[:], in_=oraw[:, hh * 128:(hh + 1) * 128], func=AF.Square,
                                                              accum_out=ssq[:, hh:hh + 1]),
                         reads=[r_oraw], writes=[r_ssq, r_junk])
                t.op("dve", lambda e: e.tensor_scalar(out=ssq[:, 0:8], in0=ssq[:, 0:8], scalar1=1.0 / 128.0, scalar2=LN_EPS,
                                                      op0=ALU.mult, op1=ALU.add), reads=[r_ssq], writes=[r_ssq])
                t.op("act", lambda e: e.activation(out=ssq[:, 0:8], in_=ssq[:, 0:8], func=AF.Sqrt), reads=[r_ssq], writes=[r_ssq])
                t.op("dve", lambda e: e.reciprocal(out=ssq[:, 8:16], in_=ssq[:, 0:8]), reads=[r_ssq], writes=[r_ssq])
                for hh in range(8):
                    t.op("dve", lambda e, hh=hh: e.scalar_tensor_tensor(
                        out=ysb[0][:, hh * 128:(hh + 1) * 128], in0=oraw[:, hh * 128:(hh + 1) * 128], scalar=ssq[:, 8 + hh:9 + hh],
                        in1=gbc[:], op0=ALU.mult, op1=ALU.mult), reads=[r_oraw, r_ssq, r_c2], writes=[r_ysb[0]])
                t.dma("sp", self.d_ya[q0:q0 + 128, :], ysb[0][:], reads=[r_ysb[0]])
                t.op("dve", lambda e: e.tensor_scalar(out=mask[:, 0:N], in0=score[:, 0:N], scalar1=col(LO), scalar2=None,
                                                      op0=ALU.is_gt), reads=[r_sm, r_sv], writes=[r_mask])
                for kg in range(ngrp):
                    t.op("dve", lambda e, kg=kg: e.scalar_tensor_tensor(
                        out=tmp512[:], in0=iota[:], scalar=float(kg * 512), in1=mask[:, kg * 512:(kg + 1) * 512],
                        op0=ALU.add, op1=ALU.mult), reads=[r_mask, r_c2], writes=[r_tmp])
                    t.op("dve", lambda e, kg=kg: e.reduce_max(out=am[:, kg:kg + 1], in_=tmp512[:], axis=AX.X),
                         reads=[r_tmp], writes=[r_am])
                MP, DD, AF_, BF_ = 8, 9, 10, 11
                t.op("dve", lambda e: e.reduce_max(out=col(MP), in_=am[:, 0:ngrp], axis=AX.X), reads=[r_am], writes=[r_sv])
                t.op("dve", lambda e: e.tensor_scalar(out=col(DD), in0=col(MP), scalar1=pidx1[:, 0:1], scalar2=float(128 * tq),
                                                      op0=ALU.subtract, op1=ALU.subtract), reads=[r_sv, r_c2], writes=[r_sv])
                t.op("act", lambda e: e.activation(out=col(DD), in_=col(DD), func=AF.Abs), reads=[r_sv], writes=[r_sv])
                t.op("dve", lambda e: e.tensor_copy(out=svi[:, 0:1], in_=col(DD)), reads=[r_sv], writes=[r_sv])
                t.op("dve", lambda e: e.tensor_single_scalar(out=svi[:, 1:2], in_=svi[:, 0:1], scalar=7,
                                                             op=ALU.arith_shift_right), reads=[r_sv], writes=[r_sv])
                t.op("dve", lambda e: e.tensor_copy(out=col(AF_), in_=svi[:, 1:2]), reads=[r_sv], writes=[r_sv])
                t.op("dve", lambda e: e.scalar_tensor_tensor(out=col(BF_), in0=col(AF_), scalar=-128.0, in1=col(DD),
                                                             op0=ALU.mult, op1=ALU.add), reads=[r_sv], writes=[r_sv])
                t.op("dve", lambda e: e.tensor_copy(out=ab[:, 0:2], in_=sv[:, AF_:AF_ + 2]), reads=[r_sv], writes=[r_sv])
                t.dma("sp", qbaug[2:7, :, :], self.I("c_qaug")[2:7, j, :, :], writes=[r_qbaug])
                t.op("pe", lambda e: e.matmul(PS[7][0:2, 0:128], lhsT=ab[:, 0:2], rhs=self.identb[:], start=True, stop=True),
                     reads=[r_sv, self.r_const], writes=[PR[7]])
                t.op("dve", lambda e: e.tensor_tensor(out=qbaug[0:2, :, :],
                                                      in0=PS[7][0:2, 0:128].unsqueeze(1).broadcast_to([2, 8, 128]),
                                                      in1=slopetab[:], op=ALU.mult),
                     reads=[PR[7], r_c2], writes=[r_qbaug])
                pbf = PS[6].bitcast(BF16)
                for g4 in range(ngrp):
                    def tr(e, g4=g4):
                        for tl in range(4):
                            tt = g4 * 4 + tl
                            ins = e.transpose(pbf[:, tl * 128:(tl + 1) * 128], mask[:, tt * 128:(tt + 1) * 128], self.identb[:])
                        return ins
                    t.op("pe", tr, reads=[r_mask, self.r_const], writes=[PR[6]])
                    if g4 % 2 == 0:
                        t.op("act", lambda e, g4=g4: e.copy(out=maskT[:, g4 * 512:(g4 + 1) * 512], in_=pbf[:, 0:512]),
                             reads=[PR[6]], writes=[r_sm])
                    else:
                        t.op("dve", lambda e, g4=g4: e.tensor_copy(out=maskT[:, g4 * 512:(g4 + 1) * 512], in_=pbf[:, 0:512]),
                             reads=[PR[6]], writes=[r_sm])
                for gi in range(2):
                    attn_group("dsa", gi, gi % 2)
                t.dma("sp", self.d_yb[q0:q0 + 128, :], ysb[1][:], reads=[r_ysb[1]])
            self.barrier()

    def load_w_bf16(self, es, name, src_ap, r):
        wsb = self.sb(es, name, [128, 8, 1024], BF16)
        v = src_ap.rearrange("(c p) n -> p c n", p=128)
        for a in range(0, 1024, 512):
            self.t.dma("pool", wsb[:, :, a:a + 512], v[:, :, a:a + 512], writes=[r])
        return wsb

    def ln_stats(self, xin, r_x, stats, mv, r_st):
        t = self.t
        for hh in range(2):
            t.op("dve", lambda e, hh=hh: e.bn_stats(out=stats[:, hh, :], in_=xin[:, hh * 512:(hh + 1) * 512]),
                 reads=[r_x], writes=[r_st])
        t.op("dve", lambda e: e.bn_aggr(out=mv[:, 0:2], in_=stats[:].rearrange("p a b -> p (a b)")),
             reads=[r_st], writes=[r_st])
        t.op("dve", lambda e: e.tensor_scalar(out=mv[:, 2:3], in0=mv[:, 1:2], scalar1=LN_EPS, scalar2=None,
                                              op0=ALU.add), reads=[r_st], writes=[r_st])
        t.op("act", lambda e: e.activation(out=mv[:, 2:3], in_=mv[:, 2:3], func=AF.Sqrt),
             reads=[r_st], writes=[r_st])
        t.op("dve", lambda e: e.reciprocal(out=mv[:, 3:4], in_=mv[:, 2:3]), reads=[r_st], writes=[r_st])

    def phase3(self):
        nc, t = self.nc, self.t
        PS, PR = self.ps, self.psr
        nslot = self.nslot
        with ExitStack() as es:
            r_w = self.R()
            wa = self.load_w_bf16(es, "wa", self.I("w_branch_a"), r_w)
            wb = self.load_w_bf16(es, "wb", self.I("w_branch_b"), r_w)
            wo = self.load_w_bf16(es, "wo", self.I("w_out"), r_w)
            g1 = self.sb(es, "g1bc", [128, D], F32)
            b1 = self.sb(es, "b1bc", [128, D], F32)
            r_c = self.R()
            self.ga_bc = self.sb(es, "ga_bc", [128, D], F32)
            t.dma("sp", self.ga_bc[:], self.d_mod[2 * D:3 * D].partition_broadcast(128), reads=[self.r_dmod], writes=[self.r_mod])
            t.dma("sp", g1[:], self.I("ln1_g").partition_broadcast(128), writes=[r_c])
            t.dma("sp", b1[:], self.I("ln1_b").partition_broadcast(128), writes=[r_c])
            yab = [self.sb(es, f"yab{i}", [128, 2, D], BF16) for i in range(2)]
            r_yab = [self.R() for _ in range(2)]
            yT = [self.sb(es, f"yT{i}", [128, 2, 8, 128], BF16) for i in range(2)]
            r_yT = [self.R() for _ in range(2)]
            gate = [self.sb(es, f"gate{i}", [128, 2 * D], F32) for i in range(2)]
            r_gate = [self.R() for _ in range(2)]
            xt = [self.sb(es, f"x3_{i}", [128, D], F32) for i in range(2)]
            r_xt = [self.R() for _ in range(2)]
            m1 = self.sb(es, "m1", [128, D], F32)
            m2 = self.sb(es, "m2", [128, D], F32)
            mg = self.sb(es, "mg", [128, D], BF16)
            mgT = self.sb(es, "mgT", [128, 8, 128], BF16)
            r_m = self.R()
            r_mg = self.R()
            r_mgT = self.R()
            xnew = self.sb(es, "xnew", [128, D], F32)
            r_xn = self.R()
            x1 = [self.sb(es, f"x1_{i}", [128, D], F32) for i in range(2)]
            r_x1 = [self.R() for _ in range(2)]
            stats = self.sb(es, "st3", [128, 2, 6], F32)
            mv = self.sb(es, "mv3", [128, 4], F32)
            r_st = self.R()
            for j in range(nslot):
                b = j % 2
                q0 = j * 128
                tok0 = (8 * j + 7) * 128
                t.dma("sp", yab[b][:, 0, :], self.d_ya[q0:q0 + 128, :], writes=[r_yab[b]])
                t.dma("sp", yab[b][:, 1, :], self.d_yb[q0:q0 + 128, :], writes=[r_yab[b]])
                t.dma("sp", gate[b][:], self.d_gate[q0:q0 + 128, :], writes=[r_gate[b]])
                t.dma("sp", xt[b][:], self.I("x")[tok0:tok0 + 128, :], writes=[r_xt[b]])
                for br in range(2):
                    pbf = PS[br].bitcast(BF16)

                    def tr(e, br=br, b=b, pbf=pbf):
                        for c in range(8):
                            ins = e.transpose(pbf[:, c * 128:(c + 1) * 128], yab[b][:, br, c * 128:(c + 1) * 128], self.identb[:])
                        return ins
                    t.op("pe", tr, reads=[r_yab[b], self.r_const], writes=[PR[br]])
                    t.op("act", lambda e, br=br, b=b, pbf=pbf: e.copy(
                        out=yT[b][:, br, :, :], in_=pbf[:, :].rearrange("p (c n) -> p c n", c=8)),
                        reads=[PR[br]], writes=[r_yT[b]])
                for cg in range(2):
                    csl = slice(cg * 512, (cg + 1) * 512)
                    for br, w_ in ((0, wa), (1, wb)):
                        pb = 2 + br

                        def mm(e, br=br, w_=w_, pb=pb, b=b, csl=csl):
                            for c in range(8):
                                ins = e.matmul(PS[pb][:, :], lhsT=yT[b][:, br, c, :], rhs=w_[:, c, csl],
                                               start=(c == 0), stop=(c == 7))
                            return ins
                        t.op("pe", mm, reads=[r_yT[b], r_w], writes=[PR[pb]])
                    t.op("dve", lambda e, b=b, csl=csl, cg=cg: e.tensor_tensor(
                        out=m1[:, csl], in0=PS[2][:, :], in1=gate[b][:, cg * 512:(cg + 1) * 512], op=ALU.mult),
                        reads=[PR[2], r_gate[b]], writes=[r_m])
                    t.op("dve", lambda e, b=b, csl=csl, cg=cg: e.tensor_tensor(
                        out=m2[:, csl], in0=PS[3][:, :], in1=gate[b][:, D + cg * 512:D + (cg + 1) * 512], op=ALU.mult),
                        reads=[PR[3], r_gate[b]], writes=[r_m])
                    t.op("dve", lambda e, csl=csl: e.tensor_tensor(out=mg[:, csl], in0=m1[:, csl], in1=m2[:, csl], op=ALU.add),
                         reads=[r_m], writes=[r_mg])
                pbf = PS[4].bitcast(BF16)

                def tr2(e, pbf=pbf):
                    for c in range(8):
                        ins = e.transpose(pbf[:, c * 128:(c + 1) * 128], mg[:, c * 128:(c + 1) * 128], self.identb[:])
                    return ins
                t.op("pe", tr2, reads=[r_mg, self.r_const], writes=[PR[4]])
                t.op("act", lambda e, pbf=pbf: e.copy(out=mgT[:], in_=pbf[:, :].rearrange("p (c n) -> p c n", c=8)),
                     reads=[PR[4]], writes=[r_mgT])
                for cg in range(2):
                    csl = slice(cg * 512, (cg + 1) * 512)
                    pb = 5 + cg

                    def mm3(e, pb=pb, csl=csl):
                        for c in range(8):
                            ins = e.matmul(PS[pb][:, :], lhsT=mgT[:, c, :], rhs=wo[:, c, csl], start=(c == 0), stop=(c == 7))
                        return ins
                    t.op("pe", mm3, reads=[r_mgT, r_w], writes=[PR[pb]])
                    t.op("dve", lambda e, pb=pb, csl=csl: e.tensor_tensor(out=m1[:, csl], in0=PS[pb][:, :],
                                                                          in1=self.ga_bc[:, csl], op=ALU.mult),
                         reads=[PR[pb], self.r_mod], writes=[r_m])
                    t.op("dve", lambda e, b=b, csl=csl: e.scalar_tensor_tensor(
                        out=xnew[:, csl], in0=xt[b][:, csl], scalar=ALPHA, in1=m1[:, csl], op0=ALU.mult, op1=ALU.add),
                        reads=[r_xt[b], r_m], writes=[r_xn])
                self.ln_stats(xnew, r_xn, stats, mv, r_st)
                t.op("dve", lambda e, b=b: e.tensor_scalar(out=x1[b][:], in0=xnew[:], scalar1=mv[:, 0:1], scalar2=mv[:, 3:4],
                                                           op0=ALU.subtract, op1=ALU.mult),
                     reads=[r_xn, r_st], writes=[r_x1[b]])
                t.op("pool", lambda e, b=b: e.tensor_tensor(out=x1[b][:], in0=x1[b][:], in1=g1[:], op=ALU.mult),
                     reads=[r_x1[b], r_c], writes=[r_x1[b]])
                t.op("pool", lambda e, b=b: e.tensor_tensor(out=x1[b][:], in0=x1[b][:], in1=b1[:], op=ALU.add),
                     reads=[r_x1[b], r_c], writes=[r_x1[b]])
                t.dma("sp", self.d_x1[q0:q0 + 128, :], x1[b][:], reads=[r_x1[b]])
            self.barrier()

    def phase4(self):
        nc, t = self.nc, self.t
        PS, PR = self.ps, self.psr
        NQ = self.NQ
        HT = min(1024, NQ)
        nhalf = NQ // HT
        TG = min(512, HT)
        ntg = HT // TG
        ntt = HT // 128
        with ExitStack() as es:
            scf = self.sb(es, "scf_bc", [128, D], F32)
            shf = self.sb(es, "shf_bc", [128, D], F32)
            g2 = scf
            b2l = shf
            wr = self.sb(es, "wr", [128, 8, NEXP], F32)
            brr = self.sb(es, "brr", [1, NEXP], F32)
            onesf = self.sb(es, "onesf4", [1, 128], F32)
            b2w = self.sb(es, "b2w", [NEXP, D], F32)
            b1raw = self.sb(es, "b1raw", [NEXP, 2 * DFF], F32)
            b1g = self.sb(es, "b1g", [128, 8, NEXP], F32)
            b1l = self.sb(es, "b1l", [128, 8, NEXP], F32)
            r_c = self.R()
            self.gf_bc = self.sb(es, "gf_bc", [128, D], F32)
            t.dma("sp", self.gf_bc[:], self.d_mod[5 * D:6 * D].partition_broadcast(128), reads=[self.r_dmod], writes=[self.r_mod])
            r_md = self.R()
            t.dma("sp", wr[:], self.I("w_router").rearrange("(c p) n -> p c n", p=128), writes=[r_c])
            t.dma("sp", brr[:], self.I("b_router").rearrange("(o n) -> o n", o=1), writes=[r_c])
            t.dma("sp", b2w[:], self.I("b_e2"), writes=[r_c])
            t.dma("sp", b1raw[:], self.I("b_e1"), writes=[r_c])
            t.op("dve", lambda e: e.memset(onesf[:], 1.0), writes=[r_c])
            b1v = b1raw[:].rearrange("e (p f two) -> e p f two", p=8, two=2)
            for p in range(8):
                for two, dst in ((0, b1g), (1, b1l)):
                    t.op("pe", lambda e, p=p, two=two: e.transpose(PS[7][:, 0:NEXP], b1v[:, p, :, two], self.identf[0:NEXP, 0:NEXP]),
                         reads=[r_c, self.r_const], writes=[PR[7]])
                    t.op("dve", lambda e, p=p, dst=dst, two=two: e.tensor_scalar(
                        out=dst[:, p, :], in0=PS[7][:, 0:NEXP], scalar1=float(two), scalar2=None, op0=ALU.add),
                        reads=[PR[7]], writes=[r_c])
            vT = self.sb(es, "vT", [128, 8, HT], BF16)
            r_vT = self.R()
            yacc = self.sb(es, "yacc", [128, ntt, D], F32)
            r_y = self.R()
            gate = self.sb(es, "gate4", [128, ntt, NEXP], F32)
            r_g = self.R()
            aT = [self.sb(es, f"aT{i}", [128, 8, HT], BF16) for i in range(2)]
            r_aT = [self.R() for _ in range(2)]
            w1p = [self.sb(es, f"w1p{i}", [128, 8, 256], BF16) for i in range(5)]
            r_w1 = [self.R() for _ in range(5)]
            w2e = [self.sb(es, f"w2e{i}", [128, 8, D], BF16) for i in range(2)]
            r_w2 = [self.R() for _ in range(2)]
            glu = [self.sb(es, f"glu{i}", [128, TG], F32) for i in range(2)]
            sig = [self.sb(es, f"sig{i}", [128, TG], F32) for i in range(2)]
            lin = [self.sb(es, f"lin{i}", [128, TG], F32) for i in range(2)]
            r_elg = [self.R() for _ in range(3)]
            r_ell = [self.R() for _ in range(3)]
            r_els = [self.R() for _ in range(3)]
            xt = [self.sb(es, f"x4_{i}", [128, D], F32) for i in range(2)]
            r_xt = [self.R() for _ in range(2)]
            vf = self.sb(es, "vf", [128, D], F32)
            vb16 = self.sb(es, "vb16", [128, D], BF16)
            vTf = self.sb(es, "vTf", [128, 8, 128], F32)
            r_v = self.R()
            stats = self.sb(es, "st4", [128, 2, 6], F32)
            mv = self.sb(es, "mv4", [128, 4], F32)
            r_st = self.R()
            rt = self.sb(es, "rt", [128, 4, NEXP], F32)
            m8 = self.sb(es, "m8", [128, 16], F32)
            gT = self.sb(es, "gT", [NEXP, 128], F32)
            r_rt = self.R()
            w1cnt = [0]
            wfc = [0]
            elc = [0]
            for hf in range(nhalf):
                h0 = hf * HT
                t.dma("sp", scf[:], self.d_mod[4 * D:5 * D].partition_broadcast(128), writes=[r_md])
                t.dma("sp", shf[:], self.d_mod[3 * D:4 * D].partition_broadcast(128), writes=[r_md])
                t.op("dve", lambda e: e.tensor_scalar(out=scf[:], in0=scf[:], scalar1=1.0, scalar2=None, op0=ALU.add),
                     reads=[r_md], writes=[r_md])
                for tt in range(ntt):
                    b = tt % 2
                    q0 = h0 + tt * 128
                    t.dma("sp", xt[b][:], self.d_x1[q0:q0 + 128, :], writes=[r_xt[b]])
                    self.ln_stats(xt[b], r_xt[b], stats, mv, r_st)
                    t.op("dve", lambda e, b=b: e.tensor_scalar(out=vf[:], in0=xt[b][:], scalar1=mv[:, 0:1], scalar2=mv[:, 3:4],
                                                               op0=ALU.subtract, op1=ALU.mult),
                         reads=[r_xt[b], r_st], writes=[r_v])
                    t.op("pool", lambda e: e.tensor_tensor(out=vf[:], in0=vf[:], in1=scf[:], op=ALU.mult),
                         reads=[r_v, r_md], writes=[r_v])
                    t.op("pool", lambda e: e.tensor_tensor(out=vf[:], in0=vf[:], in1=shf[:], op=ALU.add),
                         reads=[r_v, r_md], writes=[r_v])
                    t.op("dve", lambda e: e.tensor_copy(out=vb16[:], in_=vf[:]), reads=[r_v], writes=[r_v])
                    pbf = PS[0].bitcast(BF16)

                    def tr(e, pbf=pbf):
                        for c in range(8):
                            ins = e.transpose(pbf[:, c * 128:(c + 1) * 128], vb16[:, c * 128:(c + 1) * 128], self.identb[:])
                        return ins
                    t.op("pe", tr, reads=[r_v, self.r_const], writes=[PR[0]])
                    t.op("act", lambda e, tt=tt, pbf=pbf: e.copy(out=vT[:, :, tt * 128:(tt + 1) * 128],
                                                                 in_=pbf[:, :].rearrange("p (c n) -> p c n", c=8)),
                         reads=[PR[0]], writes=[r_vT])
                    for half2 in range(2):
                        def trf(e, half2=half2):
                            for c4 in range(4):
                                c = half2 * 4 + c4
                                ins = e.transpose(PS[1 + half2][:, c4 * 128:(c4 + 1) * 128], vf[:, c * 128:(c + 1) * 128], self.identf[:])
                            return ins
                        t.op("pe", trf, reads=[r_v, self.r_const], writes=[PR[1 + half2]])
                        t.op("dve", lambda e, half2=half2: e.tensor_copy(
                            out=vTf[:, half2 * 4:half2 * 4 + 4, :], in_=PS[1 + half2][:, :].rearrange("p (c n) -> p c n", c=4)),
                            reads=[PR[1 + half2]], writes=[r_v])

                    def mmr(e):
                        for c in range(8):
                            e.matmul(PS[3][:, 0:NEXP], lhsT=vTf[:, c, :], rhs=wr[:, c, :], start=(c == 0), stop=False)
                        return e.matmul(PS[3][:, 0:NEXP], lhsT=onesf[0:1, :], rhs=brr[0:1, :], start=False, stop=True)
                    t.op("pe", mmr, reads=[r_v, r_c], writes=[PR[3]])
                    LG, SEL, EX = 0, 1, 2
                    t.op("dve", lambda e: e.tensor_copy(out=rt[:, LG, :], in_=PS[3][:, 0:NEXP]), reads=[PR[3]], writes=[r_rt])
                    t.op("dve", lambda e: e.max(out=m8[:, 0:8], in_=rt[:, LG, :]), reads=[r_rt], writes=[r_rt])
                    t.op("dve", lambda e: e.tensor_scalar(out=rt[:, SEL, :], in0=rt[:, LG, :], scalar1=m8[:, 3:4], scalar2=None,
                                                          op0=ALU.is_ge), reads=[r_rt], writes=[r_rt])
                    t.op("dve", lambda e: e.tensor_scalar(out=m8[:, 8:9], in0=m8[:, 0:1], scalar1=-1.0, scalar2=None,
                                                          op0=ALU.mult), reads=[r_rt], writes=[r_rt])
                    t.op("act", lambda e: e.activation(out=rt[:, EX, :], in_=rt[:, LG, :], func=AF.Exp, bias=m8[:, 8:9], scale=1.0),
                         reads=[r_rt], writes=[r_rt])
                    t.op("dve", lambda e: e.tensor_tensor(out=rt[:, EX, :], in0=rt[:, EX, :], in1=rt[:, SEL, :], op=ALU.mult),
                         reads=[r_rt], writes=[r_rt])
                    t.op("dve", lambda e: e.reduce_sum(out=m8[:, 9:10], in_=rt[:, EX, :], axis=AX.X), reads=[r_rt], writes=[r_rt])
                    t.op("dve", lambda e: e.reciprocal(out=m8[:, 10:11], in_=m8[:, 9:10]), reads=[r_rt], writes=[r_rt])
                    t.op("dve", lambda e, tt=tt: e.tensor_scalar(out=gate[:, tt, :], in0=rt[:, EX, :], scalar1=m8[:, 10:11],
                                                                 scalar2=None, op0=ALU.mult), reads=[r_rt], writes=[r_g])
                    t.op("pe", lambda e, tt=tt: e.transpose(PS[3][0:NEXP, 128:256], gate[:, tt, :], self.identf[:]),
                         reads=[r_g, self.r_const], writes=[PR[3]])
                    t.op("dve", lambda e: e.tensor_copy(out=gT[:], in_=PS[3][0:NEXP, 128:256]), reads=[PR[3]], writes=[r_rt])
                    for cg in range(2):
                        t.op("pe", lambda e, cg=cg: e.matmul(PS[4 + cg][:, :], lhsT=gT[:, :], rhs=b2w[:, cg * 512:(cg + 1) * 512],
                                                            start=True, stop=True), reads=[r_rt, r_c], writes=[PR[4 + cg]])
                        t.op("dve", lambda e, cg=cg, tt=tt: e.tensor_copy(out=yacc[:, tt, cg * 512:(cg + 1) * 512], in_=PS[4 + cg][:, :]),
                             reads=[PR[4 + cg]], writes=[r_y])

                def stageA(e_, mid=None):
                    ab_ = e_ % 2
                    for p in range(8):
                        if p == 6 and mid is not None:
                            mid()
                        wb_ = w1cnt[0] % 5
                        w1cnt[0] += 1
                        t.dma("pool", w1p[wb_][:], self.I("w_e1")[e_, :, p * 256:(p + 1) * 256].rearrange("(c q) n -> q c n", q=128),
                              writes=[r_w1[wb_]])
                        for tg in range(ntg):
                            tsl = slice(tg * TG, (tg + 1) * TG)
                            pg, pl = (0, 1) if (p * ntg + tg) % 2 == 0 else (2, 3)

                            def mm(e, wb_=wb_, tsl=tsl, pg=pg, pl=pl):
                                for two, pb in ((0, pg), (1, pl)):
                                    for c in range(8):
                                        ins = e.matmul(PS[pb][:, 0:TG], lhsT=w1p[wb_][:, c, two::2], rhs=vT[:, c, tsl],
                                                       start=(c == 0), stop=(c == 7))
                                return ins
                            t.op("pe", mm, reads=[r_w1[wb_], r_vT], writes=[PR[pg], PR[pl]])
                            k = elc[0] % 2
                            elc[0] += 1
                            t.op("dve", lambda e, k=k, pg=pg, p=p, e_=e_: e.tensor_scalar(
                                out=glu[k][:], in0=PS[pg][:, 0:TG], scalar1=b1g[:, p, e_:e_ + 1], scalar2=SWIGLU_LIMIT,
                                op0=ALU.add, op1=ALU.min), reads=[PR[pg], r_c], writes=[r_elg[k]])
                            t.op("dve", lambda e, k=k, pl=pl, p=p, e_=e_: e.tensor_scalar(
                                out=lin[k][:], in0=PS[pl][:, 0:TG], scalar1=b1l[:, p, e_:e_ + 1], scalar2=1.0 - SWIGLU_LIMIT,
                                op0=ALU.add, op1=ALU.max), reads=[PR[pl], r_c], writes=[r_ell[k]])
                            t.op("act", lambda e, k=k: e.activation(out=sig[k][:], in_=glu[k][:], func=AF.Sigmoid, scale=SWIGLU_ALPHA),
                                 reads=[r_elg[k]], writes=[r_els[k]])
                            t.op("dve", lambda e, k=k: e.scalar_tensor_tensor(
                                out=lin[k][:], in0=lin[k][:], scalar=1.0 + SWIGLU_LIMIT, in1=glu[k][:], op0=ALU.min, op1=ALU.mult),
                                reads=[r_ell[k], r_elg[k]], writes=[r_ell[k]])
                            t.op("dve", lambda e, k=k, ab_=ab_, p=p, tsl=tsl: e.tensor_tensor(
                                out=aT[ab_][:, p, tsl], in0=lin[k][:], in1=sig[k][:], op=ALU.mult),
                                reads=[r_ell[k], r_els[k]], writes=[r_aT[ab_]])

                def stageB(e_):
                    ab_ = e_ % 2
                    for tt in range(ntt):
                        for cg in range(2):
                            pb = 4 + (tt * 2 + cg) % 3

                            def mm(e, tt=tt, cg=cg, pb=pb):
                                for p in range(8):
                                    ins = e.matmul(PS[pb][:, :], lhsT=aT[ab_][:, p, tt * 128:(tt + 1) * 128],
                                                   rhs=w2e[ab_][:, p, cg * 512:(cg + 1) * 512], start=(p == 0), stop=(p == 7))
                                return ins
                            t.op("pe", mm, reads=[r_aT[ab_], r_w2[ab_]], writes=[PR[pb]])
                            t.op("dve", lambda e, tt=tt, cg=cg, pb=pb: e.scalar_tensor_tensor(
                                out=yacc[:, tt, cg * 512:(cg + 1) * 512], in0=PS[pb][:, :], scalar=gate[:, tt, e_:e_ + 1],
                                in1=yacc[:, tt, cg * 512:(cg + 1) * 512], op0=ALU.mult, op1=ALU.add),
                                reads=[PR[pb], r_g, r_y], writes=[r_y])

                def loadw2(e_):
                    ab_ = e_ % 2
                    v = self.I("w_e2")[e_].rearrange("(c q) n -> q c n", q=128)
                    for a in range(0, 1024, 512):
                        t.dma("pool", w2e[ab_][:, :, a:a + 512], v[:, :, a:a + 512], writes=[r_w2[ab_]])
                loadw2(0)
                stageA(0)
                for e_ in range(NEXP):
                    if e_ + 1 < NEXP:
                        stageA(e_ + 1, mid=lambda e_=e_: loadw2(e_ + 1))
                    stageB(e_)
                t.dma("sp", g2[:], self.I("ln2_g").partition_broadcast(128), writes=[r_md])
                t.dma("sp", b2l[:], self.I("ln2_b").partition_broadcast(128), writes=[r_md])
                for tt in range(ntt):
                    b = tt % 2
                    q0 = h0 + tt * 128
                    t.dma("sp", xt[b][:], self.d_x1[q0:q0 + 128, :], writes=[r_xt[b]])
                    t.op("pool", lambda e, tt=tt: e.tensor_tensor(out=yacc[:, tt, :], in0=yacc[:, tt, :], in1=self.gf_bc[:], op=ALU.mult),
                         reads=[r_y, self.r_mod], writes=[r_y])
                    t.op("dve", lambda e, b=b, tt=tt: e.scalar_tensor_tensor(out=vf[:], in0=xt[b][:], scalar=ALPHA, in1=yacc[:, tt, :],
                                                                            op0=ALU.mult, op1=ALU.add),
                         reads=[r_xt[b], r_y], writes=[r_v])
                    self.ln_stats(vf, r_v, stats, mv, r_st)
                    t.op("dve", lambda e, b=b: e.tensor_scalar(out=xt[b][:], in0=vf[:], scalar1=mv[:, 0:1], scalar2=mv[:, 3:4],
                                                               op0=ALU.subtract, op1=ALU.mult),
                         reads=[r_v, r_st], writes=[r_xt[b]])
                    t.op("pool", lambda e, b=b: e.tensor_tensor(out=xt[b][:], in0=xt[b][:], in1=g2[:], op=ALU.mult),
                         reads=[r_xt[b], r_md], writes=[r_xt[b]])
                    t.op("pool", lambda e, b=b: e.tensor_tensor(out=xt[b][:], in0=xt[b][:], in1=b2l[:], op=ALU.add),
                         reads=[r_xt[b], r_md], writes=[r_xt[b]])
                    t.dma("sp", self.out[q0:q0 + 128, :], xt[b][:], reads=[r_xt[b]], writes=[self.r_out])
            self.barrier()


def make_consts(nslot, c):
    ntile = 8 * nslot
    S = 128 * ntile
    slopes = alibi_slopes(8)
    k = np.arange(S)
    tt = k // 128
    pk = k % 128
    ndummy = 7 - c
    kaug = np.zeros((7, S), np.float32)
    kaug[0] = 1.0
    kaug[1] = 1.0
    kaug[2] = 128.0 * tt
    kaug[3] = 1.0
    kaug[4] = 1.0
    kaug[5] = pk
    kaug[6] = (tt < ndummy).astype(np.float32)
    qaug = np.zeros((7, nslot, 8, 128), np.float32)
    ql = np.arange(128)
    for j in range(nslot):
        tq = 8 * j + 7
        for h in range(8):
            s = slopes[h]
            qaug[2, j, h] = s
            qaug[3, j, h] = -s * 128.0 * tq
            qaug[4, j, h] = -s * ql
            qaug[5, j, h] = s
            qaug[6, j, h] = NEG
    kk = np.arange(128)[:, None]
    qq = np.arange(128)[None, :]
    cend = (qq // 64 + 1) * 64
    dg = np.zeros((128, 8, 128), np.float32)
    for h in range(8):
        s = slopes[h]
        m = np.where(kk > qq, -2.0 * s * (kk - qq), 0.0)
        m = np.where(kk >= cend, NEG, m)
        dg[:, h, :] = m
    dmask = np.where(kk.T >= 0, 0.0, 0.0) * 0.0
    qq2 = np.arange(128)[:, None]
    kk2 = np.arange(128)[None, :]
    dmask = np.where(kk2 < (qq2 // 64 + 1) * 64, 0.0, -1e9).astype(np.float32)
    dummy = np.zeros((128, 8), np.float32)
    dummy[:, :ndummy] = -1e9
    iota = np.broadcast_to(np.arange(1, 513, dtype=np.float32)[None, :], (128, 512)).copy()
    slopetab = np.zeros((2, 8, 128), np.float32)
    for h in range(8):
        slopetab[0, h] = slopes[h] * 128.0
        slopetab[1, h] = slopes[h]
    return {
        "c_identb": np.eye(128, dtype=np.float32).astype(NPBF),
        "c_identf": np.eye(128, dtype=np.float32),
        "c_kaug": kaug.astype(NPBF),
        "c_qaug": qaug.astype(NPBF),
        "c_dg": dg.astype(NPBF),
        "c_dmask": dmask,
        "c_dummy": dummy,
        "c_iota": iota,
        "c_slopetab": slopetab,
        "c_pidx1": np.arange(1, 129, dtype=np.float32).reshape(128, 1),
    }


def make_in_maps(inputs, nslot, used=None):
    S = 128 * 8 * nslot
    f = lambda a: np.ascontiguousarray(np.asarray(a, dtype=np.float32))
    x = f(inputs["x"])[0]
    assert x.shape[0] == S
    shared = {
        "c": f(inputs["c"])[0],
        "w_ada": f(inputs["w_ada"])[0],
        "b_ada": f(inputs["b_ada"])[0],
        "w_in": f(inputs["w_in"])[0],
        "lamv": np.stack([f(inputs[k])[0] for k in ("lam_q1", "lam_k1", "lam_q2", "lam_k2")]),
        "diff_norm_g": f(inputs["diff_norm_g"])[0],
        "w_branch_a": f(inputs["w_branch_a"])[0],
        "w_branch_b": f(inputs["w_branch_b"])[0],
        "w_out": f(inputs["w_out"])[0],
        "ln1_g": f(inputs["ln1_g"])[0],
        "ln1_b": f(inputs["ln1_b"])[0],
        "w_router": f(inputs["w_router"])[0],
        "b_router": f(inputs["b_router"])[0],
        "w_e1": f(inputs["w_e1"])[0],
        "b_e1": f(inputs["b_e1"])[0],
        "w_e2": f(inputs["w_e2"])[0],
        "b_e2": f(inputs["b_e2"])[0],
        "ln2_g": f(inputs["ln2_g"])[0],
        "ln2_b": f(inputs["ln2_b"])[0],
    }
    maps = []
    for c in range(NCORE):
        m = dict(shared)
        m["x"] = np.ascontiguousarray(np.roll(x, 128 * (7 - c), axis=0))
        m.update(make_consts(nslot, c))
        maps.append({k: v for k, v in m.items() if used is None or k in used})
    return maps


_CACHE = {}


def run(inputs, nslot, debug=False, phases=99, trace=False):
    key = (nslot, debug, phases)
    mk = MK(nslot, debug=debug, phases=phases)
    nc = mk.build()
    in_maps = make_in_maps(inputs, nslot, used=set(mk.in_aps.keys()))
    res = run_bass_kernel_spmd(nc, in_maps, core_ids=list(range(NCORE)), trace=trace)
    return res


def kernel(**inputs):
    nslot = 16
    res = run(inputs, nslot)
    S = 128 * 8 * nslot
    out = np.zeros((1, S, D), np.float32)
    for c in range(NCORE):
        o = np.asarray(res.results[c]["out"], dtype=np.float32)
        for j in range(nslot):
            rt = 8 * j + c
            out[0, rt * 128:(rt + 1) * 128, :] = o[j * 128:(j + 1) * 128, :]
    return out
```

```python
import os
import numpy as np
import ml_dtypes
from contextlib import ExitStack
import concourse.bass as bass
import concourse.mybir as mybir
from concourse.bass_utils import run_bass_kernel_spmd

F32 = mybir.dt.float32
BF16 = mybir.dt.bfloat16
I32 = mybir.dt.int32
AF = mybir.ActivationFunctionType
ALU = mybir.AluOpType
AX = mybir.AxisListType
NPBF = ml_dtypes.bfloat16

D = 1024
NCORE = 8
NEXP = 32
DFF = 1024
TOPK = 256
LN_EPS = 1e-5
ALPHA = 2.0 ** 0.25
LAM_INIT = 0.2
NEG = -30000.0
NBISECT = 24
SWIGLU_ALPHA = 1.702
SWIGLU_LIMIT = 7.0
C_QA, C_KA, C_VA, C_QB, C_KB, C_VB, C_QI, C_KI, C_WI, C_GA, C_GB = (
    0, 1024, 2048, 3072, 4096, 5120, 6144, 7168, 7232, 7248, 8272)
PROJ_W = 9296


class Res:
    __slots__ = ("w", "r", "name")

    def __init__(self, name=""):
        self.w = None
        self.r = {}
        self.name = name


class Eng:
    def __init__(self, name, eng, sem):
        self.name = name
        self.eng = eng
        self.sem = sem
        self.cnt = 0
        self.seen = {}


class Trk:
    def __init__(self, nc, es):
        self.nc = nc
        mk = lambda n: es.enter_context(nc.semaphore(n))
        self.E = {n: Eng(n, e, mk("s_" + n)) for n, e in [
            ("pe", nc.tensor), ("act", nc.scalar), ("dve", nc.vector),
            ("pool", nc.gpsimd), ("sp", nc.sync)]}
        self.dsems = {q: [[mk(f"d_{q}{i}"), 0] for i in range(n)]
                      for q, n in [("sp", 16), ("pool", 10), ("act", 4)]}
        self.dnext = {q: 0 for q in self.dsems}
        self.nwait = 0

    def _waits(self, E, reads, writes):
        need = {}

        def add(tok, raw):
            if tok is None:
                return
            sem, val = tok
            if sem is E.sem and E.name == "pe":
                return
            k = id(sem)
            if k not in need or need[k][1] < val:
                need[k] = (sem, val)
        for r in reads:
            add(r.w, True)
        for w in writes:
            add(w.w, False)
            for tok in w.r.values():
                add(tok, False)
        for k, (sem, val) in need.items():
            if E.seen.get(k, 0) < val:
                E.eng.wait_ge(sem, val)
                E.seen[k] = val
                self.nwait += 1

    @staticmethod
    def _mark(tok, reads, writes):
        k = id(tok[0])
        for r in reads:
            r.r[k] = tok
        for w in writes:
            w.w = tok
            w.r = {}

    def op(self, en, fn, reads=(), writes=()):
        E = self.E[en]
        self._waits(E, reads, writes)
        ins = fn(E.eng)
        E.cnt += 1
        ins.then_inc(E.sem, 1)
        self._mark((E.sem, E.cnt), reads, writes)

    def dma(self, q, out, in_, reads=(), writes=(), **kw):
        E = self.E[q]
        self._waits(E, reads, writes)
        slots = self.dsems[q]
        i = self.dnext[q]
        self.dnext[q] = (i + 1) % len(slots)
        sem, val = slots[i]
        k = id(sem)
        if val > 0 and E.seen.get(k, 0) < val:
            E.eng.wait_ge(sem, val)
            E.seen[k] = val
        ins = E.eng.dma_start(out=out, in_=in_, **kw)
        val += 16
        slots[i][1] = val
        ins.then_inc(sem, 16)
        self._mark((sem, val), reads, writes)

    def barrier(self, all_res):
        toks = {}
        for r in all_res:
            for tok in [r.w] + list(r.r.values()):
                if tok is None:
                    continue
                k = id(tok[0])
                if k not in toks or toks[k][1] < tok[1]:
                    toks[k] = tok
        for E in self.E.values():
            for k, (sem, val) in toks.items():
                if sem is E.sem:
                    continue
                if E.seen.get(k, 0) < val:
                    E.eng.wait_ge(sem, val)
                    E.seen[k] = val


def alibi_slopes(n=8):
    return [2.0 ** (-8.0 * (h + 1) / n) for h in range(n)]


class MK:
    def __init__(self, nslot, debug=False, phases=99):
        self.nslot = nslot
        self.ntile = 8 * nslot
        self.S = 128 * self.ntile
        self.NQ = 128 * nslot
        self.debug = debug
        self.phases = phases
        self.nc = bass.Bass("TRN2", target_bir_lowering=False)
        self.res_all = []

    def R(self, name=""):
        r = Res(name)
        self.res_all.append(r)
        return r

    def I(self, name):
        if name not in self.in_aps:
            shape, dt = self.in_specs[name]
            self.in_aps[name] = self.nc.dram_tensor(name, list(shape), dt, kind="ExternalInput").ap()
        return self.in_aps[name]

    def dscr(self, name, shape, dt):
        kind = "ExternalOutput" if self.debug else "Internal"
        t = self.nc.dram_tensor(name, list(shape), dt, kind=kind).ap()
        return t

    def sb(self, es, name, shape, dt):
        return es.enter_context(self.nc.sbuf_tensor(name, list(shape), dt))

    def barrier(self):
        self.t.barrier(self.res_all)

    def build(self):
        nc = self.nc
        S, NQ, nslot, ntile = self.S, self.NQ, self.nslot, self.ntile
        self.in_specs = {
            "x": ([S, D], F32), "c": ([D], F32), "w_ada": ([D, 6 * D], F32), "b_ada": ([6 * D], F32),
            "w_in": ([D, PROJ_W], F32), "lamv": ([4, 64], F32), "diff_norm_g": ([128], F32),
            "w_branch_a": ([D, D], F32), "w_branch_b": ([D, D], F32), "w_out": ([D, D], F32),
            "ln1_g": ([D], F32), "ln1_b": ([D], F32), "w_router": ([D, NEXP], F32), "b_router": ([NEXP], F32),
            "w_e1": ([NEXP, D, 2 * DFF], F32), "b_e1": ([NEXP, 2 * DFF], F32),
            "w_e2": ([NEXP, DFF, D], F32), "b_e2": ([NEXP, D], F32), "ln2_g": ([D], F32), "ln2_b": ([D], F32),
            "c_identb": ([128, 128], BF16), "c_identf": ([128, 128], F32), "c_kaug": ([7, S], BF16),
            "c_qaug": ([7, nslot, 8, 128], BF16), "c_dg": ([128, 8, 128], BF16), "c_dmask": ([128, 128], F32),
            "c_dummy": ([128, 8], F32), "c_iota": ([128, 512], F32), "c_slopetab": ([2, 8, 128], F32),
            "c_pidx1": ([128, 1], F32),
        }
        self.in_aps = {}
        self.out = nc.dram_tensor("out", [NQ, D], F32, kind="ExternalOutput").ap()
        self.d_mod = self.dscr("d_mod", [6 * D], F32)
        self.d_kat = self.dscr("d_kat", [16, 64, S], BF16)
        self.d_va = self.dscr("d_va", [S, 8, 132], BF16)
        self.d_kbt = self.dscr("d_kbt", [8, 128, S], BF16)
        self.d_vb = self.dscr("d_vb", [S, 8, 132], BF16)
        self.d_kit = self.dscr("d_kit", [64, S], BF16)
        self.d_qat = self.dscr("d_qat", [16, 64, NQ], BF16)
        self.d_qbt = self.dscr("d_qbt", [8, 128, NQ], BF16)
        self.d_qit = self.dscr("d_qit", [16, 64, NQ], BF16)
        self.d_sgn = self.dscr("d_sgn", [NQ, 16], F32)
        self.d_gate = self.dscr("d_gate", [NQ, 2 * D], F32)
        self.d_ya = self.dscr("d_ya", [NQ, D], BF16)
        self.d_yb = self.dscr("d_yb", [NQ, D], BF16)
        self.d_x1 = self.dscr("d_x1", [NQ, D], F32)

        with ExitStack() as es:
            self.t = Trk(nc, es)
            self.ps = [es.enter_context(nc.psum_tensor(f"ps{i}", [128, 512], F32)) for i in range(8)]
            self.psr = [self.R(f"ps{i}") for i in range(8)]
            self.identb = self.sb(es, "identb", [128, 128], BF16)
            self.identf = self.sb(es, "identf", [128, 128], F32)
            self.r_const = self.R("const")
            self.t.dma("sp", self.identb[:], self.I("c_identb"), writes=[self.r_const])
            self.t.dma("sp", self.identf[:], self.I("c_identf"), writes=[self.r_const])
            self.modT = self.sb(es, "modT", [128, 48], F32)
            self.r_mod = self.R("mod")
            self.r_out = self.R("out")
            self.phase0()
            self.barrier()
            if self.phases >= 1:
                self.phase1a()
                self.barrier()
                self.phase1b()
                self.barrier()
            if self.phases >= 2:
                self.phase2()
                self.barrier()
            if self.phases >= 3:
                self.phase3()
                self.barrier()
            if self.phases >= 4:
                self.phase4()
            self.final_wait()
        return nc

    def final_wait(self):
        self.barrier()

    def phase0(self):
        nc, t = self.nc, self.t
        with ExitStack() as es:
            cT = self.sb(es, "cT", [128, 8], F32)
            cact = self.sb(es, "cact", [128, 8], F32)
            wbuf = [self.sb(es, f"wada{i}", [128, 8, 512], F32) for i in range(2)]
            wr = [self.R() for _ in range(2)]
            brow = self.sb(es, "brow", [1, 6 * D], F32)
            mrow = self.sb(es, "mrow", [1, 6 * D], F32)
            r_c, r_b, r_m = self.R(), self.R(), self.R()
            t.dma("sp", cT[:], self.I("c").rearrange("(c p) -> p c", p=128), writes=[r_c],
                  allow_slow_non_contiguous=True)
            t.dma("sp", brow[:], self.I("b_ada").rearrange("(o n) -> o n", o=1), writes=[r_b])
            t.op("act", lambda e: e.activation(out=cact[:], in_=cT[:], func=AF.Silu),
                 reads=[r_c], writes=[r_c])
            wv = self.I("w_ada").rearrange("(c p) n -> p c n", p=128)
            for g in range(12):
                b = g % 2
                t.dma("sp", wbuf[b][:], wv[:, :, g * 512:(g + 1) * 512], writes=[wr[b]])
                pb = g % 2

                def mm(e, b=b, pb=pb):
                    for c in range(8):
                        ins = e.matmul(self.ps[pb][0:1, :], lhsT=cact[:, c:c + 1], rhs=wbuf[b][:, c, :],
                                       start=(c == 0), stop=(c == 7))
                    return ins
                t.op("pe", mm, reads=[r_c, wr[b]], writes=[self.psr[pb]])
                t.op("dve", lambda e, g=g, pb=pb: e.tensor_tensor(
                    out=mrow[:, g * 512:(g + 1) * 512], in0=self.ps[pb][0:1, :],
                    in1=brow[:, g * 512:(g + 1) * 512], op=ALU.add),
                    reads=[self.psr[pb], r_b], writes=[r_m])
            r_d = self.R()
            t.dma("sp", self.d_mod.rearrange("(o n) -> o n", o=1), mrow[:], reads=[r_m], writes=[r_d])
            t.dma("sp", self.modT[:], self.d_mod.rearrange("(m p) -> p m", p=128), reads=[r_d],
                  writes=[self.r_mod], allow_slow_non_contiguous=True)
            self.r_dmod = r_d
            self.barrier()

    def ln_pre(self, xin_ap, xt, r_xt, xn, r_xn, stats, mv, r_st, q="sp"):
        t = self.t
        t.dma(q, xt[:], xin_ap, writes=[r_xt])
        for hh in range(2):
            t.op("dve", lambda e, hh=hh: e.bn_stats(out=stats[:, hh, :], in_=xt[:, hh * 512:(hh + 1) * 512]),
                 reads=[r_xt], writes=[r_st])
        t.op("dve", lambda e: e.bn_aggr(out=mv[:, 0:2], in_=stats[:].rearrange("p a b -> p (a b)")),
             reads=[r_st], writes=[r_st])
        t.op("dve", lambda e: e.tensor_scalar(out=mv[:, 2:3], in0=mv[:, 1:2], scalar1=LN_EPS, scalar2=None,
                                              op0=ALU.add), reads=[r_st], writes=[r_st])
        t.op("act", lambda e: e.activation(out=mv[:, 2:3], in_=mv[:, 2:3], func=AF.Sqrt),
             reads=[r_st], writes=[r_st])
        t.op("dve", lambda e: e.reciprocal(out=mv[:, 3:4], in_=mv[:, 2:3]), reads=[r_st], writes=[r_st])
        t.op("dve", lambda e: e.tensor_scalar(out=xn[:], in0=xt[:], scalar1=mv[:, 0:1], scalar2=mv[:, 3:4],
                                              op0=ALU.subtract, op1=ALU.mult),
             reads=[r_xt, r_st], writes=[r_xn])

    def ln_post(self, xn, r_xn, out_xnT, r_out, pbank):
        t = self.t
        pbf = self.ps[pbank].bitcast(BF16)

        def tr(e):
            for c in range(8):
                ins = e.transpose(pbf[:, c * 128:(c + 1) * 128], xn[:, c * 128:(c + 1) * 128], self.identb[:])
            return ins
        t.op("pe", tr, reads=[r_xn, self.r_const], writes=[self.psr[pbank]])
        t.op("act", lambda e: e.copy(out=out_xnT, in_=pbf[:, :].rearrange("p (c n) -> p c n", c=8)),
             reads=[self.psr[pbank]], writes=[r_out])

    def ln_tile(self, xin_ap, xt, r_xt, xn, r_xn, stats, mv, r_st, out_xnT, r_out, pbank, q="sp"):
        self.ln_pre(xin_ap, xt, r_xt, xn, r_xn, stats, mv, r_st, q=q)
        self.ln_post(xn, r_xn, out_xnT, r_out, pbank)

    def prep_w(self, es, tag, colranges, sc_off, sh_off):
        nc, t = self.nc, self.t
        ncols = sum(l for _, l in colranges)
        wsb = self.sb(es, "w_" + tag, [128, 8, ncols], BF16)
        r_w = self.R()
        wv = self.I("w_in").rearrange("(c p) n -> p c n", p=128)
        o = 0
        for (s0, l) in colranges:
            for a in range(0, l, 512):
                b = min(l, a + 512)
                t.dma("pool", wsb[:, :, o + a:o + b], wv[:, :, s0 + a:s0 + b], writes=[r_w])
            o += l
        nch = (ncols + 127) // 128
        biasT = self.sb(es, "bT_" + tag, [128, nch], F32)
        biasrow = self.sb(es, "br_" + tag, [1, ncols], BF16)
        onep = self.sb(es, "onep_" + tag, [128, 8], F32)
        shb = self.sb(es, "shb_" + tag, [128, 8], BF16)
        r_b = self.R()
        t.op("dve", lambda e: e.tensor_scalar(out=onep[:], in0=self.modT[:, sc_off:sc_off + 8], scalar1=1.0,
                                              scalar2=None, op0=ALU.add), reads=[self.r_mod], writes=[r_b])
        t.op("dve", lambda e: e.tensor_copy(out=shb[:], in_=self.modT[:, sh_off:sh_off + 8]),
             reads=[self.r_mod], writes=[r_b])
        for ch in range(nch):
            w0 = ch * 128
            wl = min(128, ncols - w0)
            pb = ch % 2

            def mm(e, w0=w0, wl=wl, pb=pb):
                for c in range(8):
                    ins = e.matmul(self.ps[pb][0:wl, 0:1], lhsT=wsb[:, c, w0:w0 + wl], rhs=shb[:, c:c + 1],
                                   start=(c == 0), stop=(c == 7))
                return ins
            t.op("pe", mm, reads=[r_w, r_b], writes=[self.psr[pb]])
            t.op("dve", lambda e, ch=ch, wl=wl, pb=pb: e.tensor_copy(out=biasT[0:wl, ch:ch + 1],
                                                                      in_=self.ps[pb][0:wl, 0:1]),
                 reads=[self.psr[pb]], writes=[r_b])
        for a in range(0, ncols, 512):
            b = min(ncols, a + 512)
            pb = 2 + (a // 512) % 2

            def mm2(e, a=a, b=b, pb=pb):
                for c in range(8):
                    ins = e.matmul(self.ps[pb][0:1, 0:b - a], lhsT=shb[:, c:c + 1], rhs=wsb[:, c, a:b],
                                   start=(c == 0), stop=(c == 7))
                return ins
            t.op("pe", mm2, reads=[r_w, r_b], writes=[self.psr[pb]])
            t.op("dve", lambda e, a=a, b=b, pb=pb: e.tensor_copy(out=biasrow[:, a:b], in_=self.ps[pb][0:1, 0:b - a]),
                 reads=[self.psr[pb]], writes=[r_b])
        for c in range(8):
            en = "dve" if c % 2 == 0 else "pool"
            t.op(en, lambda e, c=c: e.tensor_scalar(out=wsb[:, c, :], in0=wsb[:, c, :], scalar1=onep[:, c:c + 1],
                                                     scalar2=None, op0=ALU.mult),
                 reads=[r_w, r_b], writes=[r_w])
        return wsb, r_w, biasT, biasrow, r_b

    def phase1a(self):
        nc, t = self.nc, self.t
        S = self.S
        with ExitStack() as es:
            wsb, r_w, biasT, biasrow, r_b = self.prep_w(
                es, "p1a", [(C_KA, 1024), (C_KB, 1024), (C_KI, 64), (C_VA, 1024), (C_VB, 1024)], 8, 0)
            VOFF = 2112
            ones = self.sb(es, "ones1", [1, 128], BF16)
            r_ones = self.R()
            t.op("dve", lambda e: e.memset(ones[:], 1.0), writes=[r_ones])
            xt = [self.sb(es, f"xt{i}", [128, D], F32) for i in range(2)]
            r_xt = [self.R() for _ in range(2)]
            xn = [self.sb(es, f"xn{i}", [128, D], BF16) for i in range(2)]
            r_xn = [self.R() for _ in range(2)]
            stats = [self.sb(es, f"st{i}", [128, 2, 6], F32) for i in range(2)]
            mv = [self.sb(es, f"mv{i}", [128, 4], F32) for i in range(2)]
            r_st = [self.R() for _ in range(2)]
            xnT = [self.sb(es, f"xnT{i}", [128, 8, 512], BF16) for i in range(2)]
            r_xnT = [self.R() for _ in range(2)]
            kst = [self.sb(es, f"kst{i}", [128, 512], BF16) for i in range(4)]
            r_kst = [self.R() for _ in range(4)]
            vst = [self.sb(es, f"vst{i}", [128, 4, 132], BF16) for i in range(4)]
            r_vst = [self.R() for _ in range(4)]
            for i in range(4):
                t.op("pool", lambda e, i=i: e.memset(vst[i][:, :, 128:132], 0.0), writes=[r_vst[i]])
                t.op("pool", lambda e, i=i: e.memset(vst[i][:, :, 128:129], 1.0), writes=[r_vst[i]])
            ngrp = S // 512
            ki = 0
            vi = 0
            tcount = 0
            ev = 0
            xn4 = [self.sb(es, f"xn4_{i}", [128, D], BF16) for i in range(8)]
            r_xn4 = [self.R() for _ in range(8)]

            def ln_group_pre(g):
                for tt in range(4):
                    b = tcnt[0] % 2
                    tcnt[0] += 1
                    k = (g % 2) * 4 + tt
                    tok0 = g * 512 + tt * 128
                    self.ln_pre(self.I("x")[tok0:tok0 + 128, :], xt[b], r_xt[b], xn4[k], r_xn4[k], stats[b], mv[b], r_st[b])

            def ln_group_post(g):
                gb = g % 2
                for tt in range(4):
                    k = (g % 2) * 4 + tt
                    self.ln_post(xn4[k], r_xn4[k], xnT[gb][:, :, tt * 128:(tt + 1) * 128], r_xnT[gb], pbank=tt % 2)
            tcnt = [0]
            ln_group_pre(0)
            ln_group_post(0)
            for g in range(ngrp):
                gb = g % 2
                if g + 1 < ngrp:
                    ln_group_pre(g + 1)
                for ch in range(17):
                    wl = 128 if ch < 16 else 64
                    pb = 2 + ch % 3

                    def mm(e, ch=ch, wl=wl, pb=pb, gb=gb):
                        for c in range(8):
                            ins = e.matmul(self.ps[pb][0:wl, :], lhsT=wsb[:, c, ch * 128:ch * 128 + wl],
                                           rhs=xnT[gb][:, c, :], start=(c == 0), stop=(c == 7))
                        return ins
                    t.op("pe", mm, reads=[r_w, r_xnT[gb]], writes=[self.psr[pb]])
                    s = ki % 4
                    ki += 1
                    en = "act" if ev % 4 != 3 else "dve"
                    ev += 1
                    if en == "act":
                        t.op("act", lambda e, s=s, wl=wl, pb=pb, ch=ch: e.activation(
                            out=kst[s][0:wl, :], in_=self.ps[pb][0:wl, :], func=AF.Identity,
                            bias=biasT[0:wl, ch:ch + 1], scale=1.0),
                            reads=[self.psr[pb], r_b], writes=[r_kst[s]])
                    else:
                        t.op("dve", lambda e, s=s, wl=wl, pb=pb, ch=ch: e.tensor_scalar(
                            out=kst[s][0:wl, :], in0=self.ps[pb][0:wl, :], scalar1=biasT[0:wl, ch:ch + 1],
                            scalar2=None, op0=ALU.add),
                            reads=[self.psr[pb], r_b], writes=[r_kst[s]])
                    tsl = slice(g * 512, (g + 1) * 512)
                    if ch < 8:
                        t.dma("sp", self.d_kat[2 * ch:2 * ch + 2, :, tsl].rearrange("m d n -> (m d) n"),
                              kst[s][:, :], reads=[r_kst[s]])
                    elif ch < 16:
                        t.dma("sp", self.d_kbt[ch - 8, :, tsl], kst[s][:, :], reads=[r_kst[s]])
                    else:
                        t.dma("sp", self.d_kit[:, tsl], kst[s][0:64, :], reads=[r_kst[s]])
                if g + 1 < ngrp:
                    ln_group_post(g + 1)
                for tt in range(4):
                    for vg in range(4):
                        pb = 5 + (tt * 4 + vg) % 3
                        c0 = VOFF + vg * 512

                        def mm(e, tt=tt, c0=c0, pb=pb, gb=gb):
                            for c in range(8):
                                e.matmul(self.ps[pb][:, :], lhsT=xnT[gb][:, c, tt * 128:(tt + 1) * 128],
                                         rhs=wsb[:, c, c0:c0 + 512], start=(c == 0), stop=False)
                            return e.matmul(self.ps[pb][:, :], lhsT=ones[0:1, :], rhs=biasrow[0:1, c0:c0 + 512],
                                            start=False, stop=True)
                        t.op("pe", mm, reads=[r_w, r_xnT[gb], r_b, r_ones], writes=[self.psr[pb]])
                        s = vi % 4
                        vi += 1
                        en = "act" if ev % 4 != 3 else "dve"
                        ev += 1
                        psv = self.ps[pb][:, :].rearrange("p (h e) -> p h e", h=4)
                        if en == "act":
                            t.op("act", lambda e, s=s, psv=psv: e.copy(out=vst[s][:, :, 0:128], in_=psv),
                                 reads=[self.psr[pb]], writes=[r_vst[s]])
                        else:
                            t.op("dve", lambda e, s=s, psv=psv: e.tensor_copy(out=vst[s][:, :, 0:128], in_=psv),
                                 reads=[self.psr[pb]], writes=[r_vst[s]])
                        tok0 = g * 512 + tt * 128
                        dst = self.d_va if vg < 2 else self.d_vb
                        t.dma("sp", dst[tok0:tok0 + 128, (vg % 2) * 4:(vg % 2) * 4 + 4, :], vst[s][:],
                              reads=[r_vst[s]])
            self.barrier()

    def phase1b(self):
        nc, t = self.nc, self.t
        nslot, NQ = self.nslot, self.NQ
        with ExitStack() as es:
            wsb, r_w, biasT, biasrow, r_b = self.prep_w(
                es, "p1b", [(C_QA, 1024), (C_QB, 1024), (C_QI, 1024), (C_WI, 16), (C_GA, 1024), (C_GB, 1024)], 8, 0)
            O_QI, O_WI, O_GA = 2048, 3072, 3088
            ones = self.sb(es, "ones1b", [1, 128], BF16)
            r_ones = self.R()
            t.op("dve", lambda e: e.memset(ones[:], 1.0), writes=[r_ones])
            xt = [self.sb(es, f"xtb{i}", [128, D], F32) for i in range(2)]
            r_xt = [self.R() for _ in range(2)]
            xn = [self.sb(es, f"xnb{i}", [128, D], BF16) for i in range(2)]
            r_xn = [self.R() for _ in range(2)]
            stats = [self.sb(es, f"stb{i}", [128, 2, 6], F32) for i in range(2)]
            mv = [self.sb(es, f"mvb{i}", [128, 4], F32) for i in range(2)]
            r_st = [self.R() for _ in range(2)]
            G = min(4, nslot)
            xnT = [self.sb(es, f"xnTb{i}", [128, 8, 128 * G], BF16) for i in range(2)]
            r_xnT = [self.R() for _ in range(2)]
            kst = [self.sb(es, f"kstb{i}", [128, 128 * G], BF16) for i in range(4)]
            r_kst = [self.R() for _ in range(4)]
            gst = [self.sb(es, f"gst{i}", [128, 512], F32) for i in range(3)]
            r_gst = [self.R() for _ in range(3)]
            wis = [self.sb(es, f"wis{i}", [128, 3, 16], F32) for i in range(2)]
            r_wis = [self.R() for _ in range(2)]
            qis = [self.sb(es, f"qis{i}", [128, 1024], BF16) for i in range(2)]
            r_qis = [self.R() for _ in range(2)]
            qit = [self.sb(es, f"qit{i}", [128, 8, 128], BF16) for i in range(2)]
            r_qit = [self.R() for _ in range(2)]
            ki = 0
            gi = 0
            tcount = 0
            for g in range(nslot // G):
                gb = g % 2
                NT = 128 * G
                for tt in range(G):
                    b = tcount % 2
                    j = g * G + tt
                    tok0 = (8 * j + 7) * 128
                    self.ln_tile(self.I("x")[tok0:tok0 + 128, :], xt[b], r_xt[b], xn[b], r_xn[b], stats[b], mv[b],
                                 r_st[b], xnT[gb][:, :, tt * 128:(tt + 1) * 128], r_xnT[gb], pbank=b)
                    tcount += 1
                q0 = g * NT
                for ch in range(16):
                    pb = 2 + ch % 3

                    def mm(e, ch=ch, pb=pb, gb=gb, NT=NT):
                        for c in range(8):
                            ins = e.matmul(self.ps[pb][:, 0:NT], lhsT=wsb[:, c, ch * 128:ch * 128 + 128],
                                           rhs=xnT[gb][:, c, :], start=(c == 0), stop=(c == 7))
                        return ins
                    t.op("pe", mm, reads=[r_w, r_xnT[gb]], writes=[self.psr[pb]])
                    s = ki % 4
                    ki += 1
                    scale = 0.125 if ch < 8 else 128.0 ** -0.5
                    t.op("dve", lambda e, s=s, pb=pb, ch=ch, scale=scale, NT=NT: e.tensor_scalar(
                        out=kst[s][:, 0:NT], in0=self.ps[pb][:, 0:NT], scalar1=biasT[:, ch:ch + 1], scalar2=scale,
                        op0=ALU.add, op1=ALU.mult), reads=[self.psr[pb], r_b], writes=[r_kst[s]])
                    if ch < 8:
                        t.dma("sp", self.d_qat[2 * ch:2 * ch + 2, :, q0:q0 + NT].rearrange("m d n -> (m d) n"),
                              kst[s][:, 0:NT], reads=[r_kst[s]])
                    else:
                        t.dma("sp", self.d_qbt[ch - 8, :, q0:q0 + NT], kst[s][:, 0:NT], reads=[r_kst[s]])
                for tt in range(G):
                    j = g * G + tt
                    tq0 = j * 128
                    lhs = lambda c, tt=tt, gb=gb: xnT[gb][:, c, tt * 128:(tt + 1) * 128]
                    wb = j % 2
                    pb = 5

                    def mmw(e, lhs=lhs, pb=pb):
                        for c in range(8):
                            e.matmul(self.ps[pb][:, 0:16], lhsT=lhs(c), rhs=wsb[:, c, O_WI:O_WI + 16],
                                     start=(c == 0), stop=False)
                        return e.matmul(self.ps[pb][:, 0:16], lhsT=ones[0:1, :], rhs=biasrow[0:1, O_WI:O_WI + 16],
                                        start=False, stop=True)
                    t.op("pe", mmw, reads=[r_w, r_xnT[gb], r_b, r_ones], writes=[self.psr[pb]])
                    t.op("dve", lambda e, wb=wb, pb=pb: e.tensor_copy(out=wis[wb][:, 0, :], in_=self.ps[pb][:, 0:16]),
                         reads=[self.psr[pb]], writes=[r_wis[wb]])
                    t.op("act", lambda e, wb=wb: e.activation(out=wis[wb][:, 1, :], in_=wis[wb][:, 0, :],
                                                              func=AF.Abs, scale=1.0 / 32.0),
                         reads=[r_wis[wb]], writes=[r_wis[wb]])
                    t.op("act", lambda e, wb=wb: e.activation(out=wis[wb][:, 2, :], in_=wis[wb][:, 0, :], func=AF.Sign),
                         reads=[r_wis[wb]], writes=[r_wis[wb]])
                    t.dma("sp", self.d_sgn[tq0:tq0 + 128, :], wis[wb][:, 2, :], reads=[r_wis[wb]])
                    for qg in range(2):
                        pb = 6 + qg
                        c0 = O_QI + qg * 512

                        def mmq(e, lhs=lhs, pb=pb, c0=c0):
                            for c in range(8):
                                e.matmul(self.ps[pb][:, :], lhsT=lhs(c), rhs=wsb[:, c, c0:c0 + 512],
                                         start=(c == 0), stop=False)
                            return e.matmul(self.ps[pb][:, :], lhsT=ones[0:1, :], rhs=biasrow[0:1, c0:c0 + 512],
                                            start=False, stop=True)
                        t.op("pe", mmq, reads=[r_w, r_xnT[gb], r_b, r_ones], writes=[self.psr[pb]])
                        t.op("dve", lambda e, wb=wb, pb=pb, qg=qg: e.tensor_tensor(
                            out=qis[wb][:, qg * 512:(qg + 1) * 512].rearrange("p (h d) -> p h d", h=8),
                            in0=self.ps[pb][:, :].rearrange("p (h d) -> p h d", h=8),
                            in1=wis[wb][:, 1, qg * 8:(qg + 1) * 8].unsqueeze(2).broadcast_to([128, 8, 64]),
                            op=ALU.mult), reads=[self.psr[pb], r_wis[wb]], writes=[r_qis[wb]])
                    pbf = self.ps[2 + (j % 3)].bitcast(BF16)

                    def tr(e, wb=wb, pbf=pbf):
                        for c in range(8):
                            ins = e.transpose(pbf[:, c * 128:(c + 1) * 128], qis[wb][:, c * 128:(c + 1) * 128],
                                              self.identb[:])
                        return ins
                    t.op("pe", tr, reads=[r_qis[wb], self.r_const], writes=[self.psr[2 + (j % 3)]])
                    t.op("act", lambda e, wb=wb, pbf=pbf: e.copy(
                        out=qit[wb][:], in_=pbf[:, :].rearrange("p (c n) -> p c n", c=8)),
                        reads=[self.psr[2 + (j % 3)]], writes=[r_qit[wb]])
                    for c in range(8):
                        t.dma("sp", self.d_qit[2 * c:2 * c + 2, :, tq0:tq0 + 128].rearrange("m d n -> (m d) n"),
                              qit[wb][:, c, :], reads=[r_qit[wb]])
                    for gg in range(4):
                        pb = 5 + gg % 3
                        c0 = O_GA + gg * 512

                        def mmg(e, lhs=lhs, pb=pb, c0=c0):
                            for c in range(8):
                                e.matmul(self.ps[pb][:, :], lhsT=lhs(c), rhs=wsb[:, c, c0:c0 + 512],
                                         start=(c == 0), stop=False)
                            return e.matmul(self.ps[pb][:, :], lhsT=ones[0:1, :], rhs=biasrow[0:1, c0:c0 + 512],
                                            start=False, stop=True)
                        t.op("pe", mmg, reads=[r_w, r_xnT[gb], r_b, r_ones], writes=[self.psr[pb]])
                        s = gi % 3
                        gi += 1
                        t.op("act", lambda e, s=s, pb=pb: e.activation(out=gst[s][:], in_=self.ps[pb][:, :],
                                                                        func=AF.Sigmoid),
                             reads=[self.psr[pb]], writes=[r_gst[s]])
                        t.dma("sp", self.d_gate[tq0:tq0 + 128, gg * 512:(gg + 1) * 512], gst[s][:],
                              reads=[r_gst[s]])
            self.barrier()

    def phase2(self):
        nc, t = self.nc, self.t
        STOP = int(os.environ.get('P2STOP', '99'))
        EXP = os.environ.get('EXP', '')
        S, nslot = self.S, self.nslot
        PS = self.ps
        PR = self.psr
        with ExitStack() as es:
            dg = self.sb(es, "dg", [128, 8, 128], BF16)
            dmask = self.sb(es, "dmask", [128, 128], F32)
            dummy = self.sb(es, "dummyc", [128, 8], F32)
            iota = self.sb(es, "iotac", [128, 512], F32)
            slopetab = self.sb(es, "slopetab", [2, 8, 128], F32)
            pidx1 = self.sb(es, "pidx1", [128, 1], F32)
            nlam = self.sb(es, "nlam", [128, 1], F32)
            gbc = self.sb(es, "gbc", [128, 128], F32)
            r_c2 = self.R("c2")
            for dst, nm in [(dg, "c_dg"), (dmask, "c_dmask"), (dummy, "c_dummy"), (iota, "c_iota"),
                            (slopetab, "c_slopetab"), (pidx1, "c_pidx1")]:
                t.dma("sp", dst[:], self.I(nm), writes=[r_c2])
            lv = self.sb(es, "lv", [1, 4, 64], F32)
            lsm = self.sb(es, "lsm", [1, 8], F32)
            onesf = self.sb(es, "onesf", [1, 128], F32)
            r_l = self.R("lam")
            t.dma("sp", lv[:], self.I("lamv").rearrange("(o a) d -> o a d", o=1), writes=[r_l])
            t.op("dve", lambda e: e.memset(onesf[:], 1.0), writes=[r_l])
            t.op("dve", lambda e: e.tensor_tensor(out=lv[:, 0, :], in0=lv[:, 0, :], in1=lv[:, 1, :], op=ALU.mult),
                 reads=[r_l], writes=[r_l])
            t.op("dve", lambda e: e.tensor_tensor(out=lv[:, 2, :], in0=lv[:, 2, :], in1=lv[:, 3, :], op=ALU.mult),
                 reads=[r_l], writes=[r_l])
            t.op("dve", lambda e: e.reduce_sum(out=lsm[:, 0:1], in_=lv[:, 0, :], axis=AX.X), reads=[r_l], writes=[r_l])
            t.op("dve", lambda e: e.reduce_sum(out=lsm[:, 1:2], in_=lv[:, 2, :], axis=AX.X), reads=[r_l], writes=[r_l])
            t.op("act", lambda e: e.activation(out=lsm[:, 2:4], in_=lsm[:, 0:2], func=AF.Exp), reads=[r_l], writes=[r_l])
            t.op("dve", lambda e: e.tensor_tensor(out=lsm[:, 4:5], in0=lsm[:, 3:4], in1=lsm[:, 2:3], op=ALU.subtract),
                 reads=[r_l], writes=[r_l])
            t.op("dve", lambda e: e.tensor_scalar(out=lsm[:, 5:6], in0=lsm[:, 4:5], scalar1=-LAM_INIT, scalar2=None,
                                                  op0=ALU.add), reads=[r_l], writes=[r_l])
            t.op("pe", lambda e: e.matmul(PS[7][:, 0:1], lhsT=onesf[0:1, :], rhs=lsm[0:1, 5:6], start=True, stop=True),
                 reads=[r_l], writes=[PR[7]])
            t.op("dve", lambda e: e.tensor_copy(out=nlam[:], in_=PS[7][:, 0:1]), reads=[PR[7]], writes=[r_c2])
            t.dma("sp", gbc[:], self.I("diff_norm_g").partition_broadcast(128), writes=[r_c2])
            t.op("dve", lambda e: e.tensor_scalar(out=gbc[:], in0=gbc[:], scalar1=1.0 - LAM_INIT, scalar2=None,
                                                  op0=ALU.mult), reads=[r_c2], writes=[r_c2])

            if STOP <= 0:
                self.barrier()
                return
            score = self.sb(es, "score", [128, S], F32)
            maskT = score.bitcast(BF16)
            r_sm = self.R("score")
            mask = self.sb(es, "mask", [128, S], BF16)
            r_mask = self.R("mask")
            qi_sb = self.sb(es, "qi_sb", [128, 8, 128], BF16)
            r_qi = self.R()
            sgn = self.sb(es, "sgn", [128, 16], F32)
            dsg = self.sb(es, "dsg", [128, 16, 128], BF16)
            r_dsg = self.R()
            kit_sb = [self.sb(es, f"kit{i}", [128, 1024], BF16) for i in range(2)]
            r_kit = [self.R() for _ in range(2)]
            rbuf = [self.sb(es, f"rbuf{i}", [128, 512], BF16) for i in range(4)]
            r_rbuf = [self.R() for _ in range(4)]
            sv = self.sb(es, "sv", [128, 48], F32)
            svi = self.sb(es, "svi", [128, 4], I32)
            r_sv = self.R()
            am = self.sb(es, "am", [128, 40], F32)
            r_am = self.R()
            tmp512 = self.sb(es, "tmp512", [128, 512], F32)
            r_tmp = self.R()
            ab = self.sb(es, "ab", [128, 2], BF16)
            qb_sb = self.sb(es, "qb_sb", [128, 8, 128], BF16)
            r_qb = self.R()
            qbaug = self.sb(es, "qbaug", [128, 8, 128], BF16)
            r_qbaug = self.R()
            qa_sb = self.sb(es, "qa_sb", [69, 16, 128], BF16)
            r_qa = self.R()
            NKB = 3
            kbuf = [self.sb(es, f"kbuf{i}", [128, 4, 1024], BF16) for i in range(NKB)]
            r_kbuf = [self.R() for _ in range(NKB)]
            vbuf = [self.sb(es, f"vbuf{i}", [128, 8, 4, 132], BF16) for i in range(NKB)]
            r_vbuf = [self.R() for _ in range(NKB)]
            kaug_sb = [self.sb(es, f"kaug{i}", [128, 1024], BF16) for i in range(NKB)]
            r_kaug = [self.R() for _ in range(NKB)]
            pbuf = [self.sb(es, f"pbuf{i}", [128, 4, 128], BF16) for i in range(5)]
            r_pbuf = [self.R() for _ in range(5)]
            ysb = [self.sb(es, f"ysb{i}", [128, D], BF16) for i in range(2)]
            r_ysb = [self.R() for _ in range(2)]
            junk = self.sb(es, "junk128", [128, 128], F32)
            r_junk = self.R()
            sv2 = self.sb(es, "sv2", [128, 16], F32)
            oraw = self.sb(es, "oraw", [128, D], F32)
            r_oraw = self.R()
            ssq = self.sb(es, "ssq", [128, 16], F32)
            r_ssq = self.R()
            r_sv2 = [self.R() for _ in range(2)]
            for i in range(NKB):
                t.op("pool", lambda e, i=i: e.memset(kaug_sb[i][:], 0.0), writes=[r_kaug[i]])
            t.op("pool", lambda e: e.memset(qbaug[:], 0.0), writes=[r_qbaug])
            kcnt = [0]
            pcnt = [0]
            scnt = [0]
            kitc = [0]
            rcnt = [0]

            for j in range(nslot):
                tq = 8 * j + 7
                NT = tq + 1
                N = NT * 128
                q0 = j * 128
                ngrp = NT // 4
                qv = self.d_qit[:, :, q0:q0 + 128].rearrange("(hp two) d n -> two d hp n", two=2)
                t.dma("sp", qi_sb[0:64, :, :], qv[0], writes=[r_qi])
                t.dma("sp", qi_sb[64:128, :, :], qv[1], writes=[r_qi])
                t.dma("sp", sgn[:], self.d_sgn[q0:q0 + 128, :], writes=[r_dsg])
                for h in range(16):
                    en = "dve" if (h % 2 == 0 or os.environ.get("NOPOOL")) else "pool"
                    t.op(en, lambda e, h=h: e.tensor_scalar(out=dsg[:, h, :], in0=self.identb[:], scalar1=sgn[:, h:h + 1],
                                                            scalar2=None, op0=ALU.mult),
                         reads=[r_dsg, self.r_const], writes=[r_dsg])
                for kg in range(ngrp):
                    if kg % 2 == 0:
                        kb = kitc[0] % 2
                        kitc[0] += 1
                        w = min(1024, N - kg * 512)
                        t.dma("sp", kit_sb[kb][0:64, 0:w], self.d_kit[:, kg * 512:kg * 512 + w], writes=[r_kit[kb]])
                        t.dma("sp", kit_sb[kb][64:128, 0:w], self.d_kit[:, kg * 512:kg * 512 + w], writes=[r_kit[kb]])
                    koff = (kg % 2) * 512

                    LB = (2, 3, 4, 5)

                    def logits(h, kb=kb, koff=koff):
                        lb = LB[h % 4]
                        p0 = 64 * (h % 2)
                        t.op("pe", lambda e: e.matmul(PS[lb][:, :], lhsT=qi_sb[p0:p0 + 64, h // 2, :],
                                                      rhs=kit_sb[kb][p0:p0 + 64, koff:koff + 512], start=True, stop=True),
                             reads=[r_qi, r_kit[kb]], writes=[PR[lb]])

                    def relu(h):
                        lb = LB[h % 4]
                        rb = rcnt[0] % 4
                        rcnt[0] += 1
                        if h % 2 == 0:
                            t.op("act", lambda e: e.activation(out=rbuf[rb][:], in_=PS[lb][:, :], func=AF.Relu),
                                 reads=[PR[lb]], writes=[r_rbuf[rb]])
                        else:
                            t.op("dve", lambda e: e.tensor_scalar(out=rbuf[rb][:], in0=PS[lb][:, :], scalar1=0.0,
                                                                  scalar2=None, op0=ALU.max),
                                 reads=[PR[lb]], writes=[r_rbuf[rb]])
                        return rb

                    def hsum(h, rb):
                        t.op("pe", lambda e: e.matmul(PS[7][:, :], lhsT=dsg[:, h, :], rhs=rbuf[rb][:],
                                                      start=(h == 0), stop=(h == 15)),
                             reads=[r_dsg, r_rbuf[rb]], writes=[PR[7]])
                    for h in range(4):
                        logits(h)
                    for h in range(0, 16, 2):
                        rb0 = relu(h)
                        rb1 = relu(h + 1)
                        hsum(h, rb0)
                        hsum(h + 1, rb1)
                        if h + 4 < 16:
                            logits(h + 4)
                            logits(h + 5)
                    if "f" in EXP:
                        continue
                    sl = slice(kg * 512, (kg + 1) * 512)
                    if "g" in EXP:
                        pass
                    elif "a" in EXP:
                        t.op("dve", lambda e, kg=kg: e.reduce_max(out=am[:, kg:kg + 1], in_=PS[7][:, :], axis=AX.X),
                             reads=[PR[7]], writes=[r_am])
                    else:
                        t.op("dve", lambda e, kg=kg: e.tensor_reduce(out=am[:, kg:kg + 1], in_=PS[7][:, :], axis=AX.X,
                                                                     op=ALU.max, apply_absolute_value=True),
                             reads=[PR[7]], writes=[r_am])
                    if "h" not in EXP:
                        if "I" not in EXP:
                            t.op("dve", lambda e, sl=sl: e.tensor_copy(out=score[:, sl], in_=PS[7][:, :]),
                                 reads=[PR[7]], writes=[r_sm])
                        else:
                            t.op("act", lambda e, sl=sl: e.activation(out=score[:, sl], in_=PS[7][:, :], func=AF.Identity),
                                 reads=[PR[7]], writes=[r_sm])
                    for tl in range(4):
                        if "c" in EXP:
                            break
                        tt = kg * 4 + tl
                        ts_ = slice(tt * 128, (tt + 1) * 128)
                        if tt < 7:
                            t.op("dve", lambda e, ts_=ts_, tt=tt: e.tensor_scalar(
                                out=score[:, ts_], in0=score[:, ts_], scalar1=dummy[:, tt:tt + 1], scalar2=None,
                                op0=ALU.add), reads=[r_sm, r_c2], writes=[r_sm])
                        if tt == NT - 1:
                            t.op("dve", lambda e, ts_=ts_: e.tensor_tensor(out=score[:, ts_], in0=score[:, ts_],
                                                                           in1=dmask[:], op=ALU.add),
                                 reads=[r_sm, r_c2], writes=[r_sm])
                if STOP <= 1:
                    continue
                LO, W0, MID, CNT, PRED, AMX = 0, 1, 2, 3, 4, 7
                col = lambda i: sv[:, i:i + 1]
                t.op("dve", lambda e: e.reduce_max(out=col(AMX), in_=am[:, 0:ngrp], axis=AX.X), reads=[r_am], writes=[r_sv])
                t.op("dve", lambda e: e.tensor_scalar(out=col(LO), in0=col(AMX), scalar1=1.0, scalar2=-1.0, op0=ALU.add, op1=ALU.mult),
                     reads=[r_sv], writes=[r_sv])
                t.op("dve", lambda e: e.tensor_scalar(out=col(W0), in0=col(AMX), scalar1=1.0, scalar2=2.0, op0=ALU.add, op1=ALU.mult),
                     reads=[r_sv], writes=[r_sv])
                bit = [0]

                def bisect(nit):
                    for it in range(nit):
                        f = 0.5 ** (bit[0] + 1)
                        bit[0] += 1
                        t.op("dve", lambda e, f=f: e.scalar_tensor_tensor(out=col(MID), in0=col(W0), scalar=f, in1=col(LO),
                                                                          op0=ALU.mult, op1=ALU.add), reads=[r_sv], writes=[r_sv])
                        t.op("dve", lambda e: e.tensor_scalar(out=mask[:, 0:N], in0=score[:, 0:N], scalar1=col(MID), scalar2=None,
                                                              op0=ALU.is_gt, op1=ALU.add, accum_out=col(CNT)),
                             reads=[r_sm, r_sv], writes=[r_mask, r_sv])
                        t.op("dve", lambda e, f=f: e.tensor_scalar(out=col(PRED), in0=col(CNT), scalar1=float(TOPK) - 0.5, scalar2=f,
                                                                   op0=ALU.is_gt, op1=ALU.mult), reads=[r_sv], writes=[r_sv])
                        t.op("dve", lambda e: e.scalar_tensor_tensor(out=col(LO), in0=col(W0), scalar=col(PRED), in1=col(LO),
                                                                     op0=ALU.mult, op1=ALU.add), reads=[r_sv], writes=[r_sv])

                t.dma("sp", qb_sb[:], self.d_qbt[:, :, q0:q0 + 128].rearrange("h d n -> d h n"), writes=[r_qb])
                t.dma("sp", qa_sb[0:64, :, :], self.d_qat[:, :, q0:q0 + 128].rearrange("m d n -> d m n"), writes=[r_qa])
                for m_ in range(2):
                    t.dma("sp", qa_sb[64:69, :, :].rearrange("r (h m) n -> r h m n", m=2)[:, :, m_, :],
                          self.I("c_qaug")[2:7, j, :, :], writes=[r_qa])

                def attn_group(kind, gi, ab):
                    b0, b1_ = (2, 3) if ab == 0 else (4, 5)
                    accs = [PS[b0][:, 0:129], PS[b0][:, 129:258], PS[b0][:, 258:387], PS[b1_][:, 0:129]]
                    first_in_bank = [True, False, False, True]
                    RA = [PR[b0], PR[b1_]]
                    pendq = []
                    for tg in range(NT // 8):
                        kb = kcnt[0] % NKB
                        kcnt[0] += 1
                        ksl = slice(tg * 1024, (tg + 1) * 1024)
                        if kind == "dsa":
                            t.dma("sp", kbuf[kb][:, :, :], self.d_kbt[4 * gi:4 * gi + 4, :, ksl].rearrange("h d n -> d h n"),
                                  writes=[r_kbuf[kb]])
                            t.dma("sp", kaug_sb[kb][0:7, :], self.I("c_kaug")[:, ksl], writes=[r_kaug[kb]])
                            t.dma("sp", vbuf[kb][:, :, :, :].rearrange("p t h e -> p t (h e)"),
                                  self.d_vb[ksl, 4 * gi:4 * gi + 4, :].rearrange("(t p) h e -> p t (h e)", p=128),
                                  writes=[r_vbuf[kb]])
                        else:
                            t.dma("sp", kbuf[kb][0:64, :, :], self.d_kat[4 * gi:4 * gi + 4, :, ksl].rearrange("m d n -> d m n"),
                                  writes=[r_kbuf[kb]])
                            t.dma("sp", kbuf[kb][64:69, :, :], self.I("c_kaug")[2:7, ksl].unsqueeze(1).broadcast_to([5, 4, 1024]),
                                  writes=[r_kbuf[kb]])
                            t.dma("sp", vbuf[kb][:, :, 0:2, :].rearrange("p t h e -> p t (h e)"),
                                  self.d_va[ksl, 2 * gi:2 * gi + 2, :].rearrange("(t p) h e -> p t (h e)", p=128),
                                  writes=[r_vbuf[kb]])
                        for tl in range(8):
                            tt = tg * 8 + tl
                            sb_ = (0, 1, 6, 7)[scnt[0] % 4]
                            scnt[0] += 1
                            diag = (tt == NT - 1)

                            def qk(e, kb=kb, tl=tl, sb_=sb_, diag=diag):
                                tsl = slice(tl * 128, (tl + 1) * 128)
                                for i in range(4):
                                    reg = PS[sb_][:, i * 128:(i + 1) * 128]
                                    if kind == "dsa":
                                        ins = e.matmul(reg, lhsT=kbuf[kb][:, i, tsl], rhs=qb_sb[:, 4 * gi + i, :],
                                                       start=(i == 0), stop=False, skip_group_check=True)
                                        hh = 4 * gi + i
                                    else:
                                        ins = e.matmul(reg, lhsT=kbuf[kb][0:69, i, tsl], rhs=qa_sb[0:69, 4 * gi + i, :],
                                                       start=True, stop=not diag)
                                        hh = 2 * gi + i // 2
                                    if diag:
                                        ins = e.matmul(reg, lhsT=self.identb[:], rhs=dg[:, hh, :], start=False,
                                                       stop=(kind != "dsa"), skip_group_check=(kind == "dsa"))
                                if kind == "dsa":
                                    ins = e.matmul(PS[sb_][:, :], lhsT=kaug_sb[kb][:, tsl],
                                                   rhs=qbaug[:, 4 * gi:4 * gi + 4, :].rearrange("r h n -> r (h n)"),
                                                   start=False, stop=True, skip_group_check=True)
                                return ins
                            rd = [r_kbuf[kb], self.r_const, r_c2] + ([r_kaug[kb], r_qbaug, r_qb] if kind == "dsa" else [r_qa])
                            t.op("pe", qk, reads=rd, writes=[PR[sb_]])
                            pb_ = pcnt[0] % 5
                            pcnt[0] += 1
                            t.op("act", lambda e, sb_=sb_, pb_=pb_: e.activation(
                                out=pbuf[pb_][:].rearrange("p h n -> p (h n)"), in_=PS[sb_][:, :], func=AF.Exp),
                                reads=[PR[sb_]], writes=[r_pbuf[pb_]])
                            if kind == "dsa":
                                t.op("dve", lambda e, pb_=pb_, tt=tt: e.scalar_tensor_tensor(
                                    out=pbuf[pb_][:], in0=pbuf[pb_][:], scalar=1e30,
                                    in1=maskT[:, tt * 128:(tt + 1) * 128].unsqueeze(1).broadcast_to([128, 4, 128]),
                                    op0=ALU.min, op1=ALU.mult), reads=[r_pbuf[pb_], r_sm], writes=[r_pbuf[pb_]])

                            def pv(e, kb=kb, tl=tl, pb_=pb_, tt=tt):
                                for i in range(4):
                                    vh = i if kind == "dsa" else i // 2
                                    ins = e.matmul(accs[i], lhsT=pbuf[pb_][:, i, :], rhs=vbuf[kb][:, tl, vh, 0:129],
                                                   start=(tt == 0 and first_in_bank[i]), stop=(tt == NT - 1),
                                                   skip_group_check=True)
                                return ins
                            pendq.append(lambda pv=pv, kb=kb, pb_=pb_: t.op(
                                "pe", pv, reads=[r_pbuf[pb_], r_vbuf[kb]], writes=RA))
                            if len(pendq) > 3:
                                pendq.pop(0)()
                    while pendq:
                        pendq.pop(0)()
                    s0 = 8 * ab
                    if kind == "dsa":
                        for i in range(4):
                            hh = 4 * gi + i
                            t.op("dve", lambda e, i=i: e.reciprocal(out=sv2[:, s0 + i:s0 + i + 1], in_=accs[i][:, 128:129]),
                                 reads=RA, writes=[r_sv2[ab]])
                            t.op("dve", lambda e, i=i, hh=hh: e.tensor_scalar(
                                out=ysb[1][:, hh * 128:(hh + 1) * 128], in0=accs[i][:, 0:128], scalar1=sv2[:, s0 + i:s0 + i + 1],
                                scalar2=None, op0=ALU.mult), reads=RA + [r_sv2[ab]], writes=[r_ysb[1]])
                    else:
                        for hl in range(2):
                            hh = 2 * gi + hl
                            a0, a1 = accs[2 * hl], accs[2 * hl + 1]
                            c0 = s0 + 4 * hl
                            of_ = oraw[:, hh * 128:(hh + 1) * 128]
                            r_of_ = r_oraw
                            t.op("dve", lambda e, a0=a0, c0=c0: e.reciprocal(out=sv2[:, c0:c0 + 1], in_=a0[:, 128:129]),
                                 reads=RA, writes=[r_sv2[ab]])
                            t.op("dve", lambda e, a1=a1, c0=c0: e.reciprocal(out=sv2[:, c0 + 1:c0 + 2], in_=a1[:, 128:129]),
                                 reads=RA, writes=[r_sv2[ab]])
                            t.op("dve", lambda e, c0=c0: e.tensor_tensor(out=sv2[:, c0 + 1:c0 + 2], in0=sv2[:, c0 + 1:c0 + 2],
                                                                         in1=nlam[:], op=ALU.mult),
                                 reads=[r_sv2[ab], r_c2], writes=[r_sv2[ab]])
                            t.op("dve", lambda e, a0=a0, c0=c0, of_=of_: e.tensor_scalar(
                                out=of_, in0=a0[:, 0:128], scalar1=sv2[:, c0:c0 + 1], scalar2=None, op0=ALU.mult),
                                reads=RA + [r_sv2[ab]], writes=[r_of_])
                            t.op("dve", lambda e, a1=a1, c0=c0, of_=of_: e.scalar_tensor_tensor(
                                out=of_, in0=a1[:, 0:128], scalar=sv2[:, c0 + 1:c0 + 2], in1=of_,
                                op0=ALU.mult, op1=ALU.add), reads=RA + [r_sv2[ab], r_of_], writes=[r_of_])

                nb_per = NBISECT // 4
                for gi in range(4):
                    bisect(nb_per)
                    attn_group("diff", gi, gi % 2)
                bisect(NBISECT - 4 * nb_per)
                for hh in range(8):
                    t.op("act", lambda e, hh=hh: e.activation(out=junk[:], in_=oraw[:, hh * 128:(hh + 1) * 128], func=AF.Square,
                                                              accum_out=ssq[:, hh:hh + 1]),
                         reads=[r_oraw], writes=[r_ssq, r_junk])
                t.op("dve", lambda e: e.tensor_scalar(out=ssq[:, 0:8], in0=ssq[:, 0:8], scalar1=1.0 / 128.0, scalar2=LN_EPS,
                                                      op0=ALU.mult, op1=ALU.add), reads=[r_ssq], writes=[r_ssq])
                t.op("act", lambda e: e.activation(out=ssq[:, 0:8], in_=ssq[:, 0:8], func=AF.Sqrt), reads=[r_ssq], writes=[r_ssq])
                t.op("dve", lambda e: e.reciprocal(out=ssq[:, 8:16], in_=ssq[:, 0:8]), reads=[r_ssq], writes=[r_ssq])
                for hh in range(8):
                    t.op("dve", lambda e, hh=hh: e.scalar_tensor_tensor(
                        out=ysb[0][:, hh * 128:(hh + 1) * 128], in0=oraw[:, hh * 128:(hh + 1) * 128], scalar=ssq[:, 8 + hh:9 + hh],
                        in1=gbc[:], op0=ALU.mult, op1=ALU.mult), reads=[r_oraw, r_ssq, r_c2], writes=[r_ysb[0]])
                t.dma("sp", self.d_ya[q0:q0 + 128, :], ysb[0][:], reads=[r_ysb[0]])
                t.op("dve", lambda e: e.tensor_scalar(out=mask[:, 0:N], in0=score[:, 0:N], scalar1=col(LO), scalar2=None,
                                                      op0=ALU.is_gt), reads=[r_sm, r_sv], writes=[r_mask])
                for kg in range(ngrp):
                    t.op("dve", lambda e, kg=kg: e.scalar_tensor_tensor(
                        out=tmp512[:], in0=iota[:], scalar=float(kg * 512), in1=mask[:, kg * 512:(kg + 1) * 512],
                        op0=ALU.add, op1=ALU.mult), reads=[r_mask, r_c2], writes=[r_tmp])
                    t.op("dve", lambda e, kg=kg: e.reduce_max(out=am[:, kg:kg + 1], in_=tmp512[:], axis=AX.X),
                         reads=[r_tmp], writes=[r_am])
                MP, DD, AF_, BF_ = 8, 9, 10, 11
                t.op("dve", lambda e: e.reduce_max(out=col(MP), in_=am[:, 0:ngrp], axis=AX.X), reads=[r_am], writes=[r_sv])
                t.op("dve", lambda e: e.tensor_scalar(out=col(DD), in0=col(MP), scalar1=pidx1[:, 0:1], scalar2=float(128 * tq),
                                                      op0=ALU.subtract, op1=ALU.subtract), reads=[r_sv, r_c2], writes=[r_sv])
                t.op("act", lambda e: e.activation(out=col(DD), in_=col(DD), func=AF.Abs), reads=[r_sv], writes=[r_sv])
                t.op("dve", lambda e: e.tensor_copy(out=svi[:, 0:1], in_=col(DD)), reads=[r_sv], writes=[r_sv])
                t.op("dve", lambda e: e.tensor_single_scalar(out=svi[:, 1:2], in_=svi[:, 0:1], scalar=7,
                                                             op=ALU.arith_shift_right), reads=[r_sv], writes=[r_sv])
                t.op("dve", lambda e: e.tensor_copy(out=col(AF_), in_=svi[:, 1:2]), reads=[r_sv], writes=[r_sv])
                t.op("dve", lambda e: e.scalar_tensor_tensor(out=col(BF_), in0=col(AF_), scalar=-128.0, in1=col(DD),
                                                             op0=ALU.mult, op1=ALU.add), reads=[r_sv], writes=[r_sv])
                t.op("dve", lambda e: e.tensor_copy(out=ab[:, 0:2], in_=sv[:, AF_:AF_ + 2]), reads=[r_sv], writes=[r_sv])
                t.dma("sp", qbaug[2:7, :, :], self.I("c_qaug")[2:7, j, :, :], writes=[r_qbaug])
                t.op("pe", lambda e: e.matmul(PS[7][0:2, 0:128], lhsT=ab[:, 0:2], rhs=self.identb[:], start=True, stop=True),
                     reads=[r_sv, self.r_const], writes=[PR[7]])
                t.op("dve", lambda e: e.tensor_tensor(out=qbaug[0:2, :, :],
                                                      in0=PS[7][0:2, 0:128].unsqueeze(1).broadcast_to([2, 8, 128]),
                                                      in1=slopetab[:], op=ALU.mult),
                     reads=[PR[7], r_c2], writes=[r_qbaug])
                pbf = PS[6].bitcast(BF16)
                for g4 in range(ngrp):
                    def tr(e, g4=g4):
                        for tl in range(4):
                            tt = g4 * 4 + tl
                            ins = e.transpose(pbf[:, tl * 128:(tl + 1) * 128], mask[:, tt * 128:(tt + 1) * 128], self.identb[:])
                        return ins
                    t.op("pe", tr, reads=[r_mask, self.r_const], writes=[PR[6]])
                    if g4 % 2 == 0:
                        t.op("act", lambda e, g4=g4: e.copy(out=maskT[:, g4 * 512:(g4 + 1) * 512], in_=pbf[:, 0:512]),
                             reads=[PR[6]], writes=[r_sm])
                    else:
                        t.op("dve", lambda e, g4=g4: e.tensor_copy(out=maskT[:, g4 * 512:(g4 + 1) * 512], in_=pbf[:, 0:512]),
                             reads=[PR[6]], writes=[r_sm])
                for gi in range(2):
                    attn_group("dsa", gi, gi % 2)
                t.dma("sp", self.d_yb[q0:q0 + 128, :], ysb[1][:], reads=[r_ysb[1]])
            self.barrier()

    def load_w_bf16(self, es, name, src_ap, r):
        wsb = self.sb(es, name, [128, 8, 1024], BF16)
        v = src_ap.rearrange("(c p) n -> p c n", p=128)
        for a in range(0, 1024, 512):
            self.t.dma("pool", wsb[:, :, a:a + 512], v[:, :, a:a + 512], writes=[r])
        return wsb

    def ln_stats(self, xin, r_x, stats, mv, r_st):
        t = self.t
        for hh in range(2):
            t.op("dve", lambda e, hh=hh: e.bn_stats(out=stats[:, hh, :], in_=xin[:, hh * 512:(hh + 1) * 512]),
                 reads=[r_x], writes=[r_st])
        t.op("dve", lambda e: e.bn_aggr(out=mv[:, 0:2], in_=stats[:].rearrange("p a b -> p (a b)")),
             reads=[r_st], writes=[r_st])
        t.op("dve", lambda e: e.tensor_scalar(out=mv[:, 2:3], in0=mv[:, 1:2], scalar1=LN_EPS, scalar2=None,
                                              op0=ALU.add), reads=[r_st], writes=[r_st])
        t.op("act", lambda e: e.activation(out=mv[:, 2:3], in_=mv[:, 2:3], func=AF.Sqrt),
             reads=[r_st], writes=[r_st])
        t.op("dve", lambda e: e.reciprocal(out=mv[:, 3:4], in_=mv[:, 2:3]), reads=[r_st], writes=[r_st])

    def phase3(self):
        nc, t = self.nc, self.t
        PS, PR = self.ps, self.psr
        nslot = self.nslot
        with ExitStack() as es:
            r_w = self.R()
            wa = self.load_w_bf16(es, "wa", self.I("w_branch_a"), r_w)
            wb = self.load_w_bf16(es, "wb", self.I("w_branch_b"), r_w)
            wo = self.load_w_bf16(es, "wo", self.I("w_out"), r_w)
            g1 = self.sb(es, "g1bc", [128, D], F32)
            b1 = self.sb(es, "b1bc", [128, D], F32)
            r_c = self.R()
            self.ga_bc = self.sb(es, "ga_bc", [128, D], F32)
            t.dma("sp", self.ga_bc[:], self.d_mod[2 * D:3 * D].partition_broadcast(128), reads=[self.r_dmod], writes=[self.r_mod])
            t.dma("sp", g1[:], self.I("ln1_g").partition_broadcast(128), writes=[r_c])
            t.dma("sp", b1[:], self.I("ln1_b").partition_broadcast(128), writes=[r_c])
            yab = [self.sb(es, f"yab{i}", [128, 2, D], BF16) for i in range(2)]
            r_yab = [self.R() for _ in range(2)]
            yT = [self.sb(es, f"yT{i}", [128, 2, 8, 128], BF16) for i in range(2)]
            r_yT = [self.R() for _ in range(2)]
            gate = [self.sb(es, f"gate{i}", [128, 2 * D], F32) for i in range(2)]
            r_gate = [self.R() for _ in range(2)]
            xt = [self.sb(es, f"x3_{i}", [128, D], F32) for i in range(2)]
            r_xt = [self.R() for _ in range(2)]
            m1 = self.sb(es, "m1", [128, D], F32)
            m2 = self.sb(es, "m2", [128, D], F32)
            mg = self.sb(es, "mg", [128, D], BF16)
            mgT = self.sb(es, "mgT", [128, 8, 128], BF16)
            r_m = self.R()
            r_mg = self.R()
            r_mgT = self.R()
            xnew = self.sb(es, "xnew", [128, D], F32)
            r_xn = self.R()
            x1 = [self.sb(es, f"x1_{i}", [128, D], F32) for i in range(2)]
            r_x1 = [self.R() for _ in range(2)]
            stats = self.sb(es, "st3", [128, 2, 6], F32)
            mv = self.sb(es, "mv3", [128, 4], F32)
            r_st = self.R()
            for j in range(nslot):
                b = j % 2
                q0 = j * 128
                tok0 = (8 * j + 7) * 128
                t.dma("sp", yab[b][:, 0, :], self.d_ya[q0:q0 + 128, :], writes=[r_yab[b]])
                t.dma("sp", yab[b][:, 1, :], self.d_yb[q0:q0 + 128, :], writes=[r_yab[b]])
                t.dma("sp", gate[b][:], self.d_gate[q0:q0 + 128, :], writes=[r_gate[b]])
                t.dma("sp", xt[b][:], self.I("x")[tok0:tok0 + 128, :], writes=[r_xt[b]])
                for br in range(2):
                    pbf = PS[br].bitcast(BF16)

                    def tr(e, br=br, b=b, pbf=pbf):
                        for c in range(8):
                            ins = e.transpose(pbf[:, c * 128:(c + 1) * 128], yab[b][:, br, c * 128:(c + 1) * 128], self.identb[:])
                        return ins
                    t.op("pe", tr, reads=[r_yab[b], self.r_const], writes=[PR[br]])
                    t.op("act", lambda e, br=br, b=b, pbf=pbf: e.copy(
                        out=yT[b][:, br, :, :], in_=pbf[:, :].rearrange("p (c n) -> p c n", c=8)),
                        reads=[PR[br]], writes=[r_yT[b]])
                for cg in range(2):
                    csl = slice(cg * 512, (cg + 1) * 512)
                    for br, w_ in ((0, wa), (1, wb)):
                        pb = 2 + br

                        def mm(e, br=br, w_=w_, pb=pb, b=b, csl=csl):
                            for c in range(8):
                                ins = e.matmul(PS[pb][:, :], lhsT=yT[b][:, br, c, :], rhs=w_[:, c, csl],
                                               start=(c == 0), stop=(c == 7))
                            return ins
                        t.op("pe", mm, reads=[r_yT[b], r_w], writes=[PR[pb]])
                    t.op("dve", lambda e, b=b, csl=csl, cg=cg: e.tensor_tensor(
                        out=m1[:, csl], in0=PS[2][:, :], in1=gate[b][:, cg * 512:(cg + 1) * 512], op=ALU.mult),
                        reads=[PR[2], r_gate[b]], writes=[r_m])
                    t.op("dve", lambda e, b=b, csl=csl, cg=cg: e.tensor_tensor(
                        out=m2[:, csl], in0=PS[3][:, :], in1=gate[b][:, D + cg * 512:D + (cg + 1) * 512], op=ALU.mult),
                        reads=[PR[3], r_gate[b]], writes=[r_m])
                    t.op("dve", lambda e, csl=csl: e.tensor_tensor(out=mg[:, csl], in0=m1[:, csl], in1=m2[:, csl], op=ALU.add),
                         reads=[r_m], writes=[r_mg])
                pbf = PS[4].bitcast(BF16)

                def tr2(e, pbf=pbf):
                    for c in range(8):
                        ins = e.transpose(pbf[:, c * 128:(c + 1) * 128], mg[:, c * 128:(c + 1) * 128], self.identb[:])
                    return ins
                t.op("pe", tr2, reads=[r_mg, self.r_const], writes=[PR[4]])
                t.op("act", lambda e, pbf=pbf: e.copy(out=mgT[:], in_=pbf[:, :].rearrange("p (c n) -> p c n", c=8)),
                     reads=[PR[4]], writes=[r_mgT])
                for cg in range(2):
                    csl = slice(cg * 512, (cg + 1) * 512)
                    pb = 5 + cg

                    def mm3(e, pb=pb, csl=csl):
                        for c in range(8):
                            ins = e.matmul(PS[pb][:, :], lhsT=mgT[:, c, :], rhs=wo[:, c, csl], start=(c == 0), stop=(c == 7))
                        return ins
                    t.op("pe", mm3, reads=[r_mgT, r_w], writes=[PR[pb]])
                    t.op("dve", lambda e, pb=pb, csl=csl: e.tensor_tensor(out=m1[:, csl], in0=PS[pb][:, :],
                                                                          in1=self.ga_bc[:, csl], op=ALU.mult),
                         reads=[PR[pb], self.r_mod], writes=[r_m])
                    t.op("dve", lambda e, b=b, csl=csl: e.scalar_tensor_tensor(
                        out=xnew[:, csl], in0=xt[b][:, csl], scalar=ALPHA, in1=m1[:, csl], op0=ALU.mult, op1=ALU.add),
                        reads=[r_xt[b], r_m], writes=[r_xn])
                self.ln_stats(xnew, r_xn, stats, mv, r_st)
                t.op("dve", lambda e, b=b: e.tensor_scalar(out=x1[b][:], in0=xnew[:], scalar1=mv[:, 0:1], scalar2=mv[:, 3:4],
                                                           op0=ALU.subtract, op1=ALU.mult),
                     reads=[r_xn, r_st], writes=[r_x1[b]])
                t.op("pool", lambda e, b=b: e.tensor_tensor(out=x1[b][:], in0=x1[b][:], in1=g1[:], op=ALU.mult),
                     reads=[r_x1[b], r_c], writes=[r_x1[b]])
                t.op("pool", lambda e, b=b: e.tensor_tensor(out=x1[b][:], in0=x1[b][:], in1=b1[:], op=ALU.add),
                     reads=[r_x1[b], r_c], writes=[r_x1[b]])
                t.dma("sp", self.d_x1[q0:q0 + 128, :], x1[b][:], reads=[r_x1[b]])
            self.barrier()

    def phase4(self):
        nc, t = self.nc, self.t
        PS, PR = self.ps, self.psr
        NQ = self.NQ
        HT = min(1024, NQ)
        nhalf = NQ // HT
        TG = min(512, HT)
        ntg = HT // TG
        ntt = HT // 128
        with ExitStack() as es:
            scf = self.sb(es, "scf_bc", [128, D], F32)
            shf = self.sb(es, "shf_bc", [128, D], F32)
            g2 = scf
            b2l = shf
            wr = self.sb(es, "wr", [128, 8, NEXP], F32)
            brr = self.sb(es, "brr", [1, NEXP], F32)
            onesf = self.sb(es, "onesf4", [1, 128], F32)
            b2w = self.sb(es, "b2w", [NEXP, D], F32)
            b1raw = self.sb(es, "b1raw", [NEXP, 2 * DFF], F32)
            b1g = self.sb(es, "b1g", [128, 8, NEXP], F32)
            b1l = self.sb(es, "b1l", [128, 8, NEXP], F32)
            r_c = self.R()
            self.gf_bc = self.sb(es, "gf_bc", [128, D], F32)
            t.dma("sp", self.gf_bc[:], self.d_mod[5 * D:6 * D].partition_broadcast(128), reads=[self.r_dmod], writes=[self.r_mod])
            r_md = self.R()
            t.dma("sp", wr[:], self.I("w_router").rearrange("(c p) n -> p c n", p=128), writes=[r_c])
            t.dma("sp", brr[:], self.I("b_router").rearrange("(o n) -> o n", o=1), writes=[r_c])
            t.dma("sp", b2w[:], self.I("b_e2"), writes=[r_c])
            t.dma("sp", b1raw[:], self.I("b_e1"), writes=[r_c])
            t.op("dve", lambda e: e.memset(onesf[:], 1.0), writes=[r_c])
            b1v = b1raw[:].rearrange("e (p f two) -> e p f two", p=8, two=2)
            for p in range(8):
                for two, dst in ((0, b1g), (1, b1l)):
                    t.op("pe", lambda e, p=p, two=two: e.transpose(PS[7][:, 0:NEXP], b1v[:, p, :, two], self.identf[0:NEXP, 0:NEXP]),
                         reads=[r_c, self.r_const], writes=[PR[7]])
                    t.op("dve", lambda e, p=p, dst=dst, two=two: e.tensor_scalar(
                        out=dst[:, p, :], in0=PS[7][:, 0:NEXP], scalar1=float(two), scalar2=None, op0=ALU.add),
                        reads=[PR[7]], writes=[r_c])
            vT = self.sb(es, "vT", [128, 8, HT], BF16)
            r_vT = self.R()
            yacc = self.sb(es, "yacc", [128, ntt, D], F32)
            r_y = self.R()
            gate = self.sb(es, "gate4", [128, ntt, NEXP], F32)
            r_g = self.R()
            aT = [self.sb(es, f"aT{i}", [128, 8, HT], BF16) for i in range(2)]
            r_aT = [self.R() for _ in range(2)]
            w1p = [self.sb(es, f"w1p{i}", [128, 8, 256], BF16) for i in range(5)]
            r_w1 = [self.R() for _ in range(5)]
            w2e = [self.sb(es, f"w2e{i}", [128, 8, D], BF16) for i in range(2)]
            r_w2 = [self.R() for _ in range(2)]
            glu = [self.sb(es, f"glu{i}", [128, TG], F32) for i in range(2)]
            sig = [self.sb(es, f"sig{i}", [128, TG], F32) for i in range(2)]
            lin = [self.sb(es, f"lin{i}", [128, TG], F32) for i in range(2)]
            r_elg = [self.R() for _ in range(3)]
            r_ell = [self.R() for _ in range(3)]
            r_els = [self.R() for _ in range(3)]
            xt = [self.sb(es, f"x4_{i}", [128, D], F32) for i in range(2)]
            r_xt = [self.R() for _ in range(2)]
            vf = self.sb(es, "vf", [128, D], F32)
            vb16 = self.sb(es, "vb16", [128, D], BF16)
            vTf = self.sb(es, "vTf", [128, 8, 128], F32)
            r_v = self.R()
            stats = self.sb(es, "st4", [128, 2, 6], F32)
            mv = self.sb(es, "mv4", [128, 4], F32)
            r_st = self.R()
            rt = self.sb(es, "rt", [128, 4, NEXP], F32)
            m8 = self.sb(es, "m8", [128, 16], F32)
            gT = self.sb(es, "gT", [NEXP, 128], F32)
            r_rt = self.R()
            w1cnt = [0]
            wfc = [0]
            elc = [0]
            for hf in range(nhalf):
                h0 = hf * HT
                t.dma("sp", scf[:], self.d_mod[4 * D:5 * D].partition_broadcast(128), writes=[r_md])
                t.dma("sp", shf[:], self.d_mod[3 * D:4 * D].partition_broadcast(128), writes=[r_md])
                t.op("dve", lambda e: e.tensor_scalar(out=scf[:], in0=scf[:], scalar1=1.0, scalar2=None, op0=ALU.add),
                     reads=[r_md], writes=[r_md])
                for tt in range(ntt):
                    b = tt % 2
                    q0 = h0 + tt * 128
                    t.dma("sp", xt[b][:], self.d_x1[q0:q0 + 128, :], writes=[r_xt[b]])
                    self.ln_stats(xt[b], r_xt[b], stats, mv, r_st)
                    t.op("dve", lambda e, b=b: e.tensor_scalar(out=vf[:], in0=xt[b][:], scalar1=mv[:, 0:1], scalar2=mv[:, 3:4],
                                                               op0=ALU.subtract, op1=ALU.mult),
                         reads=[r_xt[b], r_st], writes=[r_v])
                    t.op("pool", lambda e: e.tensor_tensor(out=vf[:], in0=vf[:], in1=scf[:], op=ALU.mult),
                         reads=[r_v, r_md], writes=[r_v])
                    t.op("pool", lambda e: e.tensor_tensor(out=vf[:], in0=vf[:], in1=shf[:], op=ALU.add),
                         reads=[r_v, r_md], writes=[r_v])
                    t.op("dve", lambda e: e.tensor_copy(out=vb16[:], in_=vf[:]), reads=[r_v], writes=[r_v])
                    pbf = PS[0].bitcast(BF16)

                    def tr(e, pbf=pbf):
                        for c in range(8):
                            ins = e.transpose(pbf[:, c * 128:(c + 1) * 128], vb16[:, c * 128:(c + 1) * 128], self.identb[:])
                        return ins
                    t.op("pe", tr, reads=[r_v, self.r_const], writes=[PR[0]])
                    t.op("act", lambda e, tt=tt, pbf=pbf: e.copy(out=vT[:, :, tt * 128:(tt + 1) * 128],
                                                                 in_=pbf[:, :].rearrange("p (c n) -> p c n", c=8)),
                         reads=[PR[0]], writes=[r_vT])
                    for half2 in range(2):
                        def trf(e, half2=half2):
                            for c4 in range(4):
                                c = half2 * 4 + c4
                                ins = e.transpose(PS[1 + half2][:, c4 * 128:(c4 + 1) * 128], vf[:, c * 128:(c + 1) * 128], self.identf[:])
                            return ins
                        t.op("pe", trf, reads=[r_v, self.r_const], writes=[PR[1 + half2]])
                        t.op("dve", lambda e, half2=half2: e.tensor_copy(
                            out=vTf[:, half2 * 4:half2 * 4 + 4, :], in_=PS[1 + half2][:, :].rearrange("p (c n) -> p c n", c=4)),
                            reads=[PR[1 + half2]], writes=[r_v])

                    def mmr(e):
                        for c in range(8):
                            e.matmul(PS[3][:, 0:NEXP], lhsT=vTf[:, c, :], rhs=wr[:, c, :], start=(c == 0), stop=False)
                        return e.matmul(PS[3][:, 0:NEXP], lhsT=onesf[0:1, :], rhs=brr[0:1, :], start=False, stop=True)
                    t.op("pe", mmr, reads=[r_v, r_c], writes=[PR[3]])
                    LG, SEL, EX = 0, 1, 2
                    t.op("dve", lambda e: e.tensor_copy(out=rt[:, LG, :], in_=PS[3][:, 0:NEXP]), reads=[PR[3]], writes=[r_rt])
                    t.op("dve", lambda e: e.max(out=m8[:, 0:8], in_=rt[:, LG, :]), reads=[r_rt], writes=[r_rt])
                    t.op("dve", lambda e: e.tensor_scalar(out=rt[:, SEL, :], in0=rt[:, LG, :], scalar1=m8[:, 3:4], scalar2=None,
                                                          op0=ALU.is_ge), reads=[r_rt], writes=[r_rt])
                    t.op("dve", lambda e: e.tensor_scalar(out=m8[:, 8:9], in0=m8[:, 0:1], scalar1=-1.0, scalar2=None,
                                                          op0=ALU.mult), reads=[r_rt], writes=[r_rt])
                    t.op("act", lambda e: e.activation(out=rt[:, EX, :], in_=rt[:, LG, :], func=AF.Exp, bias=m8[:, 8:9], scale=1.0),
                         reads=[r_rt], writes=[r_rt])
                    t.op("dve", lambda e: e.tensor_tensor(out=rt[:, EX, :], in0=rt[:, EX, :], in1=rt[:, SEL, :], op=ALU.mult),
                         reads=[r_rt], writes=[r_rt])
                    t.op("dve", lambda e: e.reduce_sum(out=m8[:, 9:10], in_=rt[:, EX, :], axis=AX.X), reads=[r_rt], writes=[r_rt])
                    t.op("dve", lambda e: e.reciprocal(out=m8[:, 10:11], in_=m8[:, 9:10]), reads=[r_rt], writes=[r_rt])
                    t.op("dve", lambda e, tt=tt: e.tensor_scalar(out=gate[:, tt, :], in0=rt[:, EX, :], scalar1=m8[:, 10:11],
                                                                 scalar2=None, op0=ALU.mult), reads=[r_rt], writes=[r_g])
                    t.op("pe", lambda e, tt=tt: e.transpose(PS[3][0:NEXP, 128:256], gate[:, tt, :], self.identf[:]),
                         reads=[r_g, self.r_const], writes=[PR[3]])
                    t.op("dve", lambda e: e.tensor_copy(out=gT[:], in_=PS[3][0:NEXP, 128:256]), reads=[PR[3]], writes=[r_rt])
                    for cg in range(2):
                        t.op("pe", lambda e, cg=cg: e.matmul(PS[4 + cg][:, :], lhsT=gT[:, :], rhs=b2w[:, cg * 512:(cg + 1) * 512],
                                                            start=True, stop=True), reads=[r_rt, r_c], writes=[PR[4 + cg]])
                        t.op("dve", lambda e, cg=cg, tt=tt: e.tensor_copy(out=yacc[:, tt, cg * 512:(cg + 1) * 512], in_=PS[4 + cg][:, :]),
                             reads=[PR[4 + cg]], writes=[r_y])

                def stageA(e_, mid=None):
                    ab_ = e_ % 2
                    for p in range(8):
                        if p == 6 and mid is not None:
                            mid()
                        wb_ = w1cnt[0] % 5
                        w1cnt[0] += 1
                        t.dma("pool", w1p[wb_][:], self.I("w_e1")[e_, :, p * 256:(p + 1) * 256].rearrange("(c q) n -> q c n", q=128),
                              writes=[r_w1[wb_]])
                        for tg in range(ntg):
                            tsl = slice(tg * TG, (tg + 1) * TG)
                            pg, pl = (0, 1) if (p * ntg + tg) % 2 == 0 else (2, 3)

                            def mm(e, wb_=wb_, tsl=tsl, pg=pg, pl=pl):
                                for two, pb in ((0, pg), (1, pl)):
                                    for c in range(8):
                                        ins = e.matmul(PS[pb][:, 0:TG], lhsT=w1p[wb_][:, c, two::2], rhs=vT[:, c, tsl],
                                                       start=(c == 0), stop=(c == 7))
                                return ins
                            t.op("pe", mm, reads=[r_w1[wb_], r_vT], writes=[PR[pg], PR[pl]])
                            k = elc[0] % 2
                            elc[0] += 1
                            t.op("dve", lambda e, k=k, pg=pg, p=p, e_=e_: e.tensor_scalar(
                                out=glu[k][:], in0=PS[pg][:, 0:TG], scalar1=b1g[:, p, e_:e_ + 1], scalar2=SWIGLU_LIMIT,
                                op0=ALU.add, op1=ALU.min), reads=[PR[pg], r_c], writes=[r_elg[k]])
                            t.op("dve", lambda e, k=k, pl=pl, p=p, e_=e_: e.tensor_scalar(
                                out=lin[k][:], in0=PS[pl][:, 0:TG], scalar1=b1l[:, p, e_:e_ + 1], scalar2=1.0 - SWIGLU_LIMIT,
                                op0=ALU.add, op1=ALU.max), reads=[PR[pl], r_c], writes=[r_ell[k]])
                            t.op("act", lambda e, k=k: e.activation(out=sig[k][:], in_=glu[k][:], func=AF.Sigmoid, scale=SWIGLU_ALPHA),
                                 reads=[r_elg[k]], writes=[r_els[k]])
                            t.op("dve", lambda e, k=k: e.scalar_tensor_tensor(
                                out=lin[k][:], in0=lin[k][:], scalar=1.0 + SWIGLU_LIMIT, in1=glu[k][:], op0=ALU.min, op1=ALU.mult),
                                reads=[r_ell[k], r_elg[k]], writes=[r_ell[k]])
                            t.op("dve", lambda e, k=k, ab_=ab_, p=p, tsl=tsl: e.tensor_tensor(
                                out=aT[ab_][:, p, tsl], in0=lin[k][:], in1=sig[k][:], op=ALU.mult),
                                reads=[r_ell[k], r_els[k]], writes=[r_aT[ab_]])

                def stageB(e_):
                    ab_ = e_ % 2
                    for tt in range(ntt):
                        for cg in range(2):
                            pb = 4 + (tt * 2 + cg) % 3

                            def mm(e, tt=tt, cg=cg, pb=pb):
                                for p in range(8):
                                    ins = e.matmul(PS[pb][:, :], lhsT=aT[ab_][:, p, tt * 128:(tt + 1) * 128],
                                                   rhs=w2e[ab_][:, p, cg * 512:(cg + 1) * 512], start=(p == 0), stop=(p == 7))
                                return ins
                            t.op("pe", mm, reads=[r_aT[ab_], r_w2[ab_]], writes=[PR[pb]])
                            t.op("dve", lambda e, tt=tt, cg=cg, pb=pb: e.scalar_tensor_tensor(
                                out=yacc[:, tt, cg * 512:(cg + 1) * 512], in0=PS[pb][:, :], scalar=gate[:, tt, e_:e_ + 1],
                                in1=yacc[:, tt, cg * 512:(cg + 1) * 512], op0=ALU.mult, op1=ALU.add),
                                reads=[PR[pb], r_g, r_y], writes=[r_y])

                def loadw2(e_):
                    ab_ = e_ % 2
                    v = self.I("w_e2")[e_].rearrange("(c q) n -> q c n", q=128)
                    for a in range(0, 1024, 512):
                        t.dma("pool", w2e[ab_][:, :, a:a + 512], v[:, :, a:a + 512], writes=[r_w2[ab_]])
                loadw2(0)
                stageA(0)
                for e_ in range(NEXP):
                    if e_ + 1 < NEXP:
                        stageA(e_ + 1, mid=lambda e_=e_: loadw2(e_ + 1))
                    stageB(e_)
                t.dma("sp", g2[:], self.I("ln2_g").partition_broadcast(128), writes=[r_md])
                t.dma("sp", b2l[:], self.I("ln2_b").partition_broadcast(128), writes=[r_md])
                for tt in range(ntt):
                    b = tt % 2
                    q0 = h0 + tt * 128
                    t.dma("sp", xt[b][:], self.d_x1[q0:q0 + 128, :], writes=[r_xt[b]])
                    t.op("pool", lambda e, tt=tt: e.tensor_tensor(out=yacc[:, tt, :], in0=yacc[:, tt, :], in1=self.gf_bc[:], op=ALU.mult),
                         reads=[r_y, self.r_mod], writes=[r_y])
                    t.op("dve", lambda e, b=b, tt=tt: e.scalar_tensor_tensor(out=vf[:], in0=xt[b][:], scalar=ALPHA, in1=yacc[:, tt, :],
                                                                            op0=ALU.mult, op1=ALU.add),
                         reads=[r_xt[b], r_y], writes=[r_v])
                    self.ln_stats(vf, r_v, stats, mv, r_st)
                    t.op("dve", lambda e, b=b: e.tensor_scalar(out=xt[b][:], in0=vf[:], scalar1=mv[:, 0:1], scalar2=mv[:, 3:4],
                                                               op0=ALU.subtract, op1=ALU.mult),
                         reads=[r_v, r_st], writes=[r_xt[b]])
                    t.op("pool", lambda e, b=b: e.tensor_tensor(out=xt[b][:], in0=xt[b][:], in1=g2[:], op=ALU.mult),
                         reads=[r_xt[b], r_md], writes=[r_xt[b]])
                    t.op("pool", lambda e, b=b: e.tensor_tensor(out=xt[b][:], in0=xt[b][:], in1=b2l[:], op=ALU.add),
                         reads=[r_xt[b], r_md], writes=[r_xt[b]])
                    t.dma("sp", self.out[q0:q0 + 128, :], xt[b][:], reads=[r_xt[b]], writes=[self.r_out])
            self.barrier()


def make_consts(nslot, c):
    ntile = 8 * nslot
    S = 128 * ntile
    slopes = alibi_slopes(8)
    k = np.arange(S)
    tt = k // 128
    pk = k % 128
    ndummy = 7 - c
    kaug = np.zeros((7, S), np.float32)
    kaug[0] = 1.0
    kaug[1] = 1.0
    kaug[2] = 128.0 * tt
    kaug[3] = 1.0
    kaug[4] = 1.0
    kaug[5] = pk
    kaug[6] = (tt < ndummy).astype(np.float32)
    qaug = np.zeros((7, nslot, 8, 128), np.float32)
    ql = np.arange(128)
    for j in range(nslot):
        tq = 8 * j + 7
        for h in range(8):
            s = slopes[h]
            qaug[2, j, h] = s
            qaug[3, j, h] = -s * 128.0 * tq
            qaug[4, j, h] = -s * ql
            qaug[5, j, h] = s
            qaug[6, j, h] = NEG
    kk = np.arange(128)[:, None]
    qq = np.arange(128)[None, :]
    cend = (qq // 64 + 1) * 64
    dg = np.zeros((128, 8, 128), np.float32)
    for h in range(8):
        s = slopes[h]
        m = np.where(kk > qq, -2.0 * s * (kk - qq), 0.0)
        m = np.where(kk >= cend, NEG, m)
        dg[:, h, :] = m
    dmask = np.where(kk.T >= 0, 0.0, 0.0) * 0.0
    qq2 = np.arange(128)[:, None]
    kk2 = np.arange(128)[None, :]
    dmask = np.where(kk2 < (qq2 // 64 + 1) * 64, 0.0, -1e9).astype(np.float32)
    dummy = np.zeros((128, 8), np.float32)
    dummy[:, :ndummy] = -1e9
    iota = np.broadcast_to(np.arange(1, 513, dtype=np.float32)[None, :], (128, 512)).copy()
    slopetab = np.zeros((2, 8, 128), np.float32)
    for h in range(8):
        slopetab[0, h] = slopes[h] * 128.0
        slopetab[1, h] = slopes[h]
    return {
        "c_identb": np.eye(128, dtype=np.float32).astype(NPBF),
        "c_identf": np.eye(128, dtype=np.float32),
        "c_kaug": kaug.astype(NPBF),
        "c_qaug": qaug.astype(NPBF),
        "c_dg": dg.astype(NPBF),
        "c_dmask": dmask,
        "c_dummy": dummy,
        "c_iota": iota,
        "c_slopetab": slopetab,
        "c_pidx1": np.arange(1, 129, dtype=np.float32).reshape(128, 1),
    }


def make_in_maps(inputs, nslot, used=None):
    S = 128 * 8 * nslot
    f = lambda a: np.ascontiguousarray(np.asarray(a, dtype=np.float32))
    x = f(inputs["x"])[0]
    assert x.shape[0] == S
    shared = {
        "c": f(inputs["c"])[0],
        "w_ada": f(inputs["w_ada"])[0],
        "b_ada": f(inputs["b_ada"])[0],
        "w_in": f(inputs["w_in"])[0],
        "lamv": np.stack([f(inputs[k])[0] for k in ("lam_q1", "lam_k1", "lam_q2", "lam_k2")]),
        "diff_norm_g": f(inputs["diff_norm_g"])[0],
        "w_branch_a": f(inputs["w_branch_a"])[0],
        "w_branch_b": f(inputs["w_branch_b"])[0],
        "w_out": f(inputs["w_out"])[0],
        "ln1_g": f(inputs["ln1_g"])[0],
        "ln1_b": f(inputs["ln1_b"])[0],
        "w_router": f(inputs["w_router"])[0],
        "b_router": f(inputs["b_router"])[0],
        "w_e1": f(inputs["w_e1"])[0],
        "b_e1": f(inputs["b_e1"])[0],
        "w_e2": f(inputs["w_e2"])[0],
        "b_e2": f(inputs["b_e2"])[0],
        "ln2_g": f(inputs["ln2_g"])[0],
        "ln2_b": f(inputs["ln2_b"])[0],
    }
    maps = []
    for c in range(NCORE):
        m = dict(shared)
        m["x"] = np.ascontiguousarray(np.roll(x, 128 * (7 - c), axis=0))
        m.update(make_consts(nslot, c))
        maps.append({k: v for k, v in m.items() if used is None or k in used})
    return maps


_CACHE = {}


def run(inputs, nslot, debug=False, phases=99, trace=False):
    key = (nslot, debug, phases)
    mk = MK(nslot, debug=debug, phases=phases)
    nc = mk.build()
    in_maps = make_in_maps(inputs, nslot, used=set(mk.in_aps.keys()))
    res = run_bass_kernel_spmd(nc, in_maps, core_ids=list(range(NCORE)), trace=trace)
    return res


def kernel(**inputs):
    nslot = 16
    res = run(inputs, nslot)
    S = 128 * 8 * nslot
    out = np.zeros((1, S, D), np.float32)
    for c in range(NCORE):
        o = np.asarray(res.results[c]["out"], dtype=np.float32)
        for j in range(nslot):
            rt = 8 * j + c
            out[0, rt * 128:(rt + 1) * 128, :] = o[j * 128:(j + 1) * 128, :]
    return out
```

```python
import os
import numpy as np
import ml_dtypes
from contextlib import ExitStack
import concourse.bass as bass
import concourse.mybir as mybir
from concourse.bass_utils import run_bass_kernel_spmd

F32 = mybir.dt.float32
BF16 = mybir.dt.bfloat16
I32 = mybir.dt.int32
AF = mybir.ActivationFunctionType
ALU = mybir.AluOpType
AX = mybir.AxisListType
NPBF = ml_dtypes.bfloat16

D = 1024
NCORE = 8
NEXP = 32
DFF = 1024
TOPK = 256
LN_EPS = 1e-5
ALPHA = 2.0 ** 0.25
LAM_INIT = 0.2
NEG = -30000.0
NBISECT = 24
SWIGLU_ALPHA = 1.702
SWIGLU_LIMIT = 7.0
C_QA, C_KA, C_VA, C_QB, C_KB, C_VB, C_QI, C_KI, C_WI, C_GA, C_GB = (
    0, 1024, 2048, 3072, 4096, 5120, 6144, 7168, 7232, 7248, 8272)
PROJ_W = 9296


class Res:
    __slots__ = ("w", "r", "name")

    def __init__(self, name=""):
        self.w = None
        self.r = {}
        self.name = name


class Eng:
    def __init__(self, name, eng, sem):
        self.name = name
        self.eng = eng
        self.sem = sem
        self.cnt = 0
        self.seen = {}


class Trk:
    def __init__(self, nc, es):
        self.nc = nc
        mk = lambda n: es.enter_context(nc.semaphore(n))
        self.E = {n: Eng(n, e, mk("s_" + n)) for n, e in [
            ("pe", nc.tensor), ("act", nc.scalar), ("dve", nc.vector),
            ("pool", nc.gpsimd), ("sp", nc.sync)]}
        self.dsems = {q: [[mk(f"d_{q}{i}"), 0] for i in range(n)]
                      for q, n in [("sp", 16), ("pool", 10), ("act", 4)]}
        self.dnext = {q: 0 for q in self.dsems}
        self.nwait = 0

    def _waits(self, E, reads, writes):
        need = {}

        def add(tok, raw):
            if tok is None:
                return
            sem, val = tok
            if sem is E.sem and E.name == "pe":
                return
            k = id(sem)
            if k not in need or need[k][1] < val:
                need[k] = (sem, val)
        for r in reads:
            add(r.w, True)
        for w in writes:
            add(w.w, False)
            for tok in w.r.values():
                add(tok, False)
        for k, (sem, val) in need.items():
            if E.seen.get(k, 0) < val:
                E.eng.wait_ge(sem, val)
                E.seen[k] = val
                self.nwait += 1

    @staticmethod
    def _mark(tok, reads, writes):
        k = id(tok[0])
        for r in reads:
            r.r[k] = tok
        for w in writes:
            w.w = tok
            w.r = {}

    def op(self, en, fn, reads=(), writes=()):
        E = self.E[en]
        self._waits(E, reads, writes)
        ins = fn(E.eng)
        E.cnt += 1
        ins.then_inc(E.sem, 1)
        self._mark((E.sem, E.cnt), reads, writes)

    def dma(self, q, out, in_, reads=(), writes=(), **kw):
        E = self.E[q]
        self._waits(E, reads, writes)
        slots = self.dsems[q]
        i = self.dnext[q]
        self.dnext[q] = (i + 1) % len(slots)
        sem, val = slots[i]
        k = id(sem)
        if val > 0 and E.seen.get(k, 0) < val:
            E.eng.wait_ge(sem, val)
            E.seen[k] = val
        ins = E.eng.dma_start(out=out, in_=in_, **kw)
        val += 16
        slots[i][1] = val
        ins.then_inc(sem, 16)
        self._mark((sem, val), reads, writes)

    def barrier(self, all_res):
        toks = {}
        for r in all_res:
            for tok in [r.w] + list(r.r.values()):
                if tok is None:
                    continue
                k = id(tok[0])
                if k not in toks or toks[k][1] < tok[1]:
                    toks[k] = tok
        for E in self.E.values():
            for k, (sem, val) in toks.items():
                if sem is E.sem:
                    continue
                if E.seen.get(k, 0) < val:
                    E.eng.wait_ge(sem, val)
                    E.seen[k] = val


def alibi_slopes(n=8):
    return [2.0 ** (-8.0 * (h + 1) / n) for h in range(n)]


class MK:
    def __init__(self, nslot, debug=False, phases=99):
        self.nslot = nslot
        self.ntile = 8 * nslot
        self.S = 128 * self.ntile
        self.NQ = 128 * nslot
        self.debug = debug
        self.phases = phases
        self.nc = bass.Bass("TRN2", target_bir_lowering=False)
        self.res_all = []

    def R(self, name=""):
        r = Res(name)
        self.res_all.append(r)
        return r

    def I(self, name):
        if name not in self.in_aps:
            shape, dt = self.in_specs[name]
            self.in_aps[name] = self.nc.dram_tensor(name, list(shape), dt, kind="ExternalInput").ap()
        return self.in_aps[name]

    def dscr(self, name, shape, dt):
        kind = "ExternalOutput" if self.debug else "Internal"
        t = self.nc.dram_tensor(name, list(shape), dt, kind=kind).ap()
        return t

    def sb(self, es, name, shape, dt):
        return es.enter_context(self.nc.sbuf_tensor(name, list(shape), dt))

    def barrier(self):
        self.t.barrier(self.res_all)

    def build(self):
        nc = self.nc
        S, NQ, nslot, ntile = self.S, self.NQ, self.nslot, self.ntile
        self.in_specs = {
            "x": ([S, D], F32), "c": ([D], F32), "w_ada": ([D, 6 * D], F32), "b_ada": ([6 * D], F32),
            "w_in": ([D, PROJ_W], F32), "lamv": ([4, 64], F32), "diff_norm_g": ([128], F32),
            "w_branch_a": ([D, D], F32), "w_branch_b": ([D, D], F32), "w_out": ([D, D], F32),
            "ln1_g": ([D], F32), "ln1_b": ([D], F32), "w_router": ([D, NEXP], F32), "b_router": ([NEXP], F32),
            "w_e1": ([NEXP, D, 2 * DFF], F32), "b_e1": ([NEXP, 2 * DFF], F32),
            "w_e2": ([NEXP, DFF, D], F32), "b_e2": ([NEXP, D], F32), "ln2_g": ([D], F32), "ln2_b": ([D], F32),
            "c_identb": ([128, 128], BF16), "c_identf": ([128, 128], F32), "c_kaug": ([7, S], BF16),
            "c_qaug": ([7, nslot, 8, 128], BF16), "c_dg": ([128, 8, 128], BF16), "c_dmask": ([128, 128], F32),
            "c_dummy": ([128, 8], F32), "c_iota": ([128, 512], F32), "c_slopetab": ([2, 8, 128], F32),
            "c_pidx1": ([128, 1], F32),
        }
        self.in_aps = {}
        self.out = nc.dram_tensor("out", [NQ, D], F32, kind="ExternalOutput").ap()
        self.d_mod = self.dscr("d_mod", [6 * D], F32)
        self.d_kat = self.dscr("d_kat", [16, 64, S], BF16)
        self.d_va = self.dscr("d_va", [S, 8, 132], BF16)
        self.d_kbt = self.dscr("d_kbt", [8, 128, S], BF16)
        self.d_vb = self.dscr("d_vb", [S, 8, 132], BF16)
        self.d_kit = self.dscr("d_kit", [64, S], BF16)
        self.d_qat = self.dscr("d_qat", [16, 64, NQ], BF16)
        self.d_qbt = self.dscr("d_qbt", [8, 128, NQ], BF16)
        self.d_qit = self.dscr("d_qit", [16, 64, NQ], BF16)
        self.d_sgn = self.dscr("d_sgn", [NQ, 16], F32)
        self.d_gate = self.dscr("d_gate", [NQ, 2 * D], F32)
        self.d_ya = self.dscr("d_ya", [NQ, D], BF16)
        self.d_yb = self.dscr("d_yb", [NQ, D], BF16)
        self.d_x1 = self.dscr("d_x1", [NQ, D], F32)

        with ExitStack() as es:
            self.t = Trk(nc, es)
            self.ps = [es.enter_context(nc.psum_tensor(f"ps{i}", [128, 512], F32)) for i in range(8)]
            self.psr = [self.R(f"ps{i}") for i in range(8)]
            self.identb = self.sb(es, "identb", [128, 128], BF16)
            self.identf = self.sb(es, "identf", [128, 128], F32)
            self.r_const = self.R("const")
            self.t.dma("sp", self.identb[:], self.I("c_identb"), writes=[self.r_const])
            self.t.dma("sp", self.identf[:], self.I("c_identf"), writes=[self.r_const])
            self.modT = self.sb(es, "modT", [128, 48], F32)
            self.r_mod = self.R("mod")
            self.r_out = self.R("out")
            self.phase0()
            self.barrier()
            if self.phases >= 1:
                self.phase1a()
                self.barrier()
                self.phase1b()
                self.barrier()
            if self.phases >= 2:
                self.phase2()
                self.barrier()
            if self.phases >= 3:
                self.phase3()
                self.barrier()
            if self.phases >= 4:
                self.phase4()
            self.final_wait()
        return nc

    def final_wait(self):
        self.barrier()

    def phase0(self):
        nc, t = self.nc, self.t
        with ExitStack() as es:
            cT = self.sb(es, "cT", [128, 8], F32)
            cact = self.sb(es, "cact", [128, 8], F32)
            wbuf = [self.sb(es, f"wada{i}", [128, 8, 512], F32) for i in range(2)]
            wr = [self.R() for _ in range(2)]
            brow = self.sb(es, "brow", [1, 6 * D], F32)
            mrow = self.sb(es, "mrow", [1, 6 * D], F32)
            r_c, r_b, r_m = self.R(), self.R(), self.R()
            t.dma("sp", cT[:], self.I("c").rearrange("(c p) -> p c", p=128), writes=[r_c],
                  allow_slow_non_contiguous=True)
            t.dma("sp", brow[:], self.I("b_ada").rearrange("(o n) -> o n", o=1), writes=[r_b])
            t.op("act", lambda e: e.activation(out=cact[:], in_=cT[:], func=AF.Silu),
                 reads=[r_c], writes=[r_c])
            wv = self.I("w_ada").rearrange("(c p) n -> p c n", p=128)
            for g in range(12):
                b = g % 2
                t.dma("sp", wbuf[b][:], wv[:, :, g * 512:(g + 1) * 512], writes=[wr[b]])
                pb = g % 2

                def mm(e, b=b, pb=pb):
                    for c in range(8):
                        ins = e.matmul(self.ps[pb][0:1, :], lhsT=cact[:, c:c + 1], rhs=wbuf[b][:, c, :],
                                       start=(c == 0), stop=(c == 7))
                    return ins
                t.op("pe", mm, reads=[r_c, wr[b]], writes=[self.psr[pb]])
                t.op("dve", lambda e, g=g, pb=pb: e.tensor_tensor(
                    out=mrow[:, g * 512:(g + 1) * 512], in0=self.ps[pb][0:1, :],
                    in1=brow[:, g * 512:(g + 1) * 512], op=ALU.add),
                    reads=[self.psr[pb], r_b], writes=[r_m])
            r_d = self.R()
            t.dma("sp", self.d_mod.rearrange("(o n) -> o n", o=1), mrow[:], reads=[r_m], writes=[r_d])
            t.dma("sp", self.modT[:], self.d_mod.rearrange("(m p) -> p m", p=128), reads=[r_d],
                  writes=[self.r_mod], allow_slow_non_contiguous=True)
            self.r_dmod = r_d
            self.barrier()

    def ln_pre(self, xin_ap, xt, r_xt, xn, r_xn, stats, mv, r_st, q="sp"):
        t = self.t
        t.dma(q, xt[:], xin_ap, writes=[r_xt])
        for hh in range(2):
            t.op("dve", lambda e, hh=hh: e.bn_stats(out=stats[:, hh, :], in_=xt[:, hh * 512:(hh + 1) * 512]),
                 reads=[r_xt], writes=[r_st])
        t.op("dve", lambda e: e.bn_aggr(out=mv[:, 0:2], in_=stats[:].rearrange("p a b -> p (a b)")),
             reads=[r_st], writes=[r_st])
        t.op("dve", lambda e: e.tensor_scalar(out=mv[:, 2:3], in0=mv[:, 1:2], scalar1=LN_EPS, scalar2=None,
                                              op0=ALU.add), reads=[r_st], writes=[r_st])
        t.op("act", lambda e: e.activation(out=mv[:, 2:3], in_=mv[:, 2:3], func=AF.Sqrt),
             reads=[r_st], writes=[r_st])
        t.op("dve", lambda e: e.reciprocal(out=mv[:, 3:4], in_=mv[:, 2:3]), reads=[r_st], writes=[r_st])
        t.op("dve", lambda e: e.tensor_scalar(out=xn[:], in0=xt[:], scalar1=mv[:, 0:1], scalar2=mv[:, 3:4],
                                              op0=ALU.subtract, op1=ALU.mult),
             reads=[r_xt, r_st], writes=[r_xn])

    def ln_post(self, xn, r_xn, out_xnT, r_out, pbank):
        t = self.t
        pbf = self.ps[pbank].bitcast(BF16)

        def tr(e):
            for c in range(8):
                ins = e.transpose(pbf[:, c * 128:(c + 1) * 128], xn[:, c * 128:(c + 1) * 128], self.identb[:])
            return ins
        t.op("pe", tr, reads=[r_xn, self.r_const], writes=[self.psr[pbank]])
        t.op("act", lambda e: e.copy(out=out_xnT, in_=pbf[:, :].rearrange("p (c n) -> p c n", c=8)),
             reads=[self.psr[pbank]], writes=[r_out])

    def ln_tile(self, xin_ap, xt, r_xt, xn, r_xn, stats, mv, r_st, out_xnT, r_out, pbank, q="sp"):
        self.ln_pre(xin_ap, xt, r_xt, xn, r_xn, stats, mv, r_st, q=q)
        self.ln_post(xn, r_xn, out_xnT, r_out, pbank)

    def prep_w(self, es, tag, colranges, sc_off, sh_off):
        nc, t = self.nc, self.t
        ncols = sum(l for _, l in colranges)
        wsb = self.sb(es, "w_" + tag, [128, 8, ncols], BF16)
        r_w = self.R()
        wv = self.I("w_in").rearrange("(c p) n -> p c n", p=128)
        o = 0
        for (s0, l) in colranges:
            for a in range(0, l, 512):
                b = min(l, a + 512)
                t.dma("pool", wsb[:, :, o + a:o + b], wv[:, :, s0 + a:s0 + b], writes=[r_w])
            o += l
        nch = (ncols + 127) // 128
        biasT = self.sb(es, "bT_" + tag, [128, nch], F32)
        biasrow = self.sb(es, "br_" + tag, [1, ncols], BF16)
        onep = self.sb(es, "onep_" + tag, [128, 8], F32)
        shb = self.sb(es, "shb_" + tag, [128, 8], BF16)
        r_b = self.R()
        t.op("dve", lambda e: e.tensor_scalar(out=onep[:], in0=self.modT[:, sc_off:sc_off + 8], scalar1=1.0,
                                              scalar2=None, op0=ALU.add), reads=[self.r_mod], writes=[r_b])
        t.op("dve", lambda e: e.tensor_copy(out=shb[:], in_=self.modT[:, sh_off:sh_off + 8]),
             reads=[self.r_mod], writes=[r_b])
        for ch in range(nch):
            w0 = ch * 128
            wl = min(128, ncols - w0)
            pb = ch % 2

            def mm(e, w0=w0, wl=wl, pb=pb):
                for c in range(8):
                    ins = e.matmul(self.ps[pb][0:wl, 0:1], lhsT=wsb[:, c, w0:w0 + wl], rhs=shb[:, c:c + 1],
                                   start=(c == 0), stop=(c == 7))
                return ins
            t.op("pe", mm, reads=[r_w, r_b], writes=[self.psr[pb]])
            t.op("dve", lambda e, ch=ch, wl=wl, pb=pb: e.tensor_copy(out=biasT[0:wl, ch:ch + 1],
                                                                      in_=self.ps[pb][0:wl, 0:1]),
                 reads=[self.psr[pb]], writes=[r_b])
        for a in range(0, ncols, 512):
            b = min(ncols, a + 512)
            pb = 2 + (a // 512) % 2

            def mm2(e, a=a, b=b, pb=pb):
                for c in range(8):
                    ins = e.matmul(self.ps[pb][0:1, 0:b - a], lhsT=shb[:, c:c + 1], rhs=wsb[:, c, a:b],
                                   start=(c == 0), stop=(c == 7))
                return ins
            t.op("pe", mm2, reads=[r_w, r_b], writes=[self.psr[pb]])
            t.op("dve", lambda e, a=a, b=b, pb=pb: e.tensor_copy(out=biasrow[:, a:b], in_=self.ps[pb][0:1, 0:b - a]),
                 reads=[self.psr[pb]], writes=[r_b])
        for c in range(8):
            en = "dve"
            t.op(en, lambda e, c=c: e.tensor_scalar(out=wsb[:, c, :], in0=wsb[:, c, :], scalar1=onep[:, c:c + 1],
                                                     scalar2=None, op0=ALU.mult),
                 reads=[r_w, r_b], writes=[r_w])
        return wsb, r_w, biasT, biasrow, r_b

    def phase1a(self):
        nc, t = self.nc, self.t
        S = self.S
        with ExitStack() as es:
            wsb, r_w, biasT, biasrow, r_b = self.prep_w(
                es, "p1a", [(C_KA, 1024), (C_KB, 1024), (C_KI, 64), (C_VA, 1024), (C_VB, 1024)], 8, 0)
            VOFF = 2112
            ones = self.sb(es, "ones1", [1, 128], BF16)
            r_ones = self.R()
            t.op("dve", lambda e: e.memset(ones[:], 1.0), writes=[r_ones])
            xt = [self.sb(es, f"xt{i}", [128, D], F32) for i in range(2)]
            r_xt = [self.R() for _ in range(2)]
            xn = [self.sb(es, f"xn{i}", [128, D], BF16) for i in range(2)]
            r_xn = [self.R() for _ in range(2)]
            stats = [self.sb(es, f"st{i}", [128, 2, 6], F32) for i in range(2)]
            mv = [self.sb(es, f"mv{i}", [128, 4], F32) for i in range(2)]
            r_st = [self.R() for _ in range(2)]
            xnT = [self.sb(es, f"xnT{i}", [128, 8, 512], BF16) for i in range(2)]
            r_xnT = [self.R() for _ in range(2)]
            kst = [self.sb(es, f"kst{i}", [128, 512], BF16) for i in range(4)]
            r_kst = [self.R() for _ in range(4)]
            vst = [self.sb(es, f"vst{i}", [128, 4, 132], BF16) for i in range(4)]
            r_vst = [self.R() for _ in range(4)]
            for i in range(4):
                t.op("pool", lambda e, i=i: e.memset(vst[i][:, :, 128:132], 0.0), writes=[r_vst[i]])
                t.op("pool", lambda e, i=i: e.memset(vst[i][:, :, 128:129], 1.0), writes=[r_vst[i]])
            ngrp = S // 512
            ki = 0
            vi = 0
            tcount = 0
            ev = 0
            xn4 = [self.sb(es, f"xn4_{i}", [128, D], BF16) for i in range(8)]
            r_xn4 = [self.R() for _ in range(8)]

            def ln_group_pre(g):
                for tt in range(4):
                    b = tcnt[0] % 2
                    tcnt[0] += 1
                    k = (g % 2) * 4 + tt
                    tok0 = g * 512 + tt * 128
                    self.ln_pre(self.I("x")[tok0:tok0 + 128, :], xt[b], r_xt[b], xn4[k], r_xn4[k], stats[b], mv[b], r_st[b])

            def ln_group_post(g):
                gb = g % 2
                for tt in range(4):
                    k = (g % 2) * 4 + tt
                    self.ln_post(xn4[k], r_xn4[k], xnT[gb][:, :, tt * 128:(tt + 1) * 128], r_xnT[gb], pbank=tt % 2)
            tcnt = [0]
            ln_group_pre(0)
            ln_group_post(0)
            for g in range(ngrp):
                gb = g % 2
                if g + 1 < ngrp:
                    ln_group_pre(g + 1)
                for ch in range(17):
                    wl = 128 if ch < 16 else 64
                    pb = 2 + ch % 3

                    def mm(e, ch=ch, wl=wl, pb=pb, gb=gb):
                        for c in range(8):
                            ins = e.matmul(self.ps[pb][0:wl, :], lhsT=wsb[:, c, ch * 128:ch * 128 + wl],
                                           rhs=xnT[gb][:, c, :], start=(c == 0), stop=(c == 7))
                        return ins
                    t.op("pe", mm, reads=[r_w, r_xnT[gb]], writes=[self.psr[pb]])
                    s = ki % 4
                    ki += 1
                    en = "act" if ev % 4 != 3 else "dve"
                    ev += 1
                    if en == "act":
                        t.op("act", lambda e, s=s, wl=wl, pb=pb, ch=ch: e.activation(
                            out=kst[s][0:wl, :], in_=self.ps[pb][0:wl, :], func=AF.Identity,
                            bias=biasT[0:wl, ch:ch + 1], scale=1.0),
                            reads=[self.psr[pb], r_b], writes=[r_kst[s]])
                    else:
                        t.op("dve", lambda e, s=s, wl=wl, pb=pb, ch=ch: e.tensor_scalar(
                            out=kst[s][0:wl, :], in0=self.ps[pb][0:wl, :], scalar1=biasT[0:wl, ch:ch + 1],
                            scalar2=None, op0=ALU.add),
                            reads=[self.psr[pb], r_b], writes=[r_kst[s]])
                    tsl = slice(g * 512, (g + 1) * 512)
                    if ch < 8:
                        t.dma("sp", self.d_kat[2 * ch:2 * ch + 2, :, tsl].rearrange("m d n -> (m d) n"),
                              kst[s][:, :], reads=[r_kst[s]])
                    elif ch < 16:
                        t.dma("sp", self.d_kbt[ch - 8, :, tsl], kst[s][:, :], reads=[r_kst[s]])
                    else:
                        t.dma("sp", self.d_kit[:, tsl], kst[s][0:64, :], reads=[r_kst[s]])
                if g + 1 < ngrp:
                    ln_group_post(g + 1)
                for tt in range(4):
                    for vg in range(4):
                        pb = 5 + (tt * 4 + vg) % 3
                        c0 = VOFF + vg * 512

                        def mm(e, tt=tt, c0=c0, pb=pb, gb=gb):
                            for c in range(8):
                                e.matmul(self.ps[pb][:, :], lhsT=xnT[gb][:, c, tt * 128:(tt + 1) * 128],
                                         rhs=wsb[:, c, c0:c0 + 512], start=(c == 0), stop=False)
                            return e.matmul(self.ps[pb][:, :], lhsT=ones[0:1, :], rhs=biasrow[0:1, c0:c0 + 512],
                                            start=False, stop=True)
                        t.op("pe", mm, reads=[r_w, r_xnT[gb], r_b, r_ones], writes=[self.psr[pb]])
                        s = vi % 4
                        vi += 1
                        en = "act" if ev % 4 != 3 else "dve"
                        ev += 1
                        psv = self.ps[pb][:, :].rearrange("p (h e) -> p h e", h=4)
                        if en == "act":
                            t.op("act", lambda e, s=s, psv=psv: e.copy(out=vst[s][:, :, 0:128], in_=psv),
                                 reads=[self.psr[pb]], writes=[r_vst[s]])
                        else:
                            t.op("dve", lambda e, s=s, psv=psv: e.tensor_copy(out=vst[s][:, :, 0:128], in_=psv),
                                 reads=[self.psr[pb]], writes=[r_vst[s]])
                        tok0 = g * 512 + tt * 128
                        dst = self.d_va if vg < 2 else self.d_vb
                        t.dma("sp", dst[tok0:tok0 + 128, (vg % 2) * 4:(vg % 2) * 4 + 4, :], vst[s][:],
                              reads=[r_vst[s]])
            self.barrier()

    def phase1b(self):
        nc, t = self.nc, self.t
        nslot, NQ = self.nslot, self.NQ
        with ExitStack() as es:
            wsb, r_w, biasT, biasrow, r_b = self.prep_w(
                es, "p1b", [(C_QA, 1024), (C_QB, 1024), (C_QI, 1024), (C_WI, 16), (C_GA, 1024), (C_GB, 1024)], 8, 0)
            O_QI, O_WI, O_GA = 2048, 3072, 3088
            ones = self.sb(es, "ones1b", [1, 128], BF16)
            r_ones = self.R()
            t.op("dve", lambda e: e.memset(ones[:], 1.0), writes=[r_ones])
            xt = [self.sb(es, f"xtb{i}", [128, D], F32) for i in range(2)]
            r_xt = [self.R() for _ in range(2)]
            xn = [self.sb(es, f"xnb{i}", [128, D], BF16) for i in range(2)]
            r_xn = [self.R() for _ in range(2)]
            stats = [self.sb(es, f"stb{i}", [128, 2, 6], F32) for i in range(2)]
            mv = [self.sb(es, f"mvb{i}", [128, 4], F32) for i in range(2)]
            r_st = [self.R() for _ in range(2)]
            G = min(4, nslot)
            xnT = [self.sb(es, f"xnTb{i}", [128, 8, 128 * G], BF16) for i in range(2)]
            r_xnT = [self.R() for _ in range(2)]
            kst = [self.sb(es, f"kstb{i}", [128, 128 * G], BF16) for i in range(4)]
            r_kst = [self.R() for _ in range(4)]
            gst = [self.sb(es, f"gst{i}", [128, 512], F32) for i in range(3)]
            r_gst = [self.R() for _ in range(3)]
            wis = [self.sb(es, f"wis{i}", [128, 3, 16], F32) for i in range(2)]
            r_wis = [self.R() for _ in range(2)]
            qis = [self.sb(es, f"qis{i}", [128, 1024], BF16) for i in range(2)]
            r_qis = [self.R() for _ in range(2)]
            qit = [self.sb(es, f"qit{i}", [128, 8, 128], BF16) for i in range(2)]
            r_qit = [self.R() for _ in range(2)]
            ki = 0
            gi = 0
            tcount = 0
            for g in range(nslot // G):
                gb = g % 2
                NT = 128 * G
                for tt in range(G):
                    b = tcount % 2
                    j = g * G + tt
                    tok0 = (8 * j + 7) * 128
                    self.ln_tile(self.I("x")[tok0:tok0 + 128, :], xt[b], r_xt[b], xn[b], r_xn[b], stats[b], mv[b],
                                 r_st[b], xnT[gb][:, :, tt * 128:(tt + 1) * 128], r_xnT[gb], pbank=b)
                    tcount += 1
                q0 = g * NT
                for ch in range(16):
                    pb = 2 + ch % 3

                    def mm(e, ch=ch, pb=pb, gb=gb, NT=NT):
                        for c in range(8):
                            ins = e.matmul(self.ps[pb][:, 0:NT], lhsT=wsb[:, c, ch * 128:ch * 128 + 128],
                                           rhs=xnT[gb][:, c, :], start=(c == 0), stop=(c == 7))
                        return ins
                    t.op("pe", mm, reads=[r_w, r_xnT[gb]], writes=[self.psr[pb]])
                    s = ki % 4
                    ki += 1
                    scale = 0.125 if ch < 8 else 128.0 ** -0.5
                    t.op("dve", lambda e, s=s, pb=pb, ch=ch, scale=scale, NT=NT: e.tensor_scalar(
                        out=kst[s][:, 0:NT], in0=self.ps[pb][:, 0:NT], scalar1=biasT[:, ch:ch + 1], scalar2=scale,
                        op0=ALU.add, op1=ALU.mult), reads=[self.psr[pb], r_b], writes=[r_kst[s]])
                    if ch < 8:
                        t.dma("sp", self.d_qat[2 * ch:2 * ch + 2, :, q0:q0 + NT].rearrange("m d n -> (m d) n"),
                              kst[s][:, 0:NT], reads=[r_kst[s]])
                    else:
                        t.dma("sp", self.d_qbt[ch - 8, :, q0:q0 + NT], kst[s][:, 0:NT], reads=[r_kst[s]])
                for tt in range(G):
                    j = g * G + tt
                    tq0 = j * 128
                    lhs = lambda c, tt=tt, gb=gb: xnT[gb][:, c, tt * 128:(tt + 1) * 128]
                    wb = j % 2
                    pb = 5

                    def mmw(e, lhs=lhs, pb=pb):
                        for c in range(8):
                            e.matmul(self.ps[pb][:, 0:16], lhsT=lhs(c), rhs=wsb[:, c, O_WI:O_WI + 16],
                                     start=(c == 0), stop=False)
                        return e.matmul(self.ps[pb][:, 0:16], lhsT=ones[0:1, :], rhs=biasrow[0:1, O_WI:O_WI + 16],
                                        start=False, stop=True)
                    t.op("pe", mmw, reads=[r_w, r_xnT[gb], r_b, r_ones], writes=[self.psr[pb]])
                    t.op("dve", lambda e, wb=wb, pb=pb: e.tensor_copy(out=wis[wb][:, 0, :], in_=self.ps[pb][:, 0:16]),
                         reads=[self.psr[pb]], writes=[r_wis[wb]])
                    t.op("act", lambda e, wb=wb: e.activation(out=wis[wb][:, 1, :], in_=wis[wb][:, 0, :],
                                                              func=AF.Abs, scale=1.0 / 32.0),
                         reads=[r_wis[wb]], writes=[r_wis[wb]])
                    t.op("act", lambda e, wb=wb: e.activation(out=wis[wb][:, 2, :], in_=wis[wb][:, 0, :], func=AF.Sign),
                         reads=[r_wis[wb]], writes=[r_wis[wb]])
                    t.dma("sp", self.d_sgn[tq0:tq0 + 128, :], wis[wb][:, 2, :], reads=[r_wis[wb]])
                    for qg in range(2):
                        pb = 6 + qg
                        c0 = O_QI + qg * 512

                        def mmq(e, lhs=lhs, pb=pb, c0=c0):
                            for c in range(8):
                                e.matmul(self.ps[pb][:, :], lhsT=lhs(c), rhs=wsb[:, c, c0:c0 + 512],
                                         start=(c == 0), stop=False)
                            return e.matmul(self.ps[pb][:, :], lhsT=ones[0:1, :], rhs=biasrow[0:1, c0:c0 + 512],
                                            start=False, stop=True)
                        t.op("pe", mmq, reads=[r_w, r_xnT[gb], r_b, r_ones], writes=[self.psr[pb]])
                        t.op("dve", lambda e, wb=wb, pb=pb, qg=qg: e.tensor_tensor(
                            out=qis[wb][:, qg * 512:(qg + 1) * 512].rearrange("p (h d) -> p h d", h=8),
                            in0=self.ps[pb][:, :].rearrange("p (h d) -> p h d", h=8),
                            in1=wis[wb][:, 1, qg * 8:(qg + 1) * 8].unsqueeze(2).broadcast_to([128, 8, 64]),
                            op=ALU.mult), reads=[self.psr[pb], r_wis[wb]], writes=[r_qis[wb]])
                    pbf = self.ps[2 + (j % 3)].bitcast(BF16)

                    def tr(e, wb=wb, pbf=pbf):
                        for c in range(8):
                            ins = e.transpose(pbf[:, c * 128:(c + 1) * 128], qis[wb][:, c * 128:(c + 1) * 128],
                                              self.identb[:])
                        return ins
                    t.op("pe", tr, reads=[r_qis[wb], self.r_const], writes=[self.psr[2 + (j % 3)]])
                    t.op("act", lambda e, wb=wb, pbf=pbf: e.copy(
                        out=qit[wb][:], in_=pbf[:, :].rearrange("p (c n) -> p c n", c=8)),
                        reads=[self.psr[2 + (j % 3)]], writes=[r_qit[wb]])
                    for c in range(8):
                        t.dma("sp", self.d_qit[2 * c:2 * c + 2, :, tq0:tq0 + 128].rearrange("m d n -> (m d) n"),
                              qit[wb][:, c, :], reads=[r_qit[wb]])
                    for gg in range(4):
                        pb = 5 + gg % 3
                        c0 = O_GA + gg * 512

                        def mmg(e, lhs=lhs, pb=pb, c0=c0):
                            for c in range(8):
                                e.matmul(self.ps[pb][:, :], lhsT=lhs(c), rhs=wsb[:, c, c0:c0 + 512],
                                         start=(c == 0), stop=False)
                            return e.matmul(self.ps[pb][:, :], lhsT=ones[0:1, :], rhs=biasrow[0:1, c0:c0 + 512],
                                            start=False, stop=True)
                        t.op("pe", mmg, reads=[r_w, r_xnT[gb], r_b, r_ones], writes=[self.psr[pb]])
                        s = gi % 3
                        gi += 1
                        t.op("act", lambda e, s=s, pb=pb: e.activation(out=gst[s][:], in_=self.ps[pb][:, :],
                                                                        func=AF.Sigmoid),
                             reads=[self.psr[pb]], writes=[r_gst[s]])
                        t.dma("sp", self.d_gate[tq0:tq0 + 128, gg * 512:(gg + 1) * 512], gst[s][:],
                              reads=[r_gst[s]])
            self.barrier()

    def phase2(self):
        nc, t = self.nc, self.t
        STOP = int(os.environ.get('P2STOP', '99'))
        EXP = os.environ.get('EXP', '')
        S, nslot = self.S, self.nslot
        PS = self.ps
        PR = self.psr
        with ExitStack() as es:
            dg = self.sb(es, "dg", [128, 8, 128], BF16)
            dmask = self.sb(es, "dmask", [128, 128], F32)
            dummy = self.sb(es, "dummyc", [128, 8], F32)
            iota = self.sb(es, "iotac", [128, 512], F32)
            slopetab = self.sb(es, "slopetab", [2, 8, 128], F32)
            pidx1 = self.sb(es, "pidx1", [128, 1], F32)
            nlam = self.sb(es, "nlam", [128, 1], F32)
            gbc = self.sb(es, "gbc", [128, 128], F32)
            r_c2 = self.R("c2")
            for dst, nm in [(dg, "c_dg"), (dmask, "c_dmask"), (dummy, "c_dummy"), (iota, "c_iota"),
                            (slopetab, "c_slopetab"), (pidx1, "c_pidx1")]:
                t.dma("sp", dst[:], self.I(nm), writes=[r_c2])
            lv = self.sb(es, "lv", [1, 4, 64], F32)
            lsm = self.sb(es, "lsm", [1, 8], F32)
            onesf = self.sb(es, "onesf", [1, 128], F32)
            r_l = self.R("lam")
            t.dma("sp", lv[:], self.I("lamv").rearrange("(o a) d -> o a d", o=1), writes=[r_l])
            t.op("dve", lambda e: e.memset(onesf[:], 1.0), writes=[r_l])
            t.op("dve", lambda e: e.tensor_tensor(out=lv[:, 0, :], in0=lv[:, 0, :], in1=lv[:, 1, :], op=ALU.mult),
                 reads=[r_l], writes=[r_l])
            t.op("dve", lambda e: e.tensor_tensor(out=lv[:, 2, :], in0=lv[:, 2, :], in1=lv[:, 3, :], op=ALU.mult),
                 reads=[r_l], writes=[r_l])
            t.op("dve", lambda e: e.reduce_sum(out=lsm[:, 0:1], in_=lv[:, 0, :], axis=AX.X), reads=[r_l], writes=[r_l])
            t.op("dve", lambda e: e.reduce_sum(out=lsm[:, 1:2], in_=lv[:, 2, :], axis=AX.X), reads=[r_l], writes=[r_l])
            t.op("act", lambda e: e.activation(out=lsm[:, 2:4], in_=lsm[:, 0:2], func=AF.Exp), reads=[r_l], writes=[r_l])
            t.op("dve", lambda e: e.tensor_tensor(out=lsm[:, 4:5], in0=lsm[:, 3:4], in1=lsm[:, 2:3], op=ALU.subtract),
                 reads=[r_l], writes=[r_l])
            t.op("dve", lambda e: e.tensor_scalar(out=lsm[:, 5:6], in0=lsm[:, 4:5], scalar1=-LAM_INIT, scalar2=None,
                                                  op0=ALU.add), reads=[r_l], writes=[r_l])
            t.op("pe", lambda e: e.matmul(PS[7][:, 0:1], lhsT=onesf[0:1, :], rhs=lsm[0:1, 5:6], start=True, stop=True),
                 reads=[r_l], writes=[PR[7]])
            t.op("dve", lambda e: e.tensor_copy(out=nlam[:], in_=PS[7][:, 0:1]), reads=[PR[7]], writes=[r_c2])
            t.dma("sp", gbc[:], self.I("diff_norm_g").partition_broadcast(128), writes=[r_c2])
            t.op("dve", lambda e: e.tensor_scalar(out=gbc[:], in0=gbc[:], scalar1=1.0 - LAM_INIT, scalar2=None,
                                                  op0=ALU.mult), reads=[r_c2], writes=[r_c2])

            if STOP <= 0:
                self.barrier()
                return
            score = self.sb(es, "score", [128, S], F32)
            maskT = score.bitcast(BF16)
            r_sm = self.R("score")
            mask = self.sb(es, "mask", [128, S], BF16)
            r_mask = self.R("mask")
            qi_sb = self.sb(es, "qi_sb", [128, 8, 128], BF16)
            r_qi = self.R()
            sgn = self.sb(es, "sgn", [128, 16], F32)
            dsg = self.sb(es, "dsg", [128, 16, 128], BF16)
            r_dsg = self.R()
            kit_sb = [self.sb(es, f"kit{i}", [128, 1024], BF16) for i in range(2)]
            r_kit = [self.R() for _ in range(2)]
            rbuf = [self.sb(es, f"rbuf{i}", [128, 512], BF16) for i in range(4)]
            r_rbuf = [self.R() for _ in range(4)]
            sv = self.sb(es, "sv", [128, 48], F32)
            svi = self.sb(es, "svi", [128, 4], I32)
            r_sv = self.R()
            am = self.sb(es, "am", [128, 40], F32)
            r_am = self.R()
            tmp512 = self.sb(es, "tmp512", [128, 512], F32)
            r_tmp = self.R()
            ab = self.sb(es, "ab", [128, 2], BF16)
            qb_sb = self.sb(es, "qb_sb", [128, 8, 128], BF16)
            r_qb = self.R()
            qbaug = self.sb(es, "qbaug", [128, 8, 128], BF16)
            r_qbaug = self.R()
            qa_sb = self.sb(es, "qa_sb", [69, 16, 128], BF16)
            r_qa = self.R()
            NKB = 3
            kbuf = [self.sb(es, f"kbuf{i}", [128, 4, 1024], BF16) for i in range(NKB)]
            r_kbuf = [self.R() for _ in range(NKB)]
            vbuf = [self.sb(es, f"vbuf{i}", [128, 8, 4, 132], BF16) for i in range(NKB)]
            r_vbuf = [self.R() for _ in range(NKB)]
            kaug_sb = [self.sb(es, f"kaug{i}", [128, 1024], BF16) for i in range(NKB)]
            r_kaug = [self.R() for _ in range(NKB)]
            pbuf = [self.sb(es, f"pbuf{i}", [128, 4, 128], BF16) for i in range(5)]
            r_pbuf = [self.R() for _ in range(5)]
            ysb = [self.sb(es, f"ysb{i}", [128, D], BF16) for i in range(2)]
            r_ysb = [self.R() for _ in range(2)]
            junk = self.sb(es, "junk128", [128, 128], F32)
            r_junk = self.R()
            sv2 = self.sb(es, "sv2", [128, 16], F32)
            oraw = self.sb(es, "oraw", [128, D], F32)
            r_oraw = self.R()
            ssq = self.sb(es, "ssq", [128, 16], F32)
            r_ssq = self.R()
            r_sv2 = [self.R() for _ in range(2)]
            for i in range(NKB):
                t.op("pool", lambda e, i=i: e.memset(kaug_sb[i][:], 0.0), writes=[r_kaug[i]])
            t.op("pool", lambda e: e.memset(qbaug[:], 0.0), writes=[r_qbaug])
            kcnt = [0]
            pcnt = [0]
            scnt = [0]
            kitc = [0]
            rcnt = [0]

            for j in range(nslot):
                tq = 8 * j + 7
                NT = tq + 1
                N = NT * 128
                q0 = j * 128
                ngrp = NT // 4
                qv = self.d_qit[:, :, q0:q0 + 128].rearrange("(hp two) d n -> two d hp n", two=2)
                t.dma("sp", qi_sb[0:64, :, :], qv[0], writes=[r_qi])
                t.dma("sp", qi_sb[64:128, :, :], qv[1], writes=[r_qi])
                t.dma("sp", sgn[:], self.d_sgn[q0:q0 + 128, :], writes=[r_dsg])
                for h in range(16):
                    en = "dve"
                    t.op(en, lambda e, h=h: e.tensor_scalar(out=dsg[:, h, :], in0=self.identb[:], scalar1=sgn[:, h:h + 1],
                                                            scalar2=None, op0=ALU.mult),
                         reads=[r_dsg, self.r_const], writes=[r_dsg])
                for kg in range(ngrp):
                    if kg % 2 == 0:
                        kb = kitc[0] % 2
                        kitc[0] += 1
                        w = min(1024, N - kg * 512)
                        t.dma("sp", kit_sb[kb][0:64, 0:w], self.d_kit[:, kg * 512:kg * 512 + w], writes=[r_kit[kb]])
                        t.dma("sp", kit_sb[kb][64:128, 0:w], self.d_kit[:, kg * 512:kg * 512 + w], writes=[r_kit[kb]])
                    koff = (kg % 2) * 512

                    LB = (2, 3, 4, 5)

                    def logits(h, kb=kb, koff=koff):
                        lb = LB[h % 4]
                        p0 = 64 * (h % 2)
                        t.op("pe", lambda e: e.matmul(PS[lb][:, :], lhsT=qi_sb[p0:p0 + 64, h // 2, :],
                                                      rhs=kit_sb[kb][p0:p0 + 64, koff:koff + 512], start=True, stop=True),
                             reads=[r_qi, r_kit[kb]], writes=[PR[lb]])

                    def relu(h):
                        lb = LB[h % 4]
                        rb = rcnt[0] % 4
                        rcnt[0] += 1
                        if h % 2 == 0:
                            t.op("act", lambda e: e.activation(out=rbuf[rb][:], in_=PS[lb][:, :], func=AF.Relu),
                                 reads=[PR[lb]], writes=[r_rbuf[rb]])
                        else:
                            t.op("dve", lambda e: e.tensor_scalar(out=rbuf[rb][:], in0=PS[lb][:, :], scalar1=0.0,
                                                                  scalar2=None, op0=ALU.max),
                                 reads=[PR[lb]], writes=[r_rbuf[rb]])
                        return rb

                    def hsum(h, rb):
                        t.op("pe", lambda e: e.matmul(PS[7][:, :], lhsT=dsg[:, h, :], rhs=rbuf[rb][:],
                                                      start=(h == 0), stop=(h == 15)),
                             reads=[r_dsg, r_rbuf[rb]], writes=[PR[7]])
                    for h in range(4):
                        logits(h)
                    for h in range(0, 16, 2):
                        rb0 = relu(h)
                        rb1 = relu(h + 1)
                        hsum(h, rb0)
                        hsum(h + 1, rb1)
                        if h + 4 < 16:
                            logits(h + 4)
                            logits(h + 5)
                    if "f" in EXP:
                        continue
                    sl = slice(kg * 512, (kg + 1) * 512)
                    if "g" in EXP:
                        pass
                    elif "a" in EXP:
                        t.op("dve", lambda e, kg=kg: e.reduce_max(out=am[:, kg:kg + 1], in_=PS[7][:, :], axis=AX.X),
                             reads=[PR[7]], writes=[r_am])
                    else:
                        t.op("dve", lambda e, kg=kg: e.tensor_reduce(out=am[:, kg:kg + 1], in_=PS[7][:, :], axis=AX.X,
                                                                     op=ALU.max, apply_absolute_value=True),
                             reads=[PR[7]], writes=[r_am])
                    if "h" not in EXP:
                        if "I" not in EXP:
                            t.op("dve", lambda e, sl=sl: e.tensor_copy(out=score[:, sl], in_=PS[7][:, :]),
                                 reads=[PR[7]], writes=[r_sm])
                        else:
                            t.op("act", lambda e, sl=sl: e.activation(out=score[:, sl], in_=PS[7][:, :], func=AF.Identity),
                                 reads=[PR[7]], writes=[r_sm])
                    for tl in range(4):
                        if "c" in EXP:
                            break
                        tt = kg * 4 + tl
                        ts_ = slice(tt * 128, (tt + 1) * 128)
                        if tt < 7:
                            t.op("dve", lambda e, ts_=ts_, tt=tt: e.tensor_scalar(
                                out=score[:, ts_], in0=score[:, ts_], scalar1=dummy[:, tt:tt + 1], scalar2=None,
                                op0=ALU.add), reads=[r_sm, r_c2], writes=[r_sm])
                        if tt == NT - 1:
                            t.op("dve", lambda e, ts_=ts_: e.tensor_tensor(out=score[:, ts_], in0=score[:, ts_],
                                                                           in1=dmask[:], op=ALU.add),
                                 reads=[r_sm, r_c2], writes=[r_sm])
                if STOP <= 1:
                    continue
                LO, W0, MID, CNT, PRED, AMX = 0, 1, 2, 3, 4, 7
                col = lambda i: sv[:, i:i + 1]
                t.op("dve", lambda e: e.reduce_max(out=col(AMX), in_=am[:, 0:ngrp], axis=AX.X), reads=[r_am], writes=[r_sv])
                t.op("dve", lambda e: e.tensor_scalar(out=col(LO), in0=col(AMX), scalar1=1.0, scalar2=-1.0, op0=ALU.add, op1=ALU.mult),
                     reads=[r_sv], writes=[r_sv])
                t.op("dve", lambda e: e.tensor_scalar(out=col(W0), in0=col(AMX), scalar1=1.0, scalar2=2.0, op0=ALU.add, op1=ALU.mult),
                     reads=[r_sv], writes=[r_sv])
                bit = [0]

                def bisect(nit):
                    for it in range(nit):
                        f = 0.5 ** (bit[0] + 1)
                        bit[0] += 1
                        t.op("dve", lambda e, f=f: e.scalar_tensor_tensor(out=col(MID), in0=col(W0), scalar=f, in1=col(LO),
                                                                          op0=ALU.mult, op1=ALU.add), reads=[r_sv], writes=[r_sv])
                        t.op("dve", lambda e: e.tensor_scalar(out=mask[:, 0:N], in0=score[:, 0:N], scalar1=col(MID), scalar2=None,
                                                              op0=ALU.is_gt, op1=ALU.add, accum_out=col(CNT)),
                             reads=[r_sm, r_sv], writes=[r_mask, r_sv])
                        t.op("dve", lambda e, f=f: e.tensor_scalar(out=col(PRED), in0=col(CNT), scalar1=float(TOPK) - 0.5, scalar2=f,
                                                                   op0=ALU.is_gt, op1=ALU.mult), reads=[r_sv], writes=[r_sv])
                        t.op("dve", lambda e: e.scalar_tensor_tensor(out=col(LO), in0=col(W0), scalar=col(PRED), in1=col(LO),
                                                                     op0=ALU.mult, op1=ALU.add), reads=[r_sv], writes=[r_sv])

                t.dma("sp", qb_sb[:], self.d_qbt[:, :, q0:q0 + 128].rearrange("h d n -> d h n"), writes=[r_qb])
                t.dma("sp", qa_sb[0:64, :, :], self.d_qat[:, :, q0:q0 + 128].rearrange("m d n -> d m n"), writes=[r_qa])
                for m_ in range(2):
                    t.dma("sp", qa_sb[64:69, :, :].rearrange("r (h m) n -> r h m n", m=2)[:, :, m_, :],
                          self.I("c_qaug")[2:7, j, :, :], writes=[r_qa])

                def attn_group(kind, gi, ab):
                    b0, b1_ = (2, 3) if ab == 0 else (4, 5)
                    accs = [PS[b0][:, 0:129], PS[b0][:, 129:258], PS[b0][:, 258:387], PS[b1_][:, 0:129]]
                    first_in_bank = [True, False, False, True]
                    RA = [PR[b0], PR[b1_]]
                    pendq = []
                    for tg in range(NT // 8):
                        kb = kcnt[0] % NKB
                        kcnt[0] += 1
                        ksl = slice(tg * 1024, (tg + 1) * 1024)
                        if kind == "dsa":
                            t.dma("sp", kbuf[kb][:, :, :], self.d_kbt[4 * gi:4 * gi + 4, :, ksl].rearrange("h d n -> d h n"),
                                  writes=[r_kbuf[kb]])
                            t.dma("sp", kaug_sb[kb][0:7, :], self.I("c_kaug")[:, ksl], writes=[r_kaug[kb]])
                            t.dma("sp", vbuf[kb][:, :, :, :].rearrange("p t h e -> p t (h e)"),
                                  self.d_vb[ksl, 4 * gi:4 * gi + 4, :].rearrange("(t p) h e -> p t (h e)", p=128),
                                  writes=[r_vbuf[kb]])
                        else:
                            t.dma("sp", kbuf[kb][0:64, :, :], self.d_kat[4 * gi:4 * gi + 4, :, ksl].rearrange("m d n -> d m n"),
                                  writes=[r_kbuf[kb]])
                            t.dma("sp", kbuf[kb][64:69, :, :], self.I("c_kaug")[2:7, ksl].unsqueeze(1).broadcast_to([5, 4, 1024]),
                                  writes=[r_kbuf[kb]])
                            t.dma("sp", vbuf[kb][:, :, 0:2, :].rearrange("p t h e -> p t (h e)"),
                                  self.d_va[ksl, 2 * gi:2 * gi + 2, :].rearrange("(t p) h e -> p t (h e)", p=128),
                                  writes=[r_vbuf[kb]])
                        for tl in range(8):
                            tt = tg * 8 + tl
                            sb_ = (0, 1, 6, 7)[scnt[0] % 4]
                            scnt[0] += 1
                            diag = (tt == NT - 1)

                            def qk(e, kb=kb, tl=tl, sb_=sb_, diag=diag):
                                tsl = slice(tl * 128, (tl + 1) * 128)
                                for i in range(4):
                                    reg = PS[sb_][:, i * 128:(i + 1) * 128]
                                    if kind == "dsa":
                                        ins = e.matmul(reg, lhsT=kbuf[kb][:, i, tsl], rhs=qb_sb[:, 4 * gi + i, :],
                                                       start=(i == 0), stop=False, skip_group_check=True)
                                        hh = 4 * gi + i
                                    else:
                                        ins = e.matmul(reg, lhsT=kbuf[kb][0:69, i, tsl], rhs=qa_sb[0:69, 4 * gi + i, :],
                                                       start=True, stop=not diag)
                                        hh = 2 * gi + i // 2
                                    if diag:
                                        ins = e.matmul(reg, lhsT=self.identb[:], rhs=dg[:, hh, :], start=False,
                                                       stop=(kind != "dsa"), skip_group_check=(kind == "dsa"))
                                if kind == "dsa":
                                    ins = e.matmul(PS[sb_][:, :], lhsT=kaug_sb[kb][:, tsl],
                                                   rhs=qbaug[:, 4 * gi:4 * gi + 4, :].rearrange("r h n -> r (h n)"),
                                                   start=False, stop=True, skip_group_check=True)
                                return ins
                            rd = [r_kbuf[kb], self.r_const, r_c2] + ([r_kaug[kb], r_qbaug, r_qb] if kind == "dsa" else [r_qa])
                            t.op("pe", qk, reads=rd, writes=[PR[sb_]])
                            pb_ = pcnt[0] % 5
                            pcnt[0] += 1
                            t.op("act", lambda e, sb_=sb_, pb_=pb_: e.activation(
                                out=pbuf[pb_][:].rearrange("p h n -> p (h n)"), in_=PS[sb_][:, :], func=AF.Exp),
                                reads=[PR[sb_]], writes=[r_pbuf[pb_]])
                            if kind == "dsa":
                                t.op("dve", lambda e, pb_=pb_, tt=tt: e.scalar_tensor_tensor(
                                    out=pbuf[pb_][:], in0=pbuf[pb_][:], scalar=1e30,
                                    in1=maskT[:, tt * 128:(tt + 1) * 128].unsqueeze(1).broadcast_to([128, 4, 128]),
                                    op0=ALU.min, op1=ALU.mult), reads=[r_pbuf[pb_], r_sm], writes=[r_pbuf[pb_]])

                            def pv(e, kb=kb, tl=tl, pb_=pb_, tt=tt):
                                for i in range(4):
                                    vh = i if kind == "dsa" else i // 2
                                    ins = e.matmul(accs[i], lhsT=pbuf[pb_][:, i, :], rhs=vbuf[kb][:, tl, vh, 0:129],
                                                   start=(tt == 0 and first_in_bank[i]), stop=(tt == NT - 1),
                                                   skip_group_check=True)
                                return ins
                            pendq.append(lambda pv=pv, kb=kb, pb_=pb_: t.op(
                                "pe", pv, reads=[r_pbuf[pb_], r_vbuf[kb]], writes=RA))
                            if len(pendq) > 3:
                                pendq.pop(0)()
                    while pendq:
                        pendq.pop(0)()
                    s0 = 8 * ab
                    if kind == "dsa":
                        for i in range(4):
                            hh = 4 * gi + i
                            t.op("dve", lambda e, i=i: e.reciprocal(out=sv2[:, s0 + i:s0 + i + 1], in_=accs[i][:, 128:129]),
                                 reads=RA, writes=[r_sv2[ab]])
                            t.op("dve", lambda e, i=i, hh=hh: e.tensor_scalar(
                                out=ysb[1][:, hh * 128:(hh + 1) * 128], in0=accs[i][:, 0:128], scalar1=sv2[:, s0 + i:s0 + i + 1],
                                scalar2=None, op0=ALU.mult), reads=RA + [r_sv2[ab]], writes=[r_ysb[1]])
                    else:
                        for hl in range(2):
                            hh = 2 * gi + hl
                            a0, a1 = accs[2 * hl], accs[2 * hl + 1]
                            c0 = s0 + 4 * hl
                            of_ = oraw[:, hh * 128:(hh + 1) * 128]
                            r_of_ = r_oraw
                            t.op("dve", lambda e, a0=a0, c0=c0: e.reciprocal(out=sv2[:, c0:c0 + 1], in_=a0[:, 128:129]),
                                 reads=RA, writes=[r_sv2[ab]])
                            t.op("dve", lambda e, a1=a1, c0=c0: e.reciprocal(out=sv2[:, c0 + 1:c0 + 2], in_=a1[:, 128:129]),
                                 reads=RA, writes=[r_sv2[ab]])
                            t.op("dve", lambda e, c0=c0: e.tensor_tensor(out=sv2[:, c0 + 1:c0 + 2], in0=sv2[:, c0 + 1:c0 + 2],
                                                                         in1=nlam[:], op=ALU.mult),
                                 reads=[r_sv2[ab], r_c2], writes=[r_sv2[ab]])
                            t.op("dve", lambda e, a0=a0, c0=c0, of_=of_: e.tensor_scalar(
                                out=of_, in0=a0[:, 0:128], scalar1=sv2[:, c0:c0 + 1], scalar2=None, op0=ALU.mult),
                                reads=RA + [r_sv2[ab]], writes=[r_of_])
                            t.op("dve", lambda e, a1=a1, c0=c0, of_=of_: e.scalar_tensor_tensor(
                                out=of_, in0=a1[:, 0:128], scalar=sv2[:, c0 + 1:c0 + 2], in1=of_,
                                op0=ALU.mult, op1=ALU.add), reads=RA + [r_sv2[ab], r_of_], writes=[r_of_])

                nb_per = NBISECT // 4
                for gi in range(4):
                    bisect(nb_per)
                    attn_group("diff", gi, gi % 2)
                bisect(NBISECT - 4 * nb_per)
                for hh in range(8):
                    t.op("act", lambda e, hh=hh: e.activation(out=junk[:], in_=oraw[:, hh * 128:(hh + 1) * 128], func=AF.Square,
                                                              accum_out=ssq[:, hh:hh + 1]),
                         reads=[r_oraw], writes=[r_ssq, r_junk])
                t.op("dve", lambda e: e.tensor_scalar(out=ssq[:, 0:8], in0=ssq[:, 0:8], scalar1=1.0 / 128.0, scalar2=LN_EPS,
                                                      op0=ALU.mult, op1=ALU.add), reads=[r_ssq], writes=[r_ssq])
                t.op("act", lambda e: e.activation(out=ssq[:, 0:8], in_=ssq[:, 0:8], func=AF.Sqrt), reads=[r_ssq], writes=[r_ssq])
                t.op("dve", lambda e: e.reciprocal(out=ssq[:, 8:16], in_=ssq[:, 0:8]), reads=[r_ssq], writes=[r_ssq])
                for hh in range(8):
                    t.op("dve", lambda e, hh=hh: e.scalar_tensor_tensor(
                        out=ysb[0][:, hh * 128:(hh + 1) * 128], in0=oraw[:, hh * 128:(hh + 1) * 128], scalar=ssq[:, 8 + hh:9 + hh],
                        in1=gbc[:], op0=ALU.mult, op1=ALU.mult), reads=[r_oraw, r_ssq, r_c2], writes=[r_ysb[0]])
                t.dma("sp", self.d_ya[q0:q0 + 128, :], ysb[0][:], reads=[r_ysb[0]])
                t.op("dve", lambda e: e.tensor_scalar(out=mask[:, 0:N], in0=score[:, 0:N], scalar1=col(LO), scalar2=None,
                                                      op0=ALU.is_gt), reads=[r_sm, r_sv], writes=[r_mask])
                for kg in range(ngrp):
                    t.op("dve", lambda e, kg=kg: e.scalar_tensor_tensor(
                        out=tmp512[:], in0=iota[:], scalar=float(kg * 512), in1=mask[:, kg * 512:(kg + 1) * 512],
                        op0=ALU.add, op1=ALU.mult), reads=[r_mask, r_c2], writes=[r_tmp])
                    t.op("dve", lambda e, kg=kg: e.reduce_max(out=am[:, kg:kg + 1], in_=tmp512[:], axis=AX.X),
                         reads=[r_tmp], writes=[r_am])
                MP, DD, AF_, BF_ = 8, 9, 10, 11
                t.op("dve", lambda e: e.reduce_max(out=col(MP), in_=am[:, 0:ngrp], axis=AX.X), reads=[r_am], writes=[r_sv])
                t.op("dve", lambda e: e.tensor_scalar(out=col(DD), in0=col(MP), scalar1=pidx1[:, 0:1], scalar2=float(128 * tq),
                                                      op0=ALU.subtract, op1=ALU.subtract), reads=[r_sv, r_c2], writes=[r_sv])
                t.op("act", lambda e: e.activation(out=col(DD), in_=col(DD), func=AF.Abs), reads=[r_sv], writes=[r_sv])
                t.op("dve", lambda e: e.tensor_copy(out=svi[:, 0:1], in_=col(DD)), reads=[r_sv], writes=[r_sv])
                t.op("dve", lambda e: e.tensor_single_scalar(out=svi[:, 1:2], in_=svi[:, 0:1], scalar=7,
                                                             op=ALU.arith_shift_right), reads=[r_sv], writes=[r_sv])
                t.op("dve", lambda e: e.tensor_copy(out=col(AF_), in_=svi[:, 1:2]), reads=[r_sv], writes=[r_sv])
                t.op("dve", lambda e: e.scalar_tensor_tensor(out=col(BF_), in0=col(AF_), scalar=-128.0, in1=col(DD),
                                                             op0=ALU.mult, op1=ALU.add), reads=[r_sv], writes=[r_sv])
                t.op("dve", lambda e: e.tensor_copy(out=ab[:, 0:2], in_=sv[:, AF_:AF_ + 2]), reads=[r_sv], writes=[r_sv])
                t.dma("sp", qbaug[2:7, :, :], self.I("c_qaug")[2:7, j, :, :], writes=[r_qbaug])
                t.op("pe", lambda e: e.matmul(PS[7][0:2, 0:128], lhsT=ab[:, 0:2], rhs=self.identb[:], start=True, stop=True),
                     reads=[r_sv, self.r_const], writes=[PR[7]])
                t.op("dve", lambda e: e.tensor_tensor(out=qbaug[0:2, :, :],
                                                      in0=PS[7][0:2, 0:128].unsqueeze(1).broadcast_to([2, 8, 128]),
                                                      in1=slopetab[:], op=ALU.mult),
                     reads=[PR[7], r_c2], writes=[r_qbaug])
                pbf = PS[6].bitcast(BF16)
                for g4 in range(ngrp):
                    def tr(e, g4=g4):
                        for tl in range(4):
                            tt = g4 * 4 + tl
                            ins = e.transpose(pbf[:, tl * 128:(tl + 1) * 128], mask[:, tt * 128:(tt + 1) * 128], self.identb[:])
                        return ins
                    t.op("pe", tr, reads=[r_mask, self.r_const], writes=[PR[6]])
                    if g4 % 2 == 0:
                        t.op("act", lambda e, g4=g4: e.copy(out=maskT[:, g4 * 512:(g4 + 1) * 512], in_=pbf[:, 0:512]),
                             reads=[PR[6]], writes=[r_sm])
                    else:
                        t.op("dve", lambda e, g4=g4: e.tensor_copy(out=maskT[:, g4 * 512:(g4 + 1) * 512], in_=pbf[:, 0:512]),
                             reads=[PR[6]], writes=[r_sm])
                for gi in range(2):
                    attn_group("dsa", gi, gi % 2)
                t.dma("sp", self.d_yb[q0:q0 + 128, :], ysb[1][:], reads=[r_ysb[1]])
            self.barrier()

    def load_w_bf16(self, es, name, src_ap, r):
        wsb = self.sb(es, name, [128, 8, 1024], BF16)
        v = src_ap.rearrange("(c p) n -> p c n", p=128)
        for a in range(0, 1024, 512):
            self.t.dma("pool", wsb[:, :, a:a + 512], v[:, :, a:a + 512], writes=[r])
        return wsb

    def ln_stats(self, xin, r_x, stats, mv, r_st):
        t = self.t
        for hh in range(2):
            t.op("dve", lambda e, hh=hh: e.bn_stats(out=stats[:, hh, :], in_=xin[:, hh * 512:(hh + 1) * 512]),
                 reads=[r_x], writes=[r_st])
        t.op("dve", lambda e: e.bn_aggr(out=mv[:, 0:2], in_=stats[:].rearrange("p a b -> p (a b)")),
             reads=[r_st], writes=[r_st])
        t.op("dve", lambda e: e.tensor_scalar(out=mv[:, 2:3], in0=mv[:, 1:2], scalar1=LN_EPS, scalar2=None,
                                              op0=ALU.add), reads=[r_st], writes=[r_st])
        t.op("act", lambda e: e.activation(out=mv[:, 2:3], in_=mv[:, 2:3], func=AF.Sqrt),
             reads=[r_st], writes=[r_st])
        t.op("dve", lambda e: e.reciprocal(out=mv[:, 3:4], in_=mv[:, 2:3]), reads=[r_st], writes=[r_st])

    def phase3(self):
        nc, t = self.nc, self.t
        PS, PR = self.ps, self.psr
        nslot = self.nslot
        with ExitStack() as es:
            r_w = self.R()
            wa = self.load_w_bf16(es, "wa", self.I("w_branch_a"), r_w)
            wb = self.load_w_bf16(es, "wb", self.I("w_branch_b"), r_w)
            wo = self.load_w_bf16(es, "wo", self.I("w_out"), r_w)
            g1 = self.sb(es, "g1bc", [128, D], F32)
            b1 = self.sb(es, "b1bc", [128, D], F32)
            r_c = self.R()
            self.ga_bc = self.sb(es, "ga_bc", [128, D], F32)
            t.dma("sp", self.ga_bc[:], self.d_mod[2 * D:3 * D].partition_broadcast(128), reads=[self.r_dmod], writes=[self.r_mod])
            t.dma("sp", g1[:], self.I("ln1_g").partition_broadcast(128), writes=[r_c])
            t.dma("sp", b1[:], self.I("ln1_b").partition_broadcast(128), writes=[r_c])
            yab = [self.sb(es, f"yab{i}", [128, 2, D], BF16) for i in range(2)]
            r_yab = [self.R() for _ in range(2)]
            yT = [self.sb(es, f"yT{i}", [128, 2, 8, 128], BF16) for i in range(2)]
            r_yT = [self.R() for _ in range(2)]
            gate = [self.sb(es, f"gate{i}", [128, 2 * D], F32) for i in range(2)]
            r_gate = [self.R() for _ in range(2)]
            xt = [self.sb(es, f"x3_{i}", [128, D], F32) for i in range(2)]
            r_xt = [self.R() for _ in range(2)]
            m1 = self.sb(es, "m1", [128, D], F32)
            m2 = self.sb(es, "m2", [128, D], F32)
            mg = self.sb(es, "mg", [128, D], BF16)
            mgT = self.sb(es, "mgT", [128, 8, 128], BF16)
            r_m = self.R()
            r_mg = self.R()
            r_mgT = self.R()
            xnew = self.sb(es, "xnew", [128, D], F32)
            r_xn = self.R()
            x1 = [self.sb(es, f"x1_{i}", [128, D], F32) for i in range(2)]
            r_x1 = [self.R() for _ in range(2)]
            stats = self.sb(es, "st3", [128, 2, 6], F32)
            mv = self.sb(es, "mv3", [128, 4], F32)
            r_st = self.R()
            for j in range(nslot):
                b = j % 2
                q0 = j * 128
                tok0 = (8 * j + 7) * 128
                t.dma("sp", yab[b][:, 0, :], self.d_ya[q0:q0 + 128, :], writes=[r_yab[b]])
                t.dma("sp", yab[b][:, 1, :], self.d_yb[q0:q0 + 128, :], writes=[r_yab[b]])
                t.dma("sp", gate[b][:], self.d_gate[q0:q0 + 128, :], writes=[r_gate[b]])
                t.dma("sp", xt[b][:], self.I("x")[tok0:tok0 + 128, :], writes=[r_xt[b]])
                for br in range(2):
                    pbf = PS[br].bitcast(BF16)

                    def tr(e, br=br, b=b, pbf=pbf):
                        for c in range(8):
                            ins = e.transpose(pbf[:, c * 128:(c + 1) * 128], yab[b][:, br, c * 128:(c + 1) * 128], self.identb[:])
                        return ins
                    t.op("pe", tr, reads=[r_yab[b], self.r_const], writes=[PR[br]])
                    t.op("act", lambda e, br=br, b=b, pbf=pbf: e.copy(
                        out=yT[b][:, br, :, :], in_=pbf[:, :].rearrange("p (c n) -> p c n", c=8)),
                        reads=[PR[br]], writes=[r_yT[b]])
                for cg in range(2):
                    csl = slice(cg * 512, (cg + 1) * 512)
                    for br, w_ in ((0, wa), (1, wb)):
                        pb = 2 + br

                        def mm(e, br=br, w_=w_, pb=pb, b=b, csl=csl):
                            for c in range(8):
                                ins = e.matmul(PS[pb][:, :], lhsT=yT[b][:, br, c, :], rhs=w_[:, c, csl],
                                               start=(c == 0), stop=(c == 7))
                            return ins
                        t.op("pe", mm, reads=[r_yT[b], r_w], writes=[PR[pb]])
                    t.op("dve", lambda e, b=b, csl=csl, cg=cg: e.tensor_tensor(
                        out=m1[:, csl], in0=PS[2][:, :], in1=gate[b][:, cg * 512:(cg + 1) * 512], op=ALU.mult),
                        reads=[PR[2], r_gate[b]], writes=[r_m])
                    t.op("dve", lambda e, b=b, csl=csl, cg=cg: e.tensor_tensor(
                        out=m2[:, csl], in0=PS[3][:, :], in1=gate[b][:, D + cg * 512:D + (cg + 1) * 512], op=ALU.mult),
                        reads=[PR[3], r_gate[b]], writes=[r_m])
                    t.op("dve", lambda e, csl=csl: e.tensor_tensor(out=mg[:, csl], in0=m1[:, csl], in1=m2[:, csl], op=ALU.add),
                         reads=[r_m], writes=[r_mg])
                pbf = PS[4].bitcast(BF16)

                def tr2(e, pbf=pbf):
                    for c in range(8):
                        ins = e.transpose(pbf[:, c * 128:(c + 1) * 128], mg[:, c * 128:(c + 1) * 128], self.identb[:])
                    return ins
                t.op("pe", tr2, reads=[r_mg, self.r_const], writes=[PR[4]])
                t.op("act", lambda e, pbf=pbf: e.copy(out=mgT[:], in_=pbf[:, :].rearrange("p (c n) -> p c n", c=8)),
                     reads=[PR[4]], writes=[r_mgT])
                for cg in range(2):
                    csl = slice(cg * 512, (cg + 1) * 512)
                    pb = 5 + cg

                    def mm3(e, pb=pb, csl=csl):
                        for c in range(8):
                            ins = e.matmul(PS[pb][:, :], lhsT=mgT[:, c, :], rhs=wo[:, c, csl], start=(c == 0), stop=(c == 7))
                        return ins
                    t.op("pe", mm3, reads=[r_mgT, r_w], writes=[PR[pb]])
                    t.op("dve", lambda e, pb=pb, csl=csl: e.tensor_tensor(out=m1[:, csl], in0=PS[pb][:, :],
                                                                          in1=self.ga_bc[:, csl], op=ALU.mult),
                         reads=[PR[pb], self.r_mod], writes=[r_m])
                    t.op("dve", lambda e, b=b, csl=csl: e.scalar_tensor_tensor(
                        out=xnew[:, csl], in0=xt[b][:, csl], scalar=ALPHA, in1=m1[:, csl], op0=ALU.mult, op1=ALU.add),
                        reads=[r_xt[b], r_m], writes=[r_xn])
                self.ln_stats(xnew, r_xn, stats, mv, r_st)
                t.op("dve", lambda e, b=b: e.tensor_scalar(out=x1[b][:], in0=xnew[:], scalar1=mv[:, 0:1], scalar2=mv[:, 3:4],
                                                           op0=ALU.subtract, op1=ALU.mult),
                     reads=[r_xn, r_st], writes=[r_x1[b]])
                t.op("pool", lambda e, b=b: e.tensor_tensor(out=x1[b][:], in0=x1[b][:], in1=g1[:], op=ALU.mult),
                     reads=[r_x1[b], r_c], writes=[r_x1[b]])
                t.op("pool", lambda e, b=b: e.tensor_tensor(out=x1[b][:], in0=x1[b][:], in1=b1[:], op=ALU.add),
                     reads=[r_x1[b], r_c], writes=[r_x1[b]])
                t.dma("sp", self.d_x1[q0:q0 + 128, :], x1[b][:], reads=[r_x1[b]])
            self.barrier()

    def phase4(self):
        nc, t = self.nc, self.t
        PS, PR = self.ps, self.psr
        NQ = self.NQ
        HT = min(1024, NQ)
        nhalf = NQ // HT
        TG = min(512, HT)
        ntg = HT // TG
        ntt = HT // 128
        with ExitStack() as es:
            scf = self.sb(es, "scf_bc", [128, D], F32)
            shf = self.sb(es, "shf_bc", [128, D], F32)
            g2 = scf
            b2l = shf
            wr = self.sb(es, "wr", [128, 8, NEXP], F32)
            brr = self.sb(es, "brr", [1, NEXP], F32)
            onesf = self.sb(es, "onesf4", [1, 128], F32)
            b2w = self.sb(es, "b2w", [NEXP, D], F32)
            b1raw = self.sb(es, "b1raw", [NEXP, 2 * DFF], F32)
            b1g = self.sb(es, "b1g", [128, 8, NEXP], F32)
            b1l = self.sb(es, "b1l", [128, 8, NEXP], F32)
            r_c = self.R()
            self.gf_bc = self.sb(es, "gf_bc", [128, D], F32)
            t.dma("sp", self.gf_bc[:], self.d_mod[5 * D:6 * D].partition_broadcast(128), reads=[self.r_dmod], writes=[self.r_mod])
            r_md = self.R()
            t.dma("sp", wr[:], self.I("w_router").rearrange("(c p) n -> p c n", p=128), writes=[r_c])
            t.dma("sp", brr[:], self.I("b_router").rearrange("(o n) -> o n", o=1), writes=[r_c])
            t.dma("sp", b2w[:], self.I("b_e2"), writes=[r_c])
            t.dma("sp", b1raw[:], self.I("b_e1"), writes=[r_c])
            t.op("dve", lambda e: e.memset(onesf[:], 1.0), writes=[r_c])
            b1v = b1raw[:].rearrange("e (p f two) -> e p f two", p=8, two=2)
            for p in range(8):
                for two, dst in ((0, b1g), (1, b1l)):
                    t.op("pe", lambda e, p=p, two=two: e.transpose(PS[7][:, 0:NEXP], b1v[:, p, :, two], self.identf[0:NEXP, 0:NEXP]),
                         reads=[r_c, self.r_const], writes=[PR[7]])
                    t.op("dve", lambda e, p=p, dst=dst, two=two: e.tensor_scalar(
                        out=dst[:, p, :], in0=PS[7][:, 0:NEXP], scalar1=float(two), scalar2=None, op0=ALU.add),
                        reads=[PR[7]], writes=[r_c])
            vT = self.sb(es, "vT", [128, 8, HT], BF16)
            r_vT = self.R()
            yacc = self.sb(es, "yacc", [128, ntt, D], F32)
            r_y = self.R()
            gate = self.sb(es, "gate4", [128, ntt, NEXP], F32)
            r_g = self.R()
            aT = [self.sb(es, f"aT{i}", [128, 8, HT], BF16) for i in range(2)]
            r_aT = [self.R() for _ in range(2)]
            w1p = [self.sb(es, f"w1p{i}", [128, 8, 256], BF16) for i in range(5)]
            r_w1 = [self.R() for _ in range(5)]
            w2e = [self.sb(es, f"w2e{i}", [128, 8, D], BF16) for i in range(2)]
            r_w2 = [self.R() for _ in range(2)]
            glu = [self.sb(es, f"glu{i}", [128, TG], F32) for i in range(2)]
            sig = [self.sb(es, f"sig{i}", [128, TG], F32) for i in range(2)]
            lin = [self.sb(es, f"lin{i}", [128, TG], F32) for i in range(2)]
            r_elg = [self.R() for _ in range(3)]
            r_ell = [self.R() for _ in range(3)]
            r_els = [self.R() for _ in range(3)]
            xt = [self.sb(es, f"x4_{i}", [128, D], F32) for i in range(2)]
            r_xt = [self.R() for _ in range(2)]
            vf = self.sb(es, "vf", [128, D], F32)
            vb16 = self.sb(es, "vb16", [128, D], BF16)
            vTf = self.sb(es, "vTf", [128, 8, 128], F32)
            r_v = self.R()
            stats = self.sb(es, "st4", [128, 2, 6], F32)
            mv = self.sb(es, "mv4", [128, 4], F32)
            r_st = self.R()
            rt = self.sb(es, "rt", [128, 4, NEXP], F32)
            m8 = self.sb(es, "m8", [128, 16], F32)
            gT = self.sb(es, "gT", [NEXP, 128], F32)
            r_rt = self.R()
            w1cnt = [0]
            wfc = [0]
            elc = [0]
            for hf in range(nhalf):
                h0 = hf * HT
                t.dma("sp", scf[:], self.d_mod[4 * D:5 * D].partition_broadcast(128), writes=[r_md])
                t.dma("sp", shf[:], self.d_mod[3 * D:4 * D].partition_broadcast(128), writes=[r_md])
                t.op("dve", lambda e: e.tensor_scalar(out=scf[:], in0=scf[:], scalar1=1.0, scalar2=None, op0=ALU.add),
                     reads=[r_md], writes=[r_md])
                for tt in range(ntt):
                    b = tt % 2
                    q0 = h0 + tt * 128
                    t.dma("sp", xt[b][:], self.d_x1[q0:q0 + 128, :], writes=[r_xt[b]])
                    self.ln_stats(xt[b], r_xt[b], stats, mv, r_st)
                    t.op("dve", lambda e, b=b: e.tensor_scalar(out=vf[:], in0=xt[b][:], scalar1=mv[:, 0:1], scalar2=mv[:, 3:4],
                                                               op0=ALU.subtract, op1=ALU.mult),
                         reads=[r_xt[b], r_st], writes=[r_v])
                    t.op("pool", lambda e: e.tensor_tensor(out=vf[:], in0=vf[:], in1=scf[:], op=ALU.mult),
                         reads=[r_v, r_md], writes=[r_v])
                    t.op("pool", lambda e: e.tensor_tensor(out=vf[:], in0=vf[:], in1=shf[:], op=ALU.add),
                         reads=[r_v, r_md], writes=[r_v])
                    t.op("dve", lambda e: e.tensor_copy(out=vb16[:], in_=vf[:]), reads=[r_v], writes=[r_v])
                    pbf = PS[0].bitcast(BF16)

                    def tr(e, pbf=pbf):
                        for c in range(8):
                            ins = e.transpose(pbf[:, c * 128:(c + 1) * 128], vb16[:, c * 128:(c + 1) * 128], self.identb[:])
                        return ins
                    t.op("pe", tr, reads=[r_v, self.r_const], writes=[PR[0]])
                    t.op("act", lambda e, tt=tt, pbf=pbf: e.copy(out=vT[:, :, tt * 128:(tt + 1) * 128],
                                                                 in_=pbf[:, :].rearrange("p (c n) -> p c n", c=8)),
                         reads=[PR[0]], writes=[r_vT])
                    for half2 in range(2):
                        def trf(e, half2=half2):
                            for c4 in range(4):
                                c = half2 * 4 + c4
                                ins = e.transpose(PS[1 + half2][:, c4 * 128:(c4 + 1) * 128], vf[:, c * 128:(c + 1) * 128], self.identf[:])
                            return ins
                        t.op("pe", trf, reads=[r_v, self.r_const], writes=[PR[1 + half2]])
                        t.op("dve", lambda e, half2=half2: e.tensor_copy(
                            out=vTf[:, half2 * 4:half2 * 4 + 4, :], in_=PS[1 + half2][:, :].rearrange("p (c n) -> p c n", c=4)),
                            reads=[PR[1 + half2]], writes=[r_v])

                    def mmr(e):
                        for c in range(8):
                            e.matmul(PS[3][:, 0:NEXP], lhsT=vTf[:, c, :], rhs=wr[:, c, :], start=(c == 0), stop=False)
                        return e.matmul(PS[3][:, 0:NEXP], lhsT=onesf[0:1, :], rhs=brr[0:1, :], start=False, stop=True)
                    t.op("pe", mmr, reads=[r_v, r_c], writes=[PR[3]])
                    LG, SEL, EX = 0, 1, 2
                    t.op("dve", lambda e: e.tensor_copy(out=rt[:, LG, :], in_=PS[3][:, 0:NEXP]), reads=[PR[3]], writes=[r_rt])
                    t.op("dve", lambda e: e.max(out=m8[:, 0:8], in_=rt[:, LG, :]), reads=[r_rt], writes=[r_rt])
                    t.op("dve", lambda e: e.tensor_scalar(out=rt[:, SEL, :], in0=rt[:, LG, :], scalar1=m8[:, 3:4], scalar2=None,
                                                          op0=ALU.is_ge), reads=[r_rt], writes=[r_rt])
                    t.op("dve", lambda e: e.tensor_scalar(out=m8[:, 8:9], in0=m8[:, 0:1], scalar1=-1.0, scalar2=None,
                                                          op0=ALU.mult), reads=[r_rt], writes=[r_rt])
                    t.op("act", lambda e: e.activation(out=rt[:, EX, :], in_=rt[:, LG, :], func=AF.Exp, bias=m8[:, 8:9], scale=1.0),
                         reads=[r_rt], writes=[r_rt])
                    t.op("dve", lambda e: e.tensor_tensor(out=rt[:, EX, :], in0=rt[:, EX, :], in1=rt[:, SEL, :], op=ALU.mult),
                         reads=[r_rt], writes=[r_rt])
                    t.op("dve", lambda e: e.reduce_sum(out=m8[:, 9:10], in_=rt[:, EX, :], axis=AX.X), reads=[r_rt], writes=[r_rt])
                    t.op("dve", lambda e: e.reciprocal(out=m8[:, 10:11], in_=m8[:, 9:10]), reads=[r_rt], writes=[r_rt])
                    t.op("dve", lambda e, tt=tt: e.tensor_scalar(out=gate[:, tt, :], in0=rt[:, EX, :], scalar1=m8[:, 10:11],
                                                                 scalar2=None, op0=ALU.mult), reads=[r_rt], writes=[r_g])
                    t.op("pe", lambda e, tt=tt: e.transpose(PS[3][0:NEXP, 128:256], gate[:, tt, :], self.identf[:]),
                         reads=[r_g, self.r_const], writes=[PR[3]])
                    t.op("dve", lambda e: e.tensor_copy(out=gT[:], in_=PS[3][0:NEXP, 128:256]), reads=[PR[3]], writes=[r_rt])
                    for cg in range(2):
                        t.op("pe", lambda e, cg=cg: e.matmul(PS[4 + cg][:, :], lhsT=gT[:, :], rhs=b2w[:, cg * 512:(cg + 1) * 512],
                                                            start=True, stop=True), reads=[r_rt, r_c], writes=[PR[4 + cg]])
                        t.op("dve", lambda e, cg=cg, tt=tt: e.tensor_copy(out=yacc[:, tt, cg * 512:(cg + 1) * 512], in_=PS[4 + cg][:, :]),
                             reads=[PR[4 + cg]], writes=[r_y])

                def stageA(e_, mid=None):
                    ab_ = e_ % 2
                    for p in range(8):
                        if p == 6 and mid is not None:
                            mid()
                        wb_ = w1cnt[0] % 5
                        w1cnt[0] += 1
                        t.dma("pool", w1p[wb_][:], self.I("w_e1")[e_, :, p * 256:(p + 1) * 256].rearrange("(c q) n -> q c n", q=128),
                              writes=[r_w1[wb_]])
                        for tg in range(ntg):
                            tsl = slice(tg * TG, (tg + 1) * TG)
                            pg, pl = (0, 1) if (p * ntg + tg) % 2 == 0 else (2, 3)

                            def mm(e, wb_=wb_, tsl=tsl, pg=pg, pl=pl):
                                for two, pb in ((0, pg), (1, pl)):
                                    for c in range(8):
                                        ins = e.matmul(PS[pb][:, 0:TG], lhsT=w1p[wb_][:, c, two::2], rhs=vT[:, c, tsl],
                                                       start=(c == 0), stop=(c == 7))
                                return ins
                            t.op("pe", mm, reads=[r_w1[wb_], r_vT], writes=[PR[pg], PR[pl]])
                            k = elc[0] % 2
                            elc[0] += 1
                            t.op("dve", lambda e, k=k, pg=pg, p=p, e_=e_: e.tensor_scalar(
                                out=glu[k][:], in0=PS[pg][:, 0:TG], scalar1=b1g[:, p, e_:e_ + 1], scalar2=SWIGLU_LIMIT,
                                op0=ALU.add, op1=ALU.min), reads=[PR[pg], r_c], writes=[r_elg[k]])
                            t.op("dve", lambda e, k=k, pl=pl, p=p, e_=e_: e.tensor_scalar(
                                out=lin[k][:], in0=PS[pl][:, 0:TG], scalar1=b1l[:, p, e_:e_ + 1], scalar2=1.0 - SWIGLU_LIMIT,
                                op0=ALU.add, op1=ALU.max), reads=[PR[pl], r_c], writes=[r_ell[k]])
                            t.op("act", lambda e, k=k: e.activation(out=sig[k][:], in_=glu[k][:], func=AF.Sigmoid, scale=SWIGLU_ALPHA),
                                 reads=[r_elg[k]], writes=[r_els[k]])
                            t.op("dve", lambda e, k=k: e.scalar_tensor_tensor(
                                out=lin[k][:], in0=lin[k][:], scalar=1.0 + SWIGLU_LIMIT, in1=glu[k][:], op0=ALU.min, op1=ALU.mult),
                                reads=[r_ell[k], r_elg[k]], writes=[r_ell[k]])
                            t.op("dve", lambda e, k=k, ab_=ab_, p=p, tsl=tsl: e.tensor_tensor(
                                out=aT[ab_][:, p, tsl], in0=lin[k][:], in1=sig[k][:], op=ALU.mult),
                                reads=[r_ell[k], r_els[k]], writes=[r_aT[ab_]])

                def stageB(e_):
                    ab_ = e_ % 2
                    for tt in range(ntt):
                        for cg in range(2):
                            pb = 4 + (tt * 2 + cg) % 3

                            def mm(e, tt=tt, cg=cg, pb=pb):
                                for p in range(8):
                                    ins = e.matmul(PS[pb][:, :], lhsT=aT[ab_][:, p, tt * 128:(tt + 1) * 128],
                                                   rhs=w2e[ab_][:, p, cg * 512:(cg + 1) * 512], start=(p == 0), stop=(p == 7))
                                return ins
                            t.op("pe", mm, reads=[r_aT[ab_], r_w2[ab_]], writes=[PR[pb]])
                            t.op("dve", lambda e, tt=tt, cg=cg, pb=pb: e.scalar_tensor_tensor(
                                out=yacc[:, tt, cg * 512:(cg + 1) * 512], in0=PS[pb][:, :], scalar=gate[:, tt, e_:e_ + 1],
                                in1=yacc[:, tt, cg * 512:(cg + 1) * 512], op0=ALU.mult, op1=ALU.add),
                                reads=[PR[pb], r_g, r_y], writes=[r_y])

                def loadw2(e_):
                    ab_ = e_ % 2
                    v = self.I("w_e2")[e_].rearrange("(c q) n -> q c n", q=128)
                    for a in range(0, 1024, 512):
                        t.dma("pool", w2e[ab_][:, :, a:a + 512], v[:, :, a:a + 512], writes=[r_w2[ab_]])
                loadw2(0)
                stageA(0)
                for e_ in range(NEXP):
                    if e_ + 1 < NEXP:
                        stageA(e_ + 1, mid=lambda e_=e_: loadw2(e_ + 1))
                    stageB(e_)
                t.dma("sp", g2[:], self.I("ln2_g").partition_broadcast(128), writes=[r_md])
                t.dma("sp", b2l[:], self.I("ln2_b").partition_broadcast(128), writes=[r_md])
                for tt in range(ntt):
                    b = tt % 2
                    q0 = h0 + tt * 128
                    t.dma("sp", xt[b][:], self.d_x1[q0:q0 + 128, :], writes=[r_xt[b]])
                    t.op("pool", lambda e, tt=tt: e.tensor_tensor(out=yacc[:, tt, :], in0=yacc[:, tt, :], in1=self.gf_bc[:], op=ALU.mult),
                         reads=[r_y, self.r_mod], writes=[r_y])
                    t.op("dve", lambda e, b=b, tt=tt: e.scalar_tensor_tensor(out=vf[:], in0=xt[b][:], scalar=ALPHA, in1=yacc[:, tt, :],
                                                                            op0=ALU.mult, op1=ALU.add),
                         reads=[r_xt[b], r_y], writes=[r_v])
                    self.ln_stats(vf, r_v, stats, mv, r_st)
                    t.op("dve", lambda e, b=b: e.tensor_scalar(out=xt[b][:], in0=vf[:], scalar1=mv[:, 0:1], scalar2=mv[:, 3:4],
                                                               op0=ALU.subtract, op1=ALU.mult),
                         reads=[r_v, r_st], writes=[r_xt[b]])
                    t.op("pool", lambda e, b=b: e.tensor_tensor(out=xt[b][:], in0=xt[b][:], in1=g2[:], op=ALU.mult),
                         reads=[r_xt[b], r_md], writes=[r_xt[b]])
                    t.op("pool", lambda e, b=b: e.tensor_tensor(out=xt[b][:], in0=xt[b][:], in1=b2l[:], op=ALU.add),
                         reads=[r_xt[b], r_md], writes=[r_xt[b]])
                    t.dma("sp", self.out[q0:q0 + 128, :], xt[b][:], reads=[r_xt[b]], writes=[self.r_out])
            self.barrier()


def make_consts(nslot, c):
    ntile = 8 * nslot
    S = 128 * ntile
    slopes = alibi_slopes(8)
    k = np.arange(S)
    tt = k // 128
    pk = k % 128
    ndummy = 7 - c
    kaug = np.zeros((7, S), np.float32)
    kaug[0] = 1.0
    kaug[1] = 1.0
    kaug[2] = 128.0 * tt
    kaug[3] = 1.0
    kaug[4] = 1.0
    kaug[5] = pk
    kaug[6] = (tt < ndummy).astype(np.float32)
    qaug = np.zeros((7, nslot, 8, 128), np.float32)
    ql = np.arange(128)
    for j in range(nslot):
        tq = 8 * j + 7
        for h in range(8):
            s = slopes[h]
            qaug[2, j, h] = s
            qaug[3, j, h] = -s * 128.0 * tq
            qaug[4, j, h] = -s * ql
            qaug[5, j, h] = s
            qaug[6, j, h] = NEG
    kk = np.arange(128)[:, None]
    qq = np.arange(128)[None, :]
    cend = (qq // 64 + 1) * 64
    dg = np.zeros((128, 8, 128), np.float32)
    for h in range(8):
        s = slopes[h]
        m = np.where(kk > qq, -2.0 * s * (kk - qq), 0.0)
        m = np.where(kk >= cend, NEG, m)
        dg[:, h, :] = m
    dmask = np.where(kk.T >= 0, 0.0, 0.0) * 0.0
    qq2 = np.arange(128)[:, None]
    kk2 = np.arange(128)[None, :]
    dmask = np.where(kk2 < (qq2 // 64 + 1) * 64, 0.0, -1e9).astype(np.float32)
    dummy = np.zeros((128, 8), np.float32)
    dummy[:, :ndummy] = -1e9
    iota = np.broadcast_to(np.arange(1, 513, dtype=np.float32)[None, :], (128, 512)).copy()
    slopetab = np.zeros((2, 8, 128), np.float32)
    for h in range(8):
        slopetab[0, h] = slopes[h] * 128.0
        slopetab[1, h] = slopes[h]
    return {
        "c_identb": np.eye(128, dtype=np.float32).astype(NPBF),
        "c_identf": np.eye(128, dtype=np.float32),
        "c_kaug": kaug.astype(NPBF),
        "c_qaug": qaug.astype(NPBF),
        "c_dg": dg.astype(NPBF),
        "c_dmask": dmask,
        "c_dummy": dummy,
        "c_iota": iota,
        "c_slopetab": slopetab,
        "c_pidx1": np.arange(1, 129, dtype=np.float32).reshape(128, 1),
    }


def make_in_maps(inputs, nslot, used=None):
    S = 128 * 8 * nslot
    f = lambda a: np.ascontiguousarray(np.asarray(a, dtype=np.float32))
    x = f(inputs["x"])[0]
    assert x.shape[0] == S
    shared = {
        "c": f(inputs["c"])[0],
        "w_ada": f(inputs["w_ada"])[0],
        "b_ada": f(inputs["b_ada"])[0],
        "w_in": f(inputs["w_in"])[0],
        "lamv": np.stack([f(inputs[k])[0] for k in ("lam_q1", "lam_k1", "lam_q2", "lam_k2")]),
        "diff_norm_g": f(inputs["diff_norm_g"])[0],
        "w_branch_a": f(inputs["w_branch_a"])[0],
        "w_branch_b": f(inputs["w_branch_b"])[0],
        "w_out": f(inputs["w_out"])[0],
        "ln1_g": f(inputs["ln1_g"])[0],
        "ln1_b": f(inputs["ln1_b"])[0],
        "w_router": f(inputs["w_router"])[0],
        "b_router": f(inputs["b_router"])[0],
        "w_e1": f(inputs["w_e1"])[0],
        "b_e1": f(inputs["b_e1"])[0],
        "w_e2": f(inputs["w_e2"])[0],
        "b_e2": f(inputs["b_e2"])[0],
        "ln2_g": f(inputs["ln2_g"])[0],
        "ln2_b": f(inputs["ln2_b"])[0],
    }
    maps = []
    for c in range(NCORE):
        m = dict(shared)
        m["x"] = np.ascontiguousarray(np.roll(x, 128 * (7 - c), axis=0))
        m.update(make_consts(nslot, c))
        maps.append({k: v for k, v in m.items() if used is None or k in used})
    return maps


_CACHE = {}


def run(inputs, nslot, debug=False, phases=99, trace=False):
    key = (nslot, debug, phases)
    mk = MK(nslot, debug=debug, phases=phases)
    nc = mk.build()
    in_maps = make_in_maps(inputs, nslot, used=set(mk.in_aps.keys()))
    res = run_bass_kernel_spmd(nc, in_maps, core_ids=list(range(NCORE)), trace=trace)
    return res


def kernel(**inputs):
    nslot = 16
    res = run(inputs, nslot)
    S = 128 * 8 * nslot
    out = np.zeros((1, S, D), np.float32)
    for c in range(NCORE):
        o = np.asarray(res.results[c]["out"], dtype=np.float32)
        for j in range(nslot):
            rt = 8 * j + c
            out[0, rt * 128:(rt + 1) * 128, :] = o[j * 128:(j + 1) * 128, :]
    return out
```
